# Optimizing a Trainium2 kernel written in Bass

```python
import jax
import jax.numpy as jnp
from jax import lax
import numpy as np

D_MODEL = 1024
BATCH = 8
SEQ = 4096
DEPTH = 4

N_MIXERS = 2

SSD_D_INNER = 2 * D_MODEL
SSD_HEAD_DIM = 64
SSD_N_HEADS = SSD_D_INNER // SSD_HEAD_DIM
SSD_N_GROUPS = 4
SSD_HEADS_PER_GROUP = SSD_N_HEADS // SSD_N_GROUPS
SSD_D_STATE = 128
SSD_D_CONV = 4
SSD_CHUNK = 128
SSD_CONV_DIM = SSD_D_INNER + 2 * SSD_N_GROUPS * SSD_D_STATE
SSD_D_IN_PROJ = SSD_D_INNER + SSD_CONV_DIM + SSD_N_HEADS

ATT_HEAD_DIM = 64
ATT_HEADS_PER_GROUP = 8
ATT_PATTERNS = ((128, 1), (512, 4), (2048, 16))
ATT_N_GROUPS = len(ATT_PATTERNS)
ATT_QKV_DIM = ATT_N_GROUPS * 3 * ATT_HEADS_PER_GROUP * ATT_HEAD_DIM
ATT_OUT_DIM = ATT_HEADS_PER_GROUP * ATT_HEAD_DIM
ROPE_THETA = 500000.0
ROPE_DIM = ATT_HEAD_DIM // 4

MOE_N_GROUPS = 4
MOE_EXPERTS_PER_GROUP = 8
MOE_N_EXPERTS = MOE_N_GROUPS * MOE_EXPERTS_PER_GROUP
MOE_TOP_K = 2
MOE_HIDDEN = 512
MOE_BLOCK = 128

DEEPNORM_ALPHA = (2 * DEPTH) ** 0.25
DEEPNORM_BETA = (8 * DEPTH) ** -0.25
LN_EPS = 1e-5
RMS_EPS = 1e-5
NEG_INF = -1e30

kernel_name = 'hybrid_ssd_dilated_attn_hmoe_deepnorm'


def _layer_norm(x, g, b):
    xf = x.astype(jnp.float32)
    mu = jnp.mean(xf, -1, keepdims=True)
    var = jnp.mean(jnp.square(xf - mu), -1, keepdims=True)
    return ((xf - mu) * lax.rsqrt(var + LN_EPS) * g + b).astype(x.dtype)


def _rope_tables(positions):
    inv_freq = ROPE_THETA ** (-jnp.arange(0, ROPE_DIM, 2, dtype=jnp.float32) / ROPE_DIM)
    ang = positions.astype(jnp.float32)[..., None] * inv_freq
    return jnp.cos(ang)[:, :, None, :], jnp.sin(ang)[:, :, None, :]


def _apply_partial_rope(t, cos, sin):
    half = ROPE_DIM // 2
    t1, t2, rest = t[..., :half], t[..., half:ROPE_DIM], t[..., ROPE_DIM:]
    cos = cos.astype(t.dtype)
    sin = sin.astype(t.dtype)
    return jnp.concatenate([t1 * cos - t2 * sin, t2 * cos + t1 * sin, rest], -1)


def _causal_depthwise_conv(u, w, b):
    c = u.shape[-1]
    y = lax.conv_general_dilated(u, w.reshape(SSD_D_CONV, 1, c), window_strides=(1,),
                                 padding=((SSD_D_CONV - 1, 0),),
                                 dimension_numbers=('NWC', 'WIO', 'NWC'),
                                 feature_group_count=c)
    return y + b


def _ssd_chunked_scan(x, dt, a, b_in, c_in):
    bsz, s = x.shape[:2]
    nc = s // SSD_CHUNK
    Q, G, E, P, N = SSD_CHUNK, SSD_N_GROUPS, SSD_HEADS_PER_GROUP, SSD_HEAD_DIM, SSD_D_STATE
    xr = (x.astype(jnp.float32) * dt[..., None]).reshape(bsz, nc, Q, G, E, P)
    da = (dt * a).reshape(bsz, nc, Q, G, E).transpose(0, 1, 3, 4, 2)
    br = b_in.astype(jnp.float32).reshape(bsz, nc, Q, G, N)
    cr = c_in.astype(jnp.float32).reshape(bsz, nc, Q, G, N)
    da_cs = jnp.cumsum(da, -1)
    causal = jnp.tril(jnp.ones((Q, Q), bool))
    decay_in = jnp.exp(jnp.where(causal, da_cs[..., :, None] - da_cs[..., None, :], -jnp.inf))
    cb = jnp.einsum('bclgn,bcsgn->bcgls', cr, br)
    y_diag = jnp.einsum('bcgls,bcgels,bcsgep->bclgep', cb, decay_in, xr)
    decay_to_end = jnp.exp(da_cs[..., -1:] - da_cs)
    states = jnp.einsum('bclgn,bcgel,bclgep->bcgepn', br, decay_to_end, xr)
    chunk_decay = jnp.exp(da_cs[..., -1])

    def step(h, inp):
        s_c, dec_c = inp
        return h * dec_c[..., None, None] + s_c, h

    h0 = jnp.zeros((bsz, G, E, P, N), jnp.float32)
    _, prev = lax.scan(step, h0, (jnp.moveaxis(states, 1, 0), jnp.moveaxis(chunk_decay, 1, 0)))
    prev = jnp.moveaxis(prev, 0, 1)
    y_off = jnp.einsum('bclgn,bcgepn,bcgel->bclgep', cr, prev, jnp.exp(da_cs))
    return (y_diag + y_off).reshape(bsz, s, G * E, P)


def _ssd_mixer(x, w_in, conv_w, conv_b, dt_bias, a_log, d_skip, norm_w, w_out):
    bsz, s, _ = x.shape
    zxbcdt = x @ w_in
    z = zxbcdt[..., :SSD_D_INNER]
    xbc = zxbcdt[..., SSD_D_INNER:SSD_D_INNER + SSD_CONV_DIM]
    dt_raw = zxbcdt[..., SSD_D_INNER + SSD_CONV_DIM:]
    xbc = jax.nn.silu(_causal_depthwise_conv(xbc, conv_w, conv_b))
    gn = SSD_N_GROUPS * SSD_D_STATE
    xs = xbc[..., :SSD_D_INNER].reshape(bsz, s, SSD_N_HEADS, SSD_HEAD_DIM)
    b_in = xbc[..., SSD_D_INNER:SSD_D_INNER + gn].reshape(bsz, s, SSD_N_GROUPS, SSD_D_STATE)
    c_in = xbc[..., SSD_D_INNER + gn:].reshape(bsz, s, SSD_N_GROUPS, SSD_D_STATE)
    dt = jax.nn.softplus((dt_raw + dt_bias).astype(jnp.float32))
    a = -jnp.exp(a_log.astype(jnp.float32))
    y = _ssd_chunked_scan(xs, dt, a, b_in, c_in) + xs.astype(jnp.float32) * d_skip.astype(jnp.float32)[:, None]
    y = y.reshape(bsz, s, SSD_D_INNER) * jax.nn.silu(z.astype(jnp.float32))
    yg = y.reshape(bsz, s, SSD_N_GROUPS, -1)
    yg = yg * lax.rsqrt(jnp.mean(jnp.square(yg), -1, keepdims=True) + RMS_EPS)
    y = (yg.reshape(bsz, s, SSD_D_INNER) * norm_w).astype(x.dtype)
    return y @ w_out


def _dilated_window_attention(q, k, v, window, dilation):
    bsz, s, h, d = q.shape
    w = window // dilation
    n = s // dilation
    nb = -(-n // w)
    n_pad = nb * w - n

    def to_residues(t):
        return t.reshape(bsz, n, dilation, h, d).transpose(0, 2, 1, 3, 4)

    def key_blocks(t):
        t = jnp.pad(to_residues(t), ((0, 0), (0, 0), (w, n_pad), (0, 0), (0, 0)))
        t = t.reshape(bsz, dilation, nb + 1, w, h, d)
        return jnp.concatenate([t[:, :, :-1], t[:, :, 1:]], axis=3)

    q_r = jnp.pad(to_residues(q), ((0, 0), (0, 0), (0, n_pad), (0, 0), (0, 0)))
    q_r = q_r.reshape(bsz, dilation, nb, w, h, d).astype(jnp.float32)
    k_b = key_blocks(k).astype(jnp.float32)
    v_b = key_blocks(v).astype(jnp.float32)
    scores = jnp.einsum('brnqhd,brnkhd->brnhqk', q_r, k_b) * (ATT_HEAD_DIM ** -0.5)
    qi = jnp.arange(w)[:, None]
    ki = jnp.arange(2 * w)[None, :]
    blk = jnp.arange(nb)[:, None, None]
    valid = (ki >= qi) & (ki <= qi + w) & (blk * w + ki - w >= 0)
    scores = jnp.where(valid[None, None, :, None], scores, NEG_INF)
    m = jnp.max(scores, -1)
    p = jnp.exp(scores - m[..., None])
    l = jnp.sum(p, -1)
    m_t = m.transpose(0, 1, 2, 4, 3)
    l_t = l.transpose(0, 1, 2, 4, 3)
    o = jnp.einsum('brnhqk,brnkhd->brnqhd', p, v_b) / l_t[..., None]

    def from_residues(t):
        t = t.reshape(bsz, dilation, nb * w, *t.shape[4:])[:, :, :n]
        t = jnp.moveaxis(t, 1, 2)
        return t.reshape(bsz, s, *t.shape[3:])

    return from_residues(o), from_residues(m_t), from_residues(l_t)


def _dilated_attention_mixer(x, cos, sin, w_qkv, w_o):
    bsz, s, _ = x.shape
    qkv = (x @ w_qkv).reshape(bsz, s, ATT_N_GROUPS, 3, ATT_HEADS_PER_GROUP, ATT_HEAD_DIM)
    outs, maxes, dens = [], [], []
    for g, (window, dilation) in enumerate(ATT_PATTERNS):
        q = _apply_partial_rope(qkv[:, :, g, 0], cos, sin)
        k = _apply_partial_rope(qkv[:, :, g, 1], cos, sin)
        o, m, l = _dilated_window_attention(q, k, qkv[:, :, g, 2], window, dilation)
        outs.append(o)
        maxes.append(m)
        dens.append(l)
    o = jnp.stack(outs)
    m = jnp.stack(maxes)
    l = jnp.stack(dens)
    wgt = l * jnp.exp(m - jnp.max(m, 0, keepdims=True))
    merged = jnp.sum(wgt[..., None] * o, 0) / jnp.sum(wgt, 0)[..., None]
    return merged.reshape(bsz, s, ATT_OUT_DIM).astype(x.dtype) @ w_o


def _hierarchical_moe(x, w_rg, b_rg, w_re, b_re, w_gate, w_up, w_down):
    bsz, s, d = x.shape
    t = bsz * s
    xf = x.reshape(t, d)
    g_logits = (xf @ w_rg + b_rg).astype(jnp.float32)
    g_w, g_idx = lax.top_k(jax.nn.softmax(g_logits, axis=-1), 1)
    e_logits = (xf @ w_re + b_re).astype(jnp.float32).reshape(t, MOE_N_GROUPS, MOE_EXPERTS_PER_GROUP)
    e_logits = e_logits[jnp.arange(t), g_idx[:, 0]]
    e_w, e_idx = lax.top_k(jax.nn.softmax(e_logits, axis=-1), MOE_TOP_K)
    e_w = e_w / jnp.sum(e_w, -1, keepdims=True)
    gates = (g_w * e_w).reshape(-1)
    expert_id = (g_idx * MOE_EXPERTS_PER_GROUP + e_idx).reshape(-1).astype(jnp.int32)
    token_id = jnp.repeat(jnp.arange(t, dtype=jnp.int32), MOE_TOP_K)
    n_assign = t * MOE_TOP_K
    n_blocks = -(-n_assign // MOE_BLOCK) + MOE_N_EXPERTS
    n_rows = n_blocks * MOE_BLOCK
    order = jnp.argsort(expert_id)
    sorted_e = expert_id[order]
    counts = jnp.bincount(expert_id, length=MOE_N_EXPERTS)
    padded = (counts + MOE_BLOCK - 1) // MOE_BLOCK * MOE_BLOCK
    pad_end = jnp.cumsum(padded)
    pad_start = pad_end - padded
    cnt_start = jnp.cumsum(counts) - counts
    dest = pad_start[sorted_e] + jnp.arange(n_assign, dtype=jnp.int32) - cnt_start[sorted_e]
    row_tok = jnp.full((n_rows,), t, jnp.int32).at[dest].set(token_id[order])
    row_gate = jnp.zeros((n_rows,), x.dtype).at[dest].set(gates[order].astype(x.dtype))
    block_e = jnp.minimum(jnp.searchsorted(pad_end, jnp.arange(n_blocks) * MOE_BLOCK, side='right'),
                          MOE_N_EXPERTS - 1)
    x_rows = jnp.concatenate([xf, jnp.zeros((1, d), x.dtype)], 0)[row_tok]
    x_rows = x_rows.reshape(n_blocks, MOE_BLOCK, d)

    def expert_block(args):
        xb, e = args
        h = jax.nn.silu(xb @ w_gate[e]) * (xb @ w_up[e])
        return h @ w_down[e]

    y_rows = lax.map(expert_block, (x_rows, block_e)).reshape(n_rows, d)
    y = jax.ops.segment_sum(y_rows * row_gate[:, None], row_tok, num_segments=t + 1)[:t]
    return y.reshape(bsz, s, d)


def setup_inputs(seed: int = 0) -> dict:
    key = jax.random.key(seed)
    ks = jax.random.split(key, 24)
    f32 = jnp.float32
    n_ssd = len(range(0, DEPTH, N_MIXERS))
    n_att = len(range(1, DEPTH, N_MIXERS))

    def nrm(k, shape, scale):
        return jax.random.normal(k, shape, f32) * scale

    x = jax.random.normal(ks[0], (BATCH, SEQ, D_MODEL), f32)
    positions = jnp.broadcast_to(jnp.arange(SEQ, dtype=jnp.int32), (BATCH, SEQ))
    dt0 = jnp.exp(jax.random.uniform(ks[4], (n_ssd, SSD_N_HEADS), f32) * (np.log(0.1) - np.log(0.001)) + np.log(0.001))
    return {
        'x': x,
        'positions': positions,
        'ssd_w_in': nrm(ks[1], (n_ssd, D_MODEL, SSD_D_IN_PROJ), D_MODEL ** -0.5),
        'ssd_conv_w': nrm(ks[2], (n_ssd, SSD_D_CONV, SSD_CONV_DIM), SSD_D_CONV ** -0.5),
        'ssd_conv_b': nrm(ks[3], (n_ssd, SSD_CONV_DIM), 0.02),
        'ssd_dt_bias': dt0 + jnp.log(-jnp.expm1(-dt0)),
        'ssd_a_log': jnp.log(jax.random.uniform(ks[5], (n_ssd, SSD_N_HEADS), f32, 1.0, 16.0)),
        'ssd_d': 1.0 + nrm(ks[6], (n_ssd, SSD_N_HEADS), 0.01),
        'ssd_norm_w': 1.0 + nrm(ks[7], (n_ssd, SSD_D_INNER), 0.02),
        'ssd_w_out': nrm(ks[8], (n_ssd, SSD_D_INNER, D_MODEL), SSD_D_INNER ** -0.5 * DEEPNORM_BETA),
        'attn_w_qkv': nrm(ks[9], (n_att, D_MODEL, ATT_QKV_DIM), D_MODEL ** -0.5),
        'attn_w_o': nrm(ks[10], (n_att, ATT_OUT_DIM, D_MODEL), ATT_OUT_DIM ** -0.5 * DEEPNORM_BETA),
        'ln_g': 1.0 + nrm(ks[11], (DEPTH, 2, D_MODEL), 0.02),
        'ln_b': nrm(ks[12], (DEPTH, 2, D_MODEL), 0.02),
        'moe_w_router_group': nrm(ks[13], (DEPTH, D_MODEL, MOE_N_GROUPS), D_MODEL ** -0.5),
        'moe_b_router_group': nrm(ks[14], (DEPTH, MOE_N_GROUPS), 0.01),
        'moe_w_router_expert': nrm(ks[15], (DEPTH, D_MODEL, MOE_N_EXPERTS), D_MODEL ** -0.5),
        'moe_b_router_expert': nrm(ks[16], (DEPTH, MOE_N_EXPERTS), 0.01),
        'moe_w_gate': nrm(ks[17], (DEPTH, MOE_N_EXPERTS, D_MODEL, MOE_HIDDEN), D_MODEL ** -0.5),
        'moe_w_up': nrm(ks[18], (DEPTH, MOE_N_EXPERTS, D_MODEL, MOE_HIDDEN), D_MODEL ** -0.5),
        'moe_w_down': nrm(ks[19], (DEPTH, MOE_N_EXPERTS, MOE_HIDDEN, D_MODEL), MOE_HIDDEN ** -0.5 * DEEPNORM_BETA),
    }


def reference(x, positions, ssd_w_in, ssd_conv_w, ssd_conv_b, ssd_dt_bias, ssd_a_log, ssd_d,
              ssd_norm_w, ssd_w_out, attn_w_qkv, attn_w_o, ln_g, ln_b,
              moe_w_router_group, moe_b_router_group, moe_w_router_expert, moe_b_router_expert,
              moe_w_gate, moe_w_up, moe_w_down):
    cos, sin = _rope_tables(positions)
    for i in range(DEPTH):
        j = i // N_MIXERS
        if i % N_MIXERS == 0:
            mix = _ssd_mixer(x, ssd_w_in[j], ssd_conv_w[j], ssd_conv_b[j], ssd_dt_bias[j],
                             ssd_a_log[j], ssd_d[j], ssd_norm_w[j], ssd_w_out[j])
        else:
            mix = _dilated_attention_mixer(x, cos, sin, attn_w_qkv[j], attn_w_o[j])
        x = _layer_norm(DEEPNORM_ALPHA * x + mix, ln_g[i, 0], ln_b[i, 0])
        ffn = _hierarchical_moe(x, moe_w_router_group[i], moe_b_router_group[i],
                                moe_w_router_expert[i], moe_b_router_expert[i],
                                moe_w_gate[i], moe_w_up[i], moe_w_down[i])
        x = _layer_norm(DEEPNORM_ALPHA * x + ffn, ln_g[i, 1], ln_b[i, 1])
    return x
```

```python
import numpy as np
import concourse.bass as bass
import concourse.mybir as mybir
from concourse.bass_utils import run_bass_kernel_spmd

F32 = mybir.dt.float32
F32R = mybir.dt.float32r
I32 = mybir.dt.int32
ALU = mybir.AluOpType
AF = mybir.ActivationFunctionType
AX = mybir.AxisListType

D_MODEL = 1024
DEPTH = 4
ALPHA = (2 * DEPTH) ** 0.25
LN_EPS = 1e-5
RMS_EPS = 1e-5
NEG = -30000.0
ATT_PATTERNS = ((128, 1), (512, 4), (2048, 16))
ROPE_THETA = 500000.0
NBLK_EXTRA = 32


class Buf:
    __slots__ = ("name", "w", "r")

    def __init__(self, name, init_r=None):
        self.name = name
        self.w = None
        self.r = dict(init_r) if init_r else {}


WRITE_KEYS = ("out", "accum_out", "ap")


class FW:
    ENGS = ("pe", "dve", "act", "pool", "sp")
    EPOCH = 30000

    def __init__(self, nc, n_dma_sems=48):
        self.nc = nc
        self.q = {e: [] for e in self.ENGS}
        self.sems = {}
        self._sem_ctx = []
        self._ctxs = []
        self.bufs = {}
        self.cur = {}
        self.waited = {e: {} for e in self.ENGS}
        self.free_events = {}
        for e in self.ENGS:
            self._new_epoch(e)
        self.dma_keys = []
        self.dma_uses = {}
        for i in range(n_dma_sems):
            k = self._alloc_sem("dma%d" % i)
            self.dma_keys.append(k)
            self.dma_uses[k] = 0
        self.dma_rr = 0
        self.ninst = 0
        self.uid = 0

    def _alloc_sem(self, name):
        cm = self.nc.semaphore(name)
        h = cm.__enter__()
        self._sem_ctx.append(cm)
        self.sems[name] = h
        return name

    def _new_epoch(self, e):
        idx = sum(1 for k in self.sems if k.startswith("e_" + e + "_"))
        k = self._alloc_sem("e_%s_%d" % (e, idx))
        self.cur[e] = [k, 0]

    def T(self, name, shape, dtype=F32):
        self.uid += 1
        name = "%s_%d" % (name, self.uid)
        cm = self.nc.sbuf_tensor(name, list(shape), dtype)
        t = cm.__enter__()
        self._ctxs.append((name, cm))
        self.bufs[name] = Buf(name, self.free_events)
        return t

    def P(self, name, shape, dtype=F32):
        self.uid += 1
        name = "%s_%d" % (name, self.uid)
        cm = self.nc.psum_tensor(name, list(shape), dtype)
        t = cm.__enter__()
        self._ctxs.append((name, cm))
        self.bufs[name] = Buf(name, self.free_events)
        return t

    def dram(self, name, shape, dtype=F32, kind="Internal", track=True):
        t = self.nc.dram_tensor(name, list(shape), dtype, kind=kind)
        if track:
            self.bufs[name] = Buf(name)
        return t.ap()

    def token(self, name):
        b = Buf(name)
        self.bufs[name] = b
        return b

    def scope(self):
        return _Scope(self)

    def _collect(self, kw, xr, xw):
        reads, writes = [], []
        for k, v in kw.items():
            if isinstance(v, bass.IndirectOffsetOnAxis):
                v = v.ap
                k = "idx"
            if isinstance(v, bass.AP):
                b = self.bufs.get(v.tensor.name)
                if b is not None:
                    (writes if k in WRITE_KEYS else reads).append(b)
        for b in xr or ():
            reads.append(self.bufs[b] if isinstance(b, str) else b)
        for b in xw or ():
            writes.append(self.bufs[b] if isinstance(b, str) else b)
        return reads, writes

    def _deps(self, reads, writes):
        deps = {}
        for b in reads:
            if b.w is not None and deps.get(b.w[0], 0) < b.w[1]:
                deps[b.w[0]] = b.w[1]
        for b in writes:
            if b.w is not None and deps.get(b.w[0], 0) < b.w[1]:
                deps[b.w[0]] = b.w[1]
            for k, v in b.r.items():
                if deps.get(k, 0) < v:
                    deps[k] = v
        return deps

    def _emit_waits(self, e, deps):
        wt = self.waited[e]
        for k, v in deps.items():
            if e == "pe" and k.startswith("e_pe_"):
                continue
            if wt.get(k, 0) >= v:
                continue
            wt[k] = v
            h = self.sems[k]
            self.q[e].append(lambda eng, h=h, v=v: eng.wait_ge(h, v))

    def _update(self, ev, reads, writes):
        k, v = ev
        for b in reads:
            if b.r.get(k, 0) < v:
                b.r[k] = v
        for b in writes:
            b.w = ev
            b.r = {}

    def I(self, e, meth, _r=None, _w=None, **kw):
        reads, writes = self._collect(kw, _r, _w)
        deps = self._deps(reads, writes)
        self._emit_waits(e, deps)
        cur = self.cur[e]
        if cur[1] >= self.EPOCH:
            self._new_epoch(e)
            cur = self.cur[e]
        cur[1] += 1
        k, v = cur[0], cur[1]
        h = self.sems[k]
        self.q[e].append(lambda eng, meth=meth, kw=kw, h=h: getattr(eng, meth)(**kw).then_inc(h, 1))
        self._update((k, v), reads, writes)
        self.ninst += 1

    def D(self, e, _r=None, _w=None, _meth="dma_start", **kw):
        reads, writes = self._collect(kw, _r, _w)
        deps = self._deps(reads, writes)
        k = self.dma_keys[self.dma_rr % len(self.dma_keys)]
        self.dma_rr += 1
        prev = 16 * self.dma_uses[k]
        if prev:
            deps[k] = max(deps.get(k, 0), prev)
        self._emit_waits(e, deps)
        self.dma_uses[k] += 1
        v = 16 * self.dma_uses[k]
        h = self.sems[k]
        self.q[e].append(lambda eng, meth=_meth, kw=kw, h=h: getattr(eng, meth)(**kw).then_inc(h, 16))
        self._update((k, v), reads, writes)
        self.ninst += 1

    def barrier(self, e, tok_names):
        self.I(e, "nop", _w=list(tok_names))

    def finish(self, final_names):
        reads = [self.bufs[n] for n in final_names]
        deps = self._deps(reads, [])
        for e in self.ENGS:
            self._emit_waits(e, dict(deps))
        nc = self.nc
        with nc.Block() as block:
            @block.tensor
            def _(eng):
                for f in self.q["pe"]:
                    f(eng)

            @block.vector
            def _(eng):
                for f in self.q["dve"]:
                    f(eng)

            @block.scalar
            def _(eng):
                for f in self.q["act"]:
                    f(eng)

            @block.gpsimd
            def _(eng):
                for f in self.q["pool"]:
                    f(eng)

            @block.sync
            def _(eng):
                for f in self.q["sp"]:
                    f(eng)
        for name, cm in reversed(self._ctxs):
            cm.__exit__(None, None, None)
        for cm in reversed(self._sem_ctx):
            cm.__exit__(None, None, None)


class _Scope:
    def __init__(self, fw):
        self.fw = fw

    def __enter__(self):
        self.mark = len(self.fw._ctxs)
        return self

    def __exit__(self, *a):
        fw = self.fw
        fe = fw.free_events
        while len(fw._ctxs) > self.mark:
            name, cm = fw._ctxs.pop()
            b = fw.bufs.pop(name)
            if b.w is not None and fe.get(b.w[0], 0) < b.w[1]:
                fe[b.w[0]] = b.w[1]
            for k, v in b.r.items():
                if fe.get(k, 0) < v:
                    fe[k] = v
            cm.__exit__(None, None, None)
        return False


C_ID, C_U, C_ONES, C_L, C_PM, C_MASK, C_PIDX = 0, 128, 256, 384, 512, 640, 896
C_INVF, C_M16, C_1M16, C_SS, C_HALFPI, C_HM0, C_HM1, C_EIDX, C_BLK = 897, 898, 899, 900, 901, 902, 903, 904, 936


def make_consts(nblk):
    w = C_BLK + nblk
    c = np.zeros((128, w), np.float32)
    i = np.arange(128)
    c[:, C_ID:C_ID + 128] = np.eye(128, dtype=np.float32)
    c[:, C_U:C_U + 128] = (i[:, None] <= i[None, :])
    c[:, C_ONES:C_ONES + 128] = 1.0
    c[:, C_L:C_L + 128] = (i[:, None] < i[None, :])
    pm = np.zeros((128, 128), np.float32)
    for dp in range(128):
        if dp % 64 < 16:
            d = (dp // 64) * 64 + ((dp % 64) ^ 8)
            pm[d, dp] = 1.0
    c[:, C_PM:C_PM + 128] = pm
    c[:, C_MASK:C_MASK + 128] = np.where(i[None, :] >= i[:, None], 0.0, NEG)
    c[:, C_MASK + 128:C_MASK + 256] = np.where(i[None, :] <= i[:, None], 0.0, NEG)
    c[:, C_PIDX] = i
    dd = i % 64
    invf = (np.float32(ROPE_THETA) ** (-np.arange(0, 16, 2, dtype=np.float32) / np.float32(16))).astype(np.float32)
    m16 = (dd < 16).astype(np.float32)
    c[:, C_INVF] = np.where(dd < 16, invf[dd % 8], 0.0)
    c[:, C_M16] = m16
    c[:, C_1M16] = 1.0 - m16
    c[:, C_SS] = m16 * np.where(dd < 8, -1.0, 1.0)
    c[:, C_HALFPI] = np.float32(np.pi / 2)
    c[:, C_HM0] = (i < 64)
    c[:, C_HM1] = (i >= 64)
    c[:, C_EIDX:C_EIDX + 32] = np.arange(32)[None, :]
    c[:, C_BLK:C_BLK + nblk] = 128.0 * np.arange(nblk)[None, :]
    return c


class Prog:
    def __init__(self, S, layers=(0, 1, 2, 3), stop_after=None, with_moe=True, moe_layers=(0, 1, 2, 3)):
        self.S = S
        self.with_moe = with_moe
        self.NT = S // 128
        self.layers = tuple(layers)
        self.stop_after = stop_after
        self.NBLK = (2 * S) // 128 + NBLK_EXTRA
        nc = self.nc = bass.Bass("TRN2", target_bir_lowering=False)
        fw = self.fw = FW(nc)
        S_ = S
        ext = lambda n, s, d=F32: fw.dram(n, s, d, kind="ExternalInput")
        self.x_in = ext("x", [S_, 1024])
        self.pos_in = ext("pos", [1, S_], I32)
        self.consts_in = ext("consts", [128, C_BLK + self.NBLK])
        self.ssd_w_in = ext("ssd_w_in", [2, 1024, 5152])
        self.ssd_conv_w = ext("ssd_conv_w", [2, 4, 3072])
        self.ssd_conv_b = ext("ssd_conv_b", [2, 3072])
        self.ssd_dt_bias = ext("ssd_dt_bias", [2, 32])
        self.ssd_a_log = ext("ssd_a_log", [2, 32])
        self.ssd_d = ext("ssd_d", [2, 32])
        self.ssd_norm_w = ext("ssd_norm_w", [2, 2048])
        self.ssd_w_out = ext("ssd_w_out", [2, 2048, 1024])
        self.attn_w_qkv = ext("attn_w_qkv", [2, 1024, 4608])
        self.attn_w_o = ext("attn_w_o", [2, 512, 1024])
        self.ln_g = ext("ln_g", [4, 2, 1024])
        self.ln_b = ext("ln_b", [4, 2, 1024])
        self.w_router = ext("w_router", [4, 1024, 36])
        self.b_router = ext("b_router", [4, 36])
        self.moe_layers = tuple(moe_layers)
        if with_moe:
            nl = len(self.moe_layers)
            self.moe_gu = ext("moe_gu", [nl, 32 * 128, 8 * 1024])
            self.moe_dn = ext("moe_dn", [nl, 32 * 128, 4 * 1024])
        self.out = fw.dram("out", [S_, 1024], F32, kind="ExternalOutput")
        sc = lambda n, sh, d=F32: fw.dram(n, sh, d, track=False)
        self.XA = sc("XA", [S_, 1024])
        self.XT = [sc("XT0", [8, 128, S_]), sc("XT1", [8, 128, S_])]
        self.BCfm = sc("BCfm", [8, 128, S_])
        self.XSBtm = sc("XSBtm", [S_, 2560])
        self.YN = sc("YN", [S_, 2048])
        self.XROWS = sc("XROWS", [self.NBLK * 128, 1024])
        self.YROWS = sc("YROWS", [self.NBLK * 128, 1024])
        self.AO = sc("AO", [3, S_, 512])
        self.AML = sc("AML", [3, S_, 16])
        self.ROPE = sc("ROPE", [2, 128, S_])
        self.QK = sc("QK", [24, 128, S_])
        self.VP = sc("VP", [3, S_, 512])
        self.xa_tok = [fw.token("xa%d" % t) for t in range(self.NT)]
        self.xt_tok = [fw.token("xt0"), fw.token("xt1")]
        self.tk = {n: fw.token("tk_" + n) for n in ("BCfm", "XSBtm", "YN", "XROWS", "YROWS", "AO", "AML", "ROPE", "QK", "VP")}
        self.cst = fw.T("cst", [128, C_BLK + self.NBLK])
        fw.dummy = fw.T("fwdummy", [128, 4])
        fw.D("sp", out=self.cst[:], in_=self.consts_in[:, :])
        self.eps_ln = fw.T("eps_ln", [128, 2])
        fw.I("dve", "memset", ap=self.eps_ln[:, 0:1], constant=float(LN_EPS))
        fw.I("dve", "memset", ap=self.eps_ln[:, 1:2], constant=1.0)
        self.ident = self.cst[:, C_ID:C_ID + 128]
        self.U = self.cst[:, C_U:C_U + 128]
        self.ones = self.cst[:, C_ONES:C_ONES + 128]

    def bc_load(self, q, dst, src_row):
        self.fw.D(q, out=dst, in_=src_row.partition_broadcast(128))

    def initial(self):
        fw = self.fw
        with fw.scope():
            xt = [fw.T("i_x%d" % i, [128, 1024]) for i in range(2)]
            tp = [fw.P("i_tp%d" % i, [128, 8, 128]) for i in range(2)]
            ts = [fw.T("i_ts%d" % i, [128, 8, 128]) for i in range(2)]
            for t in range(self.NT):
                a = t % 2
                fw.D("sp", out=xt[a][:], in_=self.x_in[t * 128:(t + 1) * 128, :])
                fw.D("act", out=self.XA[t * 128:(t + 1) * 128, :], in_=xt[a][:], _w=[self.xa_tok[t]])
                self.transpose_store(xt[a], tp[a], ts[a], 0, t)

    def transpose_store(self, xtile, tp, ts, XT_dst, t):
        fw = self.fw
        for c in range(8):
            fw.I("pe", "transpose", out=tp[:, c, :], in_=xtile[:, c * 128:(c + 1) * 128], identity=self.ident)
        fw.I("act", "copy", out=ts[:], in_=tp[:])
        fw.D("sp", out=self.XT[XT_dst][:, :, t * 128:(t + 1) * 128].rearrange("k p t -> p k t"), in_=ts[:],
             _r=[self.xt_tok[XT_dst]])

    def proj_ln(self, tag, li, sub, kch, w_src, a_loader, XT_dst):
        fw = self.fw
        with fw.scope():
            w = fw.T(tag + "_w", [128, kch, 1024], F32R)
            for k in range(kch):
                fw.D("pool", out=w[:, k, :], in_=w_src[k * 128:(k + 1) * 128, :])
            gbc = fw.T(tag + "_g", [128, 1024])
            bbc = fw.T(tag + "_b", [128, 1024])
            self.bc_load("sp", gbc[:], self.ln_g[li, sub])
            self.bc_load("sp", bbc[:], self.ln_b[li, sub])
            atp = [fw.P(tag + "_atp%d" % i, [128, 4, 128]) for i in range(2)]
            aT = [fw.T(tag + "_aT%d" % i, [128, kch, 128], F32R) for i in range(2)]
            mp = [fw.P(tag + "_mp%d" % i, [128, 1024]) for i in range(1)]
            ln = self.ln_alloc(tag)
            for t in range(self.NT):
                a = t % 2
                A = a_loader(t)
                for k0 in range(0, kch, 4):
                    kn = min(4, kch - k0)
                    for k in range(kn):
                        fw.I("pe", "transpose", out=atp[(k0 // 4) % 2][:, k, :],
                             in_=A[:, (k0 + k) * 128:(k0 + k + 1) * 128], identity=self.ident)
                    fw.I("act" if (k0 // 4) % 2 else "dve", "copy" if (k0 // 4) % 2 else "tensor_copy",
                         out=aT[a][:, k0:k0 + kn, :], in_=atp[(k0 // 4) % 2][:, 0:kn, :])
                for n in range(2):
                    for k in range(kch):
                        fw.I("pe", "matmul", out=mp[0][:, n * 512:(n + 1) * 512], lhsT=aT[a][:, k, :],
                             rhs=w[:, k, n * 512:(n + 1) * 512], start=(k == 0), stop=(k == kch - 1))
                self.ln_epilogue(ln, t, mp[0][:], XT_dst, gbc, bbc)

    def ln_alloc(self, tag):
        fw = self.fw
        d = {}
        d["x"] = [fw.T(tag + "_lx%d" % i, [128, 1024]) for i in range(2)]
        d["v"] = [fw.T(tag + "_lv%d" % i, [128, 1024]) for i in range(2)]
        d["junk"] = fw.T(tag + "_lj", [128, 1024])
        d["st"] = [fw.T(tag + "_ls%d" % i, [128, 8]) for i in range(2)]
        d["tp"] = fw.P(tag + "_ltp", [128, 8, 128])
        d["ts"] = [fw.T(tag + "_lts%d" % i, [128, 8, 128]) for i in range(2)]
        return d

    def ln_epilogue(self, ln, t, mix, XT_dst, gbc, bbc):
        fw = self.fw
        a = t % 2
        x, v, st, junk = ln["x"][a], ln["v"][a], ln["st"][a], ln["junk"]
        fw.D("sp", out=x[:], in_=self.XA[t * 128:(t + 1) * 128, :], _r=[self.xa_tok[t]])
        fw.I("dve", "scalar_tensor_tensor", out=v[:], in0=x[:], scalar=float(ALPHA), in1=mix,
             op0=ALU.mult, op1=ALU.add)
        fw.I("act", "activation", out=junk[:], in_=v[:], func=AF.Identity, accum_out=st[:, 0:1])
        fw.I("act", "activation", out=junk[:], in_=v[:], func=AF.Square, accum_out=st[:, 1:2])
        fw.I("dve", "tensor_scalar", out=st[:, 2:3], in0=st[:, 0:1], scalar1=1.0 / 1024, scalar2=None, op0=ALU.mult)
        fw.I("dve", "tensor_tensor", out=st[:, 3:4], in0=st[:, 2:3], in1=st[:, 2:3], op=ALU.mult)
        fw.I("dve", "scalar_tensor_tensor", out=st[:, 4:5], in0=st[:, 1:2], scalar=1.0 / 1024, in1=st[:, 3:4],
             op0=ALU.mult, op1=ALU.subtract)
        fw.I("act", "activation", out=st[:, 6:7], in_=st[:, 4:5], func=AF.Sqrt, bias=self.eps_ln[:, 0:1])
        fw.I("dve", "reciprocal", out=st[:, 5:6], in_=st[:, 6:7])
        fw.I("dve", "tensor_scalar", out=v[:], in0=v[:], scalar1=st[:, 2:3], scalar2=st[:, 5:6],
             op0=ALU.subtract, op1=ALU.mult)
        fw.I("pool", "tensor_tensor", out=v[:], in0=v[:], in1=gbc[:], op=ALU.mult)
        fw.I("pool", "tensor_tensor", out=x[:], in0=v[:], in1=bbc[:], op=ALU.add)
        fw.D("act", out=self.XA[t * 128:(t + 1) * 128, :], in_=x[:], _w=[self.xa_tok[t]])
        if XT_dst is not None:
            self.transpose_store(x, ln["tp"], ln["ts"][a], XT_dst, t)

    def ssd_sweep1(self, j, XT_src):
        fw, S = self.fw, self.S
        w_in = self.ssd_w_in
        with fw.scope():
            wx = fw.T("s1_wx", [128, 8, 3072], F32R)
            for k in range(8):
                fw.D("pool", out=wx[:, k, :], in_=w_in[j, k * 128:(k + 1) * 128, 2048:5120])
            cw = fw.T("s1_cw", [128, 4, 24])
            cb = fw.T("s1_cb", [128, 24])
            for k in range(4):
                fw.D("sp", out=cw[:, k, :], in_=self.ssd_conv_w[j, k].rearrange("(t p) -> p t", p=128),
                     allow_slow_non_contiguous=True)
            fw.D("sp", out=cb[:], in_=self.ssd_conv_b[j].rearrange("(t p) -> p t", p=128),
                 allow_slow_non_contiguous=True)
            halo = fw.T("s1_halo", [128, 24, 3])
            fw.I("dve", "memset", ap=halo[:], constant=0.0)
            xtb = [fw.T("s1_xt%d" % i, [128, 8, 512], F32R) for i in range(2)]
            ps = [fw.P("s1_ps%d" % i, [128, 512]) for i in range(2)]
            ub = [fw.T("s1_ub%d" % i, [128, 515]) for i in range(2)]
            acc = [fw.T("s1_acc%d" % i, [128, 512]) for i in range(2)]
            so = [fw.T("s1_so%d" % i, [128, 512]) for i in range(2)]
            tps = [fw.P("s1_tp%d" % i, [128, 4, 128]) for i in range(2)]
            tsb = [fw.T("s1_ts%d" % i, [128, 4, 128]) for i in range(2)]
            for tb in range(S // 512):
                xt = xtb[tb % 2]
                fw.D("pool", out=xt[:], in_=self.XT[XT_src][:, :, tb * 512:(tb + 1) * 512].rearrange("k p t -> p k t"),
                     _r=[self.xt_tok[XT_src]])
                for ct in range(24):
                    a = ct % 2
                    for k in range(8):
                        fw.I("pe", "matmul", out=ps[a][:], lhsT=wx[:, k, ct * 128:(ct + 1) * 128], rhs=xt[:, k, :],
                             start=(k == 0), stop=(k == 7))
                    fw.I("pool", "tensor_copy", out=ub[a][:, 0:3], in_=halo[:, ct, :])
                    fw.I("act", "copy", out=ub[a][:, 3:515], in_=ps[a][:])
                    fw.I("pool", "tensor_copy", out=halo[:, ct, :], in_=ub[a][:, 512:515])
                    fw.I("dve", "tensor_scalar", out=acc[a][:], in0=ub[a][:, 3:515], scalar1=cw[:, 3, ct:ct + 1],
                         scalar2=cb[:, ct:ct + 1], op0=ALU.mult, op1=ALU.add)
                    fw.I("dve", "scalar_tensor_tensor", out=acc[a][:], in0=ub[a][:, 2:514], scalar=cw[:, 2, ct:ct + 1],
                         in1=acc[a][:], op0=ALU.mult, op1=ALU.add)
                    fw.I("dve", "scalar_tensor_tensor", out=acc[a][:], in0=ub[a][:, 1:513], scalar=cw[:, 1, ct:ct + 1],
                         in1=acc[a][:], op0=ALU.mult, op1=ALU.add)
                    fw.I("dve", "scalar_tensor_tensor", out=acc[a][:], in0=ub[a][:, 0:512], scalar=cw[:, 0, ct:ct + 1],
                         in1=acc[a][:], op0=ALU.mult, op1=ALU.add)
                    fw.I("act", "activation", out=so[a][:], in_=acc[a][:], func=AF.Silu)
                    if ct >= 16:
                        fw.D("sp", out=self.BCfm[ct - 16, :, tb * 512:(tb + 1) * 512], in_=so[a][:], _r=[self.tk["BCfm"]])
                    if ct < 20:
                        for q in range(4):
                            fw.I("pe", "transpose", out=tps[a][:, q, :], in_=so[a][:, q * 128:(q + 1) * 128],
                                 identity=self.ident)
                        fw.I("dve", "tensor_copy", out=tsb[a][:], in_=tps[a][:])
                        fw.D("sp", out=self.XSBtm[tb * 512:(tb + 1) * 512, ct * 128:(ct + 1) * 128]
                             .rearrange("(q p) c -> p q c", p=128), in_=tsb[a][:], _r=[self.tk["XSBtm"]])

    def ssd_sweep2(self, j, XT_src):
        fw, S = self.fw, self.S
        w_in = self.ssd_w_in
        with fw.scope():
            wz = fw.T("s2_wz", [128, 8, 2048], F32R)
            wdt = fw.T("s2_wdt", [128, 8, 32], F32R)
            for k in range(8):
                fw.D("pool", out=wz[:, k, :], in_=w_in[j, k * 128:(k + 1) * 128, 0:2048])
                fw.D("pool", out=wdt[:, k, :], in_=w_in[j, k * 128:(k + 1) * 128, 5120:5152])
            dtb = fw.T("s2_dtb", [128, 32])
            abc = fw.T("s2_a", [128, 32])
            dsk = fw.T("s2_dsk", [128, 32])
            nw = fw.T("s2_nw", [128, 2048])
            self.bc_load("sp", dtb[:], self.ssd_dt_bias[j])
            self.bc_load("sp", abc[:], self.ssd_a_log[j])
            self.bc_load("sp", dsk[:], self.ssd_d[j])
            self.bc_load("sp", nw[:], self.ssd_norm_w[j])
            fw.I("act", "activation", out=abc[:], in_=abc[:], func=AF.Exp)
            fw.I("dve", "tensor_scalar", out=abc[:], in0=abc[:], scalar1=-1.0, scalar2=None, op0=ALU.mult)
            prev = fw.T("s2_prev", [128, 4, 512])
            prevR = fw.T("s2_prevR", [128, 4, 512], F32R)
            fw.I("dve", "memset", ap=prev[:], constant=0.0)
            fw.I("dve", "tensor_copy", out=prevR[:], in_=prev[:])
            xTc = [fw.T("s2_xT%d" % i, [128, 8, 128], F32R) for i in range(2)]
            xs = [fw.T("s2_xs%d" % i, [128, 32, 64]) for i in range(2)]
            Btm = [fw.T("s2_Btm%d" % i, [128, 4, 128], F32R) for i in range(2)]
            Bfm = [fw.T("s2_Bfm%d" % i, [128, 4, 128], F32R) for i in range(2)]
            Cfm = [fw.T("s2_Cfm%d" % i, [128, 4, 128], F32R) for i in range(2)]
            sm = fw.T("s2_sm", [128, 12, 32])
            Uda = [fw.T("s2_Uda%d" % i, [128, 8, 128]) for i in range(2)]
            xr = fw.T("s2_xr", [128, 32, 64], F32R)
            xrd = fw.T("s2_xrd", [128, 32, 64], F32R)
            zs = [fw.T("s2_zs%d" % i, [128, 512]) for i in range(2)]
            cbm = [fw.T("s2_cbm%d" % i, [128, 128]) for i in range(2)]
            Dm = [fw.T("s2_D%d" % i, [128, 8, 128]) for i in range(2)]
            MT = [fw.T("s2_MT%d" % i, [128, 8, 128], F32R) for i in range(2)]
            t1 = [fw.T("s2_t1%d" % i, [128, 8, 64]) for i in range(2)]
            t2 = [fw.T("s2_t2%d" % i, [128, 8, 64]) for i in range(2)]
            yn = [fw.T("s2_yn0", [128, 2048])] * 2
            junk = fw.T("s2_junk", [128, 512])
            rs = fw.T("s2_rs", [128, 8])
            p_z = fw.P("s2_pz", [128, 512])
            p_sm = fw.P("s2_psm", [128, 512])
            p_R = fw.P("s2_pR", [128, 8, 128])
            p_Y = fw.P("s2_pY", [128, 8, 64])
            p_Yo = fw.P("s2_pYo", [128, 8, 64])
            p_S = fw.P("s2_pS", [128, 512])
            DT, DA, CS, ECS, DTE, CD, V0, V1, V2 = range(9)
            for c in range(S // 128):
                a = c % 2
                tok = slice(c * 128, (c + 1) * 128)
                fw.D("pool", out=xTc[a][:], in_=self.XT[XT_src][:, :, tok].rearrange("k p t -> p k t"),
                     _r=[self.xt_tok[XT_src]])
                fw.D("sp", out=xs[a][:].rearrange("p h d -> p (h d)"), in_=self.XSBtm[tok, 0:2048], _r=[self.tk["XSBtm"]])
                fw.D("pool", out=Btm[a][:].rearrange("p g n -> p (g n)"), in_=self.XSBtm[tok, 2048:2560],
                     _r=[self.tk["XSBtm"]])
                fw.D("pool", out=Bfm[a][:], in_=self.BCfm[0:4, :, tok].rearrange("g p t -> p g t"), _r=[self.tk["BCfm"]])
                fw.D("pool", out=Cfm[a][:], in_=self.BCfm[4:8, :, tok].rearrange("g p t -> p g t"), _r=[self.tk["BCfm"]])
                for k in range(8):
                    fw.I("pe", "matmul", out=p_sm[:, 0:32], lhsT=xTc[a][:, k, :], rhs=wdt[:, k, :],
                         start=(k == 0), stop=(k == 7))
                fw.I("dve", "tensor_tensor", out=sm[:, V0, :], in0=p_sm[:, 0:32], in1=dtb[:], op=ALU.add)
                fw.I("dve", "scalar_tensor_tensor", out=sm[:, V1, :], in0=sm[:, V0, :], scalar=-1.0, in1=sm[:, V0, :],
                     op0=ALU.mult, op1=ALU.max)
                fw.I("act", "activation", out=sm[:, V1, :], in_=sm[:, V1, :], func=AF.Exp, scale=-1.0)
                fw.I("act", "activation", out=sm[:, V1, :], in_=sm[:, V1, :], func=AF.Ln, bias=self.eps_ln[:, 1:2])
                fw.I("dve", "scalar_tensor_tensor", out=sm[:, DT, :], in0=sm[:, V0, :], scalar=0.0, in1=sm[:, V1, :],
                     op0=ALU.max, op1=ALU.add)
                fw.I("dve", "tensor_tensor", out=sm[:, DA, :], in0=sm[:, DT, :], in1=abc[:], op=ALU.mult)
                fw.I("pe", "matmul", out=p_sm[:, 32:64], lhsT=self.U, rhs=sm[:, DA, :], start=True, stop=True)
                fw.I("act", "copy", out=sm[:, CS, :], in_=p_sm[:, 32:64])
                fw.I("act", "activation", out=sm[:, ECS, :], in_=sm[:, CS, :], func=AF.Exp)
                fw.I("pool", "tensor_tensor", out=xr[:], in0=xs[a][:],
                     in1=sm[:, DT, :].unsqueeze(2).to_broadcast([128, 32, 64]), op=ALU.mult)
                for g in range(4):
                    b2 = g % 2
                    hs = slice(8 * g, 8 * g + 8)
                    for k in range(8):
                        fw.I("pe", "matmul", out=p_z[:], lhsT=xTc[a][:, k, :], rhs=wz[:, k, g * 512:(g + 1) * 512],
                             start=(k == 0), stop=(k == 7))
                    fw.I("act", "activation", out=zs[b2][:], in_=p_z[:], func=AF.Silu)
                    fw.I("dve", "tensor_tensor", out=Uda[b2][:], in0=self.U.unsqueeze(1).to_broadcast([128, 8, 128]),
                         in1=sm[:, DA, hs].unsqueeze(2).to_broadcast([128, 8, 128]), op=ALU.mult)
                    for hh in range(2):
                        fw.I("pe", "matmul", out=p_R[:, 4 * hh:4 * hh + 4, :], lhsT=self.ones,
                             rhs=Uda[b2][:, 4 * hh:4 * hh + 4, :], start=True, stop=True)
                    fw.I("dve", "tensor_tensor", out=sm[:, V2, hs], in0=p_R[:, :, 127], in1=sm[:, CS, hs], op=ALU.subtract)
                    fw.I("act", "activation", out=sm[:, DTE, hs], in_=sm[:, V2, hs], func=AF.Exp)
                    fw.I("act", "activation", out=sm[:, CD, hs], in_=p_R[:, :, 127], func=AF.Exp)
                    fw.I("pool", "tensor_tensor", out=xrd[:, hs, :], in0=xr[:, hs, :],
                         in1=sm[:, DTE, hs].unsqueeze(2).to_broadcast([128, 8, 64]), op=ALU.mult)
                    fw.I("pe", "matmul", out=p_sm[:, 128:256], lhsT=Bfm[a][:, g, :], rhs=Cfm[a][:, g, :],
                         start=True, stop=True)
                    fw.I("dve", "tensor_tensor", out=cbm[b2][:], in0=p_sm[:, 128:256], in1=self.U, op=ALU.mult)
                    fw.I("dve", "tensor_tensor", out=Dm[b2][:], in0=p_R[:],
                         in1=sm[:, CS, hs].unsqueeze(2).to_broadcast([128, 8, 128]), op=ALU.subtract)
                    fw.I("act", "activation", out=Dm[b2][:], in_=Dm[b2][:], func=AF.Exp)
                    fw.I("dve", "scalar_tensor_tensor", out=MT[b2][:], in0=Dm[b2][:], scalar=1.0,
                         in1=cbm[b2][:].unsqueeze(1).to_broadcast([128, 8, 128]), op0=ALU.min, op1=ALU.mult)
                    for h in range(8):
                        fw.I("pe", "matmul", out=p_Y[:, h, :], lhsT=MT[b2][:, h, :], rhs=xr[:, 8 * g + h, :],
                             start=True, stop=True)
                    fw.I("pe", "matmul", out=p_Yo[:].rearrange("p h d -> p (h d)"), lhsT=Cfm[a][:, g, :],
                         rhs=prevR[:, g, :], start=True, stop=True)
                    fw.I("pe", "matmul", out=p_S[:], lhsT=Btm[a][:, g, :],
                         rhs=xrd[:, hs, :].rearrange("p h d -> p (h d)"), start=True, stop=True)
                    fw.I("dve", "tensor_tensor", out=t1[b2][:], in0=p_Yo[:],
                         in1=sm[:, ECS, hs].unsqueeze(2).to_broadcast([128, 8, 64]), op=ALU.mult)
                    fw.I("dve", "tensor_tensor", out=t1[b2][:], in0=t1[b2][:], in1=p_Y[:], op=ALU.add)
                    fw.I("pool", "tensor_tensor", out=t2[b2][:], in0=xs[a][:, hs, :],
                         in1=dsk[:, hs].unsqueeze(2).to_broadcast([128, 8, 64]), op=ALU.mult)
                    fw.I("pool", "tensor_tensor", out=t1[b2][:], in0=t1[b2][:], in1=t2[b2][:], op=ALU.add)
                    fw.I("dve", "tensor_tensor", out=t1[b2][:].rearrange("p h d -> p (h d)"),
                         in0=t1[b2][:].rearrange("p h d -> p (h d)"), in1=zs[b2][:], op=ALU.mult)
                    fw.I("act", "activation", out=junk[:], in_=t1[b2][:].rearrange("p h d -> p (h d)"),
                         func=AF.Square, accum_out=rs[:, g:g + 1])
                    fw.I("dve", "tensor_scalar", out=rs[:, 4 + g:5 + g], in0=rs[:, g:g + 1], scalar1=1.0 / 512,
                         scalar2=float(RMS_EPS), op0=ALU.mult, op1=ALU.add)
                    fw.I("act", "activation", out=rs[:, 4 + g:5 + g], in_=rs[:, 4 + g:5 + g], func=AF.Sqrt)
                    fw.I("dve", "reciprocal", out=rs[:, 4 + g:5 + g], in_=rs[:, 4 + g:5 + g])
                    fw.I("dve", "scalar_tensor_tensor", out=yn[a][:, g * 512:(g + 1) * 512],
                         in0=t1[b2][:].rearrange("p h d -> p (h d)"), scalar=rs[:, 4 + g:5 + g],
                         in1=nw[:, g * 512:(g + 1) * 512], op0=ALU.mult, op1=ALU.mult)
                    fw.I("pool", "tensor_tensor", out=prev[:, g, :].rearrange("p (h d) -> p h d", h=8),
                         in0=prev[:, g, :].rearrange("p (h d) -> p h d", h=8),
                         in1=sm[:, CD, hs].unsqueeze(2).to_broadcast([128, 8, 64]), op=ALU.mult)
                    fw.I("dve", "tensor_tensor", out=prev[:, g, :], in0=prev[:, g, :], in1=p_S[:], op=ALU.add)
                    fw.I("act", "copy", out=prevR[:, g, :], in_=prev[:, g, :])
                fw.D("sp", out=self.YN[tok, :], in_=yn[a][:], _r=[self.tk["YN"]])

    def ssd_layer(self, li, XT_src, XT_dst):
        j = li // 2
        fw = self.fw
        fw.barrier("sp", [self.xt_tok[0], self.xt_tok[1], self.tk["BCfm"], self.tk["XSBtm"], self.tk["YN"]])
        self.ssd_sweep1(j, XT_src)
        fw.barrier("sp", [self.tk["BCfm"], self.tk["XSBtm"]])
        self.ssd_sweep2(j, XT_src)
        fw.barrier("sp", [self.tk["YN"]])
        with fw.scope():
            ab = [fw.T("s3_a%d" % i, [128, 2048]) for i in range(2)]

            def loader(t):
                fw.D("sp", out=ab[t % 2][:], in_=self.YN[t * 128:(t + 1) * 128, :], _r=[self.tk["YN"]])
                return ab[t % 2]
            self.proj_ln("s3", li, 0, 16, self.ssd_w_out[j], loader, XT_dst)

    def rope_tables(self):
        fw, S, cst = self.fw, self.S, self.cst
        CW = min(1024, S)
        TWO_PI = float(2 * np.pi)
        C1 = 6.28125
        C2 = float(2 * np.pi - 6.28125)
        with fw.scope():
            posi = fw.T("r_posi", [128, CW], I32)
            ang = fw.T("r_ang", [128, CW])
            kq = fw.T("r_kq", [128, CW])
            ki = fw.T("r_ki", [128, CW], I32)
            r = fw.T("r_r", [128, CW])
            m = fw.T("r_m", [128, CW])
            sv = fw.T("r_sv", [128, CW])
            cv = fw.T("r_cv", [128, CW])
            col = lambda c: cst[:, c:c + 1]
            for c0 in range(0, S, CW):
                fw.D("sp", out=posi[:], in_=self.pos_in[0, c0:c0 + CW].partition_broadcast(128))
                fw.I("dve", "tensor_copy", out=ang[:], in_=posi[:])
                fw.I("dve", "tensor_scalar", out=ang[:], in0=ang[:], scalar1=col(C_INVF), scalar2=None, op0=ALU.mult)
                fw.I("dve", "tensor_scalar", out=kq[:], in0=ang[:], scalar1=float(1.0 / TWO_PI), scalar2=None, op0=ALU.mult)
                fw.I("dve", "tensor_copy", out=ki[:], in_=kq[:])
                fw.I("dve", "tensor_copy", out=kq[:], in_=ki[:])
                fw.I("dve", "scalar_tensor_tensor", out=r[:], in0=kq[:], scalar=-C1, in1=ang[:], op0=ALU.mult, op1=ALU.add)
                fw.I("dve", "scalar_tensor_tensor", out=r[:], in0=kq[:], scalar=-C2, in1=r[:], op0=ALU.mult, op1=ALU.add)
                fw.I("dve", "tensor_scalar", out=m[:], in0=r[:], scalar1=float(np.pi), scalar2=None, op0=ALU.is_gt)
                fw.I("dve", "scalar_tensor_tensor", out=r[:], in0=m[:], scalar=-TWO_PI, in1=r[:], op0=ALU.mult, op1=ALU.add)
                fw.I("dve", "tensor_scalar", out=m[:], in0=r[:], scalar1=float(-np.pi), scalar2=None, op0=ALU.is_lt)
                fw.I("dve", "scalar_tensor_tensor", out=r[:], in0=m[:], scalar=TWO_PI, in1=r[:], op0=ALU.mult, op1=ALU.add)
                fw.I("dve", "tensor_scalar", out=r[:], in0=r[:], scalar1=3.1415925, scalar2=-3.1415925, op0=ALU.min, op1=ALU.max)
                fw.I("act", "activation", out=sv[:], in_=r[:], func=AF.Sin)
                fw.I("dve", "scalar_tensor_tensor", out=m[:], in0=r[:], scalar=-1.0, in1=r[:], op0=ALU.mult, op1=ALU.max)
                fw.I("act", "activation", out=cv[:], in_=m[:], func=AF.Sin, scale=-1.0, bias=col(C_HALFPI))
                fw.I("dve", "tensor_scalar", out=cv[:], in0=cv[:], scalar1=col(C_M16), scalar2=col(C_1M16), op0=ALU.mult, op1=ALU.add)
                fw.I("dve", "tensor_scalar", out=sv[:], in0=sv[:], scalar1=col(C_SS), scalar2=None, op0=ALU.mult)
                fw.D("sp", out=self.ROPE[0, :, c0:c0 + CW], in_=cv[:], _r=[self.tk["ROPE"]])
                fw.D("sp", out=self.ROPE[1, :, c0:c0 + CW], in_=sv[:], _r=[self.tk["ROPE"]])
        fw.barrier("sp", [self.tk["ROPE"]])

    def attn_proj(self, j, XT_src):
        fw, S, cst = self.fw, self.S, self.cst
        CH = min(2048, S)
        with fw.scope():
            PmR = fw.T("a1_pm", [128, 128], F32R)
            fw.I("dve", "tensor_copy", out=PmR[:], in_=cst[:, C_PM:C_PM + 128])
            w = fw.T("a1_w", [128, 8, 1536], F32R)
            xch = fw.T("a1_x", [128, 8, CH], F32R)
            tch = fw.T("a1_t", [128, 2, CH])
            ps = [fw.P("a1_ps%d" % i, [128, 512]) for i in range(2)]
            pp = [fw.P("a1_pp%d" % i, [128, 512]) for i in range(2)]
            pv = [fw.P("a1_pv%d" % i, [128, 512]) for i in range(2)]
            qsb = [fw.T("a1_q%d" % i, [128, 512], F32R) for i in range(2)]
            t1 = [fw.T("a1_t1%d" % i, [128, 512]) for i in range(2)]
            t2 = [fw.T("a1_t2%d" % i, [128, 512]) for i in range(2)]
            ob = [fw.T("a1_o%d" % i, [128, 512]) for i in range(2)]
            vb = [fw.T("a1_v%d" % i, [128, 512]) for i in range(2)]
            cnt = 0
            for g, (_, d) in enumerate(ATT_PATTERNS):
                n = S // d
                ic = CH // d
                for k in range(8):
                    fw.D("pool", out=w[:, k, :], in_=self.attn_w_qkv[j, k * 128:(k + 1) * 128, g * 1536:(g + 1) * 1536])
                for ch in range(S // CH):
                    fw.D("pool", out=xch[:], in_=self.XT[XT_src][:, :, ch * CH:(ch + 1) * CH].rearrange("k p t -> p k t"),
                         _r=[self.xt_tok[XT_src]])
                    fw.D("sp", out=tch[:], in_=self.ROPE[:, :, ch * CH:(ch + 1) * CH].rearrange("c p t -> p c t"),
                         _r=[self.tk["ROPE"]])

                    def perm(v3, c0, width):
                        vv = v3.rearrange("p (i r) -> p r i", r=d)
                        if ic >= width:
                            return vv[:, c0 // ic, (c0 % ic):(c0 % ic) + width]
                        return vv[:, c0 // ic:c0 // ic + width // ic, :]
                    for sb in range(CH // 512):
                        c0 = sb * 512
                        for which in range(2):
                            for tl in range(4):
                                a = cnt % 2
                                cnt += 1
                                wc = which * 512 + tl * 128
                                for k in range(8):
                                    fw.I("pe", "matmul", out=ps[a][:], lhsT=w[:, k, wc:wc + 128], rhs=perm(xch[:, k, :], c0, 512),
                                         start=(k == 0), stop=(k == 7))
                                fw.I("act", "copy", out=qsb[a][:], in_=ps[a][:])
                                fw.I("pe", "matmul", out=pp[a][:], lhsT=PmR[:], rhs=qsb[a][:], start=True, stop=True)
                                cview = perm(tch[:, 0, :], c0, 512)
                                sview = perm(tch[:, 1, :], c0, 512)
                                shp = list(cview.shape)
                                rs = (lambda t_: t_[:]) if len(shp) == 2 else (lambda t_: t_[:].rearrange("p (r i) -> p r i", r=shp[1]))
                                fw.I("dve", "tensor_tensor", out=rs(t1[a]), in0=rs(qsb[a]), in1=cview, op=ALU.mult)
                                fw.I("dve", "tensor_tensor", out=rs(t2[a]), in0=rs(pp[a]), in1=sview, op=ALU.mult)
                                fw.I("pool", "tensor_tensor", out=ob[a][:], in0=t1[a][:], in1=t2[a][:], op=ALU.add)
                                ct = (g * 2 + which) * 4 + tl
                                if ic >= 512:
                                    r_ = c0 // ic
                                    i0 = c0 % ic
                                    dst = self.QK[ct, :, r_ * n + ch * ic + i0:r_ * n + ch * ic + i0 + 512]
                                    src = ob[a][:]
                                else:
                                    nr = 512 // ic
                                    dst = self.QK[ct].rearrange("p (r n) -> p r n", r=d)[:, sb * nr:(sb + 1) * nr, ch * ic:(ch + 1) * ic]
                                    src = ob[a][:].rearrange("p (r i) -> p r i", r=nr)
                                fw.D("sp", out=dst, in_=src, _r=[self.tk["QK"]])
                    for bi in range(CH // 128):
                        a = bi % 2
                        c0 = bi * 128
                        for k in range(8):
                            fw.I("pe", "matmul", out=pv[a][:], lhsT=perm(xch[:, k, :], c0, 128), rhs=w[:, k, 1024:1536],
                                 start=(k == 0), stop=(k == 7))
                        fw.I("act" if a else "dve", "copy" if a else "tensor_copy", out=vb[a][:], in_=pv[a][:])
                        r_ = c0 // ic
                        i0 = c0 % ic
                        row0 = r_ * n + ch * ic + i0
                        fw.D("sp", out=self.VP[g, row0:row0 + 128, :], in_=vb[a][:], _r=[self.tk["VP"]])

    def attn_core(self):
        fw, S, cst = self.fw, self.S, self.cst
        NT = self.NT
        mask = cst[:, C_MASK:C_MASK + 256]
        with fw.scope():
            QT = [fw.T("a2_q%d" % i, [128, 4, 128], F32R) for i in range(2)]
            QZ = [fw.T("a2_qz%d" % i, [128, 4, 2, 128], F32R) for i in range(2)]
            KT = [fw.T("a2_k%d" % i, [128, 4, 128], F32R) for i in range(2)]
            V = [fw.T("a2_v%d" % i, [128, 512], F32R) for i in range(2)]
            sc = [fw.T("a2_sc%d" % i, [128, 4, 256]) for i in range(2)]
            PT = [fw.T("a2_pt%d" % i, [128, 8, 128], F32R) for i in range(2)]
            st = [fw.T("a2_st%d" % i, [128, 4, 8]) for i in range(2)]
            oh = [fw.T("a2_oh%d" % i, [128, 8, 64]) for i in range(2)]
            ml = [fw.T("a2_ml%d" % i, [128, 16]) for i in range(2)]
            p_S = [fw.P("a2_pS%d" % i, [128, 4, 256]) for i in range(2)]
            p_T = fw.P("a2_pT", [128, 8, 128])
            p_O = fw.P("a2_pO", [128, 8, 64])
            for g, (_, d) in enumerate(ATT_PATTERNS):
                n = S // d
                nbr = n // 128
                for pb in range(NT):
                    a = pb % 2
                    has_prev = (pb % nbr) > 0
                    cols = slice(pb * 128, (pb + 1) * 128)
                    qbase = (g * 2) * 4
                    fw.D("pool", out=QT[a][:], in_=self.QK[qbase:qbase + 4, :, cols].rearrange("t p c -> p t c"), _r=[self.tk["QK"]])
                    fw.D("pool", out=KT[a][:], in_=self.QK[qbase + 4:qbase + 8, :, cols].rearrange("t p c -> p t c"),
                         _r=[self.tk["QK"]])
                    fw.D("pool", out=V[a][:], in_=self.VP[g, cols, :], _r=[self.tk["VP"]])
                    k0 = 0 if has_prev else 128
                    nkc = 2 if has_prev else 1
                    for hl in range(2):
                        fw.I("pool" if hl else "dve", "tensor_scalar", out=QZ[a][:, :, hl, :], in0=QT[a][:],
                             scalar1=cst[:, C_HM0 + hl:C_HM0 + hl + 1], scalar2=None, op0=ALU.mult)
                    for h4 in range(2):
                        b2 = h4
                        for hh in range(4):
                            h = h4 * 4 + hh
                            tl, po = h // 2, (h % 2) * 64
                            if has_prev:
                                fw.I("pe", "matmul", out=p_S[b2][:, hh, 0:128], lhsT=QZ[a][:, tl, h % 2, :],
                                     rhs=KT[1 - a][:, tl, :], start=True, stop=True)
                            fw.I("pe", "matmul", out=p_S[b2][:, hh, 128:256], lhsT=QZ[a][:, tl, h % 2, :],
                                 rhs=KT[a][:, tl, :], start=True, stop=True)
                        fw.I("dve", "scalar_tensor_tensor", out=sc[b2][:, :, k0:256], in0=p_S[b2][:, :, k0:256], scalar=0.125,
                             in1=mask[:, k0:256].unsqueeze(1).to_broadcast([128, 4, 256 - k0]), op0=ALU.mult, op1=ALU.add)
                        mx = st[a][:, 0, h4 * 4:h4 * 4 + 4]
                        nmx = st[a][:, 1, h4 * 4:h4 * 4 + 4]
                        fw.I("dve", "tensor_reduce", out=mx, in_=sc[b2][:, :, k0:256], axis=AX.X, op=ALU.max)
                        fw.I("dve", "tensor_scalar", out=nmx, in0=mx, scalar1=-1.0, scalar2=None, op0=ALU.mult)
                        for hh in range(4):
                            h = h4 * 4 + hh
                            fw.I("act", "activation", out=sc[b2][:, hh, k0:256], in_=sc[b2][:, hh, k0:256], func=AF.Exp,
                                 bias=st[a][:, 1, h:h + 1], accum_out=st[a][:, 2, h:h + 1])
                        for hh in range(4):
                            for c in range(nkc):
                                kc = k0 + c * 128
                                fw.I("pe", "transpose", out=p_T[:, hh * 2 + c, :], in_=sc[b2][:, hh, kc:kc + 128],
                                     identity=self.ident)
                        if has_prev:
                            fw.I("act" if h4 else "dve", "copy" if h4 else "tensor_copy", out=PT[b2][:], in_=p_T[:])
                        else:
                            fw.I("act" if h4 else "dve", "copy" if h4 else "tensor_copy",
                                 out=PT[b2][:].rearrange("p (h c) q -> p h c q", c=2)[:, :, 0, :],
                                 in_=p_T[:].rearrange("p (h c) q -> p h c q", c=2)[:, :, 0, :])
                        for hh in range(4):
                            h = h4 * 4 + hh
                            for c in range(nkc):
                                vsrc = V[1 - a] if (has_prev and c == 0) else V[a]
                                fw.I("pe", "matmul", out=p_O[:, h, :], lhsT=PT[b2][:, hh * 2 + c, :], rhs=vsrc[:, h * 64:(h + 1) * 64],
                                     start=(c == 0), stop=(c == nkc - 1))
                    fw.I("dve", "reciprocal", out=st[a][:, 3, :], in_=st[a][:, 2, :])
                    fw.I("dve", "tensor_tensor", out=oh[a][:], in0=p_O[:],
                         in1=st[a][:, 3, :].unsqueeze(2).to_broadcast([128, 8, 64]), op=ALU.mult)
                    fw.I("pool", "tensor_copy", out=ml[a][:, 0:8], in_=st[a][:, 0, :])
                    fw.I("pool", "tensor_copy", out=ml[a][:, 8:16], in_=st[a][:, 2, :])
                    r_ = (pb * 128) // n
                    i0 = (pb * 128) % n
                    ao_dst = self.AO[g].rearrange("(i r) c -> r i c", r=d)[r_, i0:i0 + 128, :]
                    ml_dst = self.AML[g].rearrange("(i r) c -> r i c", r=d)[r_, i0:i0 + 128, :]
                    fw.D("sp", out=ao_dst, in_=oh[a][:].rearrange("p h e -> p (h e)"), _r=[self.tk["AO"]])
                    fw.D("sp", out=ml_dst, in_=ml[a][:], _r=[self.tk["AML"]])

    def attn_layer(self, li, XT_src, XT_dst):
        fw = self.fw
        j = li // 2
        fw.barrier("sp", [self.xt_tok[0], self.xt_tok[1], self.tk["QK"], self.tk["VP"], self.tk["AO"], self.tk["AML"]])
        self.attn_proj(j, XT_src)
        fw.barrier("sp", [self.tk["QK"], self.tk["VP"]])
        self.attn_core()
        fw.barrier("sp", [self.tk["AO"], self.tk["AML"]])
        with fw.scope():
            ao = [fw.T("a3_ao%d" % i, [128, 3, 512]) for i in range(2)]
            am = [fw.T("a3_ml%d" % i, [128, 3, 16]) for i in range(2)]
            sm = [fw.T("a3_sm%d" % i, [128, 4, 24]) for i in range(2)]
            mg = [fw.T("a3_mg%d" % i, [128, 512]) for i in range(2)]
            tmp = fw.T("a3_tmp", [128, 512])

            def loader(t):
                a = t % 2
                rows = slice(t * 128, (t + 1) * 128)
                fw.D("sp", out=ao[a][:], in_=self.AO[:, rows, :].rearrange("g p c -> p g c"), _r=[self.tk["AO"]])
                fw.D("sp", out=am[a][:], in_=self.AML[:, rows, :].rearrange("g p c -> p g c"), _r=[self.tk["AML"]])
                M = sm[a][:, 0, 0:8]
                fw.I("dve", "tensor_tensor", out=M, in0=am[a][:, 0, 0:8], in1=am[a][:, 1, 0:8], op=ALU.max)
                fw.I("dve", "tensor_tensor", out=M, in0=M, in1=am[a][:, 2, 0:8], op=ALU.max)
                wg = sm[a][:, 1, :].rearrange("p (g h) -> p g h", g=3)
                fw.I("dve", "tensor_tensor", out=wg, in0=am[a][:, :, 0:8], in1=M.unsqueeze(1).to_broadcast([128, 3, 8]),
                     op=ALU.subtract)
                fw.I("act", "activation", out=sm[a][:, 1, :], in_=sm[a][:, 1, :], func=AF.Exp)
                fw.I("dve", "tensor_tensor", out=wg, in0=wg, in1=am[a][:, :, 8:16], op=ALU.mult)
                den = sm[a][:, 2, 0:8]
                fw.I("dve", "tensor_tensor", out=den, in0=sm[a][:, 1, 0:8], in1=sm[a][:, 1, 8:16], op=ALU.add)
                fw.I("dve", "tensor_tensor", out=den, in0=den, in1=sm[a][:, 1, 16:24], op=ALU.add)
                fw.I("dve", "reciprocal", out=sm[a][:, 2, 8:16], in_=den)
                fw.I("dve", "tensor_tensor", out=wg, in0=wg, in1=sm[a][:, 2, 8:16].unsqueeze(1).to_broadcast([128, 3, 8]),
                     op=ALU.mult)
                v3 = lambda t_: t_.rearrange("p (h e) -> p h e", h=8)
                bc = lambda gi: sm[a][:, 1, gi * 8:(gi + 1) * 8].unsqueeze(2).to_broadcast([128, 8, 64])
                fw.I("dve", "tensor_tensor", out=v3(mg[a][:]), in0=v3(ao[a][:, 0, :]), in1=bc(0), op=ALU.mult)
                for gi in (1, 2):
                    fw.I("pool", "tensor_tensor", out=v3(tmp[:]), in0=v3(ao[a][:, gi, :]), in1=bc(gi), op=ALU.mult)
                    fw.I("dve", "tensor_tensor", out=mg[a][:], in0=mg[a][:], in1=tmp[:], op=ALU.add)
                return mg[a]
            self.proj_ln("a3", li, 0, 4, self.attn_w_o[j], loader, XT_dst)

    def zero_xrows(self):
        fw = self.fw
        with fw.scope():
            z = fw.T("z_zero", [128, 4, 1024])
            fw.I("dve", "memset", ap=z[:], constant=0.0)
            nq = self.NBLK
            for q0 in range(0, nq, 4):
                qn = min(4, nq - q0)
                fw.D("sp", out=self.XROWS[q0 * 128:(q0 + qn) * 128, :].rearrange("(q p) c -> p q c", p=128),
                     in_=z[:, 0:qn, :], _r=[self.tk["XROWS"]])

    def moe_layer(self, li, XT_src, XT_dst, lw=None):
        fw, NT, NB = self.fw, self.NT, self.NBLK
        lw = li if lw is None else lw
        cst = self.cst
        Lmat = cst[:, C_L:C_L + 128]
        fw.barrier("sp", [self.xt_tok[0], self.xt_tok[1], self.tk["XROWS"], self.tk["YROWS"]])
        with fw.scope():
            OH1 = fw.T("m_oh1", [128, NT, 32])
            OH2 = fw.T("m_oh2", [128, NT, 32])
            RANK = fw.T("m_rank", [128, NT, 32])
            GT = fw.T("m_gt", [128, NT, 2])
            DEST = fw.T("m_dest", [128, 2, NT])
            DESTi = fw.T("m_desti", [128, 2, NT], I32)
            WIDX = fw.T("m_widx", [128, NB], I32)
            run = fw.T("m_run", [128, 32])
            with fw.scope():
                wr = fw.T("m_wr", [128, 8, 36], F32R)
                fw.D("pool", out=wr[:], in_=self.w_router[li].rearrange("(k p) n -> p k n", p=128))
                brb = fw.T("m_brb", [128, 36])
                self.bc_load("sp", brb[:], self.b_router[li])
                fw.I("dve", "memset", ap=run[:], constant=0.0)
                xTc = [fw.T("m_xT%d" % i, [128, 8, 128], F32R) for i in range(2)]
                smt = [fw.T("m_sm%d" % i, [128, 128]) for i in range(2)]
                p_lg = fw.P("m_plg", [128, 64])
                p_rk = fw.P("m_prk", [128, 64])
                for t in range(NT):
                    a = t % 2
                    sm = smt[a]
                    lg = sm[:, 0:36]
                    gmax, gsum, gw, m1, m2, w1, tmp, ngmax = [sm[:, 36 + i:37 + i] for i in range(8)]
                    gmask, gexp = sm[:, 44:48], sm[:, 48:52]
                    esel, mask1, esel2, mask2 = sm[:, 52:60], sm[:, 60:68], sm[:, 68:76], sm[:, 76:84]
                    A = sm[:, 84:116]
                    fw.D("pool", out=xTc[a][:], in_=self.XT[XT_src][:, :, t * 128:(t + 1) * 128].rearrange("k p t -> p k t"),
                         _r=[self.xt_tok[XT_src]])
                    for k in range(8):
                        fw.I("pe", "matmul", out=p_lg[:, 0:36], lhsT=xTc[a][:, k, :], rhs=wr[:, k, :],
                             start=(k == 0), stop=(k == 7))
                    fw.I("dve", "tensor_tensor", out=lg, in0=p_lg[:, 0:36], in1=brb[:], op=ALU.add)
                    fw.I("dve", "tensor_reduce", out=gmax, in_=sm[:, 0:4], axis=AX.X, op=ALU.max)
                    fw.I("dve", "tensor_scalar", out=gmask, in0=sm[:, 0:4], scalar1=gmax, scalar2=None, op0=ALU.is_equal)
                    fw.I("dve", "tensor_scalar", out=ngmax, in0=gmax, scalar1=-1.0, scalar2=None, op0=ALU.mult)
                    fw.I("act", "activation", out=gexp, in_=sm[:, 0:4], func=AF.Exp, bias=ngmax, accum_out=gsum)
                    fw.I("dve", "reciprocal", out=gw, in_=gsum)
                    fw.I("dve", "tensor_scalar", out=esel, in0=sm[:, 4:12], scalar1=sm[:, 44:45], scalar2=None, op0=ALU.mult)
                    for g in range(1, 4):
                        fw.I("dve", "scalar_tensor_tensor", out=esel, in0=sm[:, 4 + 8 * g:12 + 8 * g],
                             scalar=sm[:, 44 + g:45 + g], in1=esel, op0=ALU.mult, op1=ALU.add)
                    fw.I("dve", "tensor_reduce", out=m1, in_=esel, axis=AX.X, op=ALU.max)
                    fw.I("dve", "tensor_scalar", out=mask1, in0=esel, scalar1=m1, scalar2=None, op0=ALU.is_equal)
                    fw.I("dve", "scalar_tensor_tensor", out=esel2, in0=mask1, scalar=-1e30, in1=esel,
                         op0=ALU.mult, op1=ALU.add)
                    fw.I("dve", "tensor_reduce", out=m2, in_=esel2, axis=AX.X, op=ALU.max)
                    fw.I("dve", "tensor_scalar", out=mask2, in0=esel2, scalar1=m2, scalar2=None, op0=ALU.is_equal)
                    fw.I("dve", "tensor_tensor", out=tmp, in0=m2, in1=m1, op=ALU.subtract)
                    fw.I("act", "activation", out=tmp, in_=tmp, func=AF.Exp)
                    fw.I("dve", "tensor_scalar", out=tmp, in0=tmp, scalar1=1.0, scalar2=None, op0=ALU.add)
                    fw.I("dve", "reciprocal", out=w1, in_=tmp)
                    fw.I("dve", "tensor_tensor", out=GT[:, t, 0:1], in0=gw, in1=w1, op=ALU.mult)
                    fw.I("dve", "tensor_tensor", out=GT[:, t, 1:2], in0=gw, in1=GT[:, t, 0:1], op=ALU.subtract)
                    fw.I("dve", "tensor_tensor", out=OH1[:, t, :].rearrange("p (g e) -> p g e", g=4),
                         in0=gmask.unsqueeze(2).to_broadcast([128, 4, 8]),
                         in1=mask1.unsqueeze(1).to_broadcast([128, 4, 8]), op=ALU.mult)
                    fw.I("dve", "tensor_tensor", out=OH2[:, t, :].rearrange("p (g e) -> p g e", g=4),
                         in0=gmask.unsqueeze(2).to_broadcast([128, 4, 8]),
                         in1=mask2.unsqueeze(1).to_broadcast([128, 4, 8]), op=ALU.mult)
                    fw.I("dve", "tensor_tensor", out=A, in0=OH1[:, t, :], in1=OH2[:, t, :], op=ALU.add)
                    fw.I("pe", "matmul", out=p_rk[:, 0:32], lhsT=Lmat, rhs=A, start=True, stop=True)
                    fw.I("pe", "matmul", out=p_rk[:, 32:64], lhsT=self.ones, rhs=A, start=True, stop=True)
                    fw.I("dve", "tensor_tensor", out=RANK[:, t, :], in0=p_rk[:, 0:32], in1=run[:], op=ALU.add)
                    fw.I("dve", "tensor_tensor", out=run[:], in0=run[:], in1=p_rk[:, 32:64], op=ALU.add)
            with fw.scope():
                cmp = fw.T("m_cmp", [128, 32, NT])
                nblk = fw.T("m_nblk", [128, 32])
                padded = fw.T("m_padded", [128, 32])
                padT = fw.T("m_padT", [32, 128])
                pend = fw.T("m_pend", [128, 32])
                pstart = fw.T("m_pstart", [128, 32])
                tmp3 = fw.T("m_tmp3", [128, NT, 32])
                tmp4 = fw.T("m_tmp4", [128, NT, 32])
                cmpb = fw.T("m_cmpb", [128, NB, 32])
                be = fw.T("m_be", [128, NB])
                p_a = fw.P("m_pa", [128, 128])
                p_b = fw.P("m_pb", [128, 32])
                grid = cst[:, C_BLK:C_BLK + NB]
                fw.I("dve", "tensor_tensor", out=cmp[:], in0=run[:].unsqueeze(2).to_broadcast([128, 32, NT]),
                     in1=grid[:, 0:NT].unsqueeze(1).to_broadcast([128, 32, NT]), op=ALU.is_gt)
                fw.I("dve", "tensor_reduce", out=nblk[:], in_=cmp[:], axis=AX.X, op=ALU.add)
                fw.I("dve", "tensor_scalar", out=padded[:], in0=nblk[:], scalar1=128.0, scalar2=None, op0=ALU.mult)
                fw.I("pe", "transpose", out=p_a[0:32, :], in_=padded[:, 0:32], identity=self.ident)
                fw.I("act", "copy", out=padT[:], in_=p_a[0:32, :])
                fw.I("pe", "matmul", out=p_b[:], lhsT=padT[:], rhs=cst[0:32, C_U:C_U + 32], start=True, stop=True)
                fw.I("act", "copy", out=pend[:], in_=p_b[:])
                fw.I("dve", "tensor_tensor", out=pstart[:], in0=pend[:], in1=padded[:], op=ALU.subtract)
                fw.I("dve", "tensor_tensor", out=tmp3[:], in0=RANK[:],
                     in1=pstart[:].unsqueeze(1).to_broadcast([128, NT, 32]), op=ALU.add)
                for k, OH in enumerate((OH1, OH2)):
                    fw.I("dve", "tensor_tensor", out=tmp4[:], in0=tmp3[:], in1=OH[:], op=ALU.mult)
                    fw.I("dve", "tensor_reduce", out=DEST[:, k, :], in_=tmp4[:], axis=AX.X, op=ALU.add)
                fw.I("dve", "tensor_copy", out=DESTi[:], in_=DEST[:])
                fw.I("dve", "tensor_tensor", out=cmpb[:], in0=grid.unsqueeze(2).to_broadcast([128, NB, 32]),
                     in1=pend[:].unsqueeze(1).to_broadcast([128, NB, 32]), op=ALU.is_ge)
                fw.I("dve", "tensor_reduce", out=be[:], in_=cmpb[:], axis=AX.X, op=ALU.add)
                fw.I("dve", "tensor_scalar", out=be[:], in0=be[:], scalar1=31.0, scalar2=128.0, op0=ALU.min, op1=ALU.mult)
                fw.I("dve", "tensor_scalar", out=be[:], in0=be[:], scalar1=cst[:, C_PIDX:C_PIDX + 1], scalar2=None,
                     op0=ALU.add)
                fw.I("dve", "tensor_copy", out=WIDX[:], in_=be[:])
            with fw.scope():
                xt = [fw.T("m_x%d" % i, [128, 1024]) for i in range(2)]
                for t in range(NT):
                    a = t % 2
                    fw.D("sp", out=xt[a][:], in_=self.XA[t * 128:(t + 1) * 128, :], _r=[self.xa_tok[t]])
                    for k in range(2):
                        fw.D("pool", _meth="indirect_dma_start", out=self.XROWS[:, :],
                             out_offset=bass.IndirectOffsetOnAxis(ap=DESTi[:, k, t:t + 1], axis=0),
                             in_=xt[a][:], in_offset=None, _r=[self.tk["XROWS"]])
            fw.barrier("sp", [self.tk["XROWS"]])
            with fw.scope():
                gu = [fw.T("m_gu%d" % i, [128, 8, 1024], F32R) for i in range(2)]
                dn = [fw.T("m_dn%d" % i, [128, 4, 1024], F32R) for i in range(2)]
                xb = [fw.T("m_xb%d" % i, [128, 1024]) for i in range(2)]
                xbT = [fw.T("m_xbT%d" % i, [128, 8, 128], F32R) for i in range(2)]
                sg = fw.T("m_sg", [128, 512])
                ht = fw.T("m_h", [128, 512])
                hT = [fw.T("m_hT%d" % i, [128, 4, 128], F32R) for i in range(2)]
                yb = [fw.T("m_yb%d" % i, [128, 1024]) for i in range(2)]
                p_t = fw.P("m_pt", [128, 8, 128])
                p_g = fw.P("m_pg", [128, 512])
                p_u = fw.P("m_pu", [128, 512])
                p_t2 = fw.P("m_pt2", [128, 4, 128])
                p_y = fw.P("m_py", [128, 1024])
                gu_src = self.moe_gu.rearrange("l r c -> (l r) c")
                dn_src = self.moe_dn.rearrange("l r c -> (l r) c")
                for b in range(NB):
                    a = b % 2
                    fw.D("sp", out=xb[a][:], in_=self.XROWS[b * 128:(b + 1) * 128, :], _r=[self.tk["XROWS"]])
                    fw.D("pool", _meth="indirect_dma_start", out=gu[a][:].rearrange("p k n -> p (k n)"), out_offset=None,
                         in_=gu_src, in_offset=bass.IndirectOffsetOnAxis(ap=WIDX[:, b:b + 1], axis=0),
                         element_offset=lw * 4096 * 8192)
                    fw.D("pool", _meth="indirect_dma_start", out=dn[a][:].rearrange("p k n -> p (k n)"), out_offset=None,
                         in_=dn_src, in_offset=bass.IndirectOffsetOnAxis(ap=WIDX[:, b:b + 1], axis=0),
                         element_offset=lw * 4096 * 4096)
                    for k in range(8):
                        fw.I("pe", "transpose", out=p_t[:, k, :], in_=xb[a][:, k * 128:(k + 1) * 128], identity=self.ident)
                    fw.I("act", "copy", out=xbT[a][:], in_=p_t[:])
                    for k in range(8):
                        fw.I("pe", "matmul", out=p_g[:], lhsT=xbT[a][:, k, :], rhs=gu[a][:, k, 0:512],
                             start=(k == 0), stop=(k == 7))
                    for k in range(8):
                        fw.I("pe", "matmul", out=p_u[:], lhsT=xbT[a][:, k, :], rhs=gu[a][:, k, 512:1024],
                             start=(k == 0), stop=(k == 7))
                    fw.I("act", "activation", out=sg[:], in_=p_g[:], func=AF.Silu)
                    fw.I("dve", "tensor_tensor", out=ht[:], in0=sg[:], in1=p_u[:], op=ALU.mult)
                    for k in range(4):
                        fw.I("pe", "transpose", out=p_t2[:, k, :], in_=ht[:, k * 128:(k + 1) * 128], identity=self.ident)
                    fw.I("dve", "tensor_copy", out=hT[a][:], in_=p_t2[:])
                    for n in range(2):
                        for k in range(4):
                            fw.I("pe", "matmul", out=p_y[:, n * 512:(n + 1) * 512], lhsT=hT[a][:, k, :],
                                 rhs=dn[a][:, k, n * 512:(n + 1) * 512], start=(k == 0), stop=(k == 3))
                    fw.I("act", "copy", out=yb[a][:], in_=p_y[:])
                    fw.D("sp", out=self.YROWS[b * 128:(b + 1) * 128, :], in_=yb[a][:], _r=[self.tk["YROWS"]])
            fw.barrier("sp", [self.tk["YROWS"]])
            with fw.scope():
                gbc = fw.T("m_g", [128, 1024])
                bbc = fw.T("m_b", [128, 1024])
                self.bc_load("sp", gbc[:], self.ln_g[li, 1])
                self.bc_load("sp", bbc[:], self.ln_b[li, 1])
                Y1 = [fw.T("m_y1%d" % i, [128, 1024]) for i in range(2)]
                Y2 = [fw.T("m_y2%d" % i, [128, 1024]) for i in range(2)]
                ffn = [fw.T("m_ffn%d" % i, [128, 1024]) for i in range(2)]
                ln = self.ln_alloc("m4")
                for t in range(NT):
                    a = t % 2
                    for k, Y in enumerate((Y1, Y2)):
                        fw.D("pool", _meth="indirect_dma_start", out=Y[a][:], out_offset=None, in_=self.YROWS[:, :],
                             in_offset=bass.IndirectOffsetOnAxis(ap=DESTi[:, k, t:t + 1], axis=0), _r=[self.tk["YROWS"]])
                    fw.I("dve", "tensor_scalar", out=ffn[a][:], in0=Y1[a][:], scalar1=GT[:, t, 0:1], scalar2=None,
                         op0=ALU.mult)
                    fw.I("dve", "scalar_tensor_tensor", out=ffn[a][:], in0=Y2[a][:], scalar=GT[:, t, 1:2], in1=ffn[a][:],
                         op0=ALU.mult, op1=ALU.add)
                    self.ln_epilogue(ln, t, ffn[a][:], XT_dst, gbc, bbc)

    def build(self):
        fw = self.fw
        self.initial()
        if self.with_moe:
            self.zero_xrows()
        if any(li % 2 == 1 for li in self.layers):
            self.rope_tables()
        cur = 0
        last_x = "XA"
        for li in self.layers:
            if li % 2 == 0:
                self.ssd_layer(li, cur, 1 - cur)
            else:
                self.attn_layer(li, cur, 1 - cur)
            cur = 1 - cur
            if self.stop_after == (li, 0):
                break
            self.moe_layer(li, cur, 1 - cur, self.moe_layers.index(li))
            cur = 1 - cur
            if self.stop_after == (li, 1):
                break
        with fw.scope():
            ot = [fw.T("o_t%d" % i, [128, 1024]) for i in range(2)]
            for t in range(self.NT):
                fw.D("sp", out=ot[t % 2][:], in_=self.XA[t * 128:(t + 1) * 128, :], _r=[self.xa_tok[t]])
                fw.D("act", out=self.out[t * 128:(t + 1) * 128, :], in_=ot[t % 2][:])
        fw.finish(["out"])
        return self.nc


def _lay_gu(wg, wu):
    L = wg.shape[0]
    g = wg.reshape(L, 32, 8, 128, 512).transpose(0, 1, 3, 2, 4)
    u = wu.reshape(L, 32, 8, 128, 512).transpose(0, 1, 3, 2, 4)
    return np.ascontiguousarray(np.concatenate([g, u], -1)).reshape(L, 4096, 8192)


def _lay_dn(wd):
    L = wd.shape[0]
    return np.ascontiguousarray(wd.reshape(L, 32, 4, 128, 1024).transpose(0, 1, 3, 2, 4)).reshape(L, 4096, 4096)


def _run(inputs, S, ncores, layers=(0, 1, 2, 3)):
    f32 = lambda a: np.ascontiguousarray(np.asarray(a, dtype=np.float32))
    prog = Prog(S, layers=layers)
    nc = prog.build()
    base = {k: f32(inputs[k]) for k in ("ssd_w_in", "ssd_conv_w", "ssd_conv_b", "ssd_dt_bias", "ssd_a_log", "ssd_d",
                                        "ssd_norm_w", "ssd_w_out", "attn_w_qkv", "attn_w_o", "ln_g", "ln_b")}
    base["consts"] = make_consts(prog.NBLK)
    base["w_router"] = f32(np.concatenate([np.asarray(inputs["moe_w_router_group"]),
                                           np.asarray(inputs["moe_w_router_expert"])], -1))
    base["b_router"] = f32(np.concatenate([np.asarray(inputs["moe_b_router_group"]),
                                           np.asarray(inputs["moe_b_router_expert"])], -1))
    base["moe_gu"] = _lay_gu(f32(inputs["moe_w_gate"]), f32(inputs["moe_w_up"]))
    base["moe_dn"] = _lay_dn(f32(inputs["moe_w_down"]))
    x = f32(inputs["x"])
    pos = np.ascontiguousarray(np.asarray(inputs["positions"]).astype(np.int32))
    in_maps = []
    for c in range(ncores):
        d = dict(base)
        d["x"] = np.ascontiguousarray(x[c, :S])
        d["pos"] = np.ascontiguousarray(pos[c:c + 1, :S])
        in_maps.append(d)
    res = run_bass_kernel_spmd(nc, in_maps, core_ids=list(range(ncores)))
    return np.stack([np.asarray(res.results[c]["out"]) for c in range(ncores)]).astype(np.float32)


def kernel(**inputs):
    x = np.asarray(inputs["x"])
    return _run(inputs, x.shape[1], x.shape[0])
```

```python
import numpy as np
import concourse.bass as bass
import concourse.mybir as mybir
from concourse.bass_utils import run_bass_kernel_spmd

F32 = mybir.dt.float32
F32R = mybir.dt.float32r
I32 = mybir.dt.int32
ALU = mybir.AluOpType
AF = mybir.ActivationFunctionType
AX = mybir.AxisListType

D_MODEL = 1024
DEPTH = 4
ALPHA = (2 * DEPTH) ** 0.25
LN_EPS = 1e-5
RMS_EPS = 1e-5
NEG = -30000.0
ATT_PATTERNS = ((128, 1), (512, 4), (2048, 16))
ROPE_THETA = 500000.0
NBLK_EXTRA = 32
BR = 256
RT = BR // 128


class Buf:
    __slots__ = ("name", "w", "r")

    def __init__(self, name, init_r=None):
        self.name = name
        self.w = None
        self.r = dict(init_r) if init_r else {}


WRITE_KEYS = ("out", "accum_out", "ap")


class FW:
    ENGS = ("pe", "dve", "act", "pool", "sp")
    EPOCH = 30000

    def __init__(self, nc, n_dma_sems=48):
        self.nc = nc
        self.q = {e: [] for e in self.ENGS}
        self.sems = {}
        self._sem_ctx = []
        self._ctxs = []
        self.bufs = {}
        self.cur = {}
        self.waited = {e: {} for e in self.ENGS}
        self.free_events = {}
        for e in self.ENGS:
            self._new_epoch(e)
        self.dma_keys = []
        self.dma_uses = {}
        for i in range(n_dma_sems):
            k = self._alloc_sem("dma%d" % i)
            self.dma_keys.append(k)
            self.dma_uses[k] = 0
        self.dma_rr = 0
        self.ninst = 0
        self.uid = 0

    def _alloc_sem(self, name):
        cm = self.nc.semaphore(name)
        h = cm.__enter__()
        self._sem_ctx.append(cm)
        self.sems[name] = h
        return name

    def _new_epoch(self, e):
        idx = sum(1 for k in self.sems if k.startswith("e_" + e + "_"))
        k = self._alloc_sem("e_%s_%d" % (e, idx))
        self.cur[e] = [k, 0]

    def T(self, name, shape, dtype=F32):
        self.uid += 1
        name = "%s_%d" % (name, self.uid)
        cm = self.nc.sbuf_tensor(name, list(shape), dtype)
        t = cm.__enter__()
        self._ctxs.append((name, cm))
        self.bufs[name] = Buf(name, self.free_events)
        return t

    def P(self, name, shape, dtype=F32):
        self.uid += 1
        name = "%s_%d" % (name, self.uid)
        cm = self.nc.psum_tensor(name, list(shape), dtype)
        t = cm.__enter__()
        self._ctxs.append((name, cm))
        self.bufs[name] = Buf(name, self.free_events)
        return t

    def dram(self, name, shape, dtype=F32, kind="Internal", track=True):
        t = self.nc.dram_tensor(name, list(shape), dtype, kind=kind)
        if track:
            self.bufs[name] = Buf(name)
        return t.ap()

    def token(self, name):
        b = Buf(name)
        self.bufs[name] = b
        return b

    def scope(self):
        return _Scope(self)

    def _collect(self, kw, xr, xw):
        reads, writes = [], []
        for k, v in kw.items():
            if isinstance(v, bass.IndirectOffsetOnAxis):
                v = v.ap
                k = "idx"
            if isinstance(v, bass.AP):
                b = self.bufs.get(v.tensor.name)
                if b is not None:
                    (writes if k in WRITE_KEYS else reads).append(b)
        for b in xr or ():
            reads.append(self.bufs[b] if isinstance(b, str) else b)
        for b in xw or ():
            writes.append(self.bufs[b] if isinstance(b, str) else b)
        return reads, writes

    def _deps(self, reads, writes):
        deps = {}
        for b in reads:
            if b.w is not None and deps.get(b.w[0], 0) < b.w[1]:
                deps[b.w[0]] = b.w[1]
        for b in writes:
            if b.w is not None and deps.get(b.w[0], 0) < b.w[1]:
                deps[b.w[0]] = b.w[1]
            for k, v in b.r.items():
                if deps.get(k, 0) < v:
                    deps[k] = v
        return deps

    def _emit_waits(self, e, deps):
        wt = self.waited[e]
        for k, v in deps.items():
            if e == "pe" and k.startswith("e_pe_"):
                continue
            if wt.get(k, 0) >= v:
                continue
            wt[k] = v
            h = self.sems[k]
            self.q[e].append(lambda eng, h=h, v=v: eng.wait_ge(h, v))

    def _update(self, ev, reads, writes):
        k, v = ev
        for b in reads:
            if b.r.get(k, 0) < v:
                b.r[k] = v
        for b in writes:
            b.w = ev
            b.r = {}

    def I(self, e, meth, _r=None, _w=None, **kw):
        reads, writes = self._collect(kw, _r, _w)
        deps = self._deps(reads, writes)
        self._emit_waits(e, deps)
        cur = self.cur[e]
        if cur[1] >= self.EPOCH:
            self._new_epoch(e)
            cur = self.cur[e]
        cur[1] += 1
        k, v = cur[0], cur[1]
        h = self.sems[k]
        self.q[e].append(lambda eng, meth=meth, kw=kw, h=h: getattr(eng, meth)(**kw).then_inc(h, 1))
        self._update((k, v), reads, writes)
        self.ninst += 1

    def D(self, e, _r=None, _w=None, _meth="dma_start", **kw):
        reads, writes = self._collect(kw, _r, _w)
        deps = self._deps(reads, writes)
        k = self.dma_keys[self.dma_rr % len(self.dma_keys)]
        self.dma_rr += 1
        prev = 16 * self.dma_uses[k]
        if prev:
            deps[k] = max(deps.get(k, 0), prev)
        self._emit_waits(e, deps)
        self.dma_uses[k] += 1
        v = 16 * self.dma_uses[k]
        h = self.sems[k]
        self.q[e].append(lambda eng, meth=_meth, kw=kw, h=h: getattr(eng, meth)(**kw).then_inc(h, 16))
        self._update((k, v), reads, writes)
        self.ninst += 1

    def barrier(self, e, tok_names):
        self.I(e, "nop", _w=list(tok_names))

    def finish(self, final_names):
        reads = [self.bufs[n] for n in final_names]
        deps = self._deps(reads, [])
        for e in self.ENGS:
            self._emit_waits(e, dict(deps))
        nc = self.nc
        with nc.Block() as block:
            @block.tensor
            def _(eng):
                for f in self.q["pe"]:
                    f(eng)

            @block.vector
            def _(eng):
                for f in self.q["dve"]:
                    f(eng)

            @block.scalar
            def _(eng):
                for f in self.q["act"]:
                    f(eng)

            @block.gpsimd
            def _(eng):
                for f in self.q["pool"]:
                    f(eng)

            @block.sync
            def _(eng):
                for f in self.q["sp"]:
                    f(eng)
        for name, cm in reversed(self._ctxs):
            cm.__exit__(None, None, None)
        for cm in reversed(self._sem_ctx):
            cm.__exit__(None, None, None)


class _Scope:
    def __init__(self, fw):
        self.fw = fw

    def __enter__(self):
        self.mark = len(self.fw._ctxs)
        return self

    def __exit__(self, *a):
        fw = self.fw
        fe = fw.free_events
        while len(fw._ctxs) > self.mark:
            name, cm = fw._ctxs.pop()
            b = fw.bufs.pop(name)
            if b.w is not None and fe.get(b.w[0], 0) < b.w[1]:
                fe[b.w[0]] = b.w[1]
            for k, v in b.r.items():
                if fe.get(k, 0) < v:
                    fe[k] = v
            cm.__exit__(None, None, None)
        return False


def pipeline(n, stages):
    ns = len(stages)
    for t in range(n + ns - 1):
        for si, f in enumerate(stages):
            i = t - si
            if 0 <= i < n:
                f(i)


C_ID, C_U, C_ONES, C_L, C_PM, C_MASK, C_PIDX = 0, 128, 256, 384, 512, 640, 896
C_INVF, C_M16, C_1M16, C_SS, C_HALFPI, C_HM0, C_HM1, C_EIDX, C_BLK = 897, 898, 899, 900, 901, 902, 903, 904, 936


def make_consts(nblk):
    w = C_BLK + nblk
    c = np.zeros((128, w), np.float32)
    i = np.arange(128)
    c[:, C_ID:C_ID + 128] = np.eye(128, dtype=np.float32)
    c[:, C_U:C_U + 128] = (i[:, None] <= i[None, :])
    c[:, C_ONES:C_ONES + 128] = 1.0
    c[:, C_L:C_L + 128] = (i[:, None] < i[None, :])
    pm = np.zeros((128, 128), np.float32)
    for dp in range(128):
        if dp % 64 < 16:
            d = (dp // 64) * 64 + ((dp % 64) ^ 8)
            pm[d, dp] = 1.0
    c[:, C_PM:C_PM + 128] = pm
    c[:, C_MASK:C_MASK + 128] = np.where(i[None, :] >= i[:, None], 0.0, NEG)
    c[:, C_MASK + 128:C_MASK + 256] = np.where(i[None, :] <= i[:, None], 0.0, NEG)
    c[:, C_PIDX] = i
    dd = i % 64
    invf = (np.float32(ROPE_THETA) ** (-np.arange(0, 16, 2, dtype=np.float32) / np.float32(16))).astype(np.float32)
    m16 = (dd < 16).astype(np.float32)
    c[:, C_INVF] = np.where(dd < 16, invf[dd % 8], 0.0)
    c[:, C_M16] = m16
    c[:, C_1M16] = 1.0 - m16
    c[:, C_SS] = m16 * np.where(dd < 8, -1.0, 1.0)
    c[:, C_HALFPI] = np.float32(np.pi / 2)
    c[:, C_HM0] = (i < 64)
    c[:, C_HM1] = (i >= 64)
    c[:, C_EIDX:C_EIDX + 32] = np.arange(32)[None, :]
    c[:, C_BLK:C_BLK + nblk] = float(BR) * np.arange(nblk)[None, :]
    return c


class Prog:
    def __init__(self, S, layers=(0, 1, 2, 3), stop_after=None, with_moe=True, moe_layers=(0, 1, 2, 3)):
        self.S = S
        self.with_moe = with_moe
        self.NT = S // 128
        self.layers = tuple(layers)
        self.stop_after = stop_after
        self.NBLK = -(-(2 * S + 32 * (BR - 1)) // BR)
        nc = self.nc = bass.Bass("TRN2", target_bir_lowering=False)
        fw = self.fw = FW(nc)
        S_ = S
        ext = lambda n, s, d=F32: fw.dram(n, s, d, kind="ExternalInput")
        self.x_in = ext("x", [S_, 1024])
        self.pos_in = ext("pos", [1, S_], I32)
        self.consts_in = ext("consts", [128, C_BLK + self.NBLK])
        self.ssd_w_in = ext("ssd_w_in", [2, 1024, 5152])
        self.ssd_conv_w = ext("ssd_conv_w", [2, 4, 3072])
        self.ssd_conv_b = ext("ssd_conv_b", [2, 3072])
        self.ssd_dt_bias = ext("ssd_dt_bias", [2, 32])
        self.ssd_a_log = ext("ssd_a_log", [2, 32])
        self.ssd_d = ext("ssd_d", [2, 32])
        self.ssd_norm_w = ext("ssd_norm_w", [2, 2048])
        self.ssd_w_out = ext("ssd_w_out", [2, 2048, 1024])
        self.attn_w_qkv = ext("attn_w_qkv", [2, 1024, 4608])
        self.attn_w_o = ext("attn_w_o", [2, 512, 1024])
        self.ln_g = ext("ln_g", [4, 2, 1024])
        self.ln_b = ext("ln_b", [4, 2, 1024])
        self.w_router = ext("w_router", [4, 1024, 36])
        self.b_router = ext("b_router", [4, 36])
        self.moe_layers = tuple(moe_layers)
        if with_moe:
            nl = len(self.moe_layers)
            self.moe_gu = ext("moe_gu", [nl, 32 * 128, 8 * 1024])
            self.moe_dn = ext("moe_dn", [nl, 32 * 128, 4 * 1024])
        self.out = fw.dram("out", [S_, 1024], F32, kind="ExternalOutput")
        sc = lambda n, sh, d=F32: fw.dram(n, sh, d, track=False)
        self.XA = sc("XA", [S_, 1024])
        self.XT = [sc("XT0", [8, 128, S_]), sc("XT1", [8, 128, S_])]
        self.BCfm = sc("BCfm", [8, 128, S_])
        self.XSBtm = sc("XSBtm", [S_, 2560])
        self.YN = sc("YN", [S_, 2048])
        self.XROWS = sc("XROWS", [self.NBLK * BR, 1024])
        self.YROWS = sc("YROWS", [self.NBLK * BR, 1024])
        self.AO = sc("AO", [3, S_, 512])
        self.AML = sc("AML", [3, S_, 16])
        self.ROPE = sc("ROPE", [2, 128, S_])
        self.QK = sc("QK", [24, 128, S_])
        self.VP = sc("VP", [3, S_, 512])
        self.xa_tok = [fw.token("xa%d" % t) for t in range(self.NT)]
        self.xt_tok = [fw.token("xt0"), fw.token("xt1")]
        self.tk = {n: fw.token("tk_" + n) for n in ("BCfm", "XSBtm", "YN", "XROWS", "YROWS", "AO", "AML", "ROPE", "QK", "VP")}
        self.cst = fw.T("cst", [128, C_BLK + self.NBLK])
        fw.dummy = fw.T("fwdummy", [128, 4])
        fw.D("sp", out=self.cst[:], in_=self.consts_in[:, :])
        self.eps_ln = fw.T("eps_ln", [128, 2])
        fw.I("dve", "memset", ap=self.eps_ln[:, 0:1], constant=float(LN_EPS))
        fw.I("dve", "memset", ap=self.eps_ln[:, 1:2], constant=1.0)
        self.ident = self.cst[:, C_ID:C_ID + 128]
        self.U = self.cst[:, C_U:C_U + 128]
        self.ones = self.cst[:, C_ONES:C_ONES + 128]

    def bc_load(self, q, dst, src_row):
        self.fw.D(q, out=dst, in_=src_row.partition_broadcast(128))

    def initial(self):
        fw = self.fw
        with fw.scope():
            xt = [fw.T("i_x%d" % i, [128, 1024]) for i in range(2)]
            tp = [fw.P("i_tp%d" % i, [128, 8, 128]) for i in range(2)]
            ts = [fw.T("i_ts%d" % i, [128, 8, 128]) for i in range(2)]
            for t in range(self.NT):
                a = t % 2
                fw.D("sp", out=xt[a][:], in_=self.x_in[t * 128:(t + 1) * 128, :])
                fw.D("act", out=self.XA[t * 128:(t + 1) * 128, :], in_=xt[a][:], _w=[self.xa_tok[t]])
                self.transpose_store(xt[a], tp[a], ts[a], 0, t)

    def transpose_store(self, xtile, tp, ts, XT_dst, t):
        fw = self.fw
        for c in range(8):
            fw.I("pe", "transpose", out=tp[:, c, :], in_=xtile[:, c * 128:(c + 1) * 128], identity=self.ident)
        fw.I("act", "copy", out=ts[:], in_=tp[:])
        fw.D("sp", out=self.XT[XT_dst][:, :, t * 128:(t + 1) * 128].rearrange("k p t -> p k t"), in_=ts[:],
             _r=[self.xt_tok[XT_dst]])

    def proj_ln(self, tag, li, sub, kch, w_src, a_loader, XT_dst):
        fw = self.fw
        with fw.scope():
            w = fw.T(tag + "_w", [128, kch, 1024], F32R)
            for k in range(kch):
                fw.D("pool", out=w[:, k, :], in_=w_src[k * 128:(k + 1) * 128, :])
            gbc = fw.T(tag + "_g", [128, 1024])
            bbc = fw.T(tag + "_b", [128, 1024])
            self.bc_load("sp", gbc[:], self.ln_g[li, sub])
            self.bc_load("sp", bbc[:], self.ln_b[li, sub])
            atp = [fw.P(tag + "_atp%d" % i, [128, 4, 128]) for i in range(2)]
            aT = [fw.T(tag + "_aT%d" % i, [128, kch, 128], F32R) for i in range(2)]
            mp = [fw.P(tag + "_mp%d" % i, [128, 1024]) for i in range(2)]
            ln = self.ln_alloc(tag)

            def stA(t):
                a = t % 2
                A = a_loader(t)
                for k0 in range(0, kch, 4):
                    kn = min(4, kch - k0)
                    for k in range(kn):
                        fw.I("pe", "transpose", out=atp[(k0 // 4) % 2][:, k, :],
                             in_=A[:, (k0 + k) * 128:(k0 + k + 1) * 128], identity=self.ident)
                    fw.I("act" if (k0 // 4) % 2 else "dve", "copy" if (k0 // 4) % 2 else "tensor_copy",
                         out=aT[a][:, k0:k0 + kn, :], in_=atp[(k0 // 4) % 2][:, 0:kn, :])
                for n in range(2):
                    for k in range(kch):
                        fw.I("pe", "matmul", out=mp[a][:, n * 512:(n + 1) * 512], lhsT=aT[a][:, k, :],
                             rhs=w[:, k, n * 512:(n + 1) * 512], start=(k == 0), stop=(k == kch - 1))
            pipeline(self.NT, [stA] + self.ln_stages(ln, lambda t: mp[t % 2][:], gbc, bbc, XT_dst))

    def ln_alloc(self, tag):
        fw = self.fw
        d = {}
        d["x"] = [fw.T(tag + "_lx%d" % i, [128, 1024]) for i in range(4)]
        d["v"] = [fw.T(tag + "_lv%d" % i, [128, 1024]) for i in range(4)]
        d["junk"] = fw.T(tag + "_lj", [128, 1024])
        d["st"] = [fw.T(tag + "_ls%d" % i, [128, 8]) for i in range(4)]
        d["tp"] = fw.P(tag + "_ltp", [128, 8, 128])
        d["ts"] = [fw.T(tag + "_lts%d" % i, [128, 8, 128]) for i in range(2)]
        return d

    def ln_stages(self, ln, mix_of, gbc, bbc, XT_dst):
        fw = self.fw

        def b1(t):
            a = t % 4
            x, v, st, junk = ln["x"][a], ln["v"][a], ln["st"][a], ln["junk"]
            fw.D("sp", out=x[:], in_=self.XA[t * 128:(t + 1) * 128, :], _r=[self.xa_tok[t]])
            fw.I("dve", "scalar_tensor_tensor", out=v[:], in0=x[:], scalar=float(ALPHA), in1=mix_of(t),
                 op0=ALU.mult, op1=ALU.add)
            fw.I("act", "activation", out=junk[:], in_=v[:], func=AF.Identity, accum_out=st[:, 0:1])
            fw.I("act", "activation", out=junk[:], in_=v[:], func=AF.Square, accum_out=st[:, 1:2])

        def b2(t):
            st = ln["st"][t % 4]
            fw.I("dve", "tensor_scalar", out=st[:, 2:3], in0=st[:, 0:1], scalar1=1.0 / 1024, scalar2=None, op0=ALU.mult)
            fw.I("dve", "tensor_tensor", out=st[:, 3:4], in0=st[:, 2:3], in1=st[:, 2:3], op=ALU.mult)
            fw.I("dve", "scalar_tensor_tensor", out=st[:, 4:5], in0=st[:, 1:2], scalar=1.0 / 1024, in1=st[:, 3:4],
                 op0=ALU.mult, op1=ALU.subtract)
            fw.I("act", "activation", out=st[:, 6:7], in_=st[:, 4:5], func=AF.Sqrt, bias=self.eps_ln[:, 0:1])
            fw.I("dve", "reciprocal", out=st[:, 5:6], in_=st[:, 6:7])

        def b3(t):
            a = t % 4
            x, v, st = ln["x"][a], ln["v"][a], ln["st"][a]
            fw.I("dve", "tensor_scalar", out=v[:], in0=v[:], scalar1=st[:, 2:3], scalar2=st[:, 5:6],
                 op0=ALU.subtract, op1=ALU.mult)
            fw.I("dve", "tensor_tensor", out=v[:], in0=v[:], in1=gbc[:], op=ALU.mult)
            fw.I("dve", "tensor_tensor", out=x[:], in0=v[:], in1=bbc[:], op=ALU.add)
            fw.D("act", out=self.XA[t * 128:(t + 1) * 128, :], in_=x[:], _w=[self.xa_tok[t]])

        def c1(t):
            self.transpose_store(ln["x"][t % 4], ln["tp"], ln["ts"][t % 2], XT_dst, t)
        return [b1, b2, b3, c1]

    def ssd_sweep1(self, j, XT_src):
        fw, S = self.fw, self.S
        w_in = self.ssd_w_in
        with fw.scope():
            wx = fw.T("s1_wx", [128, 8, 3072], F32R)
            for k in range(8):
                fw.D("pool", out=wx[:, k, :], in_=w_in[j, k * 128:(k + 1) * 128, 2048:5120])
            cw = fw.T("s1_cw", [128, 4, 24])
            cb = fw.T("s1_cb", [128, 24])
            for k in range(4):
                fw.D("sp", out=cw[:, k, :], in_=self.ssd_conv_w[j, k].rearrange("(t p) -> p t", p=128),
                     allow_slow_non_contiguous=True)
            fw.D("sp", out=cb[:], in_=self.ssd_conv_b[j].rearrange("(t p) -> p t", p=128),
                 allow_slow_non_contiguous=True)
            halo = fw.T("s1_halo", [128, 24, 3])
            fw.I("dve", "memset", ap=halo[:], constant=0.0)
            xtb = [fw.T("s1_xt%d" % i, [128, 8, 512], F32R) for i in range(2)]
            ps = [fw.P("s1_ps%d" % i, [128, 512]) for i in range(2)]
            ub = [fw.T("s1_ub%d" % i, [128, 515]) for i in range(2)]
            acc = [fw.T("s1_acc%d" % i, [128, 512]) for i in range(2)]
            so = [fw.T("s1_so%d" % i, [128, 512]) for i in range(2)]
            tps = [fw.P("s1_tp%d" % i, [128, 4, 128]) for i in range(2)]
            tsb = [fw.T("s1_ts%d" % i, [128, 4, 128]) for i in range(2)]
            def s0(idx):
                tb, ct = divmod(idx, 24)
                a = idx % 2
                xt = xtb[tb % 2]
                if ct == 0:
                    fw.D("pool", out=xt[:], in_=self.XT[XT_src][:, :, tb * 512:(tb + 1) * 512].rearrange("k p t -> p k t"),
                         _r=[self.xt_tok[XT_src]])
                for k in range(8):
                    fw.I("pe", "matmul", out=ps[a][:], lhsT=wx[:, k, ct * 128:(ct + 1) * 128], rhs=xt[:, k, :],
                         start=(k == 0), stop=(k == 7))

            def s1(idx):
                tb, ct = divmod(idx, 24)
                a = idx % 2
                fw.I("pool", "tensor_copy", out=ub[a][:, 0:3], in_=halo[:, ct, :])
                fw.I("act", "copy", out=ub[a][:, 3:515], in_=ps[a][:])
                fw.I("pool", "tensor_copy", out=halo[:, ct, :], in_=ub[a][:, 512:515])
                fw.I("act", "activation", out=acc[a][:], in_=ub[a][:, 3:515], func=AF.Identity, scale=cw[:, 3, ct:ct + 1],
                     bias=cb[:, ct:ct + 1])
                for kk in (2, 1, 0):
                    fw.I("dve", "scalar_tensor_tensor", out=acc[a][:], in0=ub[a][:, kk:kk + 512], scalar=cw[:, kk, ct:ct + 1],
                         in1=acc[a][:], op0=ALU.mult, op1=ALU.add)
                fw.I("act", "activation", out=so[a][:], in_=acc[a][:], func=AF.Silu)
                if ct >= 16:
                    fw.D("sp", out=self.BCfm[ct - 16, :, tb * 512:(tb + 1) * 512], in_=so[a][:], _r=[self.tk["BCfm"]])

            def s2(idx):
                tb, ct = divmod(idx, 24)
                a = idx % 2
                if ct < 20:
                    for q in range(4):
                        fw.I("pe", "transpose", out=tps[a][:, q, :], in_=so[a][:, q * 128:(q + 1) * 128],
                             identity=self.ident)
                    if idx % 2:
                        fw.I("act", "copy", out=tsb[a][:], in_=tps[a][:])
                    else:
                        fw.I("dve", "tensor_copy", out=tsb[a][:], in_=tps[a][:])
                    fw.D("sp", out=self.XSBtm[tb * 512:(tb + 1) * 512, ct * 128:(ct + 1) * 128]
                         .rearrange("(q p) c -> p q c", p=128), in_=tsb[a][:], _r=[self.tk["XSBtm"]])
            pipeline((S // 512) * 24, [s0, s1, s2])

    def ssd_sweep2(self, j, XT_src):
        fw, S = self.fw, self.S
        w_in = self.ssd_w_in
        with fw.scope():
            wz = fw.T("s2_wz", [128, 8, 2048], F32R)
            wdt = fw.T("s2_wdt", [128, 8, 32], F32R)
            for k in range(8):
                fw.D("pool", out=wz[:, k, :], in_=w_in[j, k * 128:(k + 1) * 128, 0:2048])
                fw.D("pool", out=wdt[:, k, :], in_=w_in[j, k * 128:(k + 1) * 128, 5120:5152])
            dtb = fw.T("s2_dtb", [128, 32])
            abc = fw.T("s2_a", [128, 32])
            dsk = fw.T("s2_dsk", [128, 32])
            nw = fw.T("s2_nw", [128, 2048])
            self.bc_load("sp", dtb[:], self.ssd_dt_bias[j])
            self.bc_load("sp", abc[:], self.ssd_a_log[j])
            self.bc_load("sp", dsk[:], self.ssd_d[j])
            self.bc_load("sp", nw[:], self.ssd_norm_w[j])
            fw.I("act", "activation", out=abc[:], in_=abc[:], func=AF.Exp)
            fw.I("dve", "tensor_scalar", out=abc[:], in0=abc[:], scalar1=-1.0, scalar2=None, op0=ALU.mult)
            prev = fw.T("s2_prev", [128, 4, 512])
            prevR = fw.T("s2_prevR", [128, 4, 512], F32R)
            fw.I("dve", "memset", ap=prev[:], constant=0.0)
            fw.I("dve", "tensor_copy", out=prevR[:], in_=prev[:])
            xTc = [fw.T("s2_xT%d" % i, [128, 8, 128], F32R) for i in range(2)]
            xs = [fw.T("s2_xs%d" % i, [128, 32, 64]) for i in range(2)]
            Btm = [fw.T("s2_Btm%d" % i, [128, 4, 128], F32R) for i in range(2)]
            Bfm = [fw.T("s2_Bfm%d" % i, [128, 4, 128], F32R) for i in range(2)]
            Cfm = [fw.T("s2_Cfm%d" % i, [128, 4, 128], F32R) for i in range(2)]
            sm = fw.T("s2_sm", [128, 12, 32])
            Uda = [fw.T("s2_Uda%d" % i, [128, 8, 128]) for i in range(2)]
            xr = fw.T("s2_xr", [128, 32, 64], F32R)
            xrd = fw.T("s2_xrd", [128, 32, 64], F32R)
            zs = [fw.T("s2_zs%d" % i, [128, 512]) for i in range(2)]
            cbm = [fw.T("s2_cbm%d" % i, [128, 128]) for i in range(2)]
            Dm = [fw.T("s2_D%d" % i, [128, 8, 128]) for i in range(2)]
            MT = [fw.T("s2_MT%d" % i, [128, 8, 128], F32R) for i in range(2)]
            t1 = [fw.T("s2_t1%d" % i, [128, 8, 64]) for i in range(2)]
            t2 = [fw.T("s2_t2%d" % i, [128, 8, 64]) for i in range(2)]
            yn = [fw.T("s2_yn0", [128, 2048])] * 2
            junk = fw.T("s2_junk", [128, 512])
            rs = fw.T("s2_rs", [128, 8])
            p_z = fw.P("s2_pz", [128, 512])
            p_sm = fw.P("s2_psm", [128, 512])
            p_R = fw.P("s2_pR", [128, 8, 128])
            p_Y = fw.P("s2_pY", [128, 8, 64])
            p_Yo = fw.P("s2_pYo", [128, 8, 64])
            p_S = fw.P("s2_pS", [128, 512])
            DT, DA, CS, ECS, DTE, CD, V0, V1, V2 = range(9)
            p_Y2 = [p_Y, fw.P("s2_pY2", [128, 8, 64])]

            def front(u):
                c, g = divmod(u, 4)
                a = c % 2
                tok = slice(c * 128, (c + 1) * 128)
                p_Y = p_Y2[u % 2]
                if g == 0:
                    fw.D("pool", out=xTc[a][:], in_=self.XT[XT_src][:, :, tok].rearrange("k p t -> p k t"),
                         _r=[self.xt_tok[XT_src]])
                    fw.D("sp", out=xs[a][:].rearrange("p h d -> p (h d)"), in_=self.XSBtm[tok, 0:2048], _r=[self.tk["XSBtm"]])
                    fw.D("pool", out=Btm[a][:].rearrange("p g n -> p (g n)"), in_=self.XSBtm[tok, 2048:2560],
                         _r=[self.tk["XSBtm"]])
                    fw.D("pool", out=Bfm[a][:], in_=self.BCfm[0:4, :, tok].rearrange("g p t -> p g t"), _r=[self.tk["BCfm"]])
                    fw.D("pool", out=Cfm[a][:], in_=self.BCfm[4:8, :, tok].rearrange("g p t -> p g t"), _r=[self.tk["BCfm"]])
                    for k in range(8):
                        fw.I("pe", "matmul", out=p_sm[:, 0:32], lhsT=xTc[a][:, k, :], rhs=wdt[:, k, :],
                             start=(k == 0), stop=(k == 7))
                    fw.I("dve", "tensor_tensor", out=sm[:, V0, :], in0=p_sm[:, 0:32], in1=dtb[:], op=ALU.add)
                    fw.I("dve", "scalar_tensor_tensor", out=sm[:, V1, :], in0=sm[:, V0, :], scalar=-1.0, in1=sm[:, V0, :],
                         op0=ALU.mult, op1=ALU.max)
                    fw.I("act", "activation", out=sm[:, V1, :], in_=sm[:, V1, :], func=AF.Exp, scale=-1.0)
                    fw.I("act", "activation", out=sm[:, V1, :], in_=sm[:, V1, :], func=AF.Ln, bias=self.eps_ln[:, 1:2])
                    fw.I("dve", "scalar_tensor_tensor", out=sm[:, DT, :], in0=sm[:, V0, :], scalar=0.0, in1=sm[:, V1, :],
                         op0=ALU.max, op1=ALU.add)
                    fw.I("dve", "tensor_tensor", out=sm[:, DA, :], in0=sm[:, DT, :], in1=abc[:], op=ALU.mult)
                    fw.I("pe", "matmul", out=p_sm[:, 32:64], lhsT=self.U, rhs=sm[:, DA, :], start=True, stop=True)
                    fw.I("act", "copy", out=sm[:, CS, :], in_=p_sm[:, 32:64])
                    fw.I("act", "activation", out=sm[:, ECS, :], in_=sm[:, CS, :], func=AF.Exp)
                    fw.I("pool", "tensor_tensor", out=xr[:], in0=xs[a][:],
                         in1=sm[:, DT, :].unsqueeze(2).to_broadcast([128, 32, 64]), op=ALU.mult)

                b2 = g % 2
                hs = slice(8 * g, 8 * g + 8)
                for k in range(8):
                    fw.I("pe", "matmul", out=p_z[:], lhsT=xTc[a][:, k, :], rhs=wz[:, k, g * 512:(g + 1) * 512],
                         start=(k == 0), stop=(k == 7))
                fw.I("act", "activation", out=zs[b2][:], in_=p_z[:], func=AF.Silu)
                fw.I("dve", "tensor_tensor", out=Uda[b2][:], in0=self.U.unsqueeze(1).to_broadcast([128, 8, 128]),
                     in1=sm[:, DA, hs].unsqueeze(2).to_broadcast([128, 8, 128]), op=ALU.mult)
                for hh in range(2):
                    fw.I("pe", "matmul", out=p_R[:, 4 * hh:4 * hh + 4, :], lhsT=self.ones,
                         rhs=Uda[b2][:, 4 * hh:4 * hh + 4, :], start=True, stop=True)
                fw.I("dve", "tensor_tensor", out=sm[:, V2, hs], in0=p_R[:, :, 127], in1=sm[:, CS, hs], op=ALU.subtract)
                fw.I("act", "activation", out=sm[:, DTE, hs], in_=sm[:, V2, hs], func=AF.Exp)
                fw.I("act", "activation", out=sm[:, CD, hs], in_=p_R[:, :, 127], func=AF.Exp)
                fw.I("pool", "tensor_tensor", out=xrd[:, hs, :], in0=xr[:, hs, :],
                     in1=sm[:, DTE, hs].unsqueeze(2).to_broadcast([128, 8, 64]), op=ALU.mult)
                fw.I("pe", "matmul", out=p_sm[:, 128:256], lhsT=Bfm[a][:, g, :], rhs=Cfm[a][:, g, :],
                     start=True, stop=True)
                fw.I("dve", "tensor_tensor", out=cbm[b2][:], in0=p_sm[:, 128:256], in1=self.U, op=ALU.mult)
                fw.I("dve", "tensor_tensor", out=Dm[b2][:], in0=p_R[:],
                     in1=sm[:, CS, hs].unsqueeze(2).to_broadcast([128, 8, 128]), op=ALU.subtract)
                fw.I("act", "activation", out=Dm[b2][:], in_=Dm[b2][:], func=AF.Exp)
                fw.I("dve", "scalar_tensor_tensor", out=MT[b2][:], in0=Dm[b2][:], scalar=1.0,
                     in1=cbm[b2][:].unsqueeze(1).to_broadcast([128, 8, 128]), op0=ALU.min, op1=ALU.mult)
                for h in range(8):
                    fw.I("pe", "matmul", out=p_Y[:, h, :], lhsT=MT[b2][:, h, :], rhs=xr[:, 8 * g + h, :],
                         start=True, stop=True)
                fw.I("pe", "matmul", out=p_Yo[:].rearrange("p h d -> p (h d)"), lhsT=Cfm[a][:, g, :],
                     rhs=prevR[:, g, :], start=True, stop=True)
                fw.I("pe", "matmul", out=p_S[:], lhsT=Btm[a][:, g, :],
                     rhs=xrd[:, hs, :].rearrange("p h d -> p (h d)"), start=True, stop=True)

                fw.I("dve", "tensor_tensor", out=t1[b2][:], in0=p_Yo[:],
                     in1=sm[:, ECS, hs].unsqueeze(2).to_broadcast([128, 8, 64]), op=ALU.mult)

                fw.I("pool", "tensor_tensor", out=prev[:, g, :].rearrange("p (h d) -> p h d", h=8),
                     in0=prev[:, g, :].rearrange("p (h d) -> p h d", h=8),
                     in1=sm[:, CD, hs].unsqueeze(2).to_broadcast([128, 8, 64]), op=ALU.mult)
                fw.I("dve", "tensor_tensor", out=prev[:, g, :], in0=prev[:, g, :], in1=p_S[:], op=ALU.add)
                fw.I("act", "copy", out=prevR[:, g, :], in_=prev[:, g, :])


            def tailf(u):
                c, g = divmod(u, 4)
                a = c % 2
                tok = slice(c * 128, (c + 1) * 128)
                p_Y = p_Y2[u % 2]
                b2 = g % 2
                hs = slice(8 * g, 8 * g + 8)
                fw.I("dve", "tensor_tensor", out=t1[b2][:], in0=t1[b2][:], in1=p_Y[:], op=ALU.add)
                fw.I("pool", "tensor_tensor", out=t2[b2][:], in0=xs[a][:, hs, :],
                     in1=dsk[:, hs].unsqueeze(2).to_broadcast([128, 8, 64]), op=ALU.mult)
                fw.I("pool", "tensor_tensor", out=t1[b2][:], in0=t1[b2][:], in1=t2[b2][:], op=ALU.add)
                fw.I("dve", "tensor_tensor", out=t1[b2][:].rearrange("p h d -> p (h d)"),
                     in0=t1[b2][:].rearrange("p h d -> p (h d)"), in1=zs[b2][:], op=ALU.mult)
                fw.I("act", "activation", out=junk[:], in_=t1[b2][:].rearrange("p h d -> p (h d)"),
                     func=AF.Square, accum_out=rs[:, g:g + 1])
                fw.I("dve", "tensor_scalar", out=rs[:, 4 + g:5 + g], in0=rs[:, g:g + 1], scalar1=1.0 / 512,
                     scalar2=float(RMS_EPS), op0=ALU.mult, op1=ALU.add)
                fw.I("act", "activation", out=rs[:, 4 + g:5 + g], in_=rs[:, 4 + g:5 + g], func=AF.Sqrt)
                fw.I("dve", "reciprocal", out=rs[:, 4 + g:5 + g], in_=rs[:, 4 + g:5 + g])
                fw.I("dve", "scalar_tensor_tensor", out=yn[a][:, g * 512:(g + 1) * 512],
                     in0=t1[b2][:].rearrange("p h d -> p (h d)"), scalar=rs[:, 4 + g:5 + g],
                     in1=nw[:, g * 512:(g + 1) * 512], op0=ALU.mult, op1=ALU.mult)

                if g == 3:
                    fw.D("sp", out=self.YN[tok, :], in_=yn[a][:], _r=[self.tk["YN"]])


            pipeline((S // 128) * 4, [front, tailf])

    def ssd_layer(self, li, XT_src, XT_dst):
        j = li // 2
        fw = self.fw
        fw.barrier("sp", [self.xt_tok[0], self.xt_tok[1], self.tk["BCfm"], self.tk["XSBtm"], self.tk["YN"]])
        self.ssd_sweep1(j, XT_src)
        fw.barrier("sp", [self.tk["BCfm"], self.tk["XSBtm"]])
        self.ssd_sweep2(j, XT_src)
        fw.barrier("sp", [self.tk["YN"]])
        with fw.scope():
            ab = [fw.T("s3_a%d" % i, [128, 2048]) for i in range(2)]

            def loader(t):
                fw.D("sp", out=ab[t % 2][:], in_=self.YN[t * 128:(t + 1) * 128, :], _r=[self.tk["YN"]])
                return ab[t % 2]
            self.proj_ln("s3", li, 0, 16, self.ssd_w_out[j], loader, XT_dst)

    def rope_tables(self):
        fw, S, cst = self.fw, self.S, self.cst
        CW = min(1024, S)
        TWO_PI = float(2 * np.pi)
        C1 = 6.28125
        C2 = float(2 * np.pi - 6.28125)
        with fw.scope():
            posi = fw.T("r_posi", [128, CW], I32)
            ang = fw.T("r_ang", [128, CW])
            kq = fw.T("r_kq", [128, CW])
            ki = fw.T("r_ki", [128, CW], I32)
            r = fw.T("r_r", [128, CW])
            m = fw.T("r_m", [128, CW])
            sv = fw.T("r_sv", [128, CW])
            cv = fw.T("r_cv", [128, CW])
            col = lambda c: cst[:, c:c + 1]
            for c0 in range(0, S, CW):
                fw.D("sp", out=posi[:], in_=self.pos_in[0, c0:c0 + CW].partition_broadcast(128))
                fw.I("dve", "tensor_copy", out=ang[:], in_=posi[:])
                fw.I("dve", "tensor_scalar", out=ang[:], in0=ang[:], scalar1=col(C_INVF), scalar2=None, op0=ALU.mult)
                fw.I("dve", "tensor_scalar", out=kq[:], in0=ang[:], scalar1=float(1.0 / TWO_PI), scalar2=None, op0=ALU.mult)
                fw.I("dve", "tensor_copy", out=ki[:], in_=kq[:])
                fw.I("dve", "tensor_copy", out=kq[:], in_=ki[:])
                fw.I("dve", "scalar_tensor_tensor", out=r[:], in0=kq[:], scalar=-C1, in1=ang[:], op0=ALU.mult, op1=ALU.add)
                fw.I("dve", "scalar_tensor_tensor", out=r[:], in0=kq[:], scalar=-C2, in1=r[:], op0=ALU.mult, op1=ALU.add)
                fw.I("dve", "tensor_scalar", out=m[:], in0=r[:], scalar1=float(np.pi), scalar2=None, op0=ALU.is_gt)
                fw.I("dve", "scalar_tensor_tensor", out=r[:], in0=m[:], scalar=-TWO_PI, in1=r[:], op0=ALU.mult, op1=ALU.add)
                fw.I("dve", "tensor_scalar", out=m[:], in0=r[:], scalar1=float(-np.pi), scalar2=None, op0=ALU.is_lt)
                fw.I("dve", "scalar_tensor_tensor", out=r[:], in0=m[:], scalar=TWO_PI, in1=r[:], op0=ALU.mult, op1=ALU.add)
                fw.I("dve", "tensor_scalar", out=r[:], in0=r[:], scalar1=3.1415925, scalar2=-3.1415925, op0=ALU.min, op1=ALU.max)
                fw.I("act", "activation", out=sv[:], in_=r[:], func=AF.Sin)
                fw.I("dve", "scalar_tensor_tensor", out=m[:], in0=r[:], scalar=-1.0, in1=r[:], op0=ALU.mult, op1=ALU.max)
                fw.I("act", "activation", out=cv[:], in_=m[:], func=AF.Sin, scale=-1.0, bias=col(C_HALFPI))
                fw.I("dve", "tensor_scalar", out=cv[:], in0=cv[:], scalar1=col(C_M16), scalar2=col(C_1M16), op0=ALU.mult, op1=ALU.add)
                fw.I("dve", "tensor_scalar", out=sv[:], in0=sv[:], scalar1=col(C_SS), scalar2=None, op0=ALU.mult)
                fw.D("sp", out=self.ROPE[0, :, c0:c0 + CW], in_=cv[:], _r=[self.tk["ROPE"]])
                fw.D("sp", out=self.ROPE[1, :, c0:c0 + CW], in_=sv[:], _r=[self.tk["ROPE"]])
        fw.barrier("sp", [self.tk["ROPE"]])

    def attn_proj(self, j, XT_src):
        fw, S, cst = self.fw, self.S, self.cst
        CH = min(2048, S)
        with fw.scope():
            PmR = fw.T("a1_pm", [128, 128], F32R)
            fw.I("dve", "tensor_copy", out=PmR[:], in_=cst[:, C_PM:C_PM + 128])
            w = fw.T("a1_w", [128, 8, 1536], F32R)
            xch = fw.T("a1_x", [128, 8, CH], F32R)
            tchs = [fw.T("a1_t%d" % i, [128, 2, CH]) for i in range(2)]
            ps = [fw.P("a1_ps%d" % i, [128, 512]) for i in range(2)]
            pp = [fw.P("a1_pp%d" % i, [128, 512]) for i in range(2)]
            qsb = [fw.T("a1_q%d" % i, [128, 512], F32R) for i in range(2)]
            t1 = [fw.T("a1_t1%d" % i, [128, 512]) for i in range(2)]
            t2 = [fw.T("a1_t2%d" % i, [128, 512]) for i in range(2)]
            ob = [fw.T("a1_o%d" % i, [128, 512]) for i in range(2)]
            vb = [fw.T("a1_v%d" % i, [128, 512]) for i in range(2)]
            work = []
            chidx = 0
            for g in range(3):
                for ch in range(S // CH):
                    first = True
                    for sb in range(CH // 512):
                        for which in range(2):
                            for tl in range(4):
                                work.append(("qk", g, ch, chidx, first, sb, which, tl))
                                first = False
                    for bi in range(CH // 128):
                        work.append(("v", g, ch, chidx, False, bi, 0, 0))
                    chidx += 1

            def geom(g):
                d = ATT_PATTERNS[g][1]
                return d, S // d, CH // d

            def perm(v3, c0, width, d, ic):
                vv = v3.rearrange("p (i r) -> p r i", r=d)
                if ic >= width:
                    return vv[:, c0 // ic, (c0 % ic):(c0 % ic) + width]
                return vv[:, c0 // ic:c0 // ic + width // ic, :]

            def s0(i):
                kind, g, ch, cx, first, p1, which, tl = work[i]
                d, n, ic = geom(g)
                a = i % 2
                if first:
                    if ch == 0:
                        for k in range(8):
                            fw.D("pool", out=w[:, k, :], in_=self.attn_w_qkv[j, k * 128:(k + 1) * 128, g * 1536:(g + 1) * 1536])
                    fw.D("pool", out=xch[:], in_=self.XT[XT_src][:, :, ch * CH:(ch + 1) * CH].rearrange("k p t -> p k t"),
                         _r=[self.xt_tok[XT_src]])
                    fw.D("sp", out=tchs[cx % 2][:], in_=self.ROPE[:, :, ch * CH:(ch + 1) * CH].rearrange("c p t -> p c t"),
                         _r=[self.tk["ROPE"]])
                if kind == "qk":
                    wc = which * 512 + tl * 128
                    for k in range(8):
                        fw.I("pe", "matmul", out=ps[a][:], lhsT=w[:, k, wc:wc + 128], rhs=perm(xch[:, k, :], p1 * 512, 512, d, ic),
                             start=(k == 0), stop=(k == 7))
                else:
                    for k in range(8):
                        fw.I("pe", "matmul", out=ps[a][:], lhsT=perm(xch[:, k, :], p1 * 128, 128, d, ic), rhs=w[:, k, 1024:1536],
                             start=(k == 0), stop=(k == 7))

            def s1(i):
                kind = work[i][0]
                a = i % 2
                if kind == "qk":
                    fw.I("act", "copy", out=qsb[a][:], in_=ps[a][:])
                    fw.I("pe", "matmul", out=pp[a][:], lhsT=PmR[:], rhs=qsb[a][:], start=True, stop=True)
                else:
                    fw.I("act" if (i // 2) % 2 else "dve", "copy" if (i // 2) % 2 else "tensor_copy", out=vb[a][:], in_=ps[a][:])

            def s2(i):
                kind, g, ch, cx, first, p1, which, tl = work[i]
                d, n, ic = geom(g)
                a = i % 2
                if kind == "qk":
                    sb = p1
                    c0 = sb * 512
                    tch = tchs[cx % 2]
                    cview = perm(tch[:, 0, :], c0, 512, d, ic)
                    sview = perm(tch[:, 1, :], c0, 512, d, ic)
                    shp = list(cview.shape)
                    rs = (lambda t_: t_[:]) if len(shp) == 2 else (lambda t_: t_[:].rearrange("p (r i) -> p r i", r=shp[1]))
                    fw.I("dve", "tensor_tensor", out=rs(t1[a]), in0=rs(qsb[a]), in1=cview, op=ALU.mult)
                    fw.I("dve", "tensor_tensor", out=rs(t2[a]), in0=rs(pp[a]), in1=sview, op=ALU.mult)
                    fw.I("pool", "tensor_tensor", out=ob[a][:], in0=t1[a][:], in1=t2[a][:], op=ALU.add)
                    ct = (g * 2 + which) * 4 + tl
                    if ic >= 512:
                        r_ = c0 // ic
                        i0 = c0 % ic
                        dst = self.QK[ct, :, r_ * n + ch * ic + i0:r_ * n + ch * ic + i0 + 512]
                        src = ob[a][:]
                    else:
                        nr = 512 // ic
                        dst = self.QK[ct].rearrange("p (r n) -> p r n", r=d)[:, sb * nr:(sb + 1) * nr, ch * ic:(ch + 1) * ic]
                        src = ob[a][:].rearrange("p (r i) -> p r i", r=nr)
                    fw.D("sp", out=dst, in_=src, _r=[self.tk["QK"]])
                else:
                    c0 = p1 * 128
                    r_ = c0 // ic
                    i0 = c0 % ic
                    row0 = r_ * n + ch * ic + i0
                    fw.D("sp", out=self.VP[g, row0:row0 + 128, :], in_=vb[a][:], _r=[self.tk["VP"]])
            pipeline(len(work), [s0, s1, s2])

    def attn_core(self):
        fw, S, cst = self.fw, self.S, self.cst
        NT = self.NT
        mask = cst[:, C_MASK:C_MASK + 256]
        with fw.scope():
            QT = [fw.T("a2_q%d" % i, [128, 4, 128], F32R) for i in range(2)]
            QZ = [fw.T("a2_qz%d" % i, [128, 4, 2, 128], F32R) for i in range(2)]
            KT = [fw.T("a2_k%d" % i, [128, 4, 128], F32R) for i in range(4)]
            V = [fw.T("a2_v%d" % i, [128, 512], F32R) for i in range(4)]
            sc = [fw.T("a2_sc%d" % i, [128, 4, 256]) for i in range(2)]
            PT = [fw.T("a2_pt%d" % i, [128, 8, 128], F32R) for i in range(2)]
            st = [fw.T("a2_st%d" % i, [128, 4, 8]) for i in range(2)]
            oh = [fw.T("a2_oh%d" % i, [128, 8, 64]) for i in range(2)]
            ml = [fw.T("a2_ml%d" % i, [128, 16]) for i in range(2)]
            p_S = [fw.P("a2_pS%d" % i, [128, 4, 256]) for i in range(2)]
            p_T = fw.P("a2_pT", [128, 8, 128])
            p_O = fw.P("a2_pO", [128, 8, 64])

            def info(u):
                pbi, h4 = divmod(u, 2)
                g, pb = divmod(pbi, NT)
                d = ATT_PATTERNS[g][1]
                n = S // d
                nbr = n // 128
                has_prev = (pb % nbr) > 0
                return pbi, h4, g, pb, d, n, has_prev

            def aA(u):
                pbi, h4, g, pb, d, n, has_prev = info(u)
                a, b2 = pbi % 2, u % 2
                if h4 == 0:
                    cols = slice(pb * 128, (pb + 1) * 128)
                    qbase = (g * 2) * 4
                    fw.D("pool", out=QT[a][:], in_=self.QK[qbase:qbase + 4, :, cols].rearrange("t p c -> p t c"),
                         _r=[self.tk["QK"]])
                    fw.D("pool", out=KT[pbi % 4][:], in_=self.QK[qbase + 4:qbase + 8, :, cols].rearrange("t p c -> p t c"),
                         _r=[self.tk["QK"]])
                    fw.D("pool", out=V[pbi % 4][:], in_=self.VP[g, cols, :], _r=[self.tk["VP"]])
                    for hl in range(2):
                        fw.I("act", "activation", out=QZ[a][:, :, hl, :], in_=QT[a][:], func=AF.Copy,
                             scale=cst[:, C_HM0 + hl:C_HM0 + hl + 1])
                for hh in range(4):
                    h = h4 * 4 + hh
                    tl = h // 2
                    if has_prev:
                        fw.I("pe", "matmul", out=p_S[b2][:, hh, 0:128], lhsT=QZ[a][:, tl, h % 2, :],
                             rhs=KT[(pbi - 1) % 4][:, tl, :], start=True, stop=True)
                    fw.I("pe", "matmul", out=p_S[b2][:, hh, 128:256], lhsT=QZ[a][:, tl, h % 2, :],
                         rhs=KT[pbi % 4][:, tl, :], start=True, stop=True)

            def aB(u):
                pbi, h4, g, pb, d, n, has_prev = info(u)
                a, b2 = pbi % 2, u % 2
                k0 = 0 if has_prev else 128
                fw.I("dve", "scalar_tensor_tensor", out=sc[b2][:, :, k0:256], in0=p_S[b2][:, :, k0:256], scalar=0.125,
                     in1=mask[:, k0:256].unsqueeze(1).to_broadcast([128, 4, 256 - k0]), op0=ALU.mult, op1=ALU.add)
                mx = st[a][:, 0, h4 * 4:h4 * 4 + 4]
                nmx = st[a][:, 1, h4 * 4:h4 * 4 + 4]
                fw.I("dve", "tensor_reduce", out=mx, in_=sc[b2][:, :, k0:256], axis=AX.X, op=ALU.max)
                fw.I("dve", "tensor_scalar", out=nmx, in0=mx, scalar1=-1.0, scalar2=None, op0=ALU.mult)
                for hh in range(4):
                    h = h4 * 4 + hh
                    fw.I("act", "activation", out=sc[b2][:, hh, k0:256], in_=sc[b2][:, hh, k0:256], func=AF.Exp,
                         bias=st[a][:, 1, h:h + 1], accum_out=st[a][:, 2, h:h + 1])

            def aC(u):
                pbi, h4, g, pb, d, n, has_prev = info(u)
                b2 = u % 2
                k0 = 0 if has_prev else 128
                nkc = 2 if has_prev else 1
                for hh in range(4):
                    for c in range(nkc):
                        kc = k0 + c * 128
                        fw.I("pe", "transpose", out=p_T[:, hh * 2 + c, :], in_=sc[b2][:, hh, kc:kc + 128],
                             identity=self.ident)
                eng, meth = ("act", "copy") if h4 else ("dve", "tensor_copy")
                if has_prev:
                    fw.I(eng, meth, out=PT[b2][:], in_=p_T[:])
                else:
                    fw.I(eng, meth, out=PT[b2][:].rearrange("p (h c) q -> p h c q", c=2)[:, :, 0, :],
                         in_=p_T[:].rearrange("p (h c) q -> p h c q", c=2)[:, :, 0, :])

            def aD(u):
                pbi, h4, g, pb, d, n, has_prev = info(u)
                a, b2 = pbi % 2, u % 2
                nkc = 2 if has_prev else 1
                for hh in range(4):
                    h = h4 * 4 + hh
                    for c in range(nkc):
                        vsrc = V[(pbi - 1) % 4] if (has_prev and c == 0) else V[pbi % 4]
                        fw.I("pe", "matmul", out=p_O[:, h, :], lhsT=PT[b2][:, hh * 2 + c, :], rhs=vsrc[:, h * 64:(h + 1) * 64],
                             start=(c == 0), stop=(c == nkc - 1))
                if h4 == 1:
                    fw.I("dve", "reciprocal", out=st[a][:, 3, :], in_=st[a][:, 2, :])
                    fw.I("dve", "tensor_tensor", out=oh[a][:], in0=p_O[:],
                         in1=st[a][:, 3, :].unsqueeze(2).to_broadcast([128, 8, 64]), op=ALU.mult)
                    fw.I("pool", "tensor_copy", out=ml[a][:, 0:8], in_=st[a][:, 0, :])
                    fw.I("pool", "tensor_copy", out=ml[a][:, 8:16], in_=st[a][:, 2, :])
                    r_ = (pb * 128) // n
                    i0 = (pb * 128) % n
                    ao_dst = self.AO[g].rearrange("(i r) c -> r i c", r=d)[r_, i0:i0 + 128, :]
                    ml_dst = self.AML[g].rearrange("(i r) c -> r i c", r=d)[r_, i0:i0 + 128, :]
                    fw.D("sp", out=ao_dst, in_=oh[a][:].rearrange("p h e -> p (h e)"), _r=[self.tk["AO"]])
                    fw.D("sp", out=ml_dst, in_=ml[a][:], _r=[self.tk["AML"]])
            pipeline(3 * NT * 2, [aA, aB, aC, aD])

    def attn_layer(self, li, XT_src, XT_dst):
        fw = self.fw
        j = li // 2
        fw.barrier("sp", [self.xt_tok[0], self.xt_tok[1], self.tk["QK"], self.tk["VP"], self.tk["AO"], self.tk["AML"]])
        self.attn_proj(j, XT_src)
        fw.barrier("sp", [self.tk["QK"], self.tk["VP"]])
        self.attn_core()
        fw.barrier("sp", [self.tk["AO"], self.tk["AML"]])
        with fw.scope():
            ao = [fw.T("a3_ao%d" % i, [128, 3, 512]) for i in range(2)]
            am = [fw.T("a3_ml%d" % i, [128, 3, 16]) for i in range(2)]
            sm = [fw.T("a3_sm%d" % i, [128, 4, 24]) for i in range(2)]
            mg = [fw.T("a3_mg%d" % i, [128, 512]) for i in range(2)]
            tmp = fw.T("a3_tmp", [128, 512])

            def loader(t):
                a = t % 2
                rows = slice(t * 128, (t + 1) * 128)
                fw.D("sp", out=ao[a][:], in_=self.AO[:, rows, :].rearrange("g p c -> p g c"), _r=[self.tk["AO"]])
                fw.D("sp", out=am[a][:], in_=self.AML[:, rows, :].rearrange("g p c -> p g c"), _r=[self.tk["AML"]])
                M = sm[a][:, 0, 0:8]
                fw.I("dve", "tensor_tensor", out=M, in0=am[a][:, 0, 0:8], in1=am[a][:, 1, 0:8], op=ALU.max)
                fw.I("dve", "tensor_tensor", out=M, in0=M, in1=am[a][:, 2, 0:8], op=ALU.max)
                wg = sm[a][:, 1, :].rearrange("p (g h) -> p g h", g=3)
                fw.I("dve", "tensor_tensor", out=wg, in0=am[a][:, :, 0:8], in1=M.unsqueeze(1).to_broadcast([128, 3, 8]),
                     op=ALU.subtract)
                fw.I("act", "activation", out=sm[a][:, 1, :], in_=sm[a][:, 1, :], func=AF.Exp)
                fw.I("dve", "tensor_tensor", out=wg, in0=wg, in1=am[a][:, :, 8:16], op=ALU.mult)
                den = sm[a][:, 2, 0:8]
                fw.I("dve", "tensor_tensor", out=den, in0=sm[a][:, 1, 0:8], in1=sm[a][:, 1, 8:16], op=ALU.add)
                fw.I("dve", "tensor_tensor", out=den, in0=den, in1=sm[a][:, 1, 16:24], op=ALU.add)
                fw.I("dve", "reciprocal", out=sm[a][:, 2, 8:16], in_=den)
                fw.I("dve", "tensor_tensor", out=wg, in0=wg, in1=sm[a][:, 2, 8:16].unsqueeze(1).to_broadcast([128, 3, 8]),
                     op=ALU.mult)
                v3 = lambda t_: t_.rearrange("p (h e) -> p h e", h=8)
                bc = lambda gi: sm[a][:, 1, gi * 8:(gi + 1) * 8].unsqueeze(2).to_broadcast([128, 8, 64])
                fw.I("dve", "tensor_tensor", out=v3(mg[a][:]), in0=v3(ao[a][:, 0, :]), in1=bc(0), op=ALU.mult)
                for gi in (1, 2):
                    fw.I("pool", "tensor_tensor", out=v3(tmp[:]), in0=v3(ao[a][:, gi, :]), in1=bc(gi), op=ALU.mult)
                    fw.I("dve", "tensor_tensor", out=mg[a][:], in0=mg[a][:], in1=tmp[:], op=ALU.add)
                return mg[a]
            self.proj_ln("a3", li, 0, 4, self.attn_w_o[j], loader, XT_dst)

    def zero_xrows(self):
        fw = self.fw
        with fw.scope():
            z = fw.T("z_zero", [128, 4, 1024])
            fw.I("dve", "memset", ap=z[:], constant=0.0)
            nq = self.NBLK * RT
            for q0 in range(0, nq, 4):
                qn = min(4, nq - q0)
                fw.D("act", out=self.XROWS[q0 * 128:(q0 + qn) * 128, :].rearrange("(q p) c -> p q c", p=128),
                     in_=z[:, 0:qn, :], _r=[self.tk["XROWS"]])

    def moe_layer(self, li, XT_src, XT_dst, lw=None):
        fw, NT, NB = self.fw, self.NT, self.NBLK
        lw = li if lw is None else lw
        cst = self.cst
        Lmat = cst[:, C_L:C_L + 128]
        fw.barrier("sp", [self.xt_tok[0], self.xt_tok[1], self.tk["XROWS"], self.tk["YROWS"]])
        with fw.scope():
            OH1 = fw.T("m_oh1", [128, NT, 32])
            OH2 = fw.T("m_oh2", [128, NT, 32])
            RANK = fw.T("m_rank", [128, NT, 32])
            GT = fw.T("m_gt", [128, NT, 2])
            DEST = fw.T("m_dest", [128, 2, NT])
            DESTi = fw.T("m_desti", [128, 2, NT], I32)
            WIDX = fw.T("m_widx", [128, NB], I32)
            run = fw.T("m_run", [128, 32])
            with fw.scope():
                wr = fw.T("m_wr", [128, 8, 36], F32R)
                fw.D("pool", out=wr[:], in_=self.w_router[li].rearrange("(k p) n -> p k n", p=128))
                brb = fw.T("m_brb", [128, 36])
                self.bc_load("sp", brb[:], self.b_router[li])
                fw.I("dve", "memset", ap=run[:], constant=0.0)
                xTc = [fw.T("m_xT%d" % i, [128, 8, 128], F32R) for i in range(2)]
                smt = [fw.T("m_sm%d" % i, [128, 128]) for i in range(2)]
                p_lg = fw.P("m_plg", [128, 64])
                p_rk = fw.P("m_prk", [128, 64])
                for t in range(NT):
                    a = t % 2
                    sm = smt[a]
                    lg = sm[:, 0:36]
                    gmax, gsum, gw, m1, m2, w1, tmp, ngmax = [sm[:, 36 + i:37 + i] for i in range(8)]
                    gmask, gexp = sm[:, 44:48], sm[:, 48:52]
                    esel, mask1, esel2, mask2 = sm[:, 52:60], sm[:, 60:68], sm[:, 68:76], sm[:, 76:84]
                    A = sm[:, 84:116]
                    fw.D("pool", out=xTc[a][:], in_=self.XT[XT_src][:, :, t * 128:(t + 1) * 128].rearrange("k p t -> p k t"),
                         _r=[self.xt_tok[XT_src]])
                    for k in range(8):
                        fw.I("pe", "matmul", out=p_lg[:, 0:36], lhsT=xTc[a][:, k, :], rhs=wr[:, k, :],
                             start=(k == 0), stop=(k == 7))
                    fw.I("dve", "tensor_tensor", out=lg, in0=p_lg[:, 0:36], in1=brb[:], op=ALU.add)
                    fw.I("dve", "tensor_reduce", out=gmax, in_=sm[:, 0:4], axis=AX.X, op=ALU.max)
                    fw.I("dve", "tensor_scalar", out=gmask, in0=sm[:, 0:4], scalar1=gmax, scalar2=None, op0=ALU.is_equal)
                    fw.I("dve", "tensor_scalar", out=ngmax, in0=gmax, scalar1=-1.0, scalar2=None, op0=ALU.mult)
                    fw.I("act", "activation", out=gexp, in_=sm[:, 0:4], func=AF.Exp, bias=ngmax, accum_out=gsum)
                    fw.I("dve", "reciprocal", out=gw, in_=gsum)
                    fw.I("dve", "tensor_scalar", out=esel, in0=sm[:, 4:12], scalar1=sm[:, 44:45], scalar2=None, op0=ALU.mult)
                    for g in range(1, 4):
                        fw.I("dve", "scalar_tensor_tensor", out=esel, in0=sm[:, 4 + 8 * g:12 + 8 * g],
                             scalar=sm[:, 44 + g:45 + g], in1=esel, op0=ALU.mult, op1=ALU.add)
                    fw.I("dve", "tensor_reduce", out=m1, in_=esel, axis=AX.X, op=ALU.max)
                    fw.I("dve", "tensor_scalar", out=mask1, in0=esel, scalar1=m1, scalar2=None, op0=ALU.is_equal)
                    fw.I("dve", "scalar_tensor_tensor", out=esel2, in0=mask1, scalar=-1e30, in1=esel,
                         op0=ALU.mult, op1=ALU.add)
                    fw.I("dve", "tensor_reduce", out=m2, in_=esel2, axis=AX.X, op=ALU.max)
                    fw.I("dve", "tensor_scalar", out=mask2, in0=esel2, scalar1=m2, scalar2=None, op0=ALU.is_equal)
                    fw.I("dve", "tensor_tensor", out=tmp, in0=m2, in1=m1, op=ALU.subtract)
                    fw.I("act", "activation", out=tmp, in_=tmp, func=AF.Exp)
                    fw.I("dve", "tensor_scalar", out=tmp, in0=tmp, scalar1=1.0, scalar2=None, op0=ALU.add)
                    fw.I("dve", "reciprocal", out=w1, in_=tmp)
                    fw.I("dve", "tensor_tensor", out=GT[:, t, 0:1], in0=gw, in1=w1, op=ALU.mult)
                    fw.I("dve", "tensor_tensor", out=GT[:, t, 1:2], in0=gw, in1=GT[:, t, 0:1], op=ALU.subtract)
                    fw.I("dve", "tensor_tensor", out=OH1[:, t, :].rearrange("p (g e) -> p g e", g=4),
                         in0=gmask.unsqueeze(2).to_broadcast([128, 4, 8]),
                         in1=mask1.unsqueeze(1).to_broadcast([128, 4, 8]), op=ALU.mult)
                    fw.I("dve", "tensor_tensor", out=OH2[:, t, :].rearrange("p (g e) -> p g e", g=4),
                         in0=gmask.unsqueeze(2).to_broadcast([128, 4, 8]),
                         in1=mask2.unsqueeze(1).to_broadcast([128, 4, 8]), op=ALU.mult)
                    fw.I("dve", "tensor_tensor", out=A, in0=OH1[:, t, :], in1=OH2[:, t, :], op=ALU.add)
                    fw.I("pe", "matmul", out=p_rk[:, 0:32], lhsT=Lmat, rhs=A, start=True, stop=True)
                    fw.I("pe", "matmul", out=p_rk[:, 32:64], lhsT=self.ones, rhs=A, start=True, stop=True)
                    fw.I("dve", "tensor_tensor", out=RANK[:, t, :], in0=p_rk[:, 0:32], in1=run[:], op=ALU.add)
                    fw.I("dve", "tensor_tensor", out=run[:], in0=run[:], in1=p_rk[:, 32:64], op=ALU.add)
            with fw.scope():
                NG = -(-self.S // BR)
                cmp = fw.T("m_cmp", [128, 32, NG])
                nblk = fw.T("m_nblk", [128, 32])
                padded = fw.T("m_padded", [128, 32])
                padT = fw.T("m_padT", [32, 128])
                pend = fw.T("m_pend", [128, 32])
                pstart = fw.T("m_pstart", [128, 32])
                tmp3 = fw.T("m_tmp3", [128, NT, 32])
                tmp4 = fw.T("m_tmp4", [128, NT, 32])
                cmpb = fw.T("m_cmpb", [128, NB, 32])
                be = fw.T("m_be", [128, NB])
                p_a = fw.P("m_pa", [128, 128])
                p_b = fw.P("m_pb", [128, 32])
                grid = cst[:, C_BLK:C_BLK + NB]
                fw.I("dve", "tensor_tensor", out=cmp[:], in0=run[:].unsqueeze(2).to_broadcast([128, 32, NG]),
                     in1=grid[:, 0:NG].unsqueeze(1).to_broadcast([128, 32, NG]), op=ALU.is_gt)
                fw.I("dve", "tensor_reduce", out=nblk[:], in_=cmp[:], axis=AX.X, op=ALU.add)
                fw.I("dve", "tensor_scalar", out=padded[:], in0=nblk[:], scalar1=float(BR), scalar2=None, op0=ALU.mult)
                fw.I("pe", "transpose", out=p_a[0:32, :], in_=padded[:, 0:32], identity=self.ident)
                fw.I("act", "copy", out=padT[:], in_=p_a[0:32, :])
                fw.I("pe", "matmul", out=p_b[:], lhsT=padT[:], rhs=cst[0:32, C_U:C_U + 32], start=True, stop=True)
                fw.I("act", "copy", out=pend[:], in_=p_b[:])
                fw.I("dve", "tensor_tensor", out=pstart[:], in0=pend[:], in1=padded[:], op=ALU.subtract)
                fw.I("dve", "tensor_tensor", out=tmp3[:], in0=RANK[:],
                     in1=pstart[:].unsqueeze(1).to_broadcast([128, NT, 32]), op=ALU.add)
                for k, OH in enumerate((OH1, OH2)):
                    fw.I("dve", "tensor_tensor", out=tmp4[:], in0=tmp3[:], in1=OH[:], op=ALU.mult)
                    fw.I("dve", "tensor_reduce", out=DEST[:, k, :], in_=tmp4[:], axis=AX.X, op=ALU.add)
                fw.I("dve", "tensor_copy", out=DESTi[:], in_=DEST[:])
                fw.I("dve", "tensor_tensor", out=cmpb[:], in0=grid.unsqueeze(2).to_broadcast([128, NB, 32]),
                     in1=pend[:].unsqueeze(1).to_broadcast([128, NB, 32]), op=ALU.is_ge)
                fw.I("dve", "tensor_reduce", out=be[:], in_=cmpb[:], axis=AX.X, op=ALU.add)
                fw.I("dve", "tensor_scalar", out=be[:], in0=be[:], scalar1=31.0, scalar2=128.0, op0=ALU.min, op1=ALU.mult)
                fw.I("dve", "tensor_scalar", out=be[:], in0=be[:], scalar1=cst[:, C_PIDX:C_PIDX + 1], scalar2=None,
                     op0=ALU.add)
                fw.I("dve", "tensor_copy", out=WIDX[:], in_=be[:])
            with fw.scope():
                xt = [fw.T("m_x%d" % i, [128, 1024]) for i in range(2)]
                for t in range(NT):
                    a = t % 2
                    fw.D("sp", out=xt[a][:], in_=self.XA[t * 128:(t + 1) * 128, :], _r=[self.xa_tok[t]])
                    for k in range(2):
                        fw.D("pool", _meth="indirect_dma_start", out=self.XROWS[:, :],
                             out_offset=bass.IndirectOffsetOnAxis(ap=DESTi[:, k, t:t + 1], axis=0),
                             in_=xt[a][:], in_offset=None, _r=[self.tk["XROWS"]])
            fw.barrier("sp", [self.tk["XROWS"]])
            with fw.scope():
                gu = [fw.T("m_gu%d" % i, [128, 8, 1024], F32R) for i in range(2)]
                dn = [fw.T("m_dn%d" % i, [128, 4, 1024], F32R) for i in range(3)]
                xb4 = [fw.T("m_xb%d" % i, [128, RT, 1024]) for i in range(2)]
                xbT = fw.T("m_xbT", [128, 8, BR], F32R)
                hT = [fw.T("m_hT%d" % i, [128, 4, BR], F32R) for i in range(2)]
                sg = [fw.T("m_sg%d" % i, [128, BR]) for i in range(2)]
                yb = [fw.T("m_yb%d" % i, [128, 1024]) for i in range(2)]
                p_t = [fw.P("m_pt%d" % i, [128, 4, 128]) for i in range(2)]
                p_g = [fw.P("m_pg%d" % i, [128, BR]) for i in range(2)]
                p_u = [fw.P("m_pu%d" % i, [128, BR]) for i in range(2)]
                p_y = fw.P("m_py", [128, 1024])
                gu_src = self.moe_gu.rearrange("l r c -> (l r) c")
                dn_src = self.moe_dn.rearrange("l r c -> (l r) c")

                def mL(B):
                    fw.D("pool", _meth="indirect_dma_start", out=gu[B % 2][:].rearrange("p k n -> p (k n)"), out_offset=None,
                         in_=gu_src, in_offset=bass.IndirectOffsetOnAxis(ap=WIDX[:, B:B + 1], axis=0),
                         element_offset=lw * 4096 * 8192)
                    fw.D("pool", _meth="indirect_dma_start", out=dn[B % 3][:].rearrange("p k n -> p (k n)"), out_offset=None,
                         in_=dn_src, in_offset=bass.IndirectOffsetOnAxis(ap=WIDX[:, B:B + 1], axis=0),
                         element_offset=lw * 4096 * 4096)
                    fw.D("sp", out=xb4[B % 2][:], in_=self.XROWS[B * BR:(B + 1) * BR, :].rearrange("(r p) c -> p r c", p=128),
                         _r=[self.tk["XROWS"]])

                def mAB(B):
                    x4, g_ = xb4[B % 2], gu[B % 2]
                    cnt = 0
                    for rt in range(RT):
                        for k0 in (0, 4):
                            pt = p_t[cnt % 2]
                            for k in range(4):
                                fw.I("pe", "transpose", out=pt[:, k, :], in_=x4[:, rt, (k0 + k) * 128:(k0 + k + 1) * 128],
                                     identity=self.ident)
                            eng, meth = ("act", "copy") if cnt % 2 else ("dve", "tensor_copy")
                            fw.I(eng, meth, out=xbT[:, k0:k0 + 4, rt * 128:(rt + 1) * 128], in_=pt[:])
                            cnt += 1
                    for fc in range(4):
                        b2 = fc % 2
                        for k in range(8):
                            fw.I("pe", "matmul", out=p_g[b2][:], lhsT=g_[:, k, fc * 128:(fc + 1) * 128], rhs=xbT[:, k, :],
                                 start=(k == 0), stop=(k == 7))
                        for k in range(8):
                            fw.I("pe", "matmul", out=p_u[b2][:], lhsT=g_[:, k, 512 + fc * 128:512 + (fc + 1) * 128],
                                 rhs=xbT[:, k, :], start=(k == 0), stop=(k == 7))
                        fw.I("act", "activation", out=sg[b2][:], in_=p_g[b2][:], func=AF.Silu)
                        fw.I("dve", "tensor_tensor", out=hT[B % 2][:, fc, :], in0=sg[b2][:], in1=p_u[b2][:], op=ALU.mult)

                def mC(B):
                    h_, d_ = hT[B % 2], dn[B % 3]
                    for rt in range(RT):
                        for n in range(2):
                            for k in range(4):
                                fw.I("pe", "matmul", out=p_y[:, n * 512:(n + 1) * 512], lhsT=h_[:, k, rt * 128:(rt + 1) * 128],
                                     rhs=d_[:, k, n * 512:(n + 1) * 512], start=(k == 0), stop=(k == 3))
                        fw.I("act" if rt % 2 else "dve", "copy" if rt % 2 else "tensor_copy", out=yb[rt % 2][:], in_=p_y[:])
                        u = B * RT + rt
                        fw.D("sp", out=self.YROWS[u * 128:(u + 1) * 128, :], in_=yb[rt % 2][:], _r=[self.tk["YROWS"]])
                pipeline(NB, [mL, mAB, mC])
            fw.barrier("sp", [self.tk["YROWS"]])
            with fw.scope():
                gbc = fw.T("m_g", [128, 1024])
                bbc = fw.T("m_b", [128, 1024])
                self.bc_load("sp", gbc[:], self.ln_g[li, 1])
                self.bc_load("sp", bbc[:], self.ln_b[li, 1])
                Y1 = [fw.T("m_y1%d" % i, [128, 1024]) for i in range(2)]
                Y2 = [fw.T("m_y2%d" % i, [128, 1024]) for i in range(2)]
                ffn = [fw.T("m_ffn%d" % i, [128, 1024]) for i in range(2)]
                ln = self.ln_alloc("m4")
                def cA(t):
                    a = t % 2
                    for k, Y in enumerate((Y1, Y2)):
                        fw.D("pool", _meth="indirect_dma_start", out=Y[a][:], out_offset=None, in_=self.YROWS[:, :],
                             in_offset=bass.IndirectOffsetOnAxis(ap=DESTi[:, k, t:t + 1], axis=0), _r=[self.tk["YROWS"]])
                    fw.I("dve", "tensor_scalar", out=ffn[a][:], in0=Y1[a][:], scalar1=GT[:, t, 0:1], scalar2=None,
                         op0=ALU.mult)
                    fw.I("dve", "scalar_tensor_tensor", out=ffn[a][:], in0=Y2[a][:], scalar=GT[:, t, 1:2], in1=ffn[a][:],
                         op0=ALU.mult, op1=ALU.add)
                pipeline(NT, [cA] + self.ln_stages(ln, lambda t: ffn[t % 2][:], gbc, bbc, XT_dst))

    def build(self):
        fw = self.fw
        self.initial()
        if self.with_moe:
            self.zero_xrows()
        if any(li % 2 == 1 for li in self.layers):
            self.rope_tables()
        cur = 0
        last_x = "XA"
        for li in self.layers:
            if li % 2 == 0:
                self.ssd_layer(li, cur, 1 - cur)
            else:
                self.attn_layer(li, cur, 1 - cur)
            cur = 1 - cur
            if self.stop_after == (li, 0):
                break
            self.moe_layer(li, cur, 1 - cur, self.moe_layers.index(li))
            cur = 1 - cur
            if self.stop_after == (li, 1):
                break
        with fw.scope():
            ot = [fw.T("o_t%d" % i, [128, 1024]) for i in range(2)]
            for t in range(self.NT):
                fw.D("sp", out=ot[t % 2][:], in_=self.XA[t * 128:(t + 1) * 128, :], _r=[self.xa_tok[t]])
                fw.D("act", out=self.out[t * 128:(t + 1) * 128, :], in_=ot[t % 2][:])
        fw.finish(["out"])
        return self.nc


def _lay_gu(wg, wu):
    L = wg.shape[0]
    g = wg.reshape(L, 32, 8, 128, 512).transpose(0, 1, 3, 2, 4)
    u = wu.reshape(L, 32, 8, 128, 512).transpose(0, 1, 3, 2, 4)
    return np.ascontiguousarray(np.concatenate([g, u], -1)).reshape(L, 4096, 8192)


def _lay_dn(wd):
    L = wd.shape[0]
    return np.ascontiguousarray(wd.reshape(L, 32, 4, 128, 1024).transpose(0, 1, 3, 2, 4)).reshape(L, 4096, 4096)


def _run(inputs, S, ncores, layers=(0, 1, 2, 3)):
    f32 = lambda a: np.ascontiguousarray(np.asarray(a, dtype=np.float32))
    prog = Prog(S, layers=layers)
    nc = prog.build()
    base = {k: f32(inputs[k]) for k in ("ssd_w_in", "ssd_conv_w", "ssd_conv_b", "ssd_dt_bias", "ssd_a_log", "ssd_d",
                                        "ssd_norm_w", "ssd_w_out", "attn_w_qkv", "attn_w_o", "ln_g", "ln_b")}
    base["consts"] = make_consts(prog.NBLK)
    base["w_router"] = f32(np.concatenate([np.asarray(inputs["moe_w_router_group"]),
                                           np.asarray(inputs["moe_w_router_expert"])], -1))
    base["b_router"] = f32(np.concatenate([np.asarray(inputs["moe_b_router_group"]),
                                           np.asarray(inputs["moe_b_router_expert"])], -1))
    base["moe_gu"] = _lay_gu(f32(inputs["moe_w_gate"]), f32(inputs["moe_w_up"]))
    base["moe_dn"] = _lay_dn(f32(inputs["moe_w_down"]))
    x = f32(inputs["x"])
    pos = np.ascontiguousarray(np.asarray(inputs["positions"]).astype(np.int32))
    in_maps = []
    for c in range(ncores):
        d = dict(base)
        d["x"] = np.ascontiguousarray(x[c, :S])
        d["pos"] = np.ascontiguousarray(pos[c:c + 1, :S])
        in_maps.append(d)
    res = run_bass_kernel_spmd(nc, in_maps, core_ids=list(range(ncores)))
    return np.stack([np.asarray(res.results[c]["out"]) for c in range(ncores)]).astype(np.float32)


def kernel(**inputs):
    x = np.asarray(inputs["x"])
    return _run(inputs, x.shape[1], x.shape[0])
```

```python
import numpy as np
import concourse.bass as bass
import concourse.mybir as mybir
from concourse.bass_utils import run_bass_kernel_spmd

F32 = mybir.dt.float32
F32R = mybir.dt.float32r
I32 = mybir.dt.int32
ALU = mybir.AluOpType
AF = mybir.ActivationFunctionType
AX = mybir.AxisListType

D_MODEL = 1024
DEPTH = 4
ALPHA = (2 * DEPTH) ** 0.25
LN_EPS = 1e-5
RMS_EPS = 1e-5
NEG = -30000.0
ATT_PATTERNS = ((128, 1), (512, 4), (2048, 16))
ROPE_THETA = 500000.0
NBLK_EXTRA = 32
BR = 256
RT = BR // 128


class Buf:
    __slots__ = ("name", "w", "r")

    def __init__(self, name, init_r=None):
        self.name = name
        self.w = None
        self.r = dict(init_r) if init_r else {}


WRITE_KEYS = ("out", "accum_out", "ap")


class FW:
    ENGS = ("pe", "dve", "act", "pool", "sp")
    EPOCH = 30000

    def __init__(self, nc, n_dma_sems=48):
        self.nc = nc
        self.q = {e: [] for e in self.ENGS}
        self.sems = {}
        self._sem_ctx = []
        self._ctxs = []
        self.bufs = {}
        self.cur = {}
        self.waited = {e: {} for e in self.ENGS}
        self.free_events = {}
        for e in self.ENGS:
            self._new_epoch(e)
        self.dma_keys = []
        self.dma_uses = {}
        for i in range(n_dma_sems):
            k = self._alloc_sem("dma%d" % i)
            self.dma_keys.append(k)
            self.dma_uses[k] = 0
        self.dma_rr = 0
        self.ninst = 0
        self.uid = 0
        self._regs = {}

    def _alloc_sem(self, name):
        cm = self.nc.semaphore(name)
        h = cm.__enter__()
        self._sem_ctx.append(cm)
        self.sems[name] = h
        return name

    def _new_epoch(self, e):
        idx = sum(1 for k in self.sems if k.startswith("e_" + e + "_"))
        k = self._alloc_sem("e_%s_%d" % (e, idx))
        self.cur[e] = [k, 0]

    def T(self, name, shape, dtype=F32):
        self.uid += 1
        name = "%s_%d" % (name, self.uid)
        cm = self.nc.sbuf_tensor(name, list(shape), dtype)
        t = cm.__enter__()
        self._ctxs.append((name, cm))
        self.bufs[name] = Buf(name, self.free_events)
        return t

    def P(self, name, shape, dtype=F32):
        self.uid += 1
        name = "%s_%d" % (name, self.uid)
        cm = self.nc.psum_tensor(name, list(shape), dtype)
        t = cm.__enter__()
        self._ctxs.append((name, cm))
        self.bufs[name] = Buf(name, self.free_events)
        return t

    def dram(self, name, shape, dtype=F32, kind="Internal", track=True):
        t = self.nc.dram_tensor(name, list(shape), dtype, kind=kind)
        if track:
            self.bufs[name] = Buf(name)
        return t.ap()

    def token(self, name):
        b = Buf(name)
        self.bufs[name] = b
        return b

    def scope(self):
        return _Scope(self)

    def _collect(self, kw, xr, xw):
        reads, writes = [], []
        for k, v in kw.items():
            if isinstance(v, bass.IndirectOffsetOnAxis):
                v = v.ap
                k = "idx"
            if isinstance(v, bass.AP):
                b = self.bufs.get(v.tensor.name)
                if b is not None:
                    (writes if k in WRITE_KEYS else reads).append(b)
        for b in xr or ():
            reads.append(self.bufs[b] if isinstance(b, str) else b)
        for b in xw or ():
            writes.append(self.bufs[b] if isinstance(b, str) else b)
        return reads, writes

    def _deps(self, reads, writes):
        deps = {}
        for b in reads:
            if b.w is not None and deps.get(b.w[0], 0) < b.w[1]:
                deps[b.w[0]] = b.w[1]
        for b in writes:
            if b.w is not None and deps.get(b.w[0], 0) < b.w[1]:
                deps[b.w[0]] = b.w[1]
            for k, v in b.r.items():
                if deps.get(k, 0) < v:
                    deps[k] = v
        return deps

    def _emit_waits(self, e, deps):
        wt = self.waited[e]
        for k, v in deps.items():
            if e == "pe" and k.startswith("e_pe_"):
                continue
            if wt.get(k, 0) >= v:
                continue
            wt[k] = v
            h = self.sems[k]
            self.q[e].append(lambda eng, h=h, v=v: eng.wait_ge(h, v))

    def _update(self, ev, reads, writes):
        k, v = ev
        for b in reads:
            if b.r.get(k, 0) < v:
                b.r[k] = v
        for b in writes:
            b.w = ev
            b.r = {}

    def I(self, e, meth, _r=None, _w=None, **kw):
        reads, writes = self._collect(kw, _r, _w)
        deps = self._deps(reads, writes)
        self._emit_waits(e, deps)
        cur = self.cur[e]
        if cur[1] >= self.EPOCH:
            self._new_epoch(e)
            cur = self.cur[e]
        cur[1] += 1
        k, v = cur[0], cur[1]
        h = self.sems[k]
        self.q[e].append(lambda eng, meth=meth, kw=kw, h=h: getattr(eng, meth)(**kw).then_inc(h, 1))
        self._update((k, v), reads, writes)
        self.ninst += 1

    def D(self, e, _r=None, _w=None, _meth="dma_start", **kw):
        reads, writes = self._collect(kw, _r, _w)
        deps = self._deps(reads, writes)
        k = self.dma_keys[self.dma_rr % len(self.dma_keys)]
        self.dma_rr += 1
        prev = 16 * self.dma_uses[k]
        if prev:
            deps[k] = max(deps.get(k, 0), prev)
        self._emit_waits(e, deps)
        self.dma_uses[k] += 1
        v = 16 * self.dma_uses[k]
        h = self.sems[k]
        if isinstance(kw.get("bounds_check"), int):
            bv = kw.pop("bounds_check")

            def emit(eng, meth=_meth, kw=kw, h=h, bv=bv):
                key = ("bound", e, bv)
                if key not in self._regs:
                    self._regs[key] = eng.to_reg(bv)
                return getattr(eng, meth)(bounds_check=self._regs[key], **kw).then_inc(h, 16)
            self.q[e].append(emit)
        else:
            self.q[e].append(lambda eng, meth=_meth, kw=kw, h=h: getattr(eng, meth)(**kw).then_inc(h, 16))
        self._update((k, v), reads, writes)
        self.ninst += 1

    def barrier(self, e, tok_names):
        self.I(e, "nop", _w=list(tok_names))

    def finish(self, final_names):
        reads = [self.bufs[n] for n in final_names]
        deps = self._deps(reads, [])
        for e in self.ENGS:
            self._emit_waits(e, dict(deps))
        nc = self.nc
        with nc.Block() as block:
            @block.tensor
            def _(eng):
                for f in self.q["pe"]:
                    f(eng)

            @block.vector
            def _(eng):
                for f in self.q["dve"]:
                    f(eng)

            @block.scalar
            def _(eng):
                for f in self.q["act"]:
                    f(eng)

            @block.gpsimd
            def _(eng):
                for f in self.q["pool"]:
                    f(eng)

            @block.sync
            def _(eng):
                for f in self.q["sp"]:
                    f(eng)
        for name, cm in reversed(self._ctxs):
            cm.__exit__(None, None, None)
        for cm in reversed(self._sem_ctx):
            cm.__exit__(None, None, None)


class _Scope:
    def __init__(self, fw):
        self.fw = fw

    def __enter__(self):
        self.mark = len(self.fw._ctxs)
        return self

    def __exit__(self, *a):
        fw = self.fw
        fe = fw.free_events
        while len(fw._ctxs) > self.mark:
            name, cm = fw._ctxs.pop()
            b = fw.bufs.pop(name)
            if b.w is not None and fe.get(b.w[0], 0) < b.w[1]:
                fe[b.w[0]] = b.w[1]
            for k, v in b.r.items():
                if fe.get(k, 0) < v:
                    fe[k] = v
            cm.__exit__(None, None, None)
        return False


def pipeline(n, stages):
    ns = len(stages)
    for t in range(n + ns - 1):
        for si, f in enumerate(stages):
            i = t - si
            if 0 <= i < n:
                f(i)


C_ID, C_U, C_ONES, C_L, C_PM, C_MASK, C_PIDX = 0, 128, 256, 384, 512, 640, 896
C_INVF, C_M16, C_1M16, C_SS, C_HALFPI, C_HM0, C_HM1, C_EIDX, C_BLK = 897, 898, 899, 900, 901, 902, 903, 904, 936


def make_consts(nblk):
    w = C_BLK + nblk
    c = np.zeros((128, w), np.float32)
    i = np.arange(128)
    c[:, C_ID:C_ID + 128] = np.eye(128, dtype=np.float32)
    c[:, C_U:C_U + 128] = (i[:, None] <= i[None, :])
    c[:, C_ONES:C_ONES + 128] = 1.0
    c[:, C_L:C_L + 128] = (i[:, None] < i[None, :])
    pm = np.zeros((128, 128), np.float32)
    for dp in range(128):
        if dp % 64 < 16:
            d = (dp // 64) * 64 + ((dp % 64) ^ 8)
            pm[d, dp] = 1.0
    c[:, C_PM:C_PM + 128] = pm
    c[:, C_MASK:C_MASK + 128] = np.where(i[None, :] >= i[:, None], 0.0, NEG)
    c[:, C_MASK + 128:C_MASK + 256] = np.where(i[None, :] <= i[:, None], 0.0, NEG)
    c[:, C_PIDX] = i
    dd = i % 64
    invf = (np.float32(ROPE_THETA) ** (-np.arange(0, 16, 2, dtype=np.float32) / np.float32(16))).astype(np.float32)
    m16 = (dd < 16).astype(np.float32)
    c[:, C_INVF] = np.where(dd < 16, invf[dd % 8], 0.0)
    c[:, C_M16] = m16
    c[:, C_1M16] = 1.0 - m16
    c[:, C_SS] = m16 * np.where(dd < 8, -1.0, 1.0)
    c[:, C_HALFPI] = np.float32(np.pi / 2)
    c[:, C_HM0] = (i < 64)
    c[:, C_HM1] = (i >= 64)
    c[:, C_EIDX:C_EIDX + 32] = np.arange(32)[None, :]
    c[:, C_BLK:C_BLK + nblk] = float(BR) * np.arange(nblk)[None, :]
    return c


class Prog:
    def __init__(self, S, layers=(0, 1, 2, 3), stop_after=None, with_moe=True, moe_layers=(0, 1, 2, 3)):
        self.S = S
        self.with_moe = with_moe
        self.NT = S // 128
        self.layers = tuple(layers)
        self.stop_after = stop_after
        self.NBLK = -(-(2 * S + 32 * (BR - 1)) // BR)
        nc = self.nc = bass.Bass("TRN2", target_bir_lowering=False)
        fw = self.fw = FW(nc)
        S_ = S
        ext = lambda n, s, d=F32: fw.dram(n, s, d, kind="ExternalInput")
        self.x_in = ext("x", [S_, 1024])
        self.pos_in = ext("pos", [1, S_], I32)
        self.consts_in = ext("consts", [128, C_BLK + self.NBLK])
        self.ssd_w_in = ext("ssd_w_in", [2, 1024, 5152])
        self.ssd_conv_w = ext("ssd_conv_w", [2, 4, 3072])
        self.ssd_conv_b = ext("ssd_conv_b", [2, 3072])
        self.ssd_dt_bias = ext("ssd_dt_bias", [2, 32])
        self.ssd_a_log = ext("ssd_a_log", [2, 32])
        self.ssd_d = ext("ssd_d", [2, 32])
        self.ssd_norm_w = ext("ssd_norm_w", [2, 2048])
        self.ssd_w_out = ext("ssd_w_out", [2, 2048, 1024])
        self.attn_w_qkv = ext("attn_w_qkv", [2, 1024, 4608])
        self.attn_w_o = ext("attn_w_o", [2, 512, 1024])
        self.ln_g = ext("ln_g", [4, 2, 1024])
        self.ln_b = ext("ln_b", [4, 2, 1024])
        self.w_router = ext("w_router", [4, 1024, 36])
        self.b_router = ext("b_router", [4, 36])
        self.moe_layers = tuple(moe_layers)
        if with_moe:
            nl = len(self.moe_layers)
            self.moe_gu = [ext("moe_gu%d" % i, [32 * 128, 8 * 1024]) for i in range(nl)]
            self.moe_dn = [ext("moe_dn%d" % i, [32 * 128, 4 * 1024]) for i in range(nl)]
        self.out = fw.dram("out", [S_, 1024], F32, kind="ExternalOutput")
        sc = lambda n, sh, d=F32: fw.dram(n, sh, d, track=False)
        self.XA = sc("XA", [S_, 1024])
        self.XT = [sc("XT0", [8, 128, S_]), sc("XT1", [8, 128, S_])]
        self.BCfm = sc("BCfm", [8, 128, S_])
        self.XSBtm = sc("XSBtm", [S_, 2560])
        self.YN = sc("YN", [S_, 2048])
        self.XROWS = sc("XROWS", [self.NBLK * BR, 1024])
        self.YROWS = sc("YROWS", [self.NBLK * BR, 1024])
        self.AO = sc("AO", [3, S_, 512])
        self.AML = sc("AML", [3, S_, 16])
        self.ROPE = sc("ROPE", [2, 128, S_])
        self.QK = sc("QK", [24, 128, S_])
        self.VP = sc("VP", [3, S_, 512])
        self.xa_tok = [fw.token("xa%d" % t) for t in range(self.NT)]
        self.xt_tok = [fw.token("xt0"), fw.token("xt1")]
        self.tk = {n: fw.token("tk_" + n) for n in ("BCfm", "XSBtm", "YN", "XROWS", "YROWS", "AO", "AML", "ROPE", "QK", "VP")}
        self.cst = fw.T("cst", [128, C_BLK + self.NBLK])
        fw.dummy = fw.T("fwdummy", [128, 4])
        fw.D("sp", out=self.cst[:], in_=self.consts_in[:, :])
        self.eps_ln = fw.T("eps_ln", [128, 2])
        fw.I("dve", "memset", ap=self.eps_ln[:, 0:1], constant=float(LN_EPS))
        fw.I("dve", "memset", ap=self.eps_ln[:, 1:2], constant=1.0)
        self.ident = self.cst[:, C_ID:C_ID + 128]
        self.U = self.cst[:, C_U:C_U + 128]
        self.ones = self.cst[:, C_ONES:C_ONES + 128]

    def bc_load(self, q, dst, src_row):
        self.fw.D(q, out=dst, in_=src_row.partition_broadcast(128))

    def initial(self):
        fw = self.fw
        with fw.scope():
            xt = [fw.T("i_x%d" % i, [128, 1024]) for i in range(2)]
            tp = [fw.P("i_tp%d" % i, [128, 8, 128]) for i in range(2)]
            ts = [fw.T("i_ts%d" % i, [128, 8, 128]) for i in range(2)]
            for t in range(self.NT):
                a = t % 2
                fw.D("sp", out=xt[a][:], in_=self.x_in[t * 128:(t + 1) * 128, :])
                fw.D("act", out=self.XA[t * 128:(t + 1) * 128, :], in_=xt[a][:], _w=[self.xa_tok[t]])
                self.transpose_store(xt[a], tp[a], ts[a], 0, t)

    def transpose_store(self, xtile, tp, ts, XT_dst, t):
        fw = self.fw
        for c in range(8):
            fw.I("pe", "transpose", out=tp[:, c, :], in_=xtile[:, c * 128:(c + 1) * 128], identity=self.ident)
        fw.I("act", "copy", out=ts[:], in_=tp[:])
        fw.D("sp", out=self.XT[XT_dst][:, :, t * 128:(t + 1) * 128].rearrange("k p t -> p k t"), in_=ts[:],
             _r=[self.xt_tok[XT_dst]])

    def proj_ln(self, tag, li, sub, kch, w_src, a_loader, XT_dst):
        fw = self.fw
        with fw.scope():
            w = fw.T(tag + "_w", [128, kch, 1024], F32R)
            for k in range(kch):
                fw.D("pool", out=w[:, k, :], in_=w_src[k * 128:(k + 1) * 128, :])
            gbc = fw.T(tag + "_g", [128, 1024])
            bbc = fw.T(tag + "_b", [128, 1024])
            self.bc_load("sp", gbc[:], self.ln_g[li, sub])
            self.bc_load("sp", bbc[:], self.ln_b[li, sub])
            atp = [fw.P(tag + "_atp%d" % i, [128, 4, 128]) for i in range(2)]
            aT = [fw.T(tag + "_aT%d" % i, [128, kch, 128], F32R) for i in range(2)]
            mp = [fw.P(tag + "_mp%d" % i, [128, 1024]) for i in range(2)]
            ln = self.ln_alloc(tag)

            def stA(t):
                a = t % 2
                A = a_loader(t)
                for k0 in range(0, kch, 4):
                    kn = min(4, kch - k0)
                    for k in range(kn):
                        fw.I("pe", "transpose", out=atp[(k0 // 4) % 2][:, k, :],
                             in_=A[:, (k0 + k) * 128:(k0 + k + 1) * 128], identity=self.ident)
                    fw.I("act" if (k0 // 4) % 2 else "dve", "copy" if (k0 // 4) % 2 else "tensor_copy",
                         out=aT[a][:, k0:k0 + kn, :], in_=atp[(k0 // 4) % 2][:, 0:kn, :])
                for n in range(2):
                    for k in range(kch):
                        fw.I("pe", "matmul", out=mp[a][:, n * 512:(n + 1) * 512], lhsT=aT[a][:, k, :],
                             rhs=w[:, k, n * 512:(n + 1) * 512], start=(k == 0), stop=(k == kch - 1))
            pipeline(self.NT, [stA] + self.ln_stages(ln, lambda t: mp[t % 2][:], gbc, bbc, XT_dst))

    def ln_alloc(self, tag):
        fw = self.fw
        d = {}
        d["x"] = [fw.T(tag + "_lx%d" % i, [128, 1024]) for i in range(4)]
        d["v"] = [fw.T(tag + "_lv%d" % i, [128, 1024]) for i in range(4)]
        d["junk"] = fw.T(tag + "_lj", [128, 1024])
        d["st"] = [fw.T(tag + "_ls%d" % i, [128, 8]) for i in range(4)]
        d["tp"] = fw.P(tag + "_ltp", [128, 8, 128])
        d["ts"] = [fw.T(tag + "_lts%d" % i, [128, 8, 128]) for i in range(2)]
        return d

    def ln_stages(self, ln, mix_of, gbc, bbc, XT_dst):
        fw = self.fw

        def b1(t):
            a = t % 4
            x, v, st, junk = ln["x"][a], ln["v"][a], ln["st"][a], ln["junk"]
            fw.D("sp", out=x[:], in_=self.XA[t * 128:(t + 1) * 128, :], _r=[self.xa_tok[t]])
            fw.I("dve", "scalar_tensor_tensor", out=v[:], in0=x[:], scalar=float(ALPHA), in1=mix_of(t),
                 op0=ALU.mult, op1=ALU.add)
            fw.I("act", "activation", out=junk[:], in_=v[:], func=AF.Identity, accum_out=st[:, 0:1])
            fw.I("act", "activation", out=junk[:], in_=v[:], func=AF.Square, accum_out=st[:, 1:2])

        def b2(t):
            st = ln["st"][t % 4]
            fw.I("dve", "tensor_scalar", out=st[:, 2:3], in0=st[:, 0:1], scalar1=1.0 / 1024, scalar2=None, op0=ALU.mult)
            fw.I("dve", "tensor_tensor", out=st[:, 3:4], in0=st[:, 2:3], in1=st[:, 2:3], op=ALU.mult)
            fw.I("dve", "scalar_tensor_tensor", out=st[:, 4:5], in0=st[:, 1:2], scalar=1.0 / 1024, in1=st[:, 3:4],
                 op0=ALU.mult, op1=ALU.subtract)
            fw.I("act", "activation", out=st[:, 6:7], in_=st[:, 4:5], func=AF.Sqrt, bias=self.eps_ln[:, 0:1])
            fw.I("dve", "reciprocal", out=st[:, 5:6], in_=st[:, 6:7])

        def b3(t):
            a = t % 4
            x, v, st = ln["x"][a], ln["v"][a], ln["st"][a]
            fw.I("dve", "tensor_scalar", out=v[:], in0=v[:], scalar1=st[:, 2:3], scalar2=st[:, 5:6],
                 op0=ALU.subtract, op1=ALU.mult)
            fw.I("dve", "tensor_tensor", out=v[:], in0=v[:], in1=gbc[:], op=ALU.mult)
            fw.I("dve", "tensor_tensor", out=x[:], in0=v[:], in1=bbc[:], op=ALU.add)
            fw.D("act", out=self.XA[t * 128:(t + 1) * 128, :], in_=x[:], _w=[self.xa_tok[t]])

        def c1(t):
            self.transpose_store(ln["x"][t % 4], ln["tp"], ln["ts"][t % 2], XT_dst, t)
        return [b1, b2, b3, c1]

    def ssd_sweep1(self, j, XT_src):
        fw, S = self.fw, self.S
        w_in = self.ssd_w_in
        with fw.scope():
            wx = fw.T("s1_wx", [128, 8, 3072], F32R)
            for k in range(8):
                fw.D("pool", out=wx[:, k, :], in_=w_in[j, k * 128:(k + 1) * 128, 2048:5120])
            cw = fw.T("s1_cw", [128, 4, 24])
            cb = fw.T("s1_cb", [128, 24])
            for k in range(4):
                fw.D("sp", out=cw[:, k, :], in_=self.ssd_conv_w[j, k].rearrange("(t p) -> p t", p=128),
                     allow_slow_non_contiguous=True)
            fw.D("sp", out=cb[:], in_=self.ssd_conv_b[j].rearrange("(t p) -> p t", p=128),
                 allow_slow_non_contiguous=True)
            halo = fw.T("s1_halo", [128, 24, 3])
            fw.I("dve", "memset", ap=halo[:], constant=0.0)
            xtb = [fw.T("s1_xt%d" % i, [128, 8, 512], F32R) for i in range(2)]
            ps = [fw.P("s1_ps%d" % i, [128, 512]) for i in range(2)]
            ub = [fw.T("s1_ub%d" % i, [128, 515]) for i in range(2)]
            acc = [fw.T("s1_acc%d" % i, [128, 512]) for i in range(2)]
            so = [fw.T("s1_so%d" % i, [128, 512]) for i in range(2)]
            tps = [fw.P("s1_tp%d" % i, [128, 4, 128]) for i in range(2)]
            tsb = [fw.T("s1_ts%d" % i, [128, 4, 128]) for i in range(2)]
            def s0(idx):
                tb, ct = divmod(idx, 24)
                a = idx % 2
                xt = xtb[tb % 2]
                if ct == 0:
                    fw.D("pool", out=xt[:], in_=self.XT[XT_src][:, :, tb * 512:(tb + 1) * 512].rearrange("k p t -> p k t"),
                         _r=[self.xt_tok[XT_src]])
                for k in range(8):
                    fw.I("pe", "matmul", out=ps[a][:], lhsT=wx[:, k, ct * 128:(ct + 1) * 128], rhs=xt[:, k, :],
                         start=(k == 0), stop=(k == 7))

            def s1(idx):
                tb, ct = divmod(idx, 24)
                a = idx % 2
                fw.I("pool", "tensor_copy", out=ub[a][:, 0:3], in_=halo[:, ct, :])
                fw.I("act", "copy", out=ub[a][:, 3:515], in_=ps[a][:])
                fw.I("pool", "tensor_copy", out=halo[:, ct, :], in_=ub[a][:, 512:515])
                fw.I("act", "activation", out=acc[a][:], in_=ub[a][:, 3:515], func=AF.Identity, scale=cw[:, 3, ct:ct + 1],
                     bias=cb[:, ct:ct + 1])

            def s1b(idx):
                tb, ct = divmod(idx, 24)
                a = idx % 2
                for kk in (2, 1, 0):
                    fw.I("dve", "scalar_tensor_tensor", out=acc[a][:], in0=ub[a][:, kk:kk + 512], scalar=cw[:, kk, ct:ct + 1],
                         in1=acc[a][:], op0=ALU.mult, op1=ALU.add)
                fw.I("act", "activation", out=so[a][:], in_=acc[a][:], func=AF.Silu)
                if ct >= 16:
                    fw.D("sp", out=self.BCfm[ct - 16, :, tb * 512:(tb + 1) * 512], in_=so[a][:], _r=[self.tk["BCfm"]])

            def s2(idx):
                tb, ct = divmod(idx, 24)
                a = idx % 2
                if ct < 20:
                    for q in range(4):
                        fw.I("pe", "transpose", out=tps[a][:, q, :], in_=so[a][:, q * 128:(q + 1) * 128],
                             identity=self.ident)
                    if idx % 2:
                        fw.I("act", "copy", out=tsb[a][:], in_=tps[a][:])
                    else:
                        fw.I("dve", "tensor_copy", out=tsb[a][:], in_=tps[a][:])
                    fw.D("sp", out=self.XSBtm[tb * 512:(tb + 1) * 512, ct * 128:(ct + 1) * 128]
                         .rearrange("(q p) c -> p q c", p=128), in_=tsb[a][:], _r=[self.tk["XSBtm"]])
            pipeline((S // 512) * 24, [s0, s1, s1b, s2])

    def ssd_sweep2(self, j, XT_src):
        fw, S = self.fw, self.S
        w_in = self.ssd_w_in
        with fw.scope():
            wz = fw.T("s2_wz", [128, 8, 2048], F32R)
            wdt = fw.T("s2_wdt", [128, 8, 32], F32R)
            for k in range(8):
                fw.D("pool", out=wz[:, k, :], in_=w_in[j, k * 128:(k + 1) * 128, 0:2048])
                fw.D("pool", out=wdt[:, k, :], in_=w_in[j, k * 128:(k + 1) * 128, 5120:5152])
            dtb = fw.T("s2_dtb", [128, 32])
            abc = fw.T("s2_a", [128, 32])
            dsk = fw.T("s2_dsk", [128, 32])
            nw = fw.T("s2_nw", [128, 2048])
            self.bc_load("sp", dtb[:], self.ssd_dt_bias[j])
            self.bc_load("sp", abc[:], self.ssd_a_log[j])
            self.bc_load("sp", dsk[:], self.ssd_d[j])
            self.bc_load("sp", nw[:], self.ssd_norm_w[j])
            fw.I("act", "activation", out=abc[:], in_=abc[:], func=AF.Exp)
            fw.I("dve", "tensor_scalar", out=abc[:], in0=abc[:], scalar1=-1.0, scalar2=None, op0=ALU.mult)
            prev = fw.T("s2_prev", [128, 4, 512])
            prevR = fw.T("s2_prevR", [128, 4, 512], F32R)
            fw.I("dve", "memset", ap=prev[:], constant=0.0)
            fw.I("dve", "tensor_copy", out=prevR[:], in_=prev[:])
            xTc = [fw.T("s2_xT%d" % i, [128, 8, 128], F32R) for i in range(2)]
            xs = [fw.T("s2_xs%d" % i, [128, 32, 64]) for i in range(2)]
            Btm = [fw.T("s2_Btm%d" % i, [128, 4, 128], F32R) for i in range(2)]
            Bfm = [fw.T("s2_Bfm%d" % i, [128, 4, 128], F32R) for i in range(2)]
            Cfm = [fw.T("s2_Cfm%d" % i, [128, 4, 128], F32R) for i in range(2)]
            sm = fw.T("s2_sm", [128, 12, 32])
            Uda = [fw.T("s2_Uda%d" % i, [128, 8, 128]) for i in range(2)]
            xr = fw.T("s2_xr", [128, 32, 64], F32R)
            xrd = fw.T("s2_xrd", [128, 32, 64], F32R)
            zs = [fw.T("s2_zs%d" % i, [128, 512]) for i in range(2)]
            cbm = [fw.T("s2_cbm%d" % i, [128, 128]) for i in range(2)]
            Dm = [fw.T("s2_D%d" % i, [128, 8, 128]) for i in range(2)]
            MT = [fw.T("s2_MT%d" % i, [128, 8, 128], F32R) for i in range(2)]
            t1 = [fw.T("s2_t1%d" % i, [128, 8, 64]) for i in range(2)]
            t2 = [fw.T("s2_t2%d" % i, [128, 8, 64]) for i in range(2)]
            yn = [fw.T("s2_yn0", [128, 2048])] * 2
            junk = fw.T("s2_junk", [128, 512])
            rs = fw.T("s2_rs", [128, 8])
            p_z = fw.P("s2_pz", [128, 512])
            p_sm = fw.P("s2_psm", [128, 512])
            p_R = fw.P("s2_pR", [128, 8, 128])
            p_Y = fw.P("s2_pY", [128, 8, 64])
            p_Yo = fw.P("s2_pYo", [128, 8, 64])
            p_S = fw.P("s2_pS", [128, 512])
            DT, DA, CS, ECS, DTE, CD, V0, V1, V2 = range(9)
            p_Y2 = [p_Y, fw.P("s2_pY2", [128, 8, 64])]

            def front(u):
                c, g = divmod(u, 4)
                a = c % 2
                tok = slice(c * 128, (c + 1) * 128)
                p_Y = p_Y2[u % 2]
                if g == 0:
                    fw.D("pool", out=xTc[a][:], in_=self.XT[XT_src][:, :, tok].rearrange("k p t -> p k t"),
                         _r=[self.xt_tok[XT_src]])
                    fw.D("sp", out=xs[a][:].rearrange("p h d -> p (h d)"), in_=self.XSBtm[tok, 0:2048], _r=[self.tk["XSBtm"]])
                    fw.D("pool", out=Btm[a][:].rearrange("p g n -> p (g n)"), in_=self.XSBtm[tok, 2048:2560],
                         _r=[self.tk["XSBtm"]])
                    fw.D("pool", out=Bfm[a][:], in_=self.BCfm[0:4, :, tok].rearrange("g p t -> p g t"), _r=[self.tk["BCfm"]])
                    fw.D("pool", out=Cfm[a][:], in_=self.BCfm[4:8, :, tok].rearrange("g p t -> p g t"), _r=[self.tk["BCfm"]])
                    for k in range(8):
                        fw.I("pe", "matmul", out=p_sm[:, 0:32], lhsT=xTc[a][:, k, :], rhs=wdt[:, k, :],
                             start=(k == 0), stop=(k == 7))
                    fw.I("dve", "tensor_tensor", out=sm[:, V0, :], in0=p_sm[:, 0:32], in1=dtb[:], op=ALU.add)
                    fw.I("dve", "scalar_tensor_tensor", out=sm[:, V1, :], in0=sm[:, V0, :], scalar=-1.0, in1=sm[:, V0, :],
                         op0=ALU.mult, op1=ALU.max)
                    fw.I("act", "activation", out=sm[:, V1, :], in_=sm[:, V1, :], func=AF.Exp, scale=-1.0)
                    fw.I("act", "activation", out=sm[:, V1, :], in_=sm[:, V1, :], func=AF.Ln, bias=self.eps_ln[:, 1:2])
                    fw.I("dve", "scalar_tensor_tensor", out=sm[:, DT, :], in0=sm[:, V0, :], scalar=0.0, in1=sm[:, V1, :],
                         op0=ALU.max, op1=ALU.add)
                    fw.I("dve", "tensor_tensor", out=sm[:, DA, :], in0=sm[:, DT, :], in1=abc[:], op=ALU.mult)
                    fw.I("pe", "matmul", out=p_sm[:, 32:64], lhsT=self.U, rhs=sm[:, DA, :], start=True, stop=True)
                    fw.I("act", "copy", out=sm[:, CS, :], in_=p_sm[:, 32:64])
                    fw.I("act", "activation", out=sm[:, ECS, :], in_=sm[:, CS, :], func=AF.Exp)
                    fw.I("pool", "tensor_tensor", out=xr[:], in0=xs[a][:],
                         in1=sm[:, DT, :].unsqueeze(2).to_broadcast([128, 32, 64]), op=ALU.mult)

                b2 = g % 2
                hs = slice(8 * g, 8 * g + 8)
                for k in range(8):
                    fw.I("pe", "matmul", out=p_z[:], lhsT=xTc[a][:, k, :], rhs=wz[:, k, g * 512:(g + 1) * 512],
                         start=(k == 0), stop=(k == 7))
                fw.I("act", "activation", out=zs[b2][:], in_=p_z[:], func=AF.Silu)
                fw.I("dve", "tensor_tensor", out=Uda[b2][:], in0=self.U.unsqueeze(1).to_broadcast([128, 8, 128]),
                     in1=sm[:, DA, hs].unsqueeze(2).to_broadcast([128, 8, 128]), op=ALU.mult)
                for hh in range(2):
                    fw.I("pe", "matmul", out=p_R[:, 4 * hh:4 * hh + 4, :], lhsT=self.ones,
                         rhs=Uda[b2][:, 4 * hh:4 * hh + 4, :], start=True, stop=True)
                fw.I("dve", "tensor_tensor", out=sm[:, V2, hs], in0=p_R[:, :, 127], in1=sm[:, CS, hs], op=ALU.subtract)
                fw.I("act", "activation", out=sm[:, DTE, hs], in_=sm[:, V2, hs], func=AF.Exp)
                fw.I("act", "activation", out=sm[:, CD, hs], in_=p_R[:, :, 127], func=AF.Exp)
                fw.I("pool", "tensor_tensor", out=xrd[:, hs, :], in0=xr[:, hs, :],
                     in1=sm[:, DTE, hs].unsqueeze(2).to_broadcast([128, 8, 64]), op=ALU.mult)
                fw.I("pe", "matmul", out=p_sm[:, 128:256], lhsT=Bfm[a][:, g, :], rhs=Cfm[a][:, g, :],
                     start=True, stop=True)
                fw.I("dve", "tensor_tensor", out=cbm[b2][:], in0=p_sm[:, 128:256], in1=self.U, op=ALU.mult)
                fw.I("dve", "tensor_tensor", out=Dm[b2][:], in0=p_R[:],
                     in1=sm[:, CS, hs].unsqueeze(2).to_broadcast([128, 8, 128]), op=ALU.subtract)
                fw.I("act", "activation", out=Dm[b2][:], in_=Dm[b2][:], func=AF.Exp)
                fw.I("dve", "scalar_tensor_tensor", out=MT[b2][:], in0=Dm[b2][:], scalar=1.0,
                     in1=cbm[b2][:].unsqueeze(1).to_broadcast([128, 8, 128]), op0=ALU.min, op1=ALU.mult)
                for h in range(8):
                    fw.I("pe", "matmul", out=p_Y[:, h, :], lhsT=MT[b2][:, h, :], rhs=xr[:, 8 * g + h, :],
                         start=True, stop=True)
                fw.I("pe", "matmul", out=p_Yo[:].rearrange("p h d -> p (h d)"), lhsT=Cfm[a][:, g, :],
                     rhs=prevR[:, g, :], start=True, stop=True)
                fw.I("pe", "matmul", out=p_S[:], lhsT=Btm[a][:, g, :],
                     rhs=xrd[:, hs, :].rearrange("p h d -> p (h d)"), start=True, stop=True)

                fw.I("dve", "tensor_tensor", out=t1[b2][:], in0=p_Yo[:],
                     in1=sm[:, ECS, hs].unsqueeze(2).to_broadcast([128, 8, 64]), op=ALU.mult)

                fw.I("pool", "tensor_tensor", out=prev[:, g, :].rearrange("p (h d) -> p h d", h=8),
                     in0=prev[:, g, :].rearrange("p (h d) -> p h d", h=8),
                     in1=sm[:, CD, hs].unsqueeze(2).to_broadcast([128, 8, 64]), op=ALU.mult)
                fw.I("dve", "tensor_tensor", out=prev[:, g, :], in0=prev[:, g, :], in1=p_S[:], op=ALU.add)
                fw.I("act", "copy", out=prevR[:, g, :], in_=prev[:, g, :])


            def tailf(u):
                c, g = divmod(u, 4)
                a = c % 2
                tok = slice(c * 128, (c + 1) * 128)
                p_Y = p_Y2[u % 2]
                b2 = g % 2
                hs = slice(8 * g, 8 * g + 8)
                fw.I("dve", "tensor_tensor", out=t1[b2][:], in0=t1[b2][:], in1=p_Y[:], op=ALU.add)
                fw.I("pool", "tensor_tensor", out=t2[b2][:], in0=xs[a][:, hs, :],
                     in1=dsk[:, hs].unsqueeze(2).to_broadcast([128, 8, 64]), op=ALU.mult)
                fw.I("pool", "tensor_tensor", out=t1[b2][:], in0=t1[b2][:], in1=t2[b2][:], op=ALU.add)
                fw.I("dve", "tensor_tensor", out=t1[b2][:].rearrange("p h d -> p (h d)"),
                     in0=t1[b2][:].rearrange("p h d -> p (h d)"), in1=zs[b2][:], op=ALU.mult)
                fw.I("act", "activation", out=junk[:], in_=t1[b2][:].rearrange("p h d -> p (h d)"),
                     func=AF.Square, accum_out=rs[:, g:g + 1])
                fw.I("dve", "tensor_scalar", out=rs[:, 4 + g:5 + g], in0=rs[:, g:g + 1], scalar1=1.0 / 512,
                     scalar2=float(RMS_EPS), op0=ALU.mult, op1=ALU.add)
                fw.I("act", "activation", out=rs[:, 4 + g:5 + g], in_=rs[:, 4 + g:5 + g], func=AF.Sqrt)
                fw.I("dve", "reciprocal", out=rs[:, 4 + g:5 + g], in_=rs[:, 4 + g:5 + g])
                fw.I("dve", "scalar_tensor_tensor", out=yn[a][:, g * 512:(g + 1) * 512],
                     in0=t1[b2][:].rearrange("p h d -> p (h d)"), scalar=rs[:, 4 + g:5 + g],
                     in1=nw[:, g * 512:(g + 1) * 512], op0=ALU.mult, op1=ALU.mult)

                if g == 3:
                    fw.D("sp", out=self.YN[tok, :], in_=yn[a][:], _r=[self.tk["YN"]])


            pipeline((S // 128) * 4, [front, tailf])

    def ssd_layer(self, li, XT_src, XT_dst):
        j = li // 2
        fw = self.fw
        fw.barrier("sp", [self.xt_tok[0], self.xt_tok[1], self.tk["BCfm"], self.tk["XSBtm"], self.tk["YN"]])
        self.ssd_sweep1(j, XT_src)
        fw.barrier("sp", [self.tk["BCfm"], self.tk["XSBtm"]])
        self.ssd_sweep2(j, XT_src)
        fw.barrier("sp", [self.tk["YN"]])
        with fw.scope():
            ab = [fw.T("s3_a%d" % i, [128, 2048]) for i in range(2)]

            def loader(t):
                fw.D("sp", out=ab[t % 2][:], in_=self.YN[t * 128:(t + 1) * 128, :], _r=[self.tk["YN"]])
                return ab[t % 2]
            self.proj_ln("s3", li, 0, 16, self.ssd_w_out[j], loader, XT_dst)

    def rope_tables(self):
        fw, S, cst = self.fw, self.S, self.cst
        CW = min(1024, S)
        TWO_PI = float(2 * np.pi)
        C1 = 6.28125
        C2 = float(2 * np.pi - 6.28125)
        with fw.scope():
            posi = fw.T("r_posi", [128, CW], I32)
            ang = fw.T("r_ang", [128, CW])
            kq = fw.T("r_kq", [128, CW])
            ki = fw.T("r_ki", [128, CW], I32)
            r = fw.T("r_r", [128, CW])
            m = fw.T("r_m", [128, CW])
            sv = fw.T("r_sv", [128, CW])
            cv = fw.T("r_cv", [128, CW])
            col = lambda c: cst[:, c:c + 1]
            for c0 in range(0, S, CW):
                fw.D("sp", out=posi[:], in_=self.pos_in[0, c0:c0 + CW].partition_broadcast(128))
                fw.I("dve", "tensor_copy", out=ang[:], in_=posi[:])
                fw.I("dve", "tensor_scalar", out=ang[:], in0=ang[:], scalar1=col(C_INVF), scalar2=None, op0=ALU.mult)
                fw.I("dve", "tensor_scalar", out=kq[:], in0=ang[:], scalar1=float(1.0 / TWO_PI), scalar2=None, op0=ALU.mult)
                fw.I("dve", "tensor_copy", out=ki[:], in_=kq[:])
                fw.I("dve", "tensor_copy", out=kq[:], in_=ki[:])
                fw.I("dve", "scalar_tensor_tensor", out=r[:], in0=kq[:], scalar=-C1, in1=ang[:], op0=ALU.mult, op1=ALU.add)
                fw.I("dve", "scalar_tensor_tensor", out=r[:], in0=kq[:], scalar=-C2, in1=r[:], op0=ALU.mult, op1=ALU.add)
                fw.I("dve", "tensor_scalar", out=m[:], in0=r[:], scalar1=float(np.pi), scalar2=None, op0=ALU.is_gt)
                fw.I("dve", "scalar_tensor_tensor", out=r[:], in0=m[:], scalar=-TWO_PI, in1=r[:], op0=ALU.mult, op1=ALU.add)
                fw.I("dve", "tensor_scalar", out=m[:], in0=r[:], scalar1=float(-np.pi), scalar2=None, op0=ALU.is_lt)
                fw.I("dve", "scalar_tensor_tensor", out=r[:], in0=m[:], scalar=TWO_PI, in1=r[:], op0=ALU.mult, op1=ALU.add)
                fw.I("dve", "tensor_scalar", out=r[:], in0=r[:], scalar1=3.1415925, scalar2=-3.1415925, op0=ALU.min, op1=ALU.max)
                fw.I("act", "activation", out=sv[:], in_=r[:], func=AF.Sin)
                fw.I("dve", "scalar_tensor_tensor", out=m[:], in0=r[:], scalar=-1.0, in1=r[:], op0=ALU.mult, op1=ALU.max)
                fw.I("act", "activation", out=cv[:], in_=m[:], func=AF.Sin, scale=-1.0, bias=col(C_HALFPI))
                fw.I("dve", "tensor_scalar", out=cv[:], in0=cv[:], scalar1=col(C_M16), scalar2=col(C_1M16), op0=ALU.mult, op1=ALU.add)
                fw.I("dve", "tensor_scalar", out=sv[:], in0=sv[:], scalar1=col(C_SS), scalar2=None, op0=ALU.mult)
                fw.D("sp", out=self.ROPE[0, :, c0:c0 + CW], in_=cv[:], _r=[self.tk["ROPE"]])
                fw.D("sp", out=self.ROPE[1, :, c0:c0 + CW], in_=sv[:], _r=[self.tk["ROPE"]])
        fw.barrier("sp", [self.tk["ROPE"]])

    def attn_proj(self, j, XT_src):
        fw, S, cst = self.fw, self.S, self.cst
        CH = min(2048, S)
        with fw.scope():
            PmR = fw.T("a1_pm", [128, 128], F32R)
            fw.I("dve", "tensor_copy", out=PmR[:], in_=cst[:, C_PM:C_PM + 128])
            w = fw.T("a1_w", [128, 8, 1536], F32R)
            xch = fw.T("a1_x", [128, 8, CH], F32R)
            tchs = [fw.T("a1_t%d" % i, [128, 2, CH]) for i in range(2)]
            ps = [fw.P("a1_ps%d" % i, [128, 512]) for i in range(2)]
            pp = [fw.P("a1_pp%d" % i, [128, 512]) for i in range(2)]
            qsb = [fw.T("a1_q%d" % i, [128, 512], F32R) for i in range(2)]
            t1 = [fw.T("a1_t1%d" % i, [128, 512]) for i in range(2)]
            t2 = [fw.T("a1_t2%d" % i, [128, 512]) for i in range(2)]
            ob = [fw.T("a1_o%d" % i, [128, 512]) for i in range(2)]
            vb = [fw.T("a1_v%d" % i, [128, 512]) for i in range(2)]
            work = []
            chidx = 0
            for g in range(3):
                for ch in range(S // CH):
                    first = True
                    for sb in range(CH // 512):
                        for which in range(2):
                            for tl in range(4):
                                work.append(("qk", g, ch, chidx, first, sb, which, tl))
                                first = False
                    for bi in range(CH // 128):
                        work.append(("v", g, ch, chidx, False, bi, 0, 0))
                    chidx += 1

            def geom(g):
                d = ATT_PATTERNS[g][1]
                return d, S // d, CH // d

            def perm(v3, c0, width, d, ic):
                vv = v3.rearrange("p (i r) -> p r i", r=d)
                if ic >= width:
                    return vv[:, c0 // ic, (c0 % ic):(c0 % ic) + width]
                return vv[:, c0 // ic:c0 // ic + width // ic, :]

            def s0(i):
                kind, g, ch, cx, first, p1, which, tl = work[i]
                d, n, ic = geom(g)
                a = i % 2
                if first:
                    if ch == 0:
                        for k in range(8):
                            fw.D("pool", out=w[:, k, :], in_=self.attn_w_qkv[j, k * 128:(k + 1) * 128, g * 1536:(g + 1) * 1536])
                    fw.D("pool", out=xch[:], in_=self.XT[XT_src][:, :, ch * CH:(ch + 1) * CH].rearrange("k p t -> p k t"),
                         _r=[self.xt_tok[XT_src]])
                    fw.D("sp", out=tchs[cx % 2][:], in_=self.ROPE[:, :, ch * CH:(ch + 1) * CH].rearrange("c p t -> p c t"),
                         _r=[self.tk["ROPE"]])
                if kind == "qk":
                    wc = which * 512 + tl * 128
                    for k in range(8):
                        fw.I("pe", "matmul", out=ps[a][:], lhsT=w[:, k, wc:wc + 128], rhs=perm(xch[:, k, :], p1 * 512, 512, d, ic),
                             start=(k == 0), stop=(k == 7))
                else:
                    for k in range(8):
                        fw.I("pe", "matmul", out=ps[a][:], lhsT=perm(xch[:, k, :], p1 * 128, 128, d, ic), rhs=w[:, k, 1024:1536],
                             start=(k == 0), stop=(k == 7))

            def s1(i):
                kind = work[i][0]
                a = i % 2
                if kind == "qk":
                    fw.I("act", "copy", out=qsb[a][:], in_=ps[a][:])
                    fw.I("pe", "matmul", out=pp[a][:], lhsT=PmR[:], rhs=qsb[a][:], start=True, stop=True)
                else:
                    fw.I("act" if (i // 2) % 2 else "dve", "copy" if (i // 2) % 2 else "tensor_copy", out=vb[a][:], in_=ps[a][:])

            def s2(i):
                kind, g, ch, cx, first, p1, which, tl = work[i]
                d, n, ic = geom(g)
                a = i % 2
                if kind == "qk":
                    sb = p1
                    c0 = sb * 512
                    tch = tchs[cx % 2]
                    cview = perm(tch[:, 0, :], c0, 512, d, ic)
                    sview = perm(tch[:, 1, :], c0, 512, d, ic)
                    shp = list(cview.shape)
                    rs = (lambda t_: t_[:]) if len(shp) == 2 else (lambda t_: t_[:].rearrange("p (r i) -> p r i", r=shp[1]))
                    fw.I("dve", "tensor_tensor", out=rs(t1[a]), in0=rs(qsb[a]), in1=cview, op=ALU.mult)
                    fw.I("dve", "tensor_tensor", out=rs(t2[a]), in0=rs(pp[a]), in1=sview, op=ALU.mult)
                    fw.I("pool", "tensor_tensor", out=ob[a][:], in0=t1[a][:], in1=t2[a][:], op=ALU.add)
                    ct = (g * 2 + which) * 4 + tl
                    if ic >= 512:
                        r_ = c0 // ic
                        i0 = c0 % ic
                        dst = self.QK[ct, :, r_ * n + ch * ic + i0:r_ * n + ch * ic + i0 + 512]
                        src = ob[a][:]
                    else:
                        nr = 512 // ic
                        dst = self.QK[ct].rearrange("p (r n) -> p r n", r=d)[:, sb * nr:(sb + 1) * nr, ch * ic:(ch + 1) * ic]
                        src = ob[a][:].rearrange("p (r i) -> p r i", r=nr)
                    fw.D("sp", out=dst, in_=src, _r=[self.tk["QK"]])
                else:
                    c0 = p1 * 128
                    r_ = c0 // ic
                    i0 = c0 % ic
                    row0 = r_ * n + ch * ic + i0
                    fw.D("sp", out=self.VP[g, row0:row0 + 128, :], in_=vb[a][:], _r=[self.tk["VP"]])
            pipeline(len(work), [s0, s1, s2])

    def attn_core(self):
        fw, S, cst = self.fw, self.S, self.cst
        NT = self.NT
        mask = cst[:, C_MASK:C_MASK + 256]
        with fw.scope():
            QT = [fw.T("a2_q%d" % i, [128, 4, 128], F32R) for i in range(2)]
            QZ = [fw.T("a2_qz%d" % i, [128, 4, 2, 128], F32R) for i in range(2)]
            KT = [fw.T("a2_k%d" % i, [128, 4, 128], F32R) for i in range(4)]
            V = [fw.T("a2_v%d" % i, [128, 512], F32R) for i in range(4)]
            sc = [fw.T("a2_sc%d" % i, [128, 4, 256]) for i in range(2)]
            PT = [fw.T("a2_pt%d" % i, [128, 8, 128], F32R) for i in range(2)]
            st = [fw.T("a2_st%d" % i, [128, 4, 8]) for i in range(2)]
            oh = [fw.T("a2_oh%d" % i, [128, 8, 64]) for i in range(2)]
            ml = [fw.T("a2_ml%d" % i, [128, 16]) for i in range(2)]
            p_S = [fw.P("a2_pS%d" % i, [128, 4, 256]) for i in range(2)]
            p_T = fw.P("a2_pT", [128, 8, 128])
            p_O = fw.P("a2_pO", [128, 8, 64])

            def info(u):
                pbi, h4 = divmod(u, 2)
                g, pb = divmod(pbi, NT)
                d = ATT_PATTERNS[g][1]
                n = S // d
                nbr = n // 128
                has_prev = (pb % nbr) > 0
                return pbi, h4, g, pb, d, n, has_prev

            def aA(u):
                pbi, h4, g, pb, d, n, has_prev = info(u)
                a, b2 = pbi % 2, u % 2
                if h4 == 0:
                    cols = slice(pb * 128, (pb + 1) * 128)
                    qbase = (g * 2) * 4
                    fw.D("pool", out=QT[a][:], in_=self.QK[qbase:qbase + 4, :, cols].rearrange("t p c -> p t c"),
                         _r=[self.tk["QK"]])
                    fw.D("pool", out=KT[pbi % 4][:], in_=self.QK[qbase + 4:qbase + 8, :, cols].rearrange("t p c -> p t c"),
                         _r=[self.tk["QK"]])
                    fw.D("pool", out=V[pbi % 4][:], in_=self.VP[g, cols, :], _r=[self.tk["VP"]])
                    for hl in range(2):
                        fw.I("act", "activation", out=QZ[a][:, :, hl, :], in_=QT[a][:], func=AF.Copy,
                             scale=cst[:, C_HM0 + hl:C_HM0 + hl + 1])
                for hh in range(4):
                    h = h4 * 4 + hh
                    tl = h // 2
                    if has_prev:
                        fw.I("pe", "matmul", out=p_S[b2][:, hh, 0:128], lhsT=QZ[a][:, tl, h % 2, :],
                             rhs=KT[(pbi - 1) % 4][:, tl, :], start=True, stop=True)
                    fw.I("pe", "matmul", out=p_S[b2][:, hh, 128:256], lhsT=QZ[a][:, tl, h % 2, :],
                         rhs=KT[pbi % 4][:, tl, :], start=True, stop=True)

            def aB(u):
                pbi, h4, g, pb, d, n, has_prev = info(u)
                a, b2 = pbi % 2, u % 2
                k0 = 0 if has_prev else 128
                fw.I("dve", "scalar_tensor_tensor", out=sc[b2][:, :, k0:256], in0=p_S[b2][:, :, k0:256], scalar=0.125,
                     in1=mask[:, k0:256].unsqueeze(1).to_broadcast([128, 4, 256 - k0]), op0=ALU.mult, op1=ALU.add)
                mx = st[a][:, 0, h4 * 4:h4 * 4 + 4]
                nmx = st[a][:, 1, h4 * 4:h4 * 4 + 4]
                fw.I("dve", "tensor_reduce", out=mx, in_=sc[b2][:, :, k0:256], axis=AX.X, op=ALU.max)
                fw.I("dve", "tensor_scalar", out=nmx, in0=mx, scalar1=-1.0, scalar2=None, op0=ALU.mult)
                for hh in range(4):
                    h = h4 * 4 + hh
                    fw.I("act", "activation", out=sc[b2][:, hh, k0:256], in_=sc[b2][:, hh, k0:256], func=AF.Exp,
                         bias=st[a][:, 1, h:h + 1], accum_out=st[a][:, 2, h:h + 1])

            def aC(u):
                pbi, h4, g, pb, d, n, has_prev = info(u)
                b2 = u % 2
                k0 = 0 if has_prev else 128
                nkc = 2 if has_prev else 1
                for hh in range(4):
                    for c in range(nkc):
                        kc = k0 + c * 128
                        fw.I("pe", "transpose", out=p_T[:, hh * 2 + c, :], in_=sc[b2][:, hh, kc:kc + 128],
                             identity=self.ident)
                eng, meth = ("act", "copy") if h4 else ("dve", "tensor_copy")
                if has_prev:
                    fw.I(eng, meth, out=PT[b2][:], in_=p_T[:])
                else:
                    fw.I(eng, meth, out=PT[b2][:].rearrange("p (h c) q -> p h c q", c=2)[:, :, 0, :],
                         in_=p_T[:].rearrange("p (h c) q -> p h c q", c=2)[:, :, 0, :])

            def aD(u):
                pbi, h4, g, pb, d, n, has_prev = info(u)
                a, b2 = pbi % 2, u % 2
                nkc = 2 if has_prev else 1
                for hh in range(4):
                    h = h4 * 4 + hh
                    for c in range(nkc):
                        vsrc = V[(pbi - 1) % 4] if (has_prev and c == 0) else V[pbi % 4]
                        fw.I("pe", "matmul", out=p_O[:, h, :], lhsT=PT[b2][:, hh * 2 + c, :], rhs=vsrc[:, h * 64:(h + 1) * 64],
                             start=(c == 0), stop=(c == nkc - 1))
                if h4 == 1:
                    fw.I("dve", "reciprocal", out=st[a][:, 3, :], in_=st[a][:, 2, :])
                    fw.I("dve", "tensor_tensor", out=oh[a][:], in0=p_O[:],
                         in1=st[a][:, 3, :].unsqueeze(2).to_broadcast([128, 8, 64]), op=ALU.mult)
                    fw.I("pool", "tensor_copy", out=ml[a][:, 0:8], in_=st[a][:, 0, :])
                    fw.I("pool", "tensor_copy", out=ml[a][:, 8:16], in_=st[a][:, 2, :])
                    r_ = (pb * 128) // n
                    i0 = (pb * 128) % n
                    ao_dst = self.AO[g].rearrange("(i r) c -> r i c", r=d)[r_, i0:i0 + 128, :]
                    ml_dst = self.AML[g].rearrange("(i r) c -> r i c", r=d)[r_, i0:i0 + 128, :]
                    fw.D("sp", out=ao_dst, in_=oh[a][:].rearrange("p h e -> p (h e)"), _r=[self.tk["AO"]])
                    fw.D("sp", out=ml_dst, in_=ml[a][:], _r=[self.tk["AML"]])
            pipeline(3 * NT * 2, [aA, aB, aC, aD])

    def attn_layer(self, li, XT_src, XT_dst):
        fw = self.fw
        j = li // 2
        fw.barrier("sp", [self.xt_tok[0], self.xt_tok[1], self.tk["QK"], self.tk["VP"], self.tk["AO"], self.tk["AML"]])
        self.attn_proj(j, XT_src)
        fw.barrier("sp", [self.tk["QK"], self.tk["VP"]])
        self.attn_core()
        fw.barrier("sp", [self.tk["AO"], self.tk["AML"]])
        with fw.scope():
            ao = [fw.T("a3_ao%d" % i, [128, 3, 512]) for i in range(2)]
            am = [fw.T("a3_ml%d" % i, [128, 3, 16]) for i in range(2)]
            sm = [fw.T("a3_sm%d" % i, [128, 4, 24]) for i in range(2)]
            mg = [fw.T("a3_mg%d" % i, [128, 512]) for i in range(2)]
            tmp = fw.T("a3_tmp", [128, 512])

            def loader(t):
                a = t % 2
                rows = slice(t * 128, (t + 1) * 128)
                fw.D("sp", out=ao[a][:], in_=self.AO[:, rows, :].rearrange("g p c -> p g c"), _r=[self.tk["AO"]])
                fw.D("sp", out=am[a][:], in_=self.AML[:, rows, :].rearrange("g p c -> p g c"), _r=[self.tk["AML"]])
                M = sm[a][:, 0, 0:8]
                fw.I("dve", "tensor_tensor", out=M, in0=am[a][:, 0, 0:8], in1=am[a][:, 1, 0:8], op=ALU.max)
                fw.I("dve", "tensor_tensor", out=M, in0=M, in1=am[a][:, 2, 0:8], op=ALU.max)
                wg = sm[a][:, 1, :].rearrange("p (g h) -> p g h", g=3)
                fw.I("dve", "tensor_tensor", out=wg, in0=am[a][:, :, 0:8], in1=M.unsqueeze(1).to_broadcast([128, 3, 8]),
                     op=ALU.subtract)
                fw.I("act", "activation", out=sm[a][:, 1, :], in_=sm[a][:, 1, :], func=AF.Exp)
                fw.I("dve", "tensor_tensor", out=wg, in0=wg, in1=am[a][:, :, 8:16], op=ALU.mult)
                den = sm[a][:, 2, 0:8]
                fw.I("dve", "tensor_tensor", out=den, in0=sm[a][:, 1, 0:8], in1=sm[a][:, 1, 8:16], op=ALU.add)
                fw.I("dve", "tensor_tensor", out=den, in0=den, in1=sm[a][:, 1, 16:24], op=ALU.add)
                fw.I("dve", "reciprocal", out=sm[a][:, 2, 8:16], in_=den)
                fw.I("dve", "tensor_tensor", out=wg, in0=wg, in1=sm[a][:, 2, 8:16].unsqueeze(1).to_broadcast([128, 3, 8]),
                     op=ALU.mult)
                v3 = lambda t_: t_.rearrange("p (h e) -> p h e", h=8)
                bc = lambda gi: sm[a][:, 1, gi * 8:(gi + 1) * 8].unsqueeze(2).to_broadcast([128, 8, 64])
                fw.I("dve", "tensor_tensor", out=v3(mg[a][:]), in0=v3(ao[a][:, 0, :]), in1=bc(0), op=ALU.mult)
                for gi in (1, 2):
                    fw.I("pool", "tensor_tensor", out=v3(tmp[:]), in0=v3(ao[a][:, gi, :]), in1=bc(gi), op=ALU.mult)
                    fw.I("dve", "tensor_tensor", out=mg[a][:], in0=mg[a][:], in1=tmp[:], op=ALU.add)
                return mg[a]
            self.proj_ln("a3", li, 0, 4, self.attn_w_o[j], loader, XT_dst)

    def zero_xrows(self):
        fw = self.fw
        with fw.scope():
            z = fw.T("z_zero", [128, 4, 1024])
            fw.I("dve", "memset", ap=z[:], constant=0.0)
            nq = self.NBLK * RT
            for q0 in range(0, nq, 4):
                qn = min(4, nq - q0)
                fw.D("act", out=self.XROWS[q0 * 128:(q0 + qn) * 128, :].rearrange("(q p) c -> p q c", p=128),
                     in_=z[:, 0:qn, :], _r=[self.tk["XROWS"]])

    def moe_layer(self, li, XT_src, XT_dst, lw=None):
        fw, NT, NB = self.fw, self.NT, self.NBLK
        lw = li if lw is None else lw
        cst = self.cst
        Lmat = cst[:, C_L:C_L + 128]
        fw.barrier("sp", [self.xt_tok[0], self.xt_tok[1], self.tk["XROWS"], self.tk["YROWS"]])
        with fw.scope():
            OH1 = fw.T("m_oh1", [128, NT, 32])
            OH2 = fw.T("m_oh2", [128, NT, 32])
            RANK = fw.T("m_rank", [128, NT, 32])
            GT = fw.T("m_gt", [128, NT, 2])
            DEST = fw.T("m_dest", [128, 2, NT])
            DESTi = fw.T("m_desti", [128, 2, NT], I32)
            WIDX = fw.T("m_widx", [128, NB], I32)
            run = fw.T("m_run", [128, 32])
            with fw.scope():
                wr = fw.T("m_wr", [128, 8, 36], F32R)
                fw.D("pool", out=wr[:], in_=self.w_router[li].rearrange("(k p) n -> p k n", p=128))
                brb = fw.T("m_brb", [128, 36])
                self.bc_load("sp", brb[:], self.b_router[li])
                fw.I("dve", "memset", ap=run[:], constant=0.0)
                xTc = [fw.T("m_xT%d" % i, [128, 8, 128], F32R) for i in range(2)]
                smt = [fw.T("m_sm%d" % i, [128, 128]) for i in range(2)]
                p_lg = fw.P("m_plg", [128, 64])
                p_rk = fw.P("m_prk", [128, 64])
                for t in range(NT):
                    a = t % 2
                    sm = smt[a]
                    lg = sm[:, 0:36]
                    gmax, gsum, gw, m1, m2, w1, tmp, ngmax = [sm[:, 36 + i:37 + i] for i in range(8)]
                    gmask, gexp = sm[:, 44:48], sm[:, 48:52]
                    esel, mask1, esel2, mask2 = sm[:, 52:60], sm[:, 60:68], sm[:, 68:76], sm[:, 76:84]
                    A = sm[:, 84:116]
                    fw.D("pool", out=xTc[a][:], in_=self.XT[XT_src][:, :, t * 128:(t + 1) * 128].rearrange("k p t -> p k t"),
                         _r=[self.xt_tok[XT_src]])
                    for k in range(8):
                        fw.I("pe", "matmul", out=p_lg[:, 0:36], lhsT=xTc[a][:, k, :], rhs=wr[:, k, :],
                             start=(k == 0), stop=(k == 7))
                    fw.I("dve", "tensor_tensor", out=lg, in0=p_lg[:, 0:36], in1=brb[:], op=ALU.add)
                    fw.I("dve", "tensor_reduce", out=gmax, in_=sm[:, 0:4], axis=AX.X, op=ALU.max)
                    fw.I("dve", "tensor_scalar", out=gmask, in0=sm[:, 0:4], scalar1=gmax, scalar2=None, op0=ALU.is_equal)
                    fw.I("dve", "tensor_scalar", out=ngmax, in0=gmax, scalar1=-1.0, scalar2=None, op0=ALU.mult)
                    fw.I("act", "activation", out=gexp, in_=sm[:, 0:4], func=AF.Exp, bias=ngmax, accum_out=gsum)
                    fw.I("dve", "reciprocal", out=gw, in_=gsum)
                    fw.I("dve", "tensor_scalar", out=esel, in0=sm[:, 4:12], scalar1=sm[:, 44:45], scalar2=None, op0=ALU.mult)
                    for g in range(1, 4):
                        fw.I("dve", "scalar_tensor_tensor", out=esel, in0=sm[:, 4 + 8 * g:12 + 8 * g],
                             scalar=sm[:, 44 + g:45 + g], in1=esel, op0=ALU.mult, op1=ALU.add)
                    fw.I("dve", "tensor_reduce", out=m1, in_=esel, axis=AX.X, op=ALU.max)
                    fw.I("dve", "tensor_scalar", out=mask1, in0=esel, scalar1=m1, scalar2=None, op0=ALU.is_equal)
                    fw.I("dve", "scalar_tensor_tensor", out=esel2, in0=mask1, scalar=-1e30, in1=esel,
                         op0=ALU.mult, op1=ALU.add)
                    fw.I("dve", "tensor_reduce", out=m2, in_=esel2, axis=AX.X, op=ALU.max)
                    fw.I("dve", "tensor_scalar", out=mask2, in0=esel2, scalar1=m2, scalar2=None, op0=ALU.is_equal)
                    fw.I("dve", "tensor_tensor", out=tmp, in0=m2, in1=m1, op=ALU.subtract)
                    fw.I("act", "activation", out=tmp, in_=tmp, func=AF.Exp)
                    fw.I("dve", "tensor_scalar", out=tmp, in0=tmp, scalar1=1.0, scalar2=None, op0=ALU.add)
                    fw.I("dve", "reciprocal", out=w1, in_=tmp)
                    fw.I("dve", "tensor_tensor", out=GT[:, t, 0:1], in0=gw, in1=w1, op=ALU.mult)
                    fw.I("dve", "tensor_tensor", out=GT[:, t, 1:2], in0=gw, in1=GT[:, t, 0:1], op=ALU.subtract)
                    fw.I("dve", "tensor_tensor", out=OH1[:, t, :].rearrange("p (g e) -> p g e", g=4),
                         in0=gmask.unsqueeze(2).to_broadcast([128, 4, 8]),
                         in1=mask1.unsqueeze(1).to_broadcast([128, 4, 8]), op=ALU.mult)
                    fw.I("dve", "tensor_tensor", out=OH2[:, t, :].rearrange("p (g e) -> p g e", g=4),
                         in0=gmask.unsqueeze(2).to_broadcast([128, 4, 8]),
                         in1=mask2.unsqueeze(1).to_broadcast([128, 4, 8]), op=ALU.mult)
                    fw.I("dve", "tensor_tensor", out=A, in0=OH1[:, t, :], in1=OH2[:, t, :], op=ALU.add)
                    fw.I("pe", "matmul", out=p_rk[:, 0:32], lhsT=Lmat, rhs=A, start=True, stop=True)
                    fw.I("pe", "matmul", out=p_rk[:, 32:64], lhsT=self.ones, rhs=A, start=True, stop=True)
                    fw.I("dve", "tensor_tensor", out=RANK[:, t, :], in0=p_rk[:, 0:32], in1=run[:], op=ALU.add)
                    fw.I("dve", "tensor_tensor", out=run[:], in0=run[:], in1=p_rk[:, 32:64], op=ALU.add)
            with fw.scope():
                NG = -(-self.S // BR)
                cmp = fw.T("m_cmp", [128, 32, NG])
                nblk = fw.T("m_nblk", [128, 32])
                padded = fw.T("m_padded", [128, 32])
                padT = fw.T("m_padT", [32, 128])
                pend = fw.T("m_pend", [128, 32])
                pstart = fw.T("m_pstart", [128, 32])
                tmp3 = fw.T("m_tmp3", [128, NT, 32])
                tmp4 = fw.T("m_tmp4", [128, NT, 32])
                cmpb = fw.T("m_cmpb", [128, NB, 32])
                be = fw.T("m_be", [128, NB])
                p_a = fw.P("m_pa", [128, 128])
                p_b = fw.P("m_pb", [128, 32])
                grid = cst[:, C_BLK:C_BLK + NB]
                fw.I("dve", "tensor_tensor", out=cmp[:], in0=run[:].unsqueeze(2).to_broadcast([128, 32, NG]),
                     in1=grid[:, 0:NG].unsqueeze(1).to_broadcast([128, 32, NG]), op=ALU.is_gt)
                fw.I("dve", "tensor_reduce", out=nblk[:], in_=cmp[:], axis=AX.X, op=ALU.add)
                fw.I("dve", "tensor_scalar", out=padded[:], in0=nblk[:], scalar1=float(BR), scalar2=None, op0=ALU.mult)
                fw.I("pe", "transpose", out=p_a[0:32, :], in_=padded[:, 0:32], identity=self.ident)
                fw.I("act", "copy", out=padT[:], in_=p_a[0:32, :])
                fw.I("pe", "matmul", out=p_b[:], lhsT=padT[:], rhs=cst[0:32, C_U:C_U + 32], start=True, stop=True)
                fw.I("act", "copy", out=pend[:], in_=p_b[:])
                fw.I("dve", "tensor_tensor", out=pstart[:], in0=pend[:], in1=padded[:], op=ALU.subtract)
                fw.I("dve", "tensor_tensor", out=tmp3[:], in0=RANK[:],
                     in1=pstart[:].unsqueeze(1).to_broadcast([128, NT, 32]), op=ALU.add)
                for k, OH in enumerate((OH1, OH2)):
                    fw.I("dve", "tensor_tensor", out=tmp4[:], in0=tmp3[:], in1=OH[:], op=ALU.mult)
                    fw.I("dve", "tensor_reduce", out=DEST[:, k, :], in_=tmp4[:], axis=AX.X, op=ALU.add)
                fw.I("dve", "tensor_copy", out=DESTi[:], in_=DEST[:])
                fw.I("dve", "tensor_tensor", out=cmpb[:], in0=grid.unsqueeze(2).to_broadcast([128, NB, 32]),
                     in1=pend[:].unsqueeze(1).to_broadcast([128, NB, 32]), op=ALU.is_ge)
                fw.I("dve", "tensor_reduce", out=be[:], in_=cmpb[:], axis=AX.X, op=ALU.add)
                fw.I("dve", "tensor_scalar", out=be[:], in0=be[:], scalar1=32.0, scalar2=128.0, op0=ALU.min, op1=ALU.mult)
                fw.I("dve", "tensor_scalar", out=be[:], in0=be[:], scalar1=cst[:, C_PIDX:C_PIDX + 1], scalar2=None,
                     op0=ALU.add)
                fw.I("dve", "tensor_copy", out=WIDX[:], in_=be[:])
            with fw.scope():
                xt = [fw.T("m_x%d" % i, [128, 1024]) for i in range(2)]
                for t in range(NT):
                    a = t % 2
                    fw.D("sp", out=xt[a][:], in_=self.XA[t * 128:(t + 1) * 128, :], _r=[self.xa_tok[t]])
                    for k in range(2):
                        fw.D("pool", _meth="indirect_dma_start", out=self.XROWS[:, :],
                             out_offset=bass.IndirectOffsetOnAxis(ap=DESTi[:, k, t:t + 1], axis=0),
                             in_=xt[a][:], in_offset=None, _r=[self.tk["XROWS"]])
            fw.barrier("sp", [self.tk["XROWS"]])
            with fw.scope():
                gu = [fw.T("m_gu%d" % i, [128, 8, 1024], F32R) for i in range(2)]
                dn = [fw.T("m_dn%d" % i, [128, 4, 1024], F32R) for i in range(2)]
                xb4 = [fw.T("m_xb%d" % i, [128, RT, 1024]) for i in range(2)]
                xbT = fw.T("m_xbT", [128, 8, BR], F32R)
                hT = [fw.T("m_hT%d" % i, [128, 4, BR], F32R) for i in range(2)]
                sg = [fw.T("m_sg%d" % i, [128, BR]) for i in range(2)]
                yb = [fw.T("m_yb%d" % i, [128, 1024]) for i in range(2)]
                p_t = [fw.P("m_pt%d" % i, [128, 4, 128]) for i in range(2)]
                p_g = [fw.P("m_pg%d" % i, [128, BR]) for i in range(2)]
                p_u = [fw.P("m_pu%d" % i, [128, BR]) for i in range(2)]
                p_y = fw.P("m_py", [128, 1024])
                gu_src = self.moe_gu[lw][:, :]
                dn_src = self.moe_dn[lw][:, :]

                def mL(B):
                    fw.D("pool", _meth="indirect_dma_start", out=gu[B % 2][:].rearrange("p k n -> p (k n)"), out_offset=None,
                         in_=gu_src, in_offset=bass.IndirectOffsetOnAxis(ap=WIDX[:, B:B + 1], axis=0),
                         bounds_check=4095, oob_is_err=False)
                    fw.D("sp", out=xb4[B % 2][:], in_=self.XROWS[B * BR:(B + 1) * BR, :].rearrange("(r p) c -> p r c", p=128),
                         _r=[self.tk["XROWS"]])

                def mAB(B):
                    x4, g_ = xb4[B % 2], gu[B % 2]
                    fw.D("pool", _meth="indirect_dma_start", out=dn[B % 2][:].rearrange("p k n -> p (k n)"), out_offset=None,
                         in_=dn_src, in_offset=bass.IndirectOffsetOnAxis(ap=WIDX[:, B:B + 1], axis=0),
                         bounds_check=4095, oob_is_err=False)
                    cnt = 0
                    for rt in range(RT):
                        for k0 in (0, 4):
                            pt = p_t[cnt % 2]
                            for k in range(4):
                                fw.I("pe", "transpose", out=pt[:, k, :], in_=x4[:, rt, (k0 + k) * 128:(k0 + k + 1) * 128],
                                     identity=self.ident)
                            eng, meth = ("act", "copy") if cnt % 2 else ("dve", "tensor_copy")
                            fw.I(eng, meth, out=xbT[:, k0:k0 + 4, rt * 128:(rt + 1) * 128], in_=pt[:])
                            cnt += 1
                    for fc in range(4):
                        b2 = fc % 2
                        for k in range(8):
                            fw.I("pe", "matmul", out=p_g[b2][:], lhsT=g_[:, k, fc * 128:(fc + 1) * 128], rhs=xbT[:, k, :],
                                 start=(k == 0), stop=(k == 7))
                        for k in range(8):
                            fw.I("pe", "matmul", out=p_u[b2][:], lhsT=g_[:, k, 512 + fc * 128:512 + (fc + 1) * 128],
                                 rhs=xbT[:, k, :], start=(k == 0), stop=(k == 7))
                        fw.I("act", "activation", out=sg[b2][:], in_=p_g[b2][:], func=AF.Silu)
                        fw.I("dve", "tensor_tensor", out=hT[B % 2][:, fc, :], in0=sg[b2][:], in1=p_u[b2][:], op=ALU.mult)

                def mC(B):
                    h_, d_ = hT[B % 2], dn[B % 2]
                    for rt in range(RT):
                        for n in range(2):
                            for k in range(4):
                                fw.I("pe", "matmul", out=p_y[:, n * 512:(n + 1) * 512], lhsT=h_[:, k, rt * 128:(rt + 1) * 128],
                                     rhs=d_[:, k, n * 512:(n + 1) * 512], start=(k == 0), stop=(k == 3))
                        fw.I("act" if rt % 2 else "dve", "copy" if rt % 2 else "tensor_copy", out=yb[rt % 2][:], in_=p_y[:])
                        u = B * RT + rt
                        fw.D("sp", out=self.YROWS[u * 128:(u + 1) * 128, :], in_=yb[rt % 2][:], _r=[self.tk["YROWS"]])
                pipeline(NB, [mL, mAB, mC])
            fw.barrier("sp", [self.tk["YROWS"]])
            with fw.scope():
                gbc = fw.T("m_g", [128, 1024])
                bbc = fw.T("m_b", [128, 1024])
                self.bc_load("sp", gbc[:], self.ln_g[li, 1])
                self.bc_load("sp", bbc[:], self.ln_b[li, 1])
                Y1 = [fw.T("m_y1%d" % i, [128, 1024]) for i in range(2)]
                Y2 = [fw.T("m_y2%d" % i, [128, 1024]) for i in range(2)]
                ffn = [fw.T("m_ffn%d" % i, [128, 1024]) for i in range(2)]
                ln = self.ln_alloc("m4")
                def cA(t):
                    a = t % 2
                    for k, Y in enumerate((Y1, Y2)):
                        fw.D("pool", _meth="indirect_dma_start", out=Y[a][:], out_offset=None, in_=self.YROWS[:, :],
                             in_offset=bass.IndirectOffsetOnAxis(ap=DESTi[:, k, t:t + 1], axis=0), _r=[self.tk["YROWS"]])
                    fw.I("dve", "tensor_scalar", out=ffn[a][:], in0=Y1[a][:], scalar1=GT[:, t, 0:1], scalar2=None,
                         op0=ALU.mult)
                    fw.I("dve", "scalar_tensor_tensor", out=ffn[a][:], in0=Y2[a][:], scalar=GT[:, t, 1:2], in1=ffn[a][:],
                         op0=ALU.mult, op1=ALU.add)
                pipeline(NT, [cA] + self.ln_stages(ln, lambda t: ffn[t % 2][:], gbc, bbc, XT_dst))

    def build(self):
        fw = self.fw
        self.initial()
        if self.with_moe:
            self.zero_xrows()
        if any(li % 2 == 1 for li in self.layers):
            self.rope_tables()
        cur = 0
        last_x = "XA"
        for li in self.layers:
            if li % 2 == 0:
                self.ssd_layer(li, cur, 1 - cur)
            else:
                self.attn_layer(li, cur, 1 - cur)
            cur = 1 - cur
            if self.stop_after == (li, 0):
                break
            self.moe_layer(li, cur, 1 - cur, self.moe_layers.index(li))
            cur = 1 - cur
            if self.stop_after == (li, 1):
                break
        with fw.scope():
            ot = [fw.T("o_t%d" % i, [128, 1024]) for i in range(2)]
            for t in range(self.NT):
                fw.D("sp", out=ot[t % 2][:], in_=self.XA[t * 128:(t + 1) * 128, :], _r=[self.xa_tok[t]])
                fw.D("act", out=self.out[t * 128:(t + 1) * 128, :], in_=ot[t % 2][:])
        fw.finish(["out"])
        return self.nc


def _lay_gu(wg, wu):
    L = wg.shape[0]
    g = wg.reshape(L, 32, 8, 128, 512).transpose(0, 1, 3, 2, 4)
    u = wu.reshape(L, 32, 8, 128, 512).transpose(0, 1, 3, 2, 4)
    return np.ascontiguousarray(np.concatenate([g, u], -1)).reshape(L, 4096, 8192)


def _lay_dn(wd):
    L = wd.shape[0]
    return np.ascontiguousarray(wd.reshape(L, 32, 4, 128, 1024).transpose(0, 1, 3, 2, 4)).reshape(L, 4096, 4096)


def _run(inputs, S, ncores, layers=(0, 1, 2, 3)):
    f32 = lambda a: np.ascontiguousarray(np.asarray(a, dtype=np.float32))
    prog = Prog(S, layers=layers)
    nc = prog.build()
    base = {k: f32(inputs[k]) for k in ("ssd_w_in", "ssd_conv_w", "ssd_conv_b", "ssd_dt_bias", "ssd_a_log", "ssd_d",
                                        "ssd_norm_w", "ssd_w_out", "attn_w_qkv", "attn_w_o", "ln_g", "ln_b")}
    base["consts"] = make_consts(prog.NBLK)
    base["w_router"] = f32(np.concatenate([np.asarray(inputs["moe_w_router_group"]),
                                           np.asarray(inputs["moe_w_router_expert"])], -1))
    base["b_router"] = f32(np.concatenate([np.asarray(inputs["moe_b_router_group"]),
                                           np.asarray(inputs["moe_b_router_expert"])], -1))
    gu = _lay_gu(f32(inputs["moe_w_gate"]), f32(inputs["moe_w_up"]))
    dn = _lay_dn(f32(inputs["moe_w_down"]))
    for i, li in enumerate(prog.moe_layers):
        base["moe_gu%d" % i] = gu[li]
        base["moe_dn%d" % i] = dn[li]
    x = f32(inputs["x"])
    pos = np.ascontiguousarray(np.asarray(inputs["positions"]).astype(np.int32))
    in_maps = []
    for c in range(ncores):
        d = dict(base)
        d["x"] = np.ascontiguousarray(x[c, :S])
        d["pos"] = np.ascontiguousarray(pos[c:c + 1, :S])
        in_maps.append(d)
    res = run_bass_kernel_spmd(nc, in_maps, core_ids=list(range(ncores)))
    return np.stack([np.asarray(res.results[c]["out"]) for c in range(ncores)]).astype(np.float32)


def kernel(**inputs):
    x = np.asarray(inputs["x"])
    return _run(inputs, x.shape[1], x.shape[0])
```

```python
import numpy as np
import concourse.bass as bass
import concourse.mybir as mybir
from concourse.bass_utils import run_bass_kernel_spmd

F32 = mybir.dt.float32
F32R = mybir.dt.float32r
I32 = mybir.dt.int32
ALU = mybir.AluOpType
AF = mybir.ActivationFunctionType
AX = mybir.AxisListType

D_MODEL = 1024
DEPTH = 4
ALPHA = (2 * DEPTH) ** 0.25
LN_EPS = 1e-5
RMS_EPS = 1e-5
NEG = -30000.0
ATT_PATTERNS = ((128, 1), (512, 4), (2048, 16))
ROPE_THETA = 500000.0
NBLK_EXTRA = 32
BR = 256
RT = BR // 128


class Buf:
    __slots__ = ("name", "w", "r")

    def __init__(self, name, init_r=None):
        self.name = name
        self.w = None
        self.r = dict(init_r) if init_r else {}


WRITE_KEYS = ("out", "accum_out", "ap")


class FW:
    ENGS = ("pe", "dve", "act", "pool", "sp")
    EPOCH = 30000

    def __init__(self, nc, n_dma_sems=48):
        self.nc = nc
        self.q = {e: [] for e in self.ENGS}
        self.sems = {}
        self._sem_ctx = []
        self._ctxs = []
        self.bufs = {}
        self.cur = {}
        self.waited = {e: {} for e in self.ENGS}
        self.free_events = {}
        for e in self.ENGS:
            self._new_epoch(e)
        self.dma_keys = []
        self.dma_uses = {}
        for i in range(n_dma_sems):
            k = self._alloc_sem("dma%d" % i)
            self.dma_keys.append(k)
            self.dma_uses[k] = 0
        self.dma_rr = 0
        self.ninst = 0
        self.uid = 0
        self._regs = {}

    def _alloc_sem(self, name):
        cm = self.nc.semaphore(name)
        h = cm.__enter__()
        self._sem_ctx.append(cm)
        self.sems[name] = h
        return name

    def _new_epoch(self, e):
        idx = sum(1 for k in self.sems if k.startswith("e_" + e + "_"))
        k = self._alloc_sem("e_%s_%d" % (e, idx))
        self.cur[e] = [k, 0]

    def T(self, name, shape, dtype=F32):
        self.uid += 1
        name = "%s_%d" % (name, self.uid)
        cm = self.nc.sbuf_tensor(name, list(shape), dtype)
        t = cm.__enter__()
        self._ctxs.append((name, cm))
        self.bufs[name] = Buf(name, self.free_events)
        return t

    def P(self, name, shape, dtype=F32):
        self.uid += 1
        name = "%s_%d" % (name, self.uid)
        cm = self.nc.psum_tensor(name, list(shape), dtype)
        t = cm.__enter__()
        self._ctxs.append((name, cm))
        self.bufs[name] = Buf(name, self.free_events)
        return t

    def dram(self, name, shape, dtype=F32, kind="Internal", track=True):
        t = self.nc.dram_tensor(name, list(shape), dtype, kind=kind)
        if track:
            self.bufs[name] = Buf(name)
        return t.ap()

    def token(self, name):
        b = Buf(name)
        self.bufs[name] = b
        return b

    def scope(self):
        return _Scope(self)

    def _collect(self, kw, xr, xw):
        reads, writes = [], []
        for k, v in kw.items():
            if isinstance(v, bass.IndirectOffsetOnAxis):
                v = v.ap
                k = "idx"
            if isinstance(v, bass.AP):
                b = self.bufs.get(v.tensor.name)
                if b is not None:
                    (writes if k in WRITE_KEYS else reads).append(b)
        for b in xr or ():
            reads.append(self.bufs[b] if isinstance(b, str) else b)
        for b in xw or ():
            writes.append(self.bufs[b] if isinstance(b, str) else b)
        return reads, writes

    def _deps(self, reads, writes):
        deps = {}
        for b in reads:
            if b.w is not None and deps.get(b.w[0], 0) < b.w[1]:
                deps[b.w[0]] = b.w[1]
        for b in writes:
            if b.w is not None and deps.get(b.w[0], 0) < b.w[1]:
                deps[b.w[0]] = b.w[1]
            for k, v in b.r.items():
                if deps.get(k, 0) < v:
                    deps[k] = v
        return deps

    def _emit_waits(self, e, deps):
        wt = self.waited[e]
        for k, v in deps.items():
            if e == "pe" and k.startswith("e_pe_"):
                continue
            if wt.get(k, 0) >= v:
                continue
            wt[k] = v
            h = self.sems[k]
            self.q[e].append(lambda eng, h=h, v=v: eng.wait_ge(h, v))

    def _update(self, ev, reads, writes):
        k, v = ev
        for b in reads:
            if b.r.get(k, 0) < v:
                b.r[k] = v
        for b in writes:
            b.w = ev
            b.r = {}

    def I(self, e, meth, _r=None, _w=None, **kw):
        reads, writes = self._collect(kw, _r, _w)
        deps = self._deps(reads, writes)
        self._emit_waits(e, deps)
        cur = self.cur[e]
        if cur[1] >= self.EPOCH:
            self._new_epoch(e)
            cur = self.cur[e]
        cur[1] += 1
        k, v = cur[0], cur[1]
        h = self.sems[k]
        self.q[e].append(lambda eng, meth=meth, kw=kw, h=h: getattr(eng, meth)(**kw).then_inc(h, 1))
        self._update((k, v), reads, writes)
        self.ninst += 1

    def D(self, e, _r=None, _w=None, _meth="dma_start", **kw):
        reads, writes = self._collect(kw, _r, _w)
        deps = self._deps(reads, writes)
        k = self.dma_keys[self.dma_rr % len(self.dma_keys)]
        self.dma_rr += 1
        prev = 16 * self.dma_uses[k]
        if prev:
            deps[k] = max(deps.get(k, 0), prev)
        self._emit_waits(e, deps)
        self.dma_uses[k] += 1
        v = 16 * self.dma_uses[k]
        h = self.sems[k]
        if isinstance(kw.get("bounds_check"), int):
            bv = kw.pop("bounds_check")

            def emit(eng, meth=_meth, kw=kw, h=h, bv=bv):
                key = ("bound", e, bv)
                if key not in self._regs:
                    self._regs[key] = eng.to_reg(bv)
                return getattr(eng, meth)(bounds_check=self._regs[key], **kw).then_inc(h, 16)
            self.q[e].append(emit)
        else:
            self.q[e].append(lambda eng, meth=_meth, kw=kw, h=h: getattr(eng, meth)(**kw).then_inc(h, 16))
        self._update((k, v), reads, writes)
        self.ninst += 1

    def barrier(self, e, tok_names):
        self.I(e, "nop", _w=list(tok_names))

    def finish(self, final_names):
        reads = [self.bufs[n] for n in final_names]
        deps = self._deps(reads, [])
        for e in self.ENGS:
            self._emit_waits(e, dict(deps))
        nc = self.nc
        with nc.Block() as block:
            @block.tensor
            def _(eng):
                for f in self.q["pe"]:
                    f(eng)

            @block.vector
            def _(eng):
                for f in self.q["dve"]:
                    f(eng)

            @block.scalar
            def _(eng):
                for f in self.q["act"]:
                    f(eng)

            @block.gpsimd
            def _(eng):
                for f in self.q["pool"]:
                    f(eng)

            @block.sync
            def _(eng):
                for f in self.q["sp"]:
                    f(eng)
        for name, cm in reversed(self._ctxs):
            cm.__exit__(None, None, None)
        for cm in reversed(self._sem_ctx):
            cm.__exit__(None, None, None)


class _Scope:
    def __init__(self, fw):
        self.fw = fw

    def __enter__(self):
        self.mark = len(self.fw._ctxs)
        return self

    def __exit__(self, *a):
        fw = self.fw
        fe = fw.free_events
        while len(fw._ctxs) > self.mark:
            name, cm = fw._ctxs.pop()
            b = fw.bufs.pop(name)
            if b.w is not None and fe.get(b.w[0], 0) < b.w[1]:
                fe[b.w[0]] = b.w[1]
            for k, v in b.r.items():
                if fe.get(k, 0) < v:
                    fe[k] = v
            cm.__exit__(None, None, None)
        return False


def pipeline(n, stages):
    ns = len(stages)
    for t in range(n + ns - 1):
        for si, f in enumerate(stages):
            i = t - si
            if 0 <= i < n:
                f(i)


C_ID, C_U, C_ONES, C_L, C_PM, C_MASK, C_PIDX = 0, 128, 256, 384, 512, 640, 896
C_INVF, C_M16, C_1M16, C_SS, C_HALFPI, C_HM0, C_HM1, C_EIDX, C_BLK = 897, 898, 899, 900, 901, 902, 903, 904, 936


def make_consts(nblk):
    w = C_BLK + nblk
    c = np.zeros((128, w), np.float32)
    i = np.arange(128)
    c[:, C_ID:C_ID + 128] = np.eye(128, dtype=np.float32)
    c[:, C_U:C_U + 128] = (i[:, None] <= i[None, :])
    c[:, C_ONES:C_ONES + 128] = 1.0
    c[:, C_L:C_L + 128] = (i[:, None] < i[None, :])
    pm = np.zeros((128, 128), np.float32)
    for dp in range(128):
        if dp % 64 < 16:
            d = (dp // 64) * 64 + ((dp % 64) ^ 8)
            pm[d, dp] = 1.0
    c[:, C_PM:C_PM + 128] = pm
    c[:, C_MASK:C_MASK + 128] = np.where(i[None, :] >= i[:, None], 0.0, NEG)
    c[:, C_MASK + 128:C_MASK + 256] = np.where(i[None, :] <= i[:, None], 0.0, NEG)
    c[:, C_PIDX] = i
    dd = i % 64
    invf = (np.float32(ROPE_THETA) ** (-np.arange(0, 16, 2, dtype=np.float32) / np.float32(16))).astype(np.float32)
    m16 = (dd < 16).astype(np.float32)
    c[:, C_INVF] = np.where(dd < 16, invf[dd % 8], 0.0)
    c[:, C_M16] = m16
    c[:, C_1M16] = 1.0 - m16
    c[:, C_SS] = m16 * np.where(dd < 8, -1.0, 1.0)
    c[:, C_HALFPI] = np.float32(np.pi / 2)
    c[:, C_HM0] = (i < 64)
    c[:, C_HM1] = (i >= 64)
    c[:, C_EIDX:C_EIDX + 32] = np.arange(32)[None, :]
    c[:, C_BLK:C_BLK + nblk] = float(BR) * np.arange(nblk)[None, :]
    return c


class Prog:
    def __init__(self, S, layers=(0, 1, 2, 3), stop_after=None, with_moe=True, moe_layers=(0, 1, 2, 3)):
        self.S = S
        self.with_moe = with_moe
        self.NT = S // 128
        self.layers = tuple(layers)
        self.stop_after = stop_after
        self.NBLK = -(-(2 * S + 32 * (BR - 1)) // BR)
        nc = self.nc = bass.Bass("TRN2", target_bir_lowering=False)
        fw = self.fw = FW(nc)
        S_ = S
        ext = lambda n, s, d=F32: fw.dram(n, s, d, kind="ExternalInput")
        self.x_in = ext("x", [S_, 1024])
        self.pos_in = ext("pos", [1, S_], I32)
        self.consts_in = ext("consts", [128, C_BLK + self.NBLK])
        self.ssd_w_in = ext("ssd_w_in", [2, 1024, 5152])
        self.ssd_conv_w = ext("ssd_conv_w", [2, 4, 3072])
        self.ssd_conv_b = ext("ssd_conv_b", [2, 3072])
        self.ssd_dt_bias = ext("ssd_dt_bias", [2, 32])
        self.ssd_a_log = ext("ssd_a_log", [2, 32])
        self.ssd_d = ext("ssd_d", [2, 32])
        self.ssd_norm_w = ext("ssd_norm_w", [2, 2048])
        self.ssd_w_out = ext("ssd_w_out", [2, 2048, 1024])
        self.attn_w_qkv = ext("attn_w_qkv", [2, 1024, 4608])
        self.attn_w_o = ext("attn_w_o", [2, 512, 1024])
        self.ln_g = ext("ln_g", [4, 2, 1024])
        self.ln_b = ext("ln_b", [4, 2, 1024])
        self.w_router = ext("w_router", [4, 1024, 36])
        self.b_router = ext("b_router", [4, 36])
        self.moe_layers = tuple(moe_layers)
        if with_moe:
            nl = len(self.moe_layers)
            self.moe_gu = [ext("moe_gu%d" % i, [32 * 128, 8 * 1024]) for i in range(nl)]
            self.moe_dn = [ext("moe_dn%d" % i, [32 * 128, 4 * 1024]) for i in range(nl)]
        self.out = fw.dram("out", [S_, 1024], F32, kind="ExternalOutput")
        sc = lambda n, sh, d=F32: fw.dram(n, sh, d, track=False)
        self.XA = sc("XA", [S_, 1024])
        self.XT = [sc("XT0", [8, 128, S_]), sc("XT1", [8, 128, S_])]
        self.BCfm = sc("BCfm", [8, 128, S_])
        self.XSBtm = sc("XSBtm", [S_, 2560])
        self.YN = sc("YN", [S_, 2048])
        self.XROWS = sc("XROWS", [self.NBLK * BR, 1024])
        self.YROWS = sc("YROWS", [self.NBLK * BR, 1024])
        self.AO = sc("AO", [3, S_, 512])
        self.AML = sc("AML", [3, S_, 16])
        self.ROPE = sc("ROPE", [2, 128, S_])
        self.QK = sc("QK", [24, 128, S_])
        self.VP = sc("VP", [3, S_, 512])
        self.xa_tok = [fw.token("xa%d" % t) for t in range(self.NT)]
        self.xt_tok = [fw.token("xt0"), fw.token("xt1")]
        self.tk = {n: fw.token("tk_" + n) for n in ("BCfm", "XSBtm", "YN", "XROWS", "YROWS", "AO", "AML", "ROPE", "QK", "VP")}
        self.cst = fw.T("cst", [128, C_BLK + self.NBLK])
        fw.dummy = fw.T("fwdummy", [128, 4])
        fw.D("sp", out=self.cst[:], in_=self.consts_in[:, :])
        self.eps_ln = fw.T("eps_ln", [128, 2])
        fw.I("dve", "memset", ap=self.eps_ln[:, 0:1], constant=float(LN_EPS))
        fw.I("dve", "memset", ap=self.eps_ln[:, 1:2], constant=1.0)
        self.ident = self.cst[:, C_ID:C_ID + 128]
        self.U = self.cst[:, C_U:C_U + 128]
        self.ones = self.cst[:, C_ONES:C_ONES + 128]

    def bc_load(self, q, dst, src_row):
        self.fw.D(q, out=dst, in_=src_row.partition_broadcast(128))

    def initial(self):
        fw = self.fw
        with fw.scope():
            xt = [fw.T("i_x%d" % i, [128, 1024]) for i in range(2)]
            tp = [fw.P("i_tp%d" % i, [128, 8, 128]) for i in range(2)]
            ts = [fw.T("i_ts%d" % i, [128, 8, 128]) for i in range(2)]
            for t in range(self.NT):
                a = t % 2
                fw.D("sp", out=xt[a][:], in_=self.x_in[t * 128:(t + 1) * 128, :])
                fw.D("act", out=self.XA[t * 128:(t + 1) * 128, :], in_=xt[a][:], _w=[self.xa_tok[t]])
                self.transpose_store(xt[a], tp[a], ts[a], 0, t)

    def transpose_store(self, xtile, tp, ts, XT_dst, t):
        fw = self.fw
        for c in range(8):
            fw.I("pe", "transpose", out=tp[:, c, :], in_=xtile[:, c * 128:(c + 1) * 128], identity=self.ident)
        fw.I("act", "copy", out=ts[:], in_=tp[:])
        fw.D("sp", out=self.XT[XT_dst][:, :, t * 128:(t + 1) * 128].rearrange("k p t -> p k t"), in_=ts[:],
             _r=[self.xt_tok[XT_dst]])

    def proj_ln(self, tag, li, sub, kch, w_src, a_loader, XT_dst):
        fw = self.fw
        with fw.scope():
            w = fw.T(tag + "_w", [128, kch, 1024], F32R)
            for k in range(kch):
                fw.D("pool", out=w[:, k, :], in_=w_src[k * 128:(k + 1) * 128, :])
            gbc = fw.T(tag + "_g", [128, 1024])
            bbc = fw.T(tag + "_b", [128, 1024])
            self.bc_load("sp", gbc[:], self.ln_g[li, sub])
            self.bc_load("sp", bbc[:], self.ln_b[li, sub])
            atp = [fw.P(tag + "_atp%d" % i, [128, 4, 128]) for i in range(2)]
            aT = [fw.T(tag + "_aT%d" % i, [128, kch, 128], F32R) for i in range(2)]
            mp = [fw.P(tag + "_mp%d" % i, [128, 1024]) for i in range(2)]
            ln = self.ln_alloc(tag)

            def stA(t):
                a = t % 2
                A = a_loader(t)
                for k0 in range(0, kch, 4):
                    kn = min(4, kch - k0)
                    for k in range(kn):
                        fw.I("pe", "transpose", out=atp[(k0 // 4) % 2][:, k, :],
                             in_=A[:, (k0 + k) * 128:(k0 + k + 1) * 128], identity=self.ident)
                    fw.I("act" if (k0 // 4) % 2 else "dve", "copy" if (k0 // 4) % 2 else "tensor_copy",
                         out=aT[a][:, k0:k0 + kn, :], in_=atp[(k0 // 4) % 2][:, 0:kn, :])
                for n in range(2):
                    for k in range(kch):
                        fw.I("pe", "matmul", out=mp[a][:, n * 512:(n + 1) * 512], lhsT=aT[a][:, k, :],
                             rhs=w[:, k, n * 512:(n + 1) * 512], start=(k == 0), stop=(k == kch - 1))
            pipeline(self.NT, [stA] + self.ln_stages(ln, lambda t: mp[t % 2][:], gbc, bbc, XT_dst))

    def ln_alloc(self, tag):
        fw = self.fw
        d = {}
        d["x"] = [fw.T(tag + "_lx%d" % i, [128, 1024]) for i in range(4)]
        d["v"] = [fw.T(tag + "_lv%d" % i, [128, 1024]) for i in range(4)]
        d["junk"] = fw.T(tag + "_lj", [128, 1024])
        d["st"] = [fw.T(tag + "_ls%d" % i, [128, 8]) for i in range(4)]
        d["tp"] = fw.P(tag + "_ltp", [128, 8, 128])
        d["ts"] = [fw.T(tag + "_lts%d" % i, [128, 8, 128]) for i in range(2)]
        return d

    def ln_stages(self, ln, mix_of, gbc, bbc, XT_dst):
        fw = self.fw

        def b1(t):
            a = t % 4
            x, v, st, junk = ln["x"][a], ln["v"][a], ln["st"][a], ln["junk"]
            fw.D("sp", out=x[:], in_=self.XA[t * 128:(t + 1) * 128, :], _r=[self.xa_tok[t]])
            fw.I("dve", "scalar_tensor_tensor", out=v[:], in0=x[:], scalar=float(ALPHA), in1=mix_of(t),
                 op0=ALU.mult, op1=ALU.add)
            fw.I("act", "activation", out=junk[:], in_=v[:], func=AF.Identity, accum_out=st[:, 0:1])
            fw.I("act", "activation", out=junk[:], in_=v[:], func=AF.Square, accum_out=st[:, 1:2])

        def b2(t):
            st = ln["st"][t % 4]
            fw.I("dve", "tensor_scalar", out=st[:, 2:3], in0=st[:, 0:1], scalar1=1.0 / 1024, scalar2=None, op0=ALU.mult)
            fw.I("dve", "tensor_tensor", out=st[:, 3:4], in0=st[:, 2:3], in1=st[:, 2:3], op=ALU.mult)
            fw.I("dve", "scalar_tensor_tensor", out=st[:, 4:5], in0=st[:, 1:2], scalar=1.0 / 1024, in1=st[:, 3:4],
                 op0=ALU.mult, op1=ALU.subtract)
            fw.I("act", "activation", out=st[:, 6:7], in_=st[:, 4:5], func=AF.Sqrt, bias=self.eps_ln[:, 0:1])
            fw.I("dve", "reciprocal", out=st[:, 5:6], in_=st[:, 6:7])
            fw.I("dve", "scalar_tensor_tensor", out=st[:, 7:8], in0=st[:, 2:3], scalar=-1.0, in1=st[:, 5:6],
                 op0=ALU.mult, op1=ALU.mult)

        def b3(t):
            a = t % 4
            x, v, st = ln["x"][a], ln["v"][a], ln["st"][a]
            fw.I("act", "activation", out=v[:], in_=v[:], func=AF.Identity, scale=st[:, 5:6], bias=st[:, 7:8])
            fw.I("dve", "tensor_tensor", out=v[:], in0=v[:], in1=gbc[:], op=ALU.mult)
            fw.I("dve", "tensor_tensor", out=x[:], in0=v[:], in1=bbc[:], op=ALU.add)
            if getattr(self, "direct_out", False):
                fw.D("sp", out=self.out[t * 128:(t + 1) * 128, :], in_=x[:])
            else:
                fw.D("act", out=self.XA[t * 128:(t + 1) * 128, :], in_=x[:], _w=[self.xa_tok[t]])

        def c1(t):
            self.transpose_store(ln["x"][t % 4], ln["tp"], ln["ts"][t % 2], XT_dst, t)
        if getattr(self, "direct_out", False):
            return [b1, b2, b3]
        return [b1, b2, b3, c1]

    def ssd_sweep1(self, j, XT_src):
        fw, S = self.fw, self.S
        w_in = self.ssd_w_in
        with fw.scope():
            wx = fw.T("s1_wx", [128, 8, 3072], F32R)
            for k in range(8):
                fw.D("pool", out=wx[:, k, :], in_=w_in[j, k * 128:(k + 1) * 128, 2048:5120])
            cw = fw.T("s1_cw", [128, 4, 24])
            cb = fw.T("s1_cb", [128, 24])
            for k in range(4):
                fw.D("sp", out=cw[:, k, :], in_=self.ssd_conv_w[j, k].rearrange("(t p) -> p t", p=128),
                     allow_slow_non_contiguous=True)
            fw.D("sp", out=cb[:], in_=self.ssd_conv_b[j].rearrange("(t p) -> p t", p=128),
                 allow_slow_non_contiguous=True)
            halo = fw.T("s1_halo", [128, 24, 3])
            fw.I("dve", "memset", ap=halo[:], constant=0.0)
            xtb = [fw.T("s1_xt%d" % i, [128, 8, 512], F32R) for i in range(2)]
            ps = [fw.P("s1_ps%d" % i, [128, 512]) for i in range(2)]
            ub = [fw.T("s1_ub%d" % i, [128, 515]) for i in range(2)]
            acc = [fw.T("s1_acc%d" % i, [128, 512]) for i in range(2)]
            so = [fw.T("s1_so%d" % i, [128, 512]) for i in range(2)]
            tps = [fw.P("s1_tp%d" % i, [128, 4, 128]) for i in range(2)]
            tsb = [fw.T("s1_ts%d" % i, [128, 4, 128]) for i in range(2)]
            def s0(idx):
                tb, ct = divmod(idx, 24)
                a = idx % 2
                xt = xtb[tb % 2]
                if ct == 0:
                    fw.D("pool", out=xt[:], in_=self.XT[XT_src][:, :, tb * 512:(tb + 1) * 512].rearrange("k p t -> p k t"),
                         _r=[self.xt_tok[XT_src]])
                for k in range(8):
                    fw.I("pe", "matmul", out=ps[a][:], lhsT=wx[:, k, ct * 128:(ct + 1) * 128], rhs=xt[:, k, :],
                         start=(k == 0), stop=(k == 7))

            def s1(idx):
                tb, ct = divmod(idx, 24)
                a = idx % 2
                fw.I("pool", "tensor_copy", out=ub[a][:, 0:3], in_=halo[:, ct, :])
                fw.I("act", "copy", out=ub[a][:, 3:515], in_=ps[a][:])
                fw.I("pool", "tensor_copy", out=halo[:, ct, :], in_=ub[a][:, 512:515])
                fw.I("act", "activation", out=acc[a][:], in_=ub[a][:, 3:515], func=AF.Identity, scale=cw[:, 3, ct:ct + 1],
                     bias=cb[:, ct:ct + 1])

            def s1b(idx):
                tb, ct = divmod(idx, 24)
                a = idx % 2
                for kk in (2, 1, 0):
                    fw.I("dve", "scalar_tensor_tensor", out=acc[a][:], in0=ub[a][:, kk:kk + 512], scalar=cw[:, kk, ct:ct + 1],
                         in1=acc[a][:], op0=ALU.mult, op1=ALU.add)
                fw.I("act", "activation", out=so[a][:], in_=acc[a][:], func=AF.Silu)
                if ct >= 16:
                    fw.D("sp", out=self.BCfm[ct - 16, :, tb * 512:(tb + 1) * 512], in_=so[a][:], _r=[self.tk["BCfm"]])

            def s2(idx):
                tb, ct = divmod(idx, 24)
                a = idx % 2
                if ct < 20:
                    for q in range(4):
                        fw.I("pe", "transpose", out=tps[a][:, q, :], in_=so[a][:, q * 128:(q + 1) * 128],
                             identity=self.ident)
                    if idx % 2:
                        fw.I("act", "copy", out=tsb[a][:], in_=tps[a][:])
                    else:
                        fw.I("dve", "tensor_copy", out=tsb[a][:], in_=tps[a][:])
                    fw.D("sp", out=self.XSBtm[tb * 512:(tb + 1) * 512, ct * 128:(ct + 1) * 128]
                         .rearrange("(q p) c -> p q c", p=128), in_=tsb[a][:], _r=[self.tk["XSBtm"]])
            pipeline((S // 512) * 24, [s0, s1, s1b, s2])

    def ssd_sweep2(self, j, XT_src):
        fw, S = self.fw, self.S
        w_in = self.ssd_w_in
        with fw.scope():
            wz = fw.T("s2_wz", [128, 8, 2048], F32R)
            wdt = fw.T("s2_wdt", [128, 8, 32], F32R)
            for k in range(8):
                fw.D("pool", out=wz[:, k, :], in_=w_in[j, k * 128:(k + 1) * 128, 0:2048])
                fw.D("pool", out=wdt[:, k, :], in_=w_in[j, k * 128:(k + 1) * 128, 5120:5152])
            dtb = fw.T("s2_dtb", [128, 32])
            abc = fw.T("s2_a", [128, 32])
            dsk = fw.T("s2_dsk", [128, 32])
            nw = fw.T("s2_nw", [128, 2048])
            self.bc_load("sp", dtb[:], self.ssd_dt_bias[j])
            self.bc_load("sp", abc[:], self.ssd_a_log[j])
            self.bc_load("sp", dsk[:], self.ssd_d[j])
            self.bc_load("sp", nw[:], self.ssd_norm_w[j])
            fw.I("act", "activation", out=abc[:], in_=abc[:], func=AF.Exp)
            fw.I("dve", "tensor_scalar", out=abc[:], in0=abc[:], scalar1=-1.0, scalar2=None, op0=ALU.mult)
            prev = fw.T("s2_prev", [128, 4, 512])
            prevR = fw.T("s2_prevR", [128, 4, 512], F32R)
            fw.I("dve", "memset", ap=prev[:], constant=0.0)
            fw.I("dve", "tensor_copy", out=prevR[:], in_=prev[:])
            xTc = [fw.T("s2_xT%d" % i, [128, 8, 128], F32R) for i in range(2)]
            xs = [fw.T("s2_xs%d" % i, [128, 32, 64]) for i in range(2)]
            Btm = [fw.T("s2_Btm%d" % i, [128, 4, 128], F32R) for i in range(2)]
            Bfm = [fw.T("s2_Bfm%d" % i, [128, 4, 128], F32R) for i in range(2)]
            Cfm = [fw.T("s2_Cfm%d" % i, [128, 4, 128], F32R) for i in range(2)]
            sm = fw.T("s2_sm", [128, 12, 32])
            Uda = [fw.T("s2_Uda%d" % i, [128, 8, 128]) for i in range(2)]
            xr = fw.T("s2_xr", [128, 32, 64], F32R)
            xrd = fw.T("s2_xrd", [128, 32, 64], F32R)
            zs = [fw.T("s2_zs%d" % i, [128, 512]) for i in range(2)]
            cbm = [fw.T("s2_cbm%d" % i, [128, 128]) for i in range(2)]
            Dm = [fw.T("s2_D%d" % i, [128, 8, 128]) for i in range(2)]
            MT = [fw.T("s2_MT%d" % i, [128, 8, 128], F32R) for i in range(2)]
            t1 = [fw.T("s2_t1%d" % i, [128, 8, 64]) for i in range(2)]
            t2 = [fw.T("s2_t2%d" % i, [128, 8, 64]) for i in range(2)]
            yn = [fw.T("s2_yn0", [128, 2048])] * 2
            junk = fw.T("s2_junk", [128, 512])
            rs = fw.T("s2_rs", [128, 8])
            p_z = fw.P("s2_pz", [128, 512])
            p_sm = fw.P("s2_psm", [128, 512])
            p_R = fw.P("s2_pR", [128, 8, 128])
            p_Y = fw.P("s2_pY", [128, 8, 64])
            p_Yo = fw.P("s2_pYo", [128, 8, 64])
            p_S = fw.P("s2_pS", [128, 512])
            DT, DA, CS, ECS, DTE, CD, V0, V1, V2 = range(9)
            p_Y2 = [p_Y, fw.P("s2_pY2", [128, 8, 64])]

            def front(u):
                c, g = divmod(u, 4)
                a = c % 2
                tok = slice(c * 128, (c + 1) * 128)
                p_Y = p_Y2[u % 2]
                if g == 0:
                    fw.D("pool", out=xTc[a][:], in_=self.XT[XT_src][:, :, tok].rearrange("k p t -> p k t"),
                         _r=[self.xt_tok[XT_src]])
                    fw.D("sp", out=xs[a][:].rearrange("p h d -> p (h d)"), in_=self.XSBtm[tok, 0:2048], _r=[self.tk["XSBtm"]])
                    fw.D("pool", out=Btm[a][:].rearrange("p g n -> p (g n)"), in_=self.XSBtm[tok, 2048:2560],
                         _r=[self.tk["XSBtm"]])
                    fw.D("pool", out=Bfm[a][:], in_=self.BCfm[0:4, :, tok].rearrange("g p t -> p g t"), _r=[self.tk["BCfm"]])
                    fw.D("pool", out=Cfm[a][:], in_=self.BCfm[4:8, :, tok].rearrange("g p t -> p g t"), _r=[self.tk["BCfm"]])
                    for k in range(8):
                        fw.I("pe", "matmul", out=p_sm[:, 0:32], lhsT=xTc[a][:, k, :], rhs=wdt[:, k, :],
                             start=(k == 0), stop=(k == 7))
                    fw.I("dve", "tensor_tensor", out=sm[:, V0, :], in0=p_sm[:, 0:32], in1=dtb[:], op=ALU.add)
                    fw.I("dve", "scalar_tensor_tensor", out=sm[:, V1, :], in0=sm[:, V0, :], scalar=-1.0, in1=sm[:, V0, :],
                         op0=ALU.mult, op1=ALU.max)
                    fw.I("act", "activation", out=sm[:, V1, :], in_=sm[:, V1, :], func=AF.Exp, scale=-1.0)
                    fw.I("act", "activation", out=sm[:, V1, :], in_=sm[:, V1, :], func=AF.Ln, bias=self.eps_ln[:, 1:2])
                    fw.I("dve", "scalar_tensor_tensor", out=sm[:, DT, :], in0=sm[:, V0, :], scalar=0.0, in1=sm[:, V1, :],
                         op0=ALU.max, op1=ALU.add)
                    fw.I("dve", "tensor_tensor", out=sm[:, DA, :], in0=sm[:, DT, :], in1=abc[:], op=ALU.mult)
                    fw.I("pe", "matmul", out=p_sm[:, 32:64], lhsT=self.U, rhs=sm[:, DA, :], start=True, stop=True)
                    fw.I("act", "copy", out=sm[:, CS, :], in_=p_sm[:, 32:64])
                    fw.I("act", "activation", out=sm[:, ECS, :], in_=sm[:, CS, :], func=AF.Exp)
                    fw.I("pool", "tensor_tensor", out=xr[:], in0=xs[a][:],
                         in1=sm[:, DT, :].unsqueeze(2).to_broadcast([128, 32, 64]), op=ALU.mult)

                b2 = g % 2
                hs = slice(8 * g, 8 * g + 8)
                for k in range(8):
                    fw.I("pe", "matmul", out=p_z[:], lhsT=xTc[a][:, k, :], rhs=wz[:, k, g * 512:(g + 1) * 512],
                         start=(k == 0), stop=(k == 7))
                fw.I("act", "activation", out=zs[b2][:], in_=p_z[:], func=AF.Silu)
                if g == 0:
                    fw.I("dve", "tensor_tensor", out=Uda[b2][:], in0=self.U.unsqueeze(1).to_broadcast([128, 8, 128]),
                         in1=sm[:, DA, hs].unsqueeze(2).to_broadcast([128, 8, 128]), op=ALU.mult)
                for hh in range(2):
                    fw.I("pe", "matmul", out=p_R[:, 4 * hh:4 * hh + 4, :], lhsT=self.ones,
                         rhs=Uda[b2][:, 4 * hh:4 * hh + 4, :], start=True, stop=True)
                fw.I("dve", "tensor_tensor", out=sm[:, V2, hs], in0=p_R[:, :, 127], in1=sm[:, CS, hs], op=ALU.subtract)
                fw.I("act", "activation", out=sm[:, DTE, hs], in_=sm[:, V2, hs], func=AF.Exp)
                fw.I("act", "activation", out=sm[:, CD, hs], in_=p_R[:, :, 127], func=AF.Exp)
                fw.I("pool", "tensor_tensor", out=xrd[:, hs, :], in0=xr[:, hs, :],
                     in1=sm[:, DTE, hs].unsqueeze(2).to_broadcast([128, 8, 64]), op=ALU.mult)
                fw.I("pe", "matmul", out=p_sm[:, 128:256], lhsT=Bfm[a][:, g, :], rhs=Cfm[a][:, g, :],
                     start=True, stop=True)
                fw.I("dve", "tensor_tensor", out=cbm[b2][:], in0=p_sm[:, 128:256], in1=self.U, op=ALU.mult)
                fw.I("dve", "tensor_tensor", out=Dm[b2][:], in0=p_R[:],
                     in1=sm[:, CS, hs].unsqueeze(2).to_broadcast([128, 8, 128]), op=ALU.subtract)
                fw.I("act", "activation", out=Dm[b2][:], in_=Dm[b2][:], func=AF.Exp)
                fw.I("dve", "scalar_tensor_tensor", out=MT[b2][:], in0=Dm[b2][:], scalar=1.0,
                     in1=cbm[b2][:].unsqueeze(1).to_broadcast([128, 8, 128]), op0=ALU.min, op1=ALU.mult)
                if g < 3:
                    fw.I("dve", "tensor_tensor", out=Uda[1 - b2][:], in0=self.U.unsqueeze(1).to_broadcast([128, 8, 128]),
                         in1=sm[:, DA, 8 * g + 8:8 * g + 16].unsqueeze(2).to_broadcast([128, 8, 128]), op=ALU.mult)
                for h in range(8):
                    fw.I("pe", "matmul", out=p_Y[:, h, :], lhsT=MT[b2][:, h, :], rhs=xr[:, 8 * g + h, :],
                         start=True, stop=True)
                fw.I("pe", "matmul", out=p_Yo[:].rearrange("p h d -> p (h d)"), lhsT=Cfm[a][:, g, :],
                     rhs=prevR[:, g, :], start=True, stop=True)
                fw.I("pe", "matmul", out=p_S[:], lhsT=Btm[a][:, g, :],
                     rhs=xrd[:, hs, :].rearrange("p h d -> p (h d)"), start=True, stop=True)

                fw.I("dve", "tensor_tensor", out=t1[b2][:], in0=p_Yo[:],
                     in1=sm[:, ECS, hs].unsqueeze(2).to_broadcast([128, 8, 64]), op=ALU.mult)

                fw.I("pool", "tensor_tensor", out=prev[:, g, :].rearrange("p (h d) -> p h d", h=8),
                     in0=prev[:, g, :].rearrange("p (h d) -> p h d", h=8),
                     in1=sm[:, CD, hs].unsqueeze(2).to_broadcast([128, 8, 64]), op=ALU.mult)
                fw.I("dve", "tensor_tensor", out=prev[:, g, :], in0=prev[:, g, :], in1=p_S[:], op=ALU.add)
                fw.I("act", "copy", out=prevR[:, g, :], in_=prev[:, g, :])


            def tailf(u):
                c, g = divmod(u, 4)
                a = c % 2
                tok = slice(c * 128, (c + 1) * 128)
                p_Y = p_Y2[u % 2]
                b2 = g % 2
                hs = slice(8 * g, 8 * g + 8)
                fw.I("dve", "tensor_tensor", out=t1[b2][:], in0=t1[b2][:], in1=p_Y[:], op=ALU.add)
                fw.I("pool", "tensor_tensor", out=t2[b2][:], in0=xs[a][:, hs, :],
                     in1=dsk[:, hs].unsqueeze(2).to_broadcast([128, 8, 64]), op=ALU.mult)
                fw.I("pool", "tensor_tensor", out=t1[b2][:], in0=t1[b2][:], in1=t2[b2][:], op=ALU.add)
                fw.I("dve", "tensor_tensor", out=t1[b2][:].rearrange("p h d -> p (h d)"),
                     in0=t1[b2][:].rearrange("p h d -> p (h d)"), in1=zs[b2][:], op=ALU.mult)
                fw.I("act", "activation", out=junk[:], in_=t1[b2][:].rearrange("p h d -> p (h d)"),
                     func=AF.Square, accum_out=rs[:, g:g + 1])
                fw.I("dve", "tensor_scalar", out=rs[:, 4 + g:5 + g], in0=rs[:, g:g + 1], scalar1=1.0 / 512,
                     scalar2=float(RMS_EPS), op0=ALU.mult, op1=ALU.add)
                fw.I("act", "activation", out=rs[:, 4 + g:5 + g], in_=rs[:, 4 + g:5 + g], func=AF.Sqrt)
                fw.I("dve", "reciprocal", out=rs[:, 4 + g:5 + g], in_=rs[:, 4 + g:5 + g])
                fw.I("dve", "scalar_tensor_tensor", out=yn[a][:, g * 512:(g + 1) * 512],
                     in0=t1[b2][:].rearrange("p h d -> p (h d)"), scalar=rs[:, 4 + g:5 + g],
                     in1=nw[:, g * 512:(g + 1) * 512], op0=ALU.mult, op1=ALU.mult)

                if g == 3:
                    fw.D("sp", out=self.YN[tok, :], in_=yn[a][:], _r=[self.tk["YN"]])


            pipeline((S // 128) * 4, [front, tailf])

    def ssd_layer(self, li, XT_src, XT_dst):
        j = li // 2
        fw = self.fw
        fw.barrier("sp", [self.xt_tok[0], self.xt_tok[1], self.tk["BCfm"], self.tk["XSBtm"], self.tk["YN"]])
        self.ssd_sweep1(j, XT_src)
        fw.barrier("sp", [self.tk["BCfm"], self.tk["XSBtm"]])
        self.ssd_sweep2(j, XT_src)
        fw.barrier("sp", [self.tk["YN"]])
        with fw.scope():
            ab = [fw.T("s3_a%d" % i, [128, 2048]) for i in range(2)]

            def loader(t):
                fw.D("sp", out=ab[t % 2][:], in_=self.YN[t * 128:(t + 1) * 128, :], _r=[self.tk["YN"]])
                return ab[t % 2]
            self.proj_ln("s3", li, 0, 16, self.ssd_w_out[j], loader, XT_dst)

    def rope_tables(self):
        fw, S, cst = self.fw, self.S, self.cst
        CW = min(1024, S)
        TWO_PI = float(2 * np.pi)
        C1 = 6.28125
        C2 = float(2 * np.pi - 6.28125)
        with fw.scope():
            posi = fw.T("r_posi", [128, CW], I32)
            ang = fw.T("r_ang", [128, CW])
            kq = fw.T("r_kq", [128, CW])
            ki = fw.T("r_ki", [128, CW], I32)
            r = fw.T("r_r", [128, CW])
            m = fw.T("r_m", [128, CW])
            sv = fw.T("r_sv", [128, CW])
            cv = fw.T("r_cv", [128, CW])
            col = lambda c: cst[:, c:c + 1]
            for c0 in range(0, S, CW):
                fw.D("sp", out=posi[:], in_=self.pos_in[0, c0:c0 + CW].partition_broadcast(128))
                fw.I("dve", "tensor_copy", out=ang[:], in_=posi[:])
                fw.I("dve", "tensor_scalar", out=ang[:], in0=ang[:], scalar1=col(C_INVF), scalar2=None, op0=ALU.mult)
                fw.I("dve", "tensor_scalar", out=kq[:], in0=ang[:], scalar1=float(1.0 / TWO_PI), scalar2=None, op0=ALU.mult)
                fw.I("dve", "tensor_copy", out=ki[:], in_=kq[:])
                fw.I("dve", "tensor_copy", out=kq[:], in_=ki[:])
                fw.I("dve", "scalar_tensor_tensor", out=r[:], in0=kq[:], scalar=-C1, in1=ang[:], op0=ALU.mult, op1=ALU.add)
                fw.I("dve", "scalar_tensor_tensor", out=r[:], in0=kq[:], scalar=-C2, in1=r[:], op0=ALU.mult, op1=ALU.add)
                fw.I("dve", "tensor_scalar", out=m[:], in0=r[:], scalar1=float(np.pi), scalar2=None, op0=ALU.is_gt)
                fw.I("dve", "scalar_tensor_tensor", out=r[:], in0=m[:], scalar=-TWO_PI, in1=r[:], op0=ALU.mult, op1=ALU.add)
                fw.I("dve", "tensor_scalar", out=m[:], in0=r[:], scalar1=float(-np.pi), scalar2=None, op0=ALU.is_lt)
                fw.I("dve", "scalar_tensor_tensor", out=r[:], in0=m[:], scalar=TWO_PI, in1=r[:], op0=ALU.mult, op1=ALU.add)
                fw.I("dve", "tensor_scalar", out=r[:], in0=r[:], scalar1=3.1415925, scalar2=-3.1415925, op0=ALU.min, op1=ALU.max)
                fw.I("act", "activation", out=sv[:], in_=r[:], func=AF.Sin)
                fw.I("dve", "scalar_tensor_tensor", out=m[:], in0=r[:], scalar=-1.0, in1=r[:], op0=ALU.mult, op1=ALU.max)
                fw.I("act", "activation", out=cv[:], in_=m[:], func=AF.Sin, scale=-1.0, bias=col(C_HALFPI))
                fw.I("dve", "tensor_scalar", out=cv[:], in0=cv[:], scalar1=col(C_M16), scalar2=col(C_1M16), op0=ALU.mult, op1=ALU.add)
                fw.I("dve", "tensor_scalar", out=sv[:], in0=sv[:], scalar1=col(C_SS), scalar2=None, op0=ALU.mult)
                fw.D("sp", out=self.ROPE[0, :, c0:c0 + CW], in_=cv[:], _r=[self.tk["ROPE"]])
                fw.D("sp", out=self.ROPE[1, :, c0:c0 + CW], in_=sv[:], _r=[self.tk["ROPE"]])
        fw.barrier("sp", [self.tk["ROPE"]])

    def attn_proj(self, j, XT_src):
        fw, S, cst = self.fw, self.S, self.cst
        CH = min(2048, S)
        with fw.scope():
            PmR = fw.T("a1_pm", [128, 128], F32R)
            fw.I("dve", "tensor_copy", out=PmR[:], in_=cst[:, C_PM:C_PM + 128])
            w = fw.T("a1_w", [128, 8, 1536], F32R)
            xch = fw.T("a1_x", [128, 8, CH], F32R)
            tchs = [fw.T("a1_t%d" % i, [128, 2, CH]) for i in range(2)]
            ps = [fw.P("a1_ps%d" % i, [128, 512]) for i in range(2)]
            pp = [fw.P("a1_pp%d" % i, [128, 512]) for i in range(2)]
            qsb = [fw.T("a1_q%d" % i, [128, 512], F32R) for i in range(2)]
            t1 = [fw.T("a1_t1%d" % i, [128, 512]) for i in range(2)]
            t2 = [fw.T("a1_t2%d" % i, [128, 512]) for i in range(2)]
            ob = [fw.T("a1_o%d" % i, [128, 512]) for i in range(2)]
            vb = [fw.T("a1_v%d" % i, [128, 512]) for i in range(2)]
            work = []
            chidx = 0
            for g in range(3):
                for ch in range(S // CH):
                    first = True
                    for sb in range(CH // 512):
                        for which in range(2):
                            for tl in range(4):
                                work.append(("qk", g, ch, chidx, first, sb, which, tl))
                                first = False
                    for bi in range(CH // 128):
                        work.append(("v", g, ch, chidx, False, bi, 0, 0))
                    chidx += 1

            def geom(g):
                d = ATT_PATTERNS[g][1]
                return d, S // d, CH // d

            def perm(v3, c0, width, d, ic):
                vv = v3.rearrange("p (i r) -> p r i", r=d)
                if ic >= width:
                    return vv[:, c0 // ic, (c0 % ic):(c0 % ic) + width]
                return vv[:, c0 // ic:c0 // ic + width // ic, :]

            def s0(i):
                kind, g, ch, cx, first, p1, which, tl = work[i]
                d, n, ic = geom(g)
                a = i % 2
                if first:
                    if ch == 0:
                        for k in range(8):
                            fw.D("pool", out=w[:, k, :], in_=self.attn_w_qkv[j, k * 128:(k + 1) * 128, g * 1536:(g + 1) * 1536])
                    fw.D("pool", out=xch[:], in_=self.XT[XT_src][:, :, ch * CH:(ch + 1) * CH].rearrange("k p t -> p k t"),
                         _r=[self.xt_tok[XT_src]])
                    fw.D("sp", out=tchs[cx % 2][:], in_=self.ROPE[:, :, ch * CH:(ch + 1) * CH].rearrange("c p t -> p c t"),
                         _r=[self.tk["ROPE"]])
                if kind == "qk":
                    wc = which * 512 + tl * 128
                    for k in range(8):
                        fw.I("pe", "matmul", out=ps[a][:], lhsT=w[:, k, wc:wc + 128], rhs=perm(xch[:, k, :], p1 * 512, 512, d, ic),
                             start=(k == 0), stop=(k == 7))
                else:
                    for k in range(8):
                        fw.I("pe", "matmul", out=ps[a][:], lhsT=perm(xch[:, k, :], p1 * 128, 128, d, ic), rhs=w[:, k, 1024:1536],
                             start=(k == 0), stop=(k == 7))

            def s1(i):
                kind = work[i][0]
                a = i % 2
                if kind == "qk":
                    fw.I("act", "copy", out=qsb[a][:], in_=ps[a][:])
                    fw.I("pe", "matmul", out=pp[a][:], lhsT=PmR[:], rhs=qsb[a][:], start=True, stop=True)
                else:
                    fw.I("act" if (i // 2) % 2 else "dve", "copy" if (i // 2) % 2 else "tensor_copy", out=vb[a][:], in_=ps[a][:])

            def s2(i):
                kind, g, ch, cx, first, p1, which, tl = work[i]
                d, n, ic = geom(g)
                a = i % 2
                if kind == "qk":
                    sb = p1
                    c0 = sb * 512
                    tch = tchs[cx % 2]
                    cview = perm(tch[:, 0, :], c0, 512, d, ic)
                    sview = perm(tch[:, 1, :], c0, 512, d, ic)
                    shp = list(cview.shape)
                    rs = (lambda t_: t_[:]) if len(shp) == 2 else (lambda t_: t_[:].rearrange("p (r i) -> p r i", r=shp[1]))
                    fw.I("dve", "tensor_tensor", out=rs(t1[a]), in0=rs(qsb[a]), in1=cview, op=ALU.mult)
                    fw.I("dve", "tensor_tensor", out=rs(t2[a]), in0=rs(pp[a]), in1=sview, op=ALU.mult)
                    fw.I("pool", "tensor_tensor", out=ob[a][:], in0=t1[a][:], in1=t2[a][:], op=ALU.add)
                    ct = (g * 2 + which) * 4 + tl
                    if ic >= 512:
                        r_ = c0 // ic
                        i0 = c0 % ic
                        dst = self.QK[ct, :, r_ * n + ch * ic + i0:r_ * n + ch * ic + i0 + 512]
                        src = ob[a][:]
                    else:
                        nr = 512 // ic
                        dst = self.QK[ct].rearrange("p (r n) -> p r n", r=d)[:, sb * nr:(sb + 1) * nr, ch * ic:(ch + 1) * ic]
                        src = ob[a][:].rearrange("p (r i) -> p r i", r=nr)
                    fw.D("sp", out=dst, in_=src, _r=[self.tk["QK"]])
                else:
                    c0 = p1 * 128
                    r_ = c0 // ic
                    i0 = c0 % ic
                    row0 = r_ * n + ch * ic + i0
                    fw.D("sp", out=self.VP[g, row0:row0 + 128, :], in_=vb[a][:], _r=[self.tk["VP"]])
            pipeline(len(work), [s0, s1, s2])

    def attn_core(self):
        fw, S, cst = self.fw, self.S, self.cst
        NT = self.NT
        mask = cst[:, C_MASK:C_MASK + 256]
        with fw.scope():
            QT = [fw.T("a2_q%d" % i, [128, 4, 128], F32R) for i in range(2)]
            QZ = [fw.T("a2_qz%d" % i, [128, 4, 2, 128], F32R) for i in range(2)]
            KT = [fw.T("a2_k%d" % i, [128, 4, 128], F32R) for i in range(4)]
            V = [fw.T("a2_v%d" % i, [128, 512], F32R) for i in range(4)]
            sc = [fw.T("a2_sc%d" % i, [128, 4, 256]) for i in range(2)]
            PT = [fw.T("a2_pt%d" % i, [128, 8, 128], F32R) for i in range(2)]
            st = [fw.T("a2_st%d" % i, [128, 4, 8]) for i in range(2)]
            oh = [fw.T("a2_oh%d" % i, [128, 8, 64]) for i in range(2)]
            ml = [fw.T("a2_ml%d" % i, [128, 16]) for i in range(2)]
            p_S = [fw.P("a2_pS%d" % i, [128, 4, 256]) for i in range(2)]
            p_T = fw.P("a2_pT", [128, 8, 128])
            p_O = fw.P("a2_pO", [128, 8, 64])

            def info(u):
                pbi, h4 = divmod(u, 2)
                g, pb = divmod(pbi, NT)
                d = ATT_PATTERNS[g][1]
                n = S // d
                nbr = n // 128
                has_prev = (pb % nbr) > 0
                return pbi, h4, g, pb, d, n, has_prev

            def aA(u):
                pbi, h4, g, pb, d, n, has_prev = info(u)
                a, b2 = pbi % 2, u % 2
                if h4 == 0:
                    cols = slice(pb * 128, (pb + 1) * 128)
                    qbase = (g * 2) * 4
                    fw.D("pool", out=QT[a][:], in_=self.QK[qbase:qbase + 4, :, cols].rearrange("t p c -> p t c"),
                         _r=[self.tk["QK"]])
                    fw.D("pool", out=KT[pbi % 4][:], in_=self.QK[qbase + 4:qbase + 8, :, cols].rearrange("t p c -> p t c"),
                         _r=[self.tk["QK"]])
                    fw.D("pool", out=V[pbi % 4][:], in_=self.VP[g, cols, :], _r=[self.tk["VP"]])
                    for hl in range(2):
                        fw.I("act", "activation", out=QZ[a][:, :, hl, :], in_=QT[a][:], func=AF.Copy,
                             scale=cst[:, C_HM0 + hl:C_HM0 + hl + 1])
                for hh in range(4):
                    h = h4 * 4 + hh
                    tl = h // 2
                    if has_prev:
                        fw.I("pe", "matmul", out=p_S[b2][:, hh, 0:128], lhsT=QZ[a][:, tl, h % 2, :],
                             rhs=KT[(pbi - 1) % 4][:, tl, :], start=True, stop=True)
                    fw.I("pe", "matmul", out=p_S[b2][:, hh, 128:256], lhsT=QZ[a][:, tl, h % 2, :],
                         rhs=KT[pbi % 4][:, tl, :], start=True, stop=True)

            def aB(u):
                pbi, h4, g, pb, d, n, has_prev = info(u)
                a, b2 = pbi % 2, u % 2
                k0 = 0 if has_prev else 128
                fw.I("dve", "scalar_tensor_tensor", out=sc[b2][:, :, k0:256], in0=p_S[b2][:, :, k0:256], scalar=0.125,
                     in1=mask[:, k0:256].unsqueeze(1).to_broadcast([128, 4, 256 - k0]), op0=ALU.mult, op1=ALU.add)
                mx = st[a][:, 0, h4 * 4:h4 * 4 + 4]
                nmx = st[a][:, 1, h4 * 4:h4 * 4 + 4]
                fw.I("dve", "tensor_reduce", out=mx, in_=sc[b2][:, :, k0:256], axis=AX.X, op=ALU.max)
                fw.I("dve", "tensor_scalar", out=nmx, in0=mx, scalar1=-1.0, scalar2=None, op0=ALU.mult)
                for hh in range(4):
                    h = h4 * 4 + hh
                    fw.I("act", "activation", out=sc[b2][:, hh, k0:256], in_=sc[b2][:, hh, k0:256], func=AF.Exp,
                         bias=st[a][:, 1, h:h + 1], accum_out=st[a][:, 2, h:h + 1])

            def aC(u):
                pbi, h4, g, pb, d, n, has_prev = info(u)
                b2 = u % 2
                k0 = 0 if has_prev else 128
                nkc = 2 if has_prev else 1
                for hh in range(4):
                    for c in range(nkc):
                        kc = k0 + c * 128
                        fw.I("pe", "transpose", out=p_T[:, hh * 2 + c, :], in_=sc[b2][:, hh, kc:kc + 128],
                             identity=self.ident)
                eng, meth = ("act", "copy") if h4 else ("dve", "tensor_copy")
                if has_prev:
                    fw.I(eng, meth, out=PT[b2][:], in_=p_T[:])
                else:
                    fw.I(eng, meth, out=PT[b2][:].rearrange("p (h c) q -> p h c q", c=2)[:, :, 0, :],
                         in_=p_T[:].rearrange("p (h c) q -> p h c q", c=2)[:, :, 0, :])

            def aD(u):
                pbi, h4, g, pb, d, n, has_prev = info(u)
                a, b2 = pbi % 2, u % 2
                nkc = 2 if has_prev else 1
                for hh in range(4):
                    h = h4 * 4 + hh
                    for c in range(nkc):
                        vsrc = V[(pbi - 1) % 4] if (has_prev and c == 0) else V[pbi % 4]
                        fw.I("pe", "matmul", out=p_O[:, h, :], lhsT=PT[b2][:, hh * 2 + c, :], rhs=vsrc[:, h * 64:(h + 1) * 64],
                             start=(c == 0), stop=(c == nkc - 1))
                if h4 == 1:
                    fw.I("dve", "reciprocal", out=st[a][:, 3, :], in_=st[a][:, 2, :])
                    fw.I("dve", "tensor_tensor", out=oh[a][:], in0=p_O[:],
                         in1=st[a][:, 3, :].unsqueeze(2).to_broadcast([128, 8, 64]), op=ALU.mult)
                    fw.I("pool", "tensor_copy", out=ml[a][:, 0:8], in_=st[a][:, 0, :])
                    fw.I("pool", "tensor_copy", out=ml[a][:, 8:16], in_=st[a][:, 2, :])
                    r_ = (pb * 128) // n
                    i0 = (pb * 128) % n
                    ao_dst = self.AO[g].rearrange("(i r) c -> r i c", r=d)[r_, i0:i0 + 128, :]
                    ml_dst = self.AML[g].rearrange("(i r) c -> r i c", r=d)[r_, i0:i0 + 128, :]
                    fw.D("sp", out=ao_dst, in_=oh[a][:].rearrange("p h e -> p (h e)"), _r=[self.tk["AO"]])
                    fw.D("sp", out=ml_dst, in_=ml[a][:], _r=[self.tk["AML"]])
            pipeline(3 * NT * 2, [aA, aB, aC, aD])

    def attn_layer(self, li, XT_src, XT_dst):
        fw = self.fw
        j = li // 2
        fw.barrier("sp", [self.xt_tok[0], self.xt_tok[1], self.tk["QK"], self.tk["VP"], self.tk["AO"], self.tk["AML"]])
        self.attn_proj(j, XT_src)
        fw.barrier("sp", [self.tk["QK"], self.tk["VP"]])
        self.attn_core()
        fw.barrier("sp", [self.tk["AO"], self.tk["AML"]])
        with fw.scope():
            ao = [fw.T("a3_ao%d" % i, [128, 3, 512]) for i in range(2)]
            am = [fw.T("a3_ml%d" % i, [128, 3, 16]) for i in range(2)]
            sm = [fw.T("a3_sm%d" % i, [128, 4, 24]) for i in range(2)]
            mg = [fw.T("a3_mg%d" % i, [128, 512]) for i in range(2)]
            tmp = fw.T("a3_tmp", [128, 512])

            def loader(t):
                a = t % 2
                rows = slice(t * 128, (t + 1) * 128)
                fw.D("sp", out=ao[a][:], in_=self.AO[:, rows, :].rearrange("g p c -> p g c"), _r=[self.tk["AO"]])
                fw.D("sp", out=am[a][:], in_=self.AML[:, rows, :].rearrange("g p c -> p g c"), _r=[self.tk["AML"]])
                M = sm[a][:, 0, 0:8]
                fw.I("dve", "tensor_tensor", out=M, in0=am[a][:, 0, 0:8], in1=am[a][:, 1, 0:8], op=ALU.max)
                fw.I("dve", "tensor_tensor", out=M, in0=M, in1=am[a][:, 2, 0:8], op=ALU.max)
                wg = sm[a][:, 1, :].rearrange("p (g h) -> p g h", g=3)
                fw.I("dve", "tensor_tensor", out=wg, in0=am[a][:, :, 0:8], in1=M.unsqueeze(1).to_broadcast([128, 3, 8]),
                     op=ALU.subtract)
                fw.I("act", "activation", out=sm[a][:, 1, :], in_=sm[a][:, 1, :], func=AF.Exp)
                fw.I("dve", "tensor_tensor", out=wg, in0=wg, in1=am[a][:, :, 8:16], op=ALU.mult)
                den = sm[a][:, 2, 0:8]
                fw.I("dve", "tensor_tensor", out=den, in0=sm[a][:, 1, 0:8], in1=sm[a][:, 1, 8:16], op=ALU.add)
                fw.I("dve", "tensor_tensor", out=den, in0=den, in1=sm[a][:, 1, 16:24], op=ALU.add)
                fw.I("dve", "reciprocal", out=sm[a][:, 2, 8:16], in_=den)
                fw.I("dve", "tensor_tensor", out=wg, in0=wg, in1=sm[a][:, 2, 8:16].unsqueeze(1).to_broadcast([128, 3, 8]),
                     op=ALU.mult)
                v3 = lambda t_: t_.rearrange("p (h e) -> p h e", h=8)
                bc = lambda gi: sm[a][:, 1, gi * 8:(gi + 1) * 8].unsqueeze(2).to_broadcast([128, 8, 64])
                fw.I("dve", "tensor_tensor", out=v3(mg[a][:]), in0=v3(ao[a][:, 0, :]), in1=bc(0), op=ALU.mult)
                for gi in (1, 2):
                    fw.I("pool", "tensor_tensor", out=v3(tmp[:]), in0=v3(ao[a][:, gi, :]), in1=bc(gi), op=ALU.mult)
                    fw.I("dve", "tensor_tensor", out=mg[a][:], in0=mg[a][:], in1=tmp[:], op=ALU.add)
                return mg[a]
            self.proj_ln("a3", li, 0, 4, self.attn_w_o[j], loader, XT_dst)

    def zero_xrows(self):
        fw = self.fw
        with fw.scope():
            z = fw.T("z_zero", [128, 4, 1024])
            fw.I("dve", "memset", ap=z[:], constant=0.0)
            nq = self.NBLK * RT
            for q0 in range(0, nq, 4):
                qn = min(4, nq - q0)
                fw.D("act", out=self.XROWS[q0 * 128:(q0 + qn) * 128, :].rearrange("(q p) c -> p q c", p=128),
                     in_=z[:, 0:qn, :], _r=[self.tk["XROWS"]])

    def moe_layer(self, li, XT_src, XT_dst, lw=None):
        fw, NT, NB = self.fw, self.NT, self.NBLK
        lw = li if lw is None else lw
        cst = self.cst
        Lmat = cst[:, C_L:C_L + 128]
        fw.barrier("sp", [self.xt_tok[0], self.xt_tok[1], self.tk["XROWS"], self.tk["YROWS"]])
        with fw.scope():
            OH1 = fw.T("m_oh1", [128, NT, 32])
            OH2 = fw.T("m_oh2", [128, NT, 32])
            RANK = fw.T("m_rank", [128, NT, 32])
            GT = fw.T("m_gt", [128, NT, 2])
            DEST = fw.T("m_dest", [128, 2, NT])
            DESTi = fw.T("m_desti", [128, 2, NT], I32)
            WIDX = fw.T("m_widx", [128, NB], I32)
            run = fw.T("m_run", [128, 32])
            with fw.scope():
                wr = fw.T("m_wr", [128, 8, 36], F32R)
                fw.D("pool", out=wr[:], in_=self.w_router[li].rearrange("(k p) n -> p k n", p=128))
                brb = fw.T("m_brb", [128, 36])
                self.bc_load("sp", brb[:], self.b_router[li])
                fw.I("dve", "memset", ap=run[:], constant=0.0)
                xTc = [fw.T("m_xT%d" % i, [128, 8, 128], F32R) for i in range(2)]
                smt = [fw.T("m_sm%d" % i, [128, 128]) for i in range(2)]
                p_lg = fw.P("m_plg", [128, 64])
                p_rk = fw.P("m_prk", [128, 64])
                for t in range(NT):
                    a = t % 2
                    sm = smt[a]
                    lg = sm[:, 0:36]
                    gmax, gsum, gw, m1, m2, w1, tmp, ngmax = [sm[:, 36 + i:37 + i] for i in range(8)]
                    gmask, gexp = sm[:, 44:48], sm[:, 48:52]
                    esel, mask1, esel2, mask2 = sm[:, 52:60], sm[:, 60:68], sm[:, 68:76], sm[:, 76:84]
                    A = sm[:, 84:116]
                    fw.D("pool", out=xTc[a][:], in_=self.XT[XT_src][:, :, t * 128:(t + 1) * 128].rearrange("k p t -> p k t"),
                         _r=[self.xt_tok[XT_src]])
                    for k in range(8):
                        fw.I("pe", "matmul", out=p_lg[:, 0:36], lhsT=xTc[a][:, k, :], rhs=wr[:, k, :],
                             start=(k == 0), stop=(k == 7))
                    fw.I("dve", "tensor_tensor", out=lg, in0=p_lg[:, 0:36], in1=brb[:], op=ALU.add)
                    fw.I("dve", "tensor_reduce", out=gmax, in_=sm[:, 0:4], axis=AX.X, op=ALU.max)
                    fw.I("dve", "tensor_scalar", out=gmask, in0=sm[:, 0:4], scalar1=gmax, scalar2=None, op0=ALU.is_equal)
                    fw.I("dve", "tensor_scalar", out=ngmax, in0=gmax, scalar1=-1.0, scalar2=None, op0=ALU.mult)
                    fw.I("act", "activation", out=gexp, in_=sm[:, 0:4], func=AF.Exp, bias=ngmax, accum_out=gsum)
                    fw.I("dve", "reciprocal", out=gw, in_=gsum)
                    fw.I("dve", "tensor_scalar", out=esel, in0=sm[:, 4:12], scalar1=sm[:, 44:45], scalar2=None, op0=ALU.mult)
                    for g in range(1, 4):
                        fw.I("dve", "scalar_tensor_tensor", out=esel, in0=sm[:, 4 + 8 * g:12 + 8 * g],
                             scalar=sm[:, 44 + g:45 + g], in1=esel, op0=ALU.mult, op1=ALU.add)
                    fw.I("dve", "tensor_reduce", out=m1, in_=esel, axis=AX.X, op=ALU.max)
                    fw.I("dve", "tensor_scalar", out=mask1, in0=esel, scalar1=m1, scalar2=None, op0=ALU.is_equal)
                    fw.I("dve", "scalar_tensor_tensor", out=esel2, in0=mask1, scalar=-1e30, in1=esel,
                         op0=ALU.mult, op1=ALU.add)
                    fw.I("dve", "tensor_reduce", out=m2, in_=esel2, axis=AX.X, op=ALU.max)
                    fw.I("dve", "tensor_scalar", out=mask2, in0=esel2, scalar1=m2, scalar2=None, op0=ALU.is_equal)
                    fw.I("dve", "tensor_tensor", out=tmp, in0=m2, in1=m1, op=ALU.subtract)
                    fw.I("act", "activation", out=tmp, in_=tmp, func=AF.Exp)
                    fw.I("dve", "tensor_scalar", out=tmp, in0=tmp, scalar1=1.0, scalar2=None, op0=ALU.add)
                    fw.I("dve", "reciprocal", out=w1, in_=tmp)
                    fw.I("dve", "tensor_tensor", out=GT[:, t, 0:1], in0=gw, in1=w1, op=ALU.mult)
                    fw.I("dve", "tensor_tensor", out=GT[:, t, 1:2], in0=gw, in1=GT[:, t, 0:1], op=ALU.subtract)
                    fw.I("dve", "tensor_tensor", out=OH1[:, t, :].rearrange("p (g e) -> p g e", g=4),
                         in0=gmask.unsqueeze(2).to_broadcast([128, 4, 8]),
                         in1=mask1.unsqueeze(1).to_broadcast([128, 4, 8]), op=ALU.mult)
                    fw.I("dve", "tensor_tensor", out=OH2[:, t, :].rearrange("p (g e) -> p g e", g=4),
                         in0=gmask.unsqueeze(2).to_broadcast([128, 4, 8]),
                         in1=mask2.unsqueeze(1).to_broadcast([128, 4, 8]), op=ALU.mult)
                    fw.I("dve", "tensor_tensor", out=A, in0=OH1[:, t, :], in1=OH2[:, t, :], op=ALU.add)
                    fw.I("pe", "matmul", out=p_rk[:, 0:32], lhsT=Lmat, rhs=A, start=True, stop=True)
                    fw.I("pe", "matmul", out=p_rk[:, 32:64], lhsT=self.ones, rhs=A, start=True, stop=True)
                    fw.I("dve", "tensor_tensor", out=RANK[:, t, :], in0=p_rk[:, 0:32], in1=run[:], op=ALU.add)
                    fw.I("dve", "tensor_tensor", out=run[:], in0=run[:], in1=p_rk[:, 32:64], op=ALU.add)
            with fw.scope():
                NG = -(-self.S // BR)
                cmp = fw.T("m_cmp", [128, 32, NG])
                nblk = fw.T("m_nblk", [128, 32])
                padded = fw.T("m_padded", [128, 32])
                padT = fw.T("m_padT", [32, 128])
                pend = fw.T("m_pend", [128, 32])
                pstart = fw.T("m_pstart", [128, 32])
                tmp3 = fw.T("m_tmp3", [128, NT, 32])
                tmp4 = fw.T("m_tmp4", [128, NT, 32])
                cmpb = fw.T("m_cmpb", [128, NB, 32])
                be = fw.T("m_be", [128, NB])
                p_a = fw.P("m_pa", [128, 128])
                p_b = fw.P("m_pb", [128, 32])
                grid = cst[:, C_BLK:C_BLK + NB]
                fw.I("dve", "tensor_tensor", out=cmp[:], in0=run[:].unsqueeze(2).to_broadcast([128, 32, NG]),
                     in1=grid[:, 0:NG].unsqueeze(1).to_broadcast([128, 32, NG]), op=ALU.is_gt)
                fw.I("dve", "tensor_reduce", out=nblk[:], in_=cmp[:], axis=AX.X, op=ALU.add)
                fw.I("dve", "tensor_scalar", out=padded[:], in0=nblk[:], scalar1=float(BR), scalar2=None, op0=ALU.mult)
                fw.I("pe", "transpose", out=p_a[0:32, :], in_=padded[:, 0:32], identity=self.ident)
                fw.I("act", "copy", out=padT[:], in_=p_a[0:32, :])
                fw.I("pe", "matmul", out=p_b[:], lhsT=padT[:], rhs=cst[0:32, C_U:C_U + 32], start=True, stop=True)
                fw.I("act", "copy", out=pend[:], in_=p_b[:])
                fw.I("dve", "tensor_tensor", out=pstart[:], in0=pend[:], in1=padded[:], op=ALU.subtract)
                fw.I("dve", "tensor_tensor", out=tmp3[:], in0=RANK[:],
                     in1=pstart[:].unsqueeze(1).to_broadcast([128, NT, 32]), op=ALU.add)
                for k, OH in enumerate((OH1, OH2)):
                    fw.I("dve", "tensor_tensor", out=tmp4[:], in0=tmp3[:], in1=OH[:], op=ALU.mult)
                    fw.I("dve", "tensor_reduce", out=DEST[:, k, :], in_=tmp4[:], axis=AX.X, op=ALU.add)
                fw.I("dve", "tensor_copy", out=DESTi[:], in_=DEST[:])
                fw.I("dve", "tensor_tensor", out=cmpb[:], in0=grid.unsqueeze(2).to_broadcast([128, NB, 32]),
                     in1=pend[:].unsqueeze(1).to_broadcast([128, NB, 32]), op=ALU.is_ge)
                fw.I("dve", "tensor_reduce", out=be[:], in_=cmpb[:], axis=AX.X, op=ALU.add)
                fw.I("dve", "tensor_scalar", out=be[:], in0=be[:], scalar1=32.0, scalar2=128.0, op0=ALU.min, op1=ALU.mult)
                fw.I("dve", "tensor_scalar", out=be[:], in0=be[:], scalar1=cst[:, C_PIDX:C_PIDX + 1], scalar2=None,
                     op0=ALU.add)
                fw.I("dve", "tensor_copy", out=WIDX[:], in_=be[:])
            with fw.scope():
                xt = [fw.T("m_x%d" % i, [128, 1024]) for i in range(2)]
                for t in range(NT):
                    a = t % 2
                    fw.D("sp", out=xt[a][:], in_=self.XA[t * 128:(t + 1) * 128, :], _r=[self.xa_tok[t]])
                    for k in range(2):
                        fw.D("pool", _meth="indirect_dma_start", out=self.XROWS[:, :],
                             out_offset=bass.IndirectOffsetOnAxis(ap=DESTi[:, k, t:t + 1], axis=0),
                             in_=xt[a][:], in_offset=None, _r=[self.tk["XROWS"]])
            fw.barrier("sp", [self.tk["XROWS"]])
            with fw.scope():
                gu = [fw.T("m_gu%d" % i, [128, 8, 1024], F32R) for i in range(2)]
                dn = [fw.T("m_dn%d" % i, [128, 4, 1024], F32R) for i in range(2)]
                xb4 = [fw.T("m_xb%d" % i, [128, RT, 1024]) for i in range(2)]
                xbT = fw.T("m_xbT", [128, 8, BR], F32R)
                hT = [fw.T("m_hT%d" % i, [128, 4, BR], F32R) for i in range(2)]
                sg = [fw.T("m_sg%d" % i, [128, BR]) for i in range(2)]
                yb = [fw.T("m_yb%d" % i, [128, 1024]) for i in range(2)]
                p_t = [fw.P("m_pt%d" % i, [128, 4, 128]) for i in range(2)]
                p_g = [fw.P("m_pg%d" % i, [128, BR]) for i in range(2)]
                p_u = [fw.P("m_pu%d" % i, [128, BR]) for i in range(2)]
                p_y = fw.P("m_py", [128, 1024])
                gu_src = self.moe_gu[lw][:, :]
                dn_src = self.moe_dn[lw][:, :]

                def mL(B):
                    fw.D("pool", _meth="indirect_dma_start", out=gu[B % 2][:].rearrange("p k n -> p (k n)"), out_offset=None,
                         in_=gu_src, in_offset=bass.IndirectOffsetOnAxis(ap=WIDX[:, B:B + 1], axis=0),
                         bounds_check=4095, oob_is_err=False)
                    fw.D("sp", out=xb4[B % 2][:], in_=self.XROWS[B * BR:(B + 1) * BR, :].rearrange("(r p) c -> p r c", p=128),
                         _r=[self.tk["XROWS"]])

                def mAB(B):
                    x4, g_ = xb4[B % 2], gu[B % 2]
                    fw.D("pool", _meth="indirect_dma_start", out=dn[B % 2][:].rearrange("p k n -> p (k n)"), out_offset=None,
                         in_=dn_src, in_offset=bass.IndirectOffsetOnAxis(ap=WIDX[:, B:B + 1], axis=0),
                         bounds_check=4095, oob_is_err=False)
                    cnt = 0
                    for rt in range(RT):
                        for k0 in (0, 4):
                            pt = p_t[cnt % 2]
                            for k in range(4):
                                fw.I("pe", "transpose", out=pt[:, k, :], in_=x4[:, rt, (k0 + k) * 128:(k0 + k + 1) * 128],
                                     identity=self.ident)
                            eng, meth = ("act", "copy") if cnt % 2 else ("dve", "tensor_copy")
                            fw.I(eng, meth, out=xbT[:, k0:k0 + 4, rt * 128:(rt + 1) * 128], in_=pt[:])
                            cnt += 1
                    for fc in range(4):
                        b2 = fc % 2
                        for k in range(8):
                            fw.I("pe", "matmul", out=p_g[b2][:], lhsT=g_[:, k, fc * 128:(fc + 1) * 128], rhs=xbT[:, k, :],
                                 start=(k == 0), stop=(k == 7))
                        for k in range(8):
                            fw.I("pe", "matmul", out=p_u[b2][:], lhsT=g_[:, k, 512 + fc * 128:512 + (fc + 1) * 128],
                                 rhs=xbT[:, k, :], start=(k == 0), stop=(k == 7))
                        fw.I("act", "activation", out=sg[b2][:], in_=p_g[b2][:], func=AF.Silu)
                        fw.I("dve", "tensor_tensor", out=hT[B % 2][:, fc, :], in0=sg[b2][:], in1=p_u[b2][:], op=ALU.mult)

                def mC(B):
                    h_, d_ = hT[B % 2], dn[B % 2]
                    for rt in range(RT):
                        for n in range(2):
                            for k in range(4):
                                fw.I("pe", "matmul", out=p_y[:, n * 512:(n + 1) * 512], lhsT=h_[:, k, rt * 128:(rt + 1) * 128],
                                     rhs=d_[:, k, n * 512:(n + 1) * 512], start=(k == 0), stop=(k == 3))
                        fw.I("act" if rt % 2 else "dve", "copy" if rt % 2 else "tensor_copy", out=yb[rt % 2][:], in_=p_y[:])
                        u = B * RT + rt
                        fw.D("sp", out=self.YROWS[u * 128:(u + 1) * 128, :], in_=yb[rt % 2][:], _r=[self.tk["YROWS"]])
                pipeline(NB, [mL, mAB, mC])
            fw.barrier("sp", [self.tk["YROWS"]])
            with fw.scope():
                gbc = fw.T("m_g", [128, 1024])
                bbc = fw.T("m_b", [128, 1024])
                self.bc_load("sp", gbc[:], self.ln_g[li, 1])
                self.bc_load("sp", bbc[:], self.ln_b[li, 1])
                Y1 = [fw.T("m_y1%d" % i, [128, 1024]) for i in range(2)]
                Y2 = [fw.T("m_y2%d" % i, [128, 1024]) for i in range(2)]
                ffn = [fw.T("m_ffn%d" % i, [128, 1024]) for i in range(2)]
                ln = self.ln_alloc("m4")
                def cA(t):
                    a = t % 2
                    for k, Y in enumerate((Y1, Y2)):
                        fw.D("pool", _meth="indirect_dma_start", out=Y[a][:], out_offset=None, in_=self.YROWS[:, :],
                             in_offset=bass.IndirectOffsetOnAxis(ap=DESTi[:, k, t:t + 1], axis=0), _r=[self.tk["YROWS"]])
                    fw.I("dve", "tensor_scalar", out=ffn[a][:], in0=Y1[a][:], scalar1=GT[:, t, 0:1], scalar2=None,
                         op0=ALU.mult)
                    fw.I("dve", "scalar_tensor_tensor", out=ffn[a][:], in0=Y2[a][:], scalar=GT[:, t, 1:2], in1=ffn[a][:],
                         op0=ALU.mult, op1=ALU.add)
                pipeline(NT, [cA] + self.ln_stages(ln, lambda t: ffn[t % 2][:], gbc, bbc, XT_dst))

    def build(self):
        fw = self.fw
        self.initial()
        if self.with_moe:
            self.zero_xrows()
        if any(li % 2 == 1 for li in self.layers):
            self.rope_tables()
        cur = 0
        last_x = "XA"
        for li in self.layers:
            if li % 2 == 0:
                self.ssd_layer(li, cur, 1 - cur)
            else:
                self.attn_layer(li, cur, 1 - cur)
            cur = 1 - cur
            if self.stop_after == (li, 0):
                break
            self.direct_out = (li == self.layers[-1] and self.stop_after is None)
            self.moe_layer(li, cur, 1 - cur, self.moe_layers.index(li))
            wrote_out = self.direct_out
            self.direct_out = False
            cur = 1 - cur
            if self.stop_after == (li, 1):
                break
        if 'wrote_out' in dir() and wrote_out:
            fw.finish(["out"])
            return self.nc
        with fw.scope():
            ot = [fw.T("o_t%d" % i, [128, 1024]) for i in range(2)]
            for t in range(self.NT):
                fw.D("sp", out=ot[t % 2][:], in_=self.XA[t * 128:(t + 1) * 128, :], _r=[self.xa_tok[t]])
                fw.D("act", out=self.out[t * 128:(t + 1) * 128, :], in_=ot[t % 2][:])
        fw.finish(["out"])
        return self.nc


def _lay_gu(wg, wu):
    L = wg.shape[0]
    g = wg.reshape(L, 32, 8, 128, 512).transpose(0, 1, 3, 2, 4)
    u = wu.reshape(L, 32, 8, 128, 512).transpose(0, 1, 3, 2, 4)
    return np.ascontiguousarray(np.concatenate([g, u], -1)).reshape(L, 4096, 8192)


def _lay_dn(wd):
    L = wd.shape[0]
    return np.ascontiguousarray(wd.reshape(L, 32, 4, 128, 1024).transpose(0, 1, 3, 2, 4)).reshape(L, 4096, 4096)


def _run(inputs, S, ncores, layers=(0, 1, 2, 3)):
    f32 = lambda a: np.ascontiguousarray(np.asarray(a, dtype=np.float32))
    prog = Prog(S, layers=layers)
    nc = prog.build()
    base = {k: f32(inputs[k]) for k in ("ssd_w_in", "ssd_conv_w", "ssd_conv_b", "ssd_dt_bias", "ssd_a_log", "ssd_d",
                                        "ssd_norm_w", "ssd_w_out", "attn_w_qkv", "attn_w_o", "ln_g", "ln_b")}
    base["consts"] = make_consts(prog.NBLK)
    base["w_router"] = f32(np.concatenate([np.asarray(inputs["moe_w_router_group"]),
                                           np.asarray(inputs["moe_w_router_expert"])], -1))
    base["b_router"] = f32(np.concatenate([np.asarray(inputs["moe_b_router_group"]),
                                           np.asarray(inputs["moe_b_router_expert"])], -1))
    gu = _lay_gu(f32(inputs["moe_w_gate"]), f32(inputs["moe_w_up"]))
    dn = _lay_dn(f32(inputs["moe_w_down"]))
    for i, li in enumerate(prog.moe_layers):
        base["moe_gu%d" % i] = gu[li]
        base["moe_dn%d" % i] = dn[li]
    x = f32(inputs["x"])
    pos = np.ascontiguousarray(np.asarray(inputs["positions"]).astype(np.int32))
    in_maps = []
    for c in range(ncores):
        d = dict(base)
        d["x"] = np.ascontiguousarray(x[c, :S])
        d["pos"] = np.ascontiguousarray(pos[c:c + 1, :S])
        in_maps.append(d)
    res = run_bass_kernel_spmd(nc, in_maps, core_ids=list(range(ncores)))
    return np.stack([np.asarray(res.results[c]["out"]) for c in range(ncores)]).astype(np.float32)


def kernel(**inputs):
    x = np.asarray(inputs["x"])
    return _run(inputs, x.shape[1], x.shape[0])
```

```python
import numpy as np
import concourse.bass as bass
import concourse.mybir as mybir
from concourse.bass_utils import run_bass_kernel_spmd

F32 = mybir.dt.float32
F32R = mybir.dt.float32r
I32 = mybir.dt.int32
ALU = mybir.AluOpType
AF = mybir.ActivationFunctionType
AX = mybir.AxisListType

D_MODEL = 1024
DEPTH = 4
ALPHA = (2 * DEPTH) ** 0.25
LN_EPS = 1e-5
RMS_EPS = 1e-5
NEG = -30000.0
ATT_PATTERNS = ((128, 1), (512, 4), (2048, 16))
ROPE_THETA = 500000.0
NBLK_EXTRA = 32
BR = 256
RT = BR // 128


class Buf:
    __slots__ = ("name", "w", "r")

    def __init__(self, name, init_r=None):
        self.name = name
        self.w = None
        self.r = dict(init_r) if init_r else {}


WRITE_KEYS = ("out", "accum_out", "ap")


class FW:
    ENGS = ("pe", "dve", "act", "pool", "sp")
    EPOCH = 30000

    def __init__(self, nc, n_dma_sems=48):
        self.nc = nc
        self.q = {e: [] for e in self.ENGS}
        self.sems = {}
        self._sem_ctx = []
        self._ctxs = []
        self.bufs = {}
        self.cur = {}
        self.waited = {e: {} for e in self.ENGS}
        self.free_events = {}
        for e in self.ENGS:
            self._new_epoch(e)
        self.dma_keys = []
        self.dma_uses = {}
        for i in range(n_dma_sems):
            k = self._alloc_sem("dma%d" % i)
            self.dma_keys.append(k)
            self.dma_uses[k] = 0
        self.dma_rr = 0
        self.ninst = 0
        self.uid = 0
        self._regs = {}

    def _alloc_sem(self, name):
        cm = self.nc.semaphore(name)
        h = cm.__enter__()
        self._sem_ctx.append(cm)
        self.sems[name] = h
        return name

    def _new_epoch(self, e):
        idx = sum(1 for k in self.sems if k.startswith("e_" + e + "_"))
        k = self._alloc_sem("e_%s_%d" % (e, idx))
        self.cur[e] = [k, 0]

    def T(self, name, shape, dtype=F32):
        self.uid += 1
        name = "%s_%d" % (name, self.uid)
        cm = self.nc.sbuf_tensor(name, list(shape), dtype)
        t = cm.__enter__()
        self._ctxs.append((name, cm))
        self.bufs[name] = Buf(name, self.free_events)
        return t

    def P(self, name, shape, dtype=F32):
        self.uid += 1
        name = "%s_%d" % (name, self.uid)
        cm = self.nc.psum_tensor(name, list(shape), dtype)
        t = cm.__enter__()
        self._ctxs.append((name, cm))
        self.bufs[name] = Buf(name, self.free_events)
        return t

    def dram(self, name, shape, dtype=F32, kind="Internal", track=True):
        t = self.nc.dram_tensor(name, list(shape), dtype, kind=kind)
        if track:
            self.bufs[name] = Buf(name)
        return t.ap()

    def token(self, name):
        b = Buf(name)
        self.bufs[name] = b
        return b

    def scope(self):
        return _Scope(self)

    def _collect(self, kw, xr, xw):
        reads, writes = [], []
        for k, v in kw.items():
            if isinstance(v, bass.IndirectOffsetOnAxis):
                v = v.ap
                k = "idx"
            if isinstance(v, bass.AP):
                b = self.bufs.get(v.tensor.name)
                if b is not None:
                    (writes if k in WRITE_KEYS else reads).append(b)
        for b in xr or ():
            reads.append(self.bufs[b] if isinstance(b, str) else b)
        for b in xw or ():
            writes.append(self.bufs[b] if isinstance(b, str) else b)
        return reads, writes

    def _deps(self, reads, writes):
        deps = {}
        for b in reads:
            if b.w is not None and deps.get(b.w[0], 0) < b.w[1]:
                deps[b.w[0]] = b.w[1]
        for b in writes:
            if b.w is not None and deps.get(b.w[0], 0) < b.w[1]:
                deps[b.w[0]] = b.w[1]
            for k, v in b.r.items():
                if deps.get(k, 0) < v:
                    deps[k] = v
        return deps

    def _emit_waits(self, e, deps):
        wt = self.waited[e]
        for k, v in deps.items():
            if e == "pe" and k.startswith("e_pe_"):
                continue
            if wt.get(k, 0) >= v:
                continue
            wt[k] = v
            h = self.sems[k]
            self.q[e].append(lambda eng, h=h, v=v: eng.wait_ge(h, v))

    def _update(self, ev, reads, writes):
        k, v = ev
        for b in reads:
            if b.r.get(k, 0) < v:
                b.r[k] = v
        for b in writes:
            b.w = ev
            b.r = {}

    def I(self, e, meth, _r=None, _w=None, **kw):
        reads, writes = self._collect(kw, _r, _w)
        deps = self._deps(reads, writes)
        self._emit_waits(e, deps)
        cur = self.cur[e]
        if cur[1] >= self.EPOCH:
            self._new_epoch(e)
            cur = self.cur[e]
        cur[1] += 1
        k, v = cur[0], cur[1]
        h = self.sems[k]
        self.q[e].append(lambda eng, meth=meth, kw=kw, h=h: getattr(eng, meth)(**kw).then_inc(h, 1))
        self._update((k, v), reads, writes)
        self.ninst += 1

    def D(self, e, _r=None, _w=None, _meth="dma_start", **kw):
        reads, writes = self._collect(kw, _r, _w)
        deps = self._deps(reads, writes)
        k = self.dma_keys[self.dma_rr % len(self.dma_keys)]
        self.dma_rr += 1
        prev = 16 * self.dma_uses[k]
        if prev:
            deps[k] = max(deps.get(k, 0), prev)
        self._emit_waits(e, deps)
        self.dma_uses[k] += 1
        v = 16 * self.dma_uses[k]
        h = self.sems[k]
        if isinstance(kw.get("bounds_check"), int):
            bv = kw.pop("bounds_check")

            def emit(eng, meth=_meth, kw=kw, h=h, bv=bv):
                key = ("bound", e, bv)
                if key not in self._regs:
                    self._regs[key] = eng.to_reg(bv)
                return getattr(eng, meth)(bounds_check=self._regs[key], **kw).then_inc(h, 16)
            self.q[e].append(emit)
        else:
            self.q[e].append(lambda eng, meth=_meth, kw=kw, h=h: getattr(eng, meth)(**kw).then_inc(h, 16))
        self._update((k, v), reads, writes)
        self.ninst += 1

    def barrier(self, e, tok_names):
        self.I(e, "nop", _w=list(tok_names))

    def finish(self, final_names):
        reads = [self.bufs[n] for n in final_names]
        deps = self._deps(reads, [])
        for e in self.ENGS:
            self._emit_waits(e, dict(deps))
        nc = self.nc
        with nc.Block() as block:
            @block.tensor
            def _(eng):
                for f in self.q["pe"]:
                    f(eng)

            @block.vector
            def _(eng):
                for f in self.q["dve"]:
                    f(eng)

            @block.scalar
            def _(eng):
                for f in self.q["act"]:
                    f(eng)

            @block.gpsimd
            def _(eng):
                for f in self.q["pool"]:
                    f(eng)

            @block.sync
            def _(eng):
                for f in self.q["sp"]:
                    f(eng)
        for name, cm in reversed(self._ctxs):
            cm.__exit__(None, None, None)
        for cm in reversed(self._sem_ctx):
            cm.__exit__(None, None, None)


class _Scope:
    def __init__(self, fw):
        self.fw = fw

    def __enter__(self):
        self.mark = len(self.fw._ctxs)
        return self

    def __exit__(self, *a):
        fw = self.fw
        fe = fw.free_events
        while len(fw._ctxs) > self.mark:
            name, cm = fw._ctxs.pop()
            b = fw.bufs.pop(name)
            if b.w is not None and fe.get(b.w[0], 0) < b.w[1]:
                fe[b.w[0]] = b.w[1]
            for k, v in b.r.items():
                if fe.get(k, 0) < v:
                    fe[k] = v
            cm.__exit__(None, None, None)
        return False


def pipeline(n, stages):
    ns = len(stages)
    for t in range(n + ns - 1):
        for si, f in enumerate(stages):
            i = t - si
            if 0 <= i < n:
                f(i)


C_ID, C_U, C_ONES, C_L, C_PM, C_MASK, C_PIDX = 0, 128, 256, 384, 512, 640, 896
C_INVF, C_M16, C_1M16, C_SS, C_HALFPI, C_HM0, C_HM1, C_EIDX, C_BLK = 897, 898, 899, 900, 901, 902, 903, 904, 936


def make_consts(nblk):
    w = C_BLK + nblk
    c = np.zeros((128, w), np.float32)
    i = np.arange(128)
    c[:, C_ID:C_ID + 128] = np.eye(128, dtype=np.float32)
    c[:, C_U:C_U + 128] = (i[:, None] <= i[None, :])
    c[:, C_ONES:C_ONES + 128] = 1.0
    c[:, C_L:C_L + 128] = (i[:, None] < i[None, :])
    pm = np.zeros((128, 128), np.float32)
    for dp in range(128):
        if dp % 64 < 16:
            d = (dp // 64) * 64 + ((dp % 64) ^ 8)
            pm[d, dp] = 1.0
    c[:, C_PM:C_PM + 128] = pm
    c[:, C_MASK:C_MASK + 128] = np.where(i[None, :] >= i[:, None], 0.0, NEG)
    c[:, C_MASK + 128:C_MASK + 256] = np.where(i[None, :] <= i[:, None], 0.0, NEG)
    c[:, C_PIDX] = i
    dd = i % 64
    invf = (np.float32(ROPE_THETA) ** (-np.arange(0, 16, 2, dtype=np.float32) / np.float32(16))).astype(np.float32)
    m16 = (dd < 16).astype(np.float32)
    c[:, C_INVF] = np.where(dd < 16, invf[dd % 8], 0.0)
    c[:, C_M16] = m16
    c[:, C_1M16] = 1.0 - m16
    c[:, C_SS] = m16 * np.where(dd < 8, -1.0, 1.0)
    c[:, C_HALFPI] = np.float32(np.pi / 2)
    c[:, C_HM0] = (i < 64)
    c[:, C_HM1] = (i >= 64)
    c[:, C_EIDX:C_EIDX + 32] = np.arange(32)[None, :]
    c[:, C_BLK:C_BLK + nblk] = float(BR) * np.arange(nblk)[None, :]
    return c


class Prog:
    def __init__(self, S, layers=(0, 1, 2, 3), stop_after=None, with_moe=True, moe_layers=(0, 1, 2, 3)):
        self.S = S
        self.with_moe = with_moe
        self.NT = S // 128
        self.layers = tuple(layers)
        self.stop_after = stop_after
        self.NBLK = -(-(2 * S + 32 * (BR - 1)) // BR)
        nc = self.nc = bass.Bass("TRN2", target_bir_lowering=False)
        fw = self.fw = FW(nc)
        S_ = S
        ext = lambda n, s, d=F32: fw.dram(n, s, d, kind="ExternalInput")
        self.x_in = ext("x", [S_, 1024])
        self.pos_in = ext("pos", [1, S_], I32)
        self.consts_in = ext("consts", [128, C_BLK + self.NBLK])
        self.ssd_w_in = ext("ssd_w_in", [2, 1024, 5152])
        self.ssd_conv_w = ext("ssd_conv_w", [2, 4, 3072])
        self.ssd_conv_b = ext("ssd_conv_b", [2, 3072])
        self.ssd_dt_bias = ext("ssd_dt_bias", [2, 32])
        self.ssd_a_log = ext("ssd_a_log", [2, 32])
        self.ssd_d = ext("ssd_d", [2, 32])
        self.ssd_norm_w = ext("ssd_norm_w", [2, 2048])
        self.ssd_w_out = ext("ssd_w_out", [2, 2048, 1024])
        self.attn_w_qkv = ext("attn_w_qkv", [2, 1024, 4608])
        self.attn_w_o = ext("attn_w_o", [2, 512, 1024])
        self.ln_g = ext("ln_g", [4, 2, 1024])
        self.ln_b = ext("ln_b", [4, 2, 1024])
        self.w_router = ext("w_router", [4, 1024, 36])
        self.b_router = ext("b_router", [4, 36])
        self.moe_layers = tuple(moe_layers)
        if with_moe:
            nl = len(self.moe_layers)
            self.moe_gu = [ext("moe_gu%d" % i, [32 * 128, 8 * 1024]) for i in range(nl)]
            self.moe_dn = [ext("moe_dn%d" % i, [32 * 128, 4 * 1024]) for i in range(nl)]
        self.out = fw.dram("out", [S_, 1024], F32, kind="ExternalOutput")
        sc = lambda n, sh, d=F32: fw.dram(n, sh, d, track=False)
        self.XA = sc("XA", [S_, 1024])
        self.XT = [sc("XT0", [8, 128, S_]), sc("XT1", [8, 128, S_])]
        self.BCfm = sc("BCfm", [8, 128, S_])
        self.XSBtm = sc("XSBtm", [S_, 2560])
        self.YN = sc("YN", [S_, 2048])
        self.XROWS = sc("XROWS", [self.NBLK * BR, 1024])
        self.YROWS = sc("YROWS", [self.NBLK * BR, 1024])
        self.AO = sc("AO", [3, S_, 512])
        self.AML = sc("AML", [3, S_, 16])
        self.ROPE = sc("ROPE", [2, 128, S_])
        self.QK = sc("QK", [24, 128, S_])
        self.VP = sc("VP", [3, S_, 512])
        self.xa_tok = [fw.token("xa%d" % t) for t in range(self.NT)]
        self.xt_tok = [fw.token("xt0"), fw.token("xt1")]
        self.tk = {n: fw.token("tk_" + n) for n in ("BCfm", "XSBtm", "YN", "XROWS", "YROWS", "AO", "AML", "ROPE", "QK", "VP")}
        self.cst = fw.T("cst", [128, C_BLK + self.NBLK])
        fw.dummy = fw.T("fwdummy", [128, 4])
        fw.D("sp", out=self.cst[:], in_=self.consts_in[:, :])
        self.eps_ln = fw.T("eps_ln", [128, 2])
        fw.I("dve", "memset", ap=self.eps_ln[:, 0:1], constant=float(LN_EPS))
        fw.I("dve", "memset", ap=self.eps_ln[:, 1:2], constant=1.0)
        self.ident = self.cst[:, C_ID:C_ID + 128]
        self.U = self.cst[:, C_U:C_U + 128]
        self.ones = self.cst[:, C_ONES:C_ONES + 128]

    def bc_load(self, q, dst, src_row):
        self.fw.D(q, out=dst, in_=src_row.partition_broadcast(128))

    def initial(self):
        fw = self.fw
        self.first_ln = True
        with fw.scope():
            xt = [fw.T("i_x%d" % i, [128, 1024]) for i in range(3)]
            tp = [fw.P("i_tp%d" % i, [128, 8, 128]) for i in range(2)]
            ts = [fw.T("i_ts%d" % i, [128, 8, 128]) for i in range(2)]

            def l0(t):
                fw.D("sp", out=xt[t % 3][:], in_=self.x_in[t * 128:(t + 1) * 128, :])

            def l1(t):
                for c in range(8):
                    fw.I("pe", "transpose", out=tp[t % 2][:, c, :], in_=xt[t % 3][:, c * 128:(c + 1) * 128],
                         identity=self.ident)

            def l2(t):
                if t % 2:
                    fw.I("act", "copy", out=ts[t % 2][:], in_=tp[t % 2][:])
                else:
                    fw.I("dve", "tensor_copy", out=ts[t % 2][:], in_=tp[t % 2][:])
                fw.D("sp", out=self.XT[0][:, :, t * 128:(t + 1) * 128].rearrange("k p t -> p k t"), in_=ts[t % 2][:],
                     _r=[self.xt_tok[0]])
            pipeline(self.NT, [l0, l1, l2])

    def transpose_store(self, xtile, tp, ts, XT_dst, t):
        fw = self.fw
        for c in range(8):
            fw.I("pe", "transpose", out=tp[:, c, :], in_=xtile[:, c * 128:(c + 1) * 128], identity=self.ident)
        fw.I("act", "copy", out=ts[:], in_=tp[:])
        fw.D("sp", out=self.XT[XT_dst][:, :, t * 128:(t + 1) * 128].rearrange("k p t -> p k t"), in_=ts[:],
             _r=[self.xt_tok[XT_dst]])

    def proj_ln(self, tag, li, sub, kch, w_src, a_loader, XT_dst):
        fw = self.fw
        with fw.scope():
            w = fw.T(tag + "_w", [128, kch, 1024], F32R)
            for k in range(kch):
                fw.D("pool", out=w[:, k, :], in_=w_src[k * 128:(k + 1) * 128, :])
            gbc = fw.T(tag + "_g", [128, 1024])
            bbc = fw.T(tag + "_b", [128, 1024])
            self.bc_load("sp", gbc[:], self.ln_g[li, sub])
            self.bc_load("sp", bbc[:], self.ln_b[li, sub])
            atp = [fw.P(tag + "_atp%d" % i, [128, 4, 128]) for i in range(2)]
            aT = [fw.T(tag + "_aT%d" % i, [128, kch, 128], F32R) for i in range(2)]
            mp = [fw.P(tag + "_mp%d" % i, [128, 1024]) for i in range(2)]
            ln = self.ln_alloc(tag)

            def stA(t):
                a = t % 2
                A = a_loader(t)
                for k0 in range(0, kch, 4):
                    kn = min(4, kch - k0)
                    for k in range(kn):
                        fw.I("pe", "transpose", out=atp[(k0 // 4) % 2][:, k, :],
                             in_=A[:, (k0 + k) * 128:(k0 + k + 1) * 128], identity=self.ident)
                    fw.I("act" if (k0 // 4) % 2 else "dve", "copy" if (k0 // 4) % 2 else "tensor_copy",
                         out=aT[a][:, k0:k0 + kn, :], in_=atp[(k0 // 4) % 2][:, 0:kn, :])
                for n in range(2):
                    for k in range(kch):
                        fw.I("pe", "matmul", out=mp[a][:, n * 512:(n + 1) * 512], lhsT=aT[a][:, k, :],
                             rhs=w[:, k, n * 512:(n + 1) * 512], start=(k == 0), stop=(k == kch - 1))
            pipeline(self.NT, [stA] + self.ln_stages(ln, lambda t: mp[t % 2][:], gbc, bbc, XT_dst))

    def ln_alloc(self, tag):
        fw = self.fw
        d = {}
        d["x"] = [fw.T(tag + "_lx%d" % i, [128, 1024]) for i in range(4)]
        d["v"] = [fw.T(tag + "_lv%d" % i, [128, 1024]) for i in range(4)]
        d["junk"] = fw.T(tag + "_lj", [128, 1024])
        d["st"] = [fw.T(tag + "_ls%d" % i, [128, 8]) for i in range(4)]
        d["tp"] = fw.P(tag + "_ltp", [128, 8, 128])
        d["ts"] = [fw.T(tag + "_lts%d" % i, [128, 8, 128]) for i in range(2)]
        return d

    def ln_stages(self, ln, mix_of, gbc, bbc, XT_dst):
        fw = self.fw
        first = getattr(self, "first_ln", False)
        self.first_ln = False

        def b1(t):
            a = t % 4
            x, v, st, junk = ln["x"][a], ln["v"][a], ln["st"][a], ln["junk"]
            if first:
                fw.D("sp", out=x[:], in_=self.x_in[t * 128:(t + 1) * 128, :])
            else:
                fw.D("sp", out=x[:], in_=self.XA[t * 128:(t + 1) * 128, :], _r=[self.xa_tok[t]])
            fw.I("dve", "scalar_tensor_tensor", out=v[:], in0=x[:], scalar=float(ALPHA), in1=mix_of(t),
                 op0=ALU.mult, op1=ALU.add)
            fw.I("act", "activation", out=junk[:], in_=v[:], func=AF.Identity, accum_out=st[:, 0:1])
            fw.I("act", "activation", out=junk[:], in_=v[:], func=AF.Square, accum_out=st[:, 1:2])

        def b2(t):
            st = ln["st"][t % 4]
            fw.I("dve", "tensor_scalar", out=st[:, 2:3], in0=st[:, 0:1], scalar1=1.0 / 1024, scalar2=None, op0=ALU.mult)
            fw.I("dve", "tensor_tensor", out=st[:, 3:4], in0=st[:, 2:3], in1=st[:, 2:3], op=ALU.mult)
            fw.I("dve", "scalar_tensor_tensor", out=st[:, 4:5], in0=st[:, 1:2], scalar=1.0 / 1024, in1=st[:, 3:4],
                 op0=ALU.mult, op1=ALU.subtract)
            fw.I("act", "activation", out=st[:, 6:7], in_=st[:, 4:5], func=AF.Sqrt, bias=self.eps_ln[:, 0:1])
            fw.I("dve", "reciprocal", out=st[:, 5:6], in_=st[:, 6:7])
            fw.I("dve", "scalar_tensor_tensor", out=st[:, 7:8], in0=st[:, 2:3], scalar=-1.0, in1=st[:, 5:6],
                 op0=ALU.mult, op1=ALU.mult)

        def b3(t):
            a = t % 4
            x, v, st = ln["x"][a], ln["v"][a], ln["st"][a]
            fw.I("act", "activation", out=v[:], in_=v[:], func=AF.Identity, scale=st[:, 5:6], bias=st[:, 7:8])
            fw.I("dve", "tensor_tensor", out=v[:], in0=v[:], in1=gbc[:], op=ALU.mult)
            fw.I("dve", "tensor_tensor", out=x[:], in0=v[:], in1=bbc[:], op=ALU.add)
            if getattr(self, "direct_out", False):
                fw.D("sp", out=self.out[t * 128:(t + 1) * 128, :], in_=x[:])
            else:
                fw.D("act", out=self.XA[t * 128:(t + 1) * 128, :], in_=x[:], _w=[self.xa_tok[t]])

        def c1(t):
            self.transpose_store(ln["x"][t % 4], ln["tp"], ln["ts"][t % 2], XT_dst, t)
        if getattr(self, "direct_out", False):
            return [b1, b2, b3]
        return [b1, b2, b3, c1]

    def ssd_sweep1(self, j, XT_src):
        fw, S = self.fw, self.S
        w_in = self.ssd_w_in
        with fw.scope():
            wx = fw.T("s1_wx", [128, 8, 3072], F32R)
            for k in range(8):
                fw.D("pool", out=wx[:, k, :], in_=w_in[j, k * 128:(k + 1) * 128, 2048:5120])
            cw = fw.T("s1_cw", [128, 4, 24])
            cb = fw.T("s1_cb", [128, 24])
            for k in range(4):
                fw.D("sp", out=cw[:, k, :], in_=self.ssd_conv_w[j, k].rearrange("(t p) -> p t", p=128),
                     allow_slow_non_contiguous=True)
            fw.D("sp", out=cb[:], in_=self.ssd_conv_b[j].rearrange("(t p) -> p t", p=128),
                 allow_slow_non_contiguous=True)
            halo = fw.T("s1_halo", [128, 24, 3])
            fw.I("dve", "memset", ap=halo[:], constant=0.0)
            xtb = [fw.T("s1_xt%d" % i, [128, 8, 512], F32R) for i in range(2)]
            ps = [fw.P("s1_ps%d" % i, [128, 512]) for i in range(2)]
            ub = [fw.T("s1_ub%d" % i, [128, 515]) for i in range(2)]
            acc = [fw.T("s1_acc%d" % i, [128, 512]) for i in range(2)]
            so = [fw.T("s1_so%d" % i, [128, 512]) for i in range(2)]
            tps = [fw.P("s1_tp%d" % i, [128, 4, 128]) for i in range(2)]
            tsb = [fw.T("s1_ts%d" % i, [128, 4, 128]) for i in range(2)]
            def s0(idx):
                tb, ct = divmod(idx, 24)
                a = idx % 2
                xt = xtb[tb % 2]
                if ct == 0:
                    fw.D("pool", out=xt[:], in_=self.XT[XT_src][:, :, tb * 512:(tb + 1) * 512].rearrange("k p t -> p k t"),
                         _r=[self.xt_tok[XT_src]])
                for k in range(8):
                    fw.I("pe", "matmul", out=ps[a][:], lhsT=wx[:, k, ct * 128:(ct + 1) * 128], rhs=xt[:, k, :],
                         start=(k == 0), stop=(k == 7))

            def s1(idx):
                tb, ct = divmod(idx, 24)
                a = idx % 2
                fw.I("pool", "tensor_copy", out=ub[a][:, 0:3], in_=halo[:, ct, :])
                fw.I("act", "copy", out=ub[a][:, 3:515], in_=ps[a][:])
                fw.I("pool", "tensor_copy", out=halo[:, ct, :], in_=ub[a][:, 512:515])
                fw.I("act", "activation", out=acc[a][:], in_=ub[a][:, 3:515], func=AF.Identity, scale=cw[:, 3, ct:ct + 1],
                     bias=cb[:, ct:ct + 1])

            def s1b(idx):
                tb, ct = divmod(idx, 24)
                a = idx % 2
                for kk in (2, 1, 0):
                    fw.I("dve", "scalar_tensor_tensor", out=acc[a][:], in0=ub[a][:, kk:kk + 512], scalar=cw[:, kk, ct:ct + 1],
                         in1=acc[a][:], op0=ALU.mult, op1=ALU.add)
                fw.I("act", "activation", out=so[a][:], in_=acc[a][:], func=AF.Silu)
                if ct >= 16:
                    fw.D("sp", out=self.BCfm[ct - 16, :, tb * 512:(tb + 1) * 512], in_=so[a][:], _r=[self.tk["BCfm"]])

            def s2(idx):
                tb, ct = divmod(idx, 24)
                a = idx % 2
                if ct < 20:
                    for q in range(4):
                        fw.I("pe", "transpose", out=tps[a][:, q, :], in_=so[a][:, q * 128:(q + 1) * 128],
                             identity=self.ident)
                    if idx % 2:
                        fw.I("act", "copy", out=tsb[a][:], in_=tps[a][:])
                    else:
                        fw.I("dve", "tensor_copy", out=tsb[a][:], in_=tps[a][:])
                    fw.D("sp", out=self.XSBtm[tb * 512:(tb + 1) * 512, ct * 128:(ct + 1) * 128]
                         .rearrange("(q p) c -> p q c", p=128), in_=tsb[a][:], _r=[self.tk["XSBtm"]])
            pipeline((S // 512) * 24, [s0, s1, s1b, s2])

    def ssd_sweep2(self, j, XT_src):
        fw, S = self.fw, self.S
        w_in = self.ssd_w_in
        with fw.scope():
            wz = fw.T("s2_wz", [128, 8, 2048], F32R)
            wdt = fw.T("s2_wdt", [128, 8, 32], F32R)
            for k in range(8):
                fw.D("pool", out=wz[:, k, :], in_=w_in[j, k * 128:(k + 1) * 128, 0:2048])
                fw.D("pool", out=wdt[:, k, :], in_=w_in[j, k * 128:(k + 1) * 128, 5120:5152])
            dtb = fw.T("s2_dtb", [128, 32])
            abc = fw.T("s2_a", [128, 32])
            dsk = fw.T("s2_dsk", [128, 32])
            nw = fw.T("s2_nw", [128, 2048])
            self.bc_load("sp", dtb[:], self.ssd_dt_bias[j])
            self.bc_load("sp", abc[:], self.ssd_a_log[j])
            self.bc_load("sp", dsk[:], self.ssd_d[j])
            self.bc_load("sp", nw[:], self.ssd_norm_w[j])
            fw.I("act", "activation", out=abc[:], in_=abc[:], func=AF.Exp)
            fw.I("dve", "tensor_scalar", out=abc[:], in0=abc[:], scalar1=-1.0, scalar2=None, op0=ALU.mult)
            prev = fw.T("s2_prev", [128, 4, 512])
            prevR = fw.T("s2_prevR", [128, 4, 512], F32R)
            fw.I("dve", "memset", ap=prev[:], constant=0.0)
            fw.I("dve", "tensor_copy", out=prevR[:], in_=prev[:])
            xTc = [fw.T("s2_xT%d" % i, [128, 8, 128], F32R) for i in range(2)]
            xs = [fw.T("s2_xs%d" % i, [128, 32, 64]) for i in range(2)]
            Btm = [fw.T("s2_Btm%d" % i, [128, 4, 128], F32R) for i in range(2)]
            Bfm = [fw.T("s2_Bfm%d" % i, [128, 4, 128], F32R) for i in range(2)]
            Cfm = [fw.T("s2_Cfm%d" % i, [128, 4, 128], F32R) for i in range(2)]
            sm = fw.T("s2_sm", [128, 12, 32])
            Uda = [fw.T("s2_Uda%d" % i, [128, 8, 128]) for i in range(2)]
            xr = fw.T("s2_xr", [128, 32, 64], F32R)
            xrd = fw.T("s2_xrd", [128, 32, 64], F32R)
            zs = [fw.T("s2_zs%d" % i, [128, 512]) for i in range(2)]
            cbm = [fw.T("s2_cbm%d" % i, [128, 128]) for i in range(2)]
            Dm = [fw.T("s2_D%d" % i, [128, 8, 128]) for i in range(2)]
            MT = [fw.T("s2_MT%d" % i, [128, 8, 128], F32R) for i in range(2)]
            t1 = [fw.T("s2_t1%d" % i, [128, 8, 64]) for i in range(2)]
            t2 = [fw.T("s2_t2%d" % i, [128, 8, 64]) for i in range(2)]
            yn = [fw.T("s2_yn0", [128, 2048])] * 2
            junk = fw.T("s2_junk", [128, 512])
            rs = fw.T("s2_rs", [128, 8])
            p_z = fw.P("s2_pz", [128, 512])
            p_sm = fw.P("s2_psm", [128, 512])
            p_R = fw.P("s2_pR", [128, 8, 128])
            p_Y = fw.P("s2_pY", [128, 8, 64])
            p_Yo = fw.P("s2_pYo", [128, 8, 64])
            p_S = fw.P("s2_pS", [128, 512])
            DT, DA, CS, ECS, DTE, CD, V0, V1, V2 = range(9)
            p_Y2 = [p_Y, fw.P("s2_pY2", [128, 8, 64])]

            def front(u):
                c, g = divmod(u, 4)
                a = c % 2
                tok = slice(c * 128, (c + 1) * 128)
                p_Y = p_Y2[u % 2]
                if g == 0:
                    fw.D("pool", out=xTc[a][:], in_=self.XT[XT_src][:, :, tok].rearrange("k p t -> p k t"),
                         _r=[self.xt_tok[XT_src]])
                    fw.D("sp", out=xs[a][:].rearrange("p h d -> p (h d)"), in_=self.XSBtm[tok, 0:2048], _r=[self.tk["XSBtm"]])
                    fw.D("pool", out=Btm[a][:].rearrange("p g n -> p (g n)"), in_=self.XSBtm[tok, 2048:2560],
                         _r=[self.tk["XSBtm"]])
                    fw.D("pool", out=Bfm[a][:], in_=self.BCfm[0:4, :, tok].rearrange("g p t -> p g t"), _r=[self.tk["BCfm"]])
                    fw.D("pool", out=Cfm[a][:], in_=self.BCfm[4:8, :, tok].rearrange("g p t -> p g t"), _r=[self.tk["BCfm"]])
                    for k in range(8):
                        fw.I("pe", "matmul", out=p_sm[:, 0:32], lhsT=xTc[a][:, k, :], rhs=wdt[:, k, :],
                             start=(k == 0), stop=(k == 7))
                    fw.I("dve", "tensor_tensor", out=sm[:, V0, :], in0=p_sm[:, 0:32], in1=dtb[:], op=ALU.add)
                    fw.I("dve", "scalar_tensor_tensor", out=sm[:, V1, :], in0=sm[:, V0, :], scalar=-1.0, in1=sm[:, V0, :],
                         op0=ALU.mult, op1=ALU.max)
                    fw.I("act", "activation", out=sm[:, V1, :], in_=sm[:, V1, :], func=AF.Exp, scale=-1.0)
                    fw.I("act", "activation", out=sm[:, V1, :], in_=sm[:, V1, :], func=AF.Ln, bias=self.eps_ln[:, 1:2])
                    fw.I("dve", "scalar_tensor_tensor", out=sm[:, DT, :], in0=sm[:, V0, :], scalar=0.0, in1=sm[:, V1, :],
                         op0=ALU.max, op1=ALU.add)
                    fw.I("dve", "tensor_tensor", out=sm[:, DA, :], in0=sm[:, DT, :], in1=abc[:], op=ALU.mult)
                    fw.I("pe", "matmul", out=p_sm[:, 32:64], lhsT=self.U, rhs=sm[:, DA, :], start=True, stop=True)
                    fw.I("act", "copy", out=sm[:, CS, :], in_=p_sm[:, 32:64])
                    fw.I("act", "activation", out=sm[:, ECS, :], in_=sm[:, CS, :], func=AF.Exp)
                    fw.I("pool", "tensor_tensor", out=xr[:], in0=xs[a][:],
                         in1=sm[:, DT, :].unsqueeze(2).to_broadcast([128, 32, 64]), op=ALU.mult)

                b2 = g % 2
                hs = slice(8 * g, 8 * g + 8)
                for k in range(8):
                    fw.I("pe", "matmul", out=p_z[:], lhsT=xTc[a][:, k, :], rhs=wz[:, k, g * 512:(g + 1) * 512],
                         start=(k == 0), stop=(k == 7))
                fw.I("act", "activation", out=zs[b2][:], in_=p_z[:], func=AF.Silu)
                if g == 0:
                    fw.I("dve", "tensor_tensor", out=Uda[b2][:], in0=self.U.unsqueeze(1).to_broadcast([128, 8, 128]),
                         in1=sm[:, DA, hs].unsqueeze(2).to_broadcast([128, 8, 128]), op=ALU.mult)
                for hh in range(2):
                    fw.I("pe", "matmul", out=p_R[:, 4 * hh:4 * hh + 4, :], lhsT=self.ones,
                         rhs=Uda[b2][:, 4 * hh:4 * hh + 4, :], start=True, stop=True)
                fw.I("dve", "tensor_tensor", out=sm[:, V2, hs], in0=p_R[:, :, 127], in1=sm[:, CS, hs], op=ALU.subtract)
                fw.I("act", "activation", out=sm[:, DTE, hs], in_=sm[:, V2, hs], func=AF.Exp)
                fw.I("act", "activation", out=sm[:, CD, hs], in_=p_R[:, :, 127], func=AF.Exp)
                fw.I("pool", "tensor_tensor", out=xrd[:, hs, :], in0=xr[:, hs, :],
                     in1=sm[:, DTE, hs].unsqueeze(2).to_broadcast([128, 8, 64]), op=ALU.mult)
                fw.I("pe", "matmul", out=p_sm[:, 128:256], lhsT=Bfm[a][:, g, :], rhs=Cfm[a][:, g, :],
                     start=True, stop=True)
                fw.I("dve", "tensor_tensor", out=cbm[b2][:], in0=p_sm[:, 128:256], in1=self.U, op=ALU.mult)
                fw.I("dve", "tensor_tensor", out=Dm[b2][:], in0=p_R[:],
                     in1=sm[:, CS, hs].unsqueeze(2).to_broadcast([128, 8, 128]), op=ALU.subtract)
                fw.I("act", "activation", out=Dm[b2][:], in_=Dm[b2][:], func=AF.Exp)
                fw.I("dve", "scalar_tensor_tensor", out=MT[b2][:], in0=Dm[b2][:], scalar=1.0,
                     in1=cbm[b2][:].unsqueeze(1).to_broadcast([128, 8, 128]), op0=ALU.min, op1=ALU.mult)
                if g < 3:
                    fw.I("dve", "tensor_tensor", out=Uda[1 - b2][:], in0=self.U.unsqueeze(1).to_broadcast([128, 8, 128]),
                         in1=sm[:, DA, 8 * g + 8:8 * g + 16].unsqueeze(2).to_broadcast([128, 8, 128]), op=ALU.mult)
                for h in range(8):
                    fw.I("pe", "matmul", out=p_Y[:, h, :], lhsT=MT[b2][:, h, :], rhs=xr[:, 8 * g + h, :],
                         start=True, stop=True)
                fw.I("pe", "matmul", out=p_Yo[:].rearrange("p h d -> p (h d)"), lhsT=Cfm[a][:, g, :],
                     rhs=prevR[:, g, :], start=True, stop=True)
                fw.I("pe", "matmul", out=p_S[:], lhsT=Btm[a][:, g, :],
                     rhs=xrd[:, hs, :].rearrange("p h d -> p (h d)"), start=True, stop=True)

                fw.I("dve", "tensor_tensor", out=t1[b2][:], in0=p_Yo[:],
                     in1=sm[:, ECS, hs].unsqueeze(2).to_broadcast([128, 8, 64]), op=ALU.mult)

                fw.I("pool", "tensor_tensor", out=prev[:, g, :].rearrange("p (h d) -> p h d", h=8),
                     in0=prev[:, g, :].rearrange("p (h d) -> p h d", h=8),
                     in1=sm[:, CD, hs].unsqueeze(2).to_broadcast([128, 8, 64]), op=ALU.mult)
                fw.I("dve", "tensor_tensor", out=prev[:, g, :], in0=prev[:, g, :], in1=p_S[:], op=ALU.add)
                fw.I("act", "copy", out=prevR[:, g, :], in_=prev[:, g, :])


            def tailf(u):
                c, g = divmod(u, 4)
                a = c % 2
                tok = slice(c * 128, (c + 1) * 128)
                p_Y = p_Y2[u % 2]
                b2 = g % 2
                hs = slice(8 * g, 8 * g + 8)
                fw.I("dve", "tensor_tensor", out=t1[b2][:], in0=t1[b2][:], in1=p_Y[:], op=ALU.add)
                fw.I("pool", "tensor_tensor", out=t2[b2][:], in0=xs[a][:, hs, :],
                     in1=dsk[:, hs].unsqueeze(2).to_broadcast([128, 8, 64]), op=ALU.mult)
                fw.I("pool", "tensor_tensor", out=t1[b2][:], in0=t1[b2][:], in1=t2[b2][:], op=ALU.add)
                fw.I("dve", "tensor_tensor", out=t1[b2][:].rearrange("p h d -> p (h d)"),
                     in0=t1[b2][:].rearrange("p h d -> p (h d)"), in1=zs[b2][:], op=ALU.mult)
                fw.I("act", "activation", out=junk[:], in_=t1[b2][:].rearrange("p h d -> p (h d)"),
                     func=AF.Square, accum_out=rs[:, g:g + 1])
                fw.I("dve", "tensor_scalar", out=rs[:, 4 + g:5 + g], in0=rs[:, g:g + 1], scalar1=1.0 / 512,
                     scalar2=float(RMS_EPS), op0=ALU.mult, op1=ALU.add)
                fw.I("act", "activation", out=rs[:, 4 + g:5 + g], in_=rs[:, 4 + g:5 + g], func=AF.Sqrt)
                fw.I("dve", "reciprocal", out=rs[:, 4 + g:5 + g], in_=rs[:, 4 + g:5 + g])
                fw.I("dve", "scalar_tensor_tensor", out=yn[a][:, g * 512:(g + 1) * 512],
                     in0=t1[b2][:].rearrange("p h d -> p (h d)"), scalar=rs[:, 4 + g:5 + g],
                     in1=nw[:, g * 512:(g + 1) * 512], op0=ALU.mult, op1=ALU.mult)

                if g == 3:
                    fw.D("sp", out=self.YN[tok, :], in_=yn[a][:], _r=[self.tk["YN"]])


            pipeline((S // 128) * 4, [front, tailf])

    def ssd_layer(self, li, XT_src, XT_dst):
        j = li // 2
        fw = self.fw
        fw.barrier("sp", [self.xt_tok[0], self.xt_tok[1], self.tk["BCfm"], self.tk["XSBtm"], self.tk["YN"]])
        self.ssd_sweep1(j, XT_src)
        fw.barrier("sp", [self.tk["BCfm"], self.tk["XSBtm"]])
        self.ssd_sweep2(j, XT_src)
        fw.barrier("sp", [self.tk["YN"]])
        with fw.scope():
            ab = [fw.T("s3_a%d" % i, [128, 2048]) for i in range(2)]

            def loader(t):
                fw.D("sp", out=ab[t % 2][:], in_=self.YN[t * 128:(t + 1) * 128, :], _r=[self.tk["YN"]])
                return ab[t % 2]
            self.proj_ln("s3", li, 0, 16, self.ssd_w_out[j], loader, XT_dst)

    def rope_tables(self):
        fw, S, cst = self.fw, self.S, self.cst
        CW = min(1024, S)
        TWO_PI = float(2 * np.pi)
        C1 = 6.28125
        C2 = float(2 * np.pi - 6.28125)
        with fw.scope():
            posi = fw.T("r_posi", [128, CW], I32)
            ang = fw.T("r_ang", [128, CW])
            kq = fw.T("r_kq", [128, CW])
            ki = fw.T("r_ki", [128, CW], I32)
            r = fw.T("r_r", [128, CW])
            m = fw.T("r_m", [128, CW])
            sv = fw.T("r_sv", [128, CW])
            cv = fw.T("r_cv", [128, CW])
            col = lambda c: cst[:, c:c + 1]
            for c0 in range(0, S, CW):
                fw.D("sp", out=posi[:], in_=self.pos_in[0, c0:c0 + CW].partition_broadcast(128))
                fw.I("dve", "tensor_copy", out=ang[:], in_=posi[:])
                fw.I("dve", "tensor_scalar", out=ang[:], in0=ang[:], scalar1=col(C_INVF), scalar2=None, op0=ALU.mult)
                fw.I("dve", "tensor_scalar", out=kq[:], in0=ang[:], scalar1=float(1.0 / TWO_PI), scalar2=None, op0=ALU.mult)
                fw.I("dve", "tensor_copy", out=ki[:], in_=kq[:])
                fw.I("dve", "tensor_copy", out=kq[:], in_=ki[:])
                fw.I("dve", "scalar_tensor_tensor", out=r[:], in0=kq[:], scalar=-C1, in1=ang[:], op0=ALU.mult, op1=ALU.add)
                fw.I("dve", "scalar_tensor_tensor", out=r[:], in0=kq[:], scalar=-C2, in1=r[:], op0=ALU.mult, op1=ALU.add)
                fw.I("dve", "tensor_scalar", out=m[:], in0=r[:], scalar1=float(np.pi), scalar2=None, op0=ALU.is_gt)
                fw.I("dve", "scalar_tensor_tensor", out=r[:], in0=m[:], scalar=-TWO_PI, in1=r[:], op0=ALU.mult, op1=ALU.add)
                fw.I("dve", "tensor_scalar", out=m[:], in0=r[:], scalar1=float(-np.pi), scalar2=None, op0=ALU.is_lt)
                fw.I("dve", "scalar_tensor_tensor", out=r[:], in0=m[:], scalar=TWO_PI, in1=r[:], op0=ALU.mult, op1=ALU.add)
                fw.I("dve", "tensor_scalar", out=r[:], in0=r[:], scalar1=3.1415925, scalar2=-3.1415925, op0=ALU.min, op1=ALU.max)
                fw.I("act", "activation", out=sv[:], in_=r[:], func=AF.Sin)
                fw.I("dve", "scalar_tensor_tensor", out=m[:], in0=r[:], scalar=-1.0, in1=r[:], op0=ALU.mult, op1=ALU.max)
                fw.I("act", "activation", out=cv[:], in_=m[:], func=AF.Sin, scale=-1.0, bias=col(C_HALFPI))
                fw.I("dve", "tensor_scalar", out=cv[:], in0=cv[:], scalar1=col(C_M16), scalar2=col(C_1M16), op0=ALU.mult, op1=ALU.add)
                fw.I("dve", "tensor_scalar", out=sv[:], in0=sv[:], scalar1=col(C_SS), scalar2=None, op0=ALU.mult)
                fw.D("sp", out=self.ROPE[0, :, c0:c0 + CW], in_=cv[:], _r=[self.tk["ROPE"]])
                fw.D("sp", out=self.ROPE[1, :, c0:c0 + CW], in_=sv[:], _r=[self.tk["ROPE"]])
        fw.barrier("sp", [self.tk["ROPE"]])

    def attn_proj(self, j, XT_src):
        fw, S, cst = self.fw, self.S, self.cst
        CH = min(2048, S)
        with fw.scope():
            PmR = fw.T("a1_pm", [128, 128], F32R)
            fw.I("dve", "tensor_copy", out=PmR[:], in_=cst[:, C_PM:C_PM + 128])
            w = fw.T("a1_w", [128, 8, 1536], F32R)
            xch = fw.T("a1_x", [128, 8, CH], F32R)
            tchs = [fw.T("a1_t%d" % i, [128, 2, CH]) for i in range(2)]
            ps = [fw.P("a1_ps%d" % i, [128, 512]) for i in range(2)]
            pp = [fw.P("a1_pp%d" % i, [128, 512]) for i in range(2)]
            qsb = [fw.T("a1_q%d" % i, [128, 512], F32R) for i in range(2)]
            t1 = [fw.T("a1_t1%d" % i, [128, 512]) for i in range(2)]
            t2 = [fw.T("a1_t2%d" % i, [128, 512]) for i in range(2)]
            ob = [fw.T("a1_o%d" % i, [128, 512]) for i in range(2)]
            vb = [fw.T("a1_v%d" % i, [128, 512]) for i in range(2)]
            work = []
            chidx = 0
            for g in range(3):
                for ch in range(S // CH):
                    first = True
                    for sb in range(CH // 512):
                        for which in range(2):
                            for tl in range(4):
                                work.append(("qk", g, ch, chidx, first, sb, which, tl))
                                first = False
                    for bi in range(CH // 128):
                        work.append(("v", g, ch, chidx, False, bi, 0, 0))
                    chidx += 1

            def geom(g):
                d = ATT_PATTERNS[g][1]
                return d, S // d, CH // d

            def perm(v3, c0, width, d, ic):
                vv = v3.rearrange("p (i r) -> p r i", r=d)
                if ic >= width:
                    return vv[:, c0 // ic, (c0 % ic):(c0 % ic) + width]
                return vv[:, c0 // ic:c0 // ic + width // ic, :]

            def s0(i):
                kind, g, ch, cx, first, p1, which, tl = work[i]
                d, n, ic = geom(g)
                a = i % 2
                if first:
                    if ch == 0:
                        for k in range(8):
                            fw.D("pool", out=w[:, k, :], in_=self.attn_w_qkv[j, k * 128:(k + 1) * 128, g * 1536:(g + 1) * 1536])
                    fw.D("pool", out=xch[:], in_=self.XT[XT_src][:, :, ch * CH:(ch + 1) * CH].rearrange("k p t -> p k t"),
                         _r=[self.xt_tok[XT_src]])
                    fw.D("sp", out=tchs[cx % 2][:], in_=self.ROPE[:, :, ch * CH:(ch + 1) * CH].rearrange("c p t -> p c t"),
                         _r=[self.tk["ROPE"]])
                if kind == "qk":
                    wc = which * 512 + tl * 128
                    for k in range(8):
                        fw.I("pe", "matmul", out=ps[a][:], lhsT=w[:, k, wc:wc + 128], rhs=perm(xch[:, k, :], p1 * 512, 512, d, ic),
                             start=(k == 0), stop=(k == 7))
                else:
                    for k in range(8):
                        fw.I("pe", "matmul", out=ps[a][:], lhsT=perm(xch[:, k, :], p1 * 128, 128, d, ic), rhs=w[:, k, 1024:1536],
                             start=(k == 0), stop=(k == 7))

            def s1(i):
                kind = work[i][0]
                a = i % 2
                if kind == "qk":
                    fw.I("act", "copy", out=qsb[a][:], in_=ps[a][:])
                    fw.I("pe", "matmul", out=pp[a][:], lhsT=PmR[:], rhs=qsb[a][:], start=True, stop=True)
                else:
                    fw.I("act" if (i // 2) % 2 else "dve", "copy" if (i // 2) % 2 else "tensor_copy", out=vb[a][:], in_=ps[a][:])

            def s2(i):
                kind, g, ch, cx, first, p1, which, tl = work[i]
                d, n, ic = geom(g)
                a = i % 2
                if kind == "qk":
                    sb = p1
                    c0 = sb * 512
                    tch = tchs[cx % 2]
                    cview = perm(tch[:, 0, :], c0, 512, d, ic)
                    sview = perm(tch[:, 1, :], c0, 512, d, ic)
                    shp = list(cview.shape)
                    rs = (lambda t_: t_[:]) if len(shp) == 2 else (lambda t_: t_[:].rearrange("p (r i) -> p r i", r=shp[1]))
                    fw.I("dve", "tensor_tensor", out=rs(t1[a]), in0=rs(qsb[a]), in1=cview, op=ALU.mult)
                    fw.I("dve", "tensor_tensor", out=rs(t2[a]), in0=rs(pp[a]), in1=sview, op=ALU.mult)
                    fw.I("pool", "tensor_tensor", out=ob[a][:], in0=t1[a][:], in1=t2[a][:], op=ALU.add)
                    ct = (g * 2 + which) * 4 + tl
                    if ic >= 512:
                        r_ = c0 // ic
                        i0 = c0 % ic
                        dst = self.QK[ct, :, r_ * n + ch * ic + i0:r_ * n + ch * ic + i0 + 512]
                        src = ob[a][:]
                    else:
                        nr = 512 // ic
                        dst = self.QK[ct].rearrange("p (r n) -> p r n", r=d)[:, sb * nr:(sb + 1) * nr, ch * ic:(ch + 1) * ic]
                        src = ob[a][:].rearrange("p (r i) -> p r i", r=nr)
                    fw.D("sp", out=dst, in_=src, _r=[self.tk["QK"]])
                else:
                    c0 = p1 * 128
                    r_ = c0 // ic
                    i0 = c0 % ic
                    row0 = r_ * n + ch * ic + i0
                    fw.D("sp", out=self.VP[g, row0:row0 + 128, :], in_=vb[a][:], _r=[self.tk["VP"]])
            pipeline(len(work), [s0, s1, s2])

    def attn_core(self):
        fw, S, cst = self.fw, self.S, self.cst
        NT = self.NT
        mask = cst[:, C_MASK:C_MASK + 256]
        with fw.scope():
            QT = [fw.T("a2_q%d" % i, [128, 4, 128], F32R) for i in range(2)]
            QZ = [fw.T("a2_qz%d" % i, [128, 4, 2, 128], F32R) for i in range(2)]
            KT = [fw.T("a2_k%d" % i, [128, 4, 128], F32R) for i in range(4)]
            V = [fw.T("a2_v%d" % i, [128, 512], F32R) for i in range(4)]
            sc = [fw.T("a2_sc%d" % i, [128, 4, 256]) for i in range(2)]
            PT = [fw.T("a2_pt%d" % i, [128, 8, 128], F32R) for i in range(2)]
            st = [fw.T("a2_st%d" % i, [128, 4, 8]) for i in range(2)]
            oh = [fw.T("a2_oh%d" % i, [128, 8, 64]) for i in range(2)]
            ml = [fw.T("a2_ml%d" % i, [128, 16]) for i in range(2)]
            p_S = [fw.P("a2_pS%d" % i, [128, 4, 256]) for i in range(2)]
            p_T = fw.P("a2_pT", [128, 8, 128])
            p_O = fw.P("a2_pO", [128, 8, 64])

            def info(u):
                pbi, h4 = divmod(u, 2)
                g, pb = divmod(pbi, NT)
                d = ATT_PATTERNS[g][1]
                n = S // d
                nbr = n // 128
                has_prev = (pb % nbr) > 0
                return pbi, h4, g, pb, d, n, has_prev

            def aA(u):
                pbi, h4, g, pb, d, n, has_prev = info(u)
                a, b2 = pbi % 2, u % 2
                if h4 == 0:
                    cols = slice(pb * 128, (pb + 1) * 128)
                    qbase = (g * 2) * 4
                    fw.D("pool", out=QT[a][:], in_=self.QK[qbase:qbase + 4, :, cols].rearrange("t p c -> p t c"),
                         _r=[self.tk["QK"]])
                    fw.D("pool", out=KT[pbi % 4][:], in_=self.QK[qbase + 4:qbase + 8, :, cols].rearrange("t p c -> p t c"),
                         _r=[self.tk["QK"]])
                    fw.D("pool", out=V[pbi % 4][:], in_=self.VP[g, cols, :], _r=[self.tk["VP"]])
                    for hl in range(2):
                        fw.I("act", "activation", out=QZ[a][:, :, hl, :], in_=QT[a][:], func=AF.Copy,
                             scale=cst[:, C_HM0 + hl:C_HM0 + hl + 1])
                for hh in range(4):
                    h = h4 * 4 + hh
                    tl = h // 2
                    if has_prev:
                        fw.I("pe", "matmul", out=p_S[b2][:, hh, 0:128], lhsT=QZ[a][:, tl, h % 2, :],
                             rhs=KT[(pbi - 1) % 4][:, tl, :], start=True, stop=True)
                    fw.I("pe", "matmul", out=p_S[b2][:, hh, 128:256], lhsT=QZ[a][:, tl, h % 2, :],
                         rhs=KT[pbi % 4][:, tl, :], start=True, stop=True)

            def aB(u):
                pbi, h4, g, pb, d, n, has_prev = info(u)
                a, b2 = pbi % 2, u % 2
                k0 = 0 if has_prev else 128
                fw.I("dve", "scalar_tensor_tensor", out=sc[b2][:, :, k0:256], in0=p_S[b2][:, :, k0:256], scalar=0.125,
                     in1=mask[:, k0:256].unsqueeze(1).to_broadcast([128, 4, 256 - k0]), op0=ALU.mult, op1=ALU.add)
                mx = st[a][:, 0, h4 * 4:h4 * 4 + 4]
                nmx = st[a][:, 1, h4 * 4:h4 * 4 + 4]
                fw.I("dve", "tensor_reduce", out=mx, in_=sc[b2][:, :, k0:256], axis=AX.X, op=ALU.max)
                fw.I("dve", "tensor_scalar", out=nmx, in0=mx, scalar1=-1.0, scalar2=None, op0=ALU.mult)
                for hh in range(4):
                    h = h4 * 4 + hh
                    fw.I("act", "activation", out=sc[b2][:, hh, k0:256], in_=sc[b2][:, hh, k0:256], func=AF.Exp,
                         bias=st[a][:, 1, h:h + 1], accum_out=st[a][:, 2, h:h + 1])

            def aC(u):
                pbi, h4, g, pb, d, n, has_prev = info(u)
                b2 = u % 2
                k0 = 0 if has_prev else 128
                nkc = 2 if has_prev else 1
                for hh in range(4):
                    for c in range(nkc):
                        kc = k0 + c * 128
                        fw.I("pe", "transpose", out=p_T[:, hh * 2 + c, :], in_=sc[b2][:, hh, kc:kc + 128],
                             identity=self.ident)
                eng, meth = ("act", "copy") if h4 else ("dve", "tensor_copy")
                if has_prev:
                    fw.I(eng, meth, out=PT[b2][:], in_=p_T[:])
                else:
                    fw.I(eng, meth, out=PT[b2][:].rearrange("p (h c) q -> p h c q", c=2)[:, :, 0, :],
                         in_=p_T[:].rearrange("p (h c) q -> p h c q", c=2)[:, :, 0, :])

            def aD(u):
                pbi, h4, g, pb, d, n, has_prev = info(u)
                a, b2 = pbi % 2, u % 2
                nkc = 2 if has_prev else 1
                for hh in range(4):
                    h = h4 * 4 + hh
                    for c in range(nkc):
                        vsrc = V[(pbi - 1) % 4] if (has_prev and c == 0) else V[pbi % 4]
                        fw.I("pe", "matmul", out=p_O[:, h, :], lhsT=PT[b2][:, hh * 2 + c, :], rhs=vsrc[:, h * 64:(h + 1) * 64],
                             start=(c == 0), stop=(c == nkc - 1))
                if h4 == 1:
                    fw.I("dve", "reciprocal", out=st[a][:, 3, :], in_=st[a][:, 2, :])
                    fw.I("dve", "tensor_tensor", out=oh[a][:], in0=p_O[:],
                         in1=st[a][:, 3, :].unsqueeze(2).to_broadcast([128, 8, 64]), op=ALU.mult)
                    fw.I("pool", "tensor_copy", out=ml[a][:, 0:8], in_=st[a][:, 0, :])
                    fw.I("pool", "tensor_copy", out=ml[a][:, 8:16], in_=st[a][:, 2, :])
                    r_ = (pb * 128) // n
                    i0 = (pb * 128) % n
                    ao_dst = self.AO[g].rearrange("(i r) c -> r i c", r=d)[r_, i0:i0 + 128, :]
                    ml_dst = self.AML[g].rearrange("(i r) c -> r i c", r=d)[r_, i0:i0 + 128, :]
                    fw.D("sp", out=ao_dst, in_=oh[a][:].rearrange("p h e -> p (h e)"), _r=[self.tk["AO"]])
                    fw.D("sp", out=ml_dst, in_=ml[a][:], _r=[self.tk["AML"]])
            pipeline(3 * NT * 2, [aA, aB, aC, aD])

    def attn_layer(self, li, XT_src, XT_dst):
        fw = self.fw
        j = li // 2
        fw.barrier("sp", [self.xt_tok[0], self.xt_tok[1], self.tk["QK"], self.tk["VP"], self.tk["AO"], self.tk["AML"]])
        self.attn_proj(j, XT_src)
        fw.barrier("sp", [self.tk["QK"], self.tk["VP"]])
        self.attn_core()
        fw.barrier("sp", [self.tk["AO"], self.tk["AML"]])
        with fw.scope():
            ao = [fw.T("a3_ao%d" % i, [128, 3, 512]) for i in range(2)]
            am = [fw.T("a3_ml%d" % i, [128, 3, 16]) for i in range(2)]
            sm = [fw.T("a3_sm%d" % i, [128, 4, 24]) for i in range(2)]
            mg = [fw.T("a3_mg%d" % i, [128, 512]) for i in range(2)]
            tmp = fw.T("a3_tmp", [128, 512])

            def loader(t):
                a = t % 2
                rows = slice(t * 128, (t + 1) * 128)
                fw.D("sp", out=ao[a][:], in_=self.AO[:, rows, :].rearrange("g p c -> p g c"), _r=[self.tk["AO"]])
                fw.D("sp", out=am[a][:], in_=self.AML[:, rows, :].rearrange("g p c -> p g c"), _r=[self.tk["AML"]])
                M = sm[a][:, 0, 0:8]
                fw.I("dve", "tensor_tensor", out=M, in0=am[a][:, 0, 0:8], in1=am[a][:, 1, 0:8], op=ALU.max)
                fw.I("dve", "tensor_tensor", out=M, in0=M, in1=am[a][:, 2, 0:8], op=ALU.max)
                wg = sm[a][:, 1, :].rearrange("p (g h) -> p g h", g=3)
                fw.I("dve", "tensor_tensor", out=wg, in0=am[a][:, :, 0:8], in1=M.unsqueeze(1).to_broadcast([128, 3, 8]),
                     op=ALU.subtract)
                fw.I("act", "activation", out=sm[a][:, 1, :], in_=sm[a][:, 1, :], func=AF.Exp)
                fw.I("dve", "tensor_tensor", out=wg, in0=wg, in1=am[a][:, :, 8:16], op=ALU.mult)
                den = sm[a][:, 2, 0:8]
                fw.I("dve", "tensor_tensor", out=den, in0=sm[a][:, 1, 0:8], in1=sm[a][:, 1, 8:16], op=ALU.add)
                fw.I("dve", "tensor_tensor", out=den, in0=den, in1=sm[a][:, 1, 16:24], op=ALU.add)
                fw.I("dve", "reciprocal", out=sm[a][:, 2, 8:16], in_=den)
                fw.I("dve", "tensor_tensor", out=wg, in0=wg, in1=sm[a][:, 2, 8:16].unsqueeze(1).to_broadcast([128, 3, 8]),
                     op=ALU.mult)
                v3 = lambda t_: t_.rearrange("p (h e) -> p h e", h=8)
                bc = lambda gi: sm[a][:, 1, gi * 8:(gi + 1) * 8].unsqueeze(2).to_broadcast([128, 8, 64])
                fw.I("dve", "tensor_tensor", out=v3(mg[a][:]), in0=v3(ao[a][:, 0, :]), in1=bc(0), op=ALU.mult)
                for gi in (1, 2):
                    fw.I("pool", "tensor_tensor", out=v3(tmp[:]), in0=v3(ao[a][:, gi, :]), in1=bc(gi), op=ALU.mult)
                    fw.I("dve", "tensor_tensor", out=mg[a][:], in0=mg[a][:], in1=tmp[:], op=ALU.add)
                return mg[a]
            self.proj_ln("a3", li, 0, 4, self.attn_w_o[j], loader, XT_dst)

    def zero_xrows(self):
        fw = self.fw
        with fw.scope():
            z = fw.T("z_zero", [128, 4, 1024])
            fw.I("dve", "memset", ap=z[:], constant=0.0)
            nq = self.NBLK * RT
            for q0 in range(0, nq, 4):
                qn = min(4, nq - q0)
                fw.D("act", out=self.XROWS[q0 * 128:(q0 + qn) * 128, :].rearrange("(q p) c -> p q c", p=128),
                     in_=z[:, 0:qn, :], _r=[self.tk["XROWS"]])

    def moe_layer(self, li, XT_src, XT_dst, lw=None):
        fw, NT, NB = self.fw, self.NT, self.NBLK
        lw = li if lw is None else lw
        cst = self.cst
        Lmat = cst[:, C_L:C_L + 128]
        fw.barrier("sp", [self.xt_tok[0], self.xt_tok[1], self.tk["XROWS"], self.tk["YROWS"]])
        with fw.scope():
            OH1 = fw.T("m_oh1", [128, NT, 32])
            OH2 = fw.T("m_oh2", [128, NT, 32])
            RANK = fw.T("m_rank", [128, NT, 32])
            GT = fw.T("m_gt", [128, NT, 2])
            DEST = fw.T("m_dest", [128, 2, NT])
            DESTi = fw.T("m_desti", [128, 2, NT], I32)
            WIDX = fw.T("m_widx", [128, NB], I32)
            run = fw.T("m_run", [128, 32])
            with fw.scope():
                wr = fw.T("m_wr", [128, 8, 36], F32R)
                fw.D("pool", out=wr[:], in_=self.w_router[li].rearrange("(k p) n -> p k n", p=128))
                brb = fw.T("m_brb", [128, 36])
                self.bc_load("sp", brb[:], self.b_router[li])
                fw.I("dve", "memset", ap=run[:], constant=0.0)
                xTc = [fw.T("m_xT%d" % i, [128, 8, 128], F32R) for i in range(2)]
                smt = [fw.T("m_sm%d" % i, [128, 128]) for i in range(2)]
                p_lg = fw.P("m_plg", [128, 64])
                p_rk = fw.P("m_prk", [128, 64])
                for t in range(NT):
                    a = t % 2
                    sm = smt[a]
                    lg = sm[:, 0:36]
                    gmax, gsum, gw, m1, m2, w1, tmp, ngmax = [sm[:, 36 + i:37 + i] for i in range(8)]
                    gmask, gexp = sm[:, 44:48], sm[:, 48:52]
                    esel, mask1, esel2, mask2 = sm[:, 52:60], sm[:, 60:68], sm[:, 68:76], sm[:, 76:84]
                    A = sm[:, 84:116]
                    fw.D("pool", out=xTc[a][:], in_=self.XT[XT_src][:, :, t * 128:(t + 1) * 128].rearrange("k p t -> p k t"),
                         _r=[self.xt_tok[XT_src]])
                    for k in range(8):
                        fw.I("pe", "matmul", out=p_lg[:, 0:36], lhsT=xTc[a][:, k, :], rhs=wr[:, k, :],
                             start=(k == 0), stop=(k == 7))
                    fw.I("dve", "tensor_tensor", out=lg, in0=p_lg[:, 0:36], in1=brb[:], op=ALU.add)
                    fw.I("dve", "tensor_reduce", out=gmax, in_=sm[:, 0:4], axis=AX.X, op=ALU.max)
                    fw.I("dve", "tensor_scalar", out=gmask, in0=sm[:, 0:4], scalar1=gmax, scalar2=None, op0=ALU.is_equal)
                    fw.I("dve", "tensor_scalar", out=ngmax, in0=gmax, scalar1=-1.0, scalar2=None, op0=ALU.mult)
                    fw.I("act", "activation", out=gexp, in_=sm[:, 0:4], func=AF.Exp, bias=ngmax, accum_out=gsum)
                    fw.I("dve", "reciprocal", out=gw, in_=gsum)
                    fw.I("dve", "tensor_scalar", out=esel, in0=sm[:, 4:12], scalar1=sm[:, 44:45], scalar2=None, op0=ALU.mult)
                    for g in range(1, 4):
                        fw.I("dve", "scalar_tensor_tensor", out=esel, in0=sm[:, 4 + 8 * g:12 + 8 * g],
                             scalar=sm[:, 44 + g:45 + g], in1=esel, op0=ALU.mult, op1=ALU.add)
                    fw.I("dve", "tensor_reduce", out=m1, in_=esel, axis=AX.X, op=ALU.max)
                    fw.I("dve", "tensor_scalar", out=mask1, in0=esel, scalar1=m1, scalar2=None, op0=ALU.is_equal)
                    fw.I("dve", "scalar_tensor_tensor", out=esel2, in0=mask1, scalar=-1e30, in1=esel,
                         op0=ALU.mult, op1=ALU.add)
                    fw.I("dve", "tensor_reduce", out=m2, in_=esel2, axis=AX.X, op=ALU.max)
                    fw.I("dve", "tensor_scalar", out=mask2, in0=esel2, scalar1=m2, scalar2=None, op0=ALU.is_equal)
                    fw.I("dve", "tensor_tensor", out=tmp, in0=m2, in1=m1, op=ALU.subtract)
                    fw.I("act", "activation", out=tmp, in_=tmp, func=AF.Exp)
                    fw.I("dve", "tensor_scalar", out=tmp, in0=tmp, scalar1=1.0, scalar2=None, op0=ALU.add)
                    fw.I("dve", "reciprocal", out=w1, in_=tmp)
                    fw.I("dve", "tensor_tensor", out=GT[:, t, 0:1], in0=gw, in1=w1, op=ALU.mult)
                    fw.I("dve", "tensor_tensor", out=GT[:, t, 1:2], in0=gw, in1=GT[:, t, 0:1], op=ALU.subtract)
                    fw.I("dve", "tensor_tensor", out=OH1[:, t, :].rearrange("p (g e) -> p g e", g=4),
                         in0=gmask.unsqueeze(2).to_broadcast([128, 4, 8]),
                         in1=mask1.unsqueeze(1).to_broadcast([128, 4, 8]), op=ALU.mult)
                    fw.I("dve", "tensor_tensor", out=OH2[:, t, :].rearrange("p (g e) -> p g e", g=4),
                         in0=gmask.unsqueeze(2).to_broadcast([128, 4, 8]),
                         in1=mask2.unsqueeze(1).to_broadcast([128, 4, 8]), op=ALU.mult)
                    fw.I("dve", "tensor_tensor", out=A, in0=OH1[:, t, :], in1=OH2[:, t, :], op=ALU.add)
                    fw.I("pe", "matmul", out=p_rk[:, 0:32], lhsT=Lmat, rhs=A, start=True, stop=True)
                    fw.I("pe", "matmul", out=p_rk[:, 32:64], lhsT=self.ones, rhs=A, start=True, stop=True)
                    fw.I("dve", "tensor_tensor", out=RANK[:, t, :], in0=p_rk[:, 0:32], in1=run[:], op=ALU.add)
                    fw.I("dve", "tensor_tensor", out=run[:], in0=run[:], in1=p_rk[:, 32:64], op=ALU.add)
            with fw.scope():
                NG = -(-self.S // BR)
                cmp = fw.T("m_cmp", [128, 32, NG])
                nblk = fw.T("m_nblk", [128, 32])
                padded = fw.T("m_padded", [128, 32])
                padT = fw.T("m_padT", [32, 128])
                pend = fw.T("m_pend", [128, 32])
                pstart = fw.T("m_pstart", [128, 32])
                tmp3 = fw.T("m_tmp3", [128, NT, 32])
                tmp4 = fw.T("m_tmp4", [128, NT, 32])
                cmpb = fw.T("m_cmpb", [128, NB, 32])
                be = fw.T("m_be", [128, NB])
                p_a = fw.P("m_pa", [128, 128])
                p_b = fw.P("m_pb", [128, 32])
                grid = cst[:, C_BLK:C_BLK + NB]
                fw.I("dve", "tensor_tensor", out=cmp[:], in0=run[:].unsqueeze(2).to_broadcast([128, 32, NG]),
                     in1=grid[:, 0:NG].unsqueeze(1).to_broadcast([128, 32, NG]), op=ALU.is_gt)
                fw.I("dve", "tensor_reduce", out=nblk[:], in_=cmp[:], axis=AX.X, op=ALU.add)
                fw.I("dve", "tensor_scalar", out=padded[:], in0=nblk[:], scalar1=float(BR), scalar2=None, op0=ALU.mult)
                fw.I("pe", "transpose", out=p_a[0:32, :], in_=padded[:, 0:32], identity=self.ident)
                fw.I("act", "copy", out=padT[:], in_=p_a[0:32, :])
                fw.I("pe", "matmul", out=p_b[:], lhsT=padT[:], rhs=cst[0:32, C_U:C_U + 32], start=True, stop=True)
                fw.I("act", "copy", out=pend[:], in_=p_b[:])
                fw.I("dve", "tensor_tensor", out=pstart[:], in0=pend[:], in1=padded[:], op=ALU.subtract)
                fw.I("dve", "tensor_tensor", out=tmp3[:], in0=RANK[:],
                     in1=pstart[:].unsqueeze(1).to_broadcast([128, NT, 32]), op=ALU.add)
                for k, OH in enumerate((OH1, OH2)):
                    fw.I("dve", "tensor_tensor", out=tmp4[:], in0=tmp3[:], in1=OH[:], op=ALU.mult)
                    fw.I("dve", "tensor_reduce", out=DEST[:, k, :], in_=tmp4[:], axis=AX.X, op=ALU.add)
                fw.I("dve", "tensor_copy", out=DESTi[:], in_=DEST[:])
                fw.I("dve", "tensor_tensor", out=cmpb[:], in0=grid.unsqueeze(2).to_broadcast([128, NB, 32]),
                     in1=pend[:].unsqueeze(1).to_broadcast([128, NB, 32]), op=ALU.is_ge)
                fw.I("dve", "tensor_reduce", out=be[:], in_=cmpb[:], axis=AX.X, op=ALU.add)
                fw.I("dve", "tensor_scalar", out=be[:], in0=be[:], scalar1=32.0, scalar2=128.0, op0=ALU.min, op1=ALU.mult)
                fw.I("dve", "tensor_scalar", out=be[:], in0=be[:], scalar1=cst[:, C_PIDX:C_PIDX + 1], scalar2=None,
                     op0=ALU.add)
                fw.I("dve", "tensor_copy", out=WIDX[:], in_=be[:])
            with fw.scope():
                xt = [fw.T("m_x%d" % i, [128, 1024]) for i in range(2)]
                for t in range(NT):
                    a = t % 2
                    fw.D("sp", out=xt[a][:], in_=self.XA[t * 128:(t + 1) * 128, :], _r=[self.xa_tok[t]])
                    for k in range(2):
                        fw.D("pool", _meth="indirect_dma_start", out=self.XROWS[:, :],
                             out_offset=bass.IndirectOffsetOnAxis(ap=DESTi[:, k, t:t + 1], axis=0),
                             in_=xt[a][:], in_offset=None, _r=[self.tk["XROWS"]])
            fw.barrier("sp", [self.tk["XROWS"]])
            with fw.scope():
                gu = [fw.T("m_gu%d" % i, [128, 8, 1024], F32R) for i in range(2)]
                dn = [fw.T("m_dn%d" % i, [128, 4, 1024], F32R) for i in range(2)]
                xb4 = [fw.T("m_xb%d" % i, [128, RT, 1024]) for i in range(2)]
                xbT = fw.T("m_xbT", [128, 8, BR], F32R)
                hT = [fw.T("m_hT%d" % i, [128, 4, BR], F32R) for i in range(2)]
                sg = [fw.T("m_sg%d" % i, [128, BR]) for i in range(2)]
                yb = [fw.T("m_yb%d" % i, [128, 1024]) for i in range(2)]
                p_t = [fw.P("m_pt%d" % i, [128, 4, 128]) for i in range(2)]
                p_g = [fw.P("m_pg%d" % i, [128, BR]) for i in range(2)]
                p_u = [fw.P("m_pu%d" % i, [128, BR]) for i in range(2)]
                p_y = fw.P("m_py", [128, 1024])
                gu_src = self.moe_gu[lw][:, :]
                dn_src = self.moe_dn[lw][:, :]

                def mL(B):
                    fw.D("pool", _meth="indirect_dma_start", out=gu[B % 2][:].rearrange("p k n -> p (k n)"), out_offset=None,
                         in_=gu_src, in_offset=bass.IndirectOffsetOnAxis(ap=WIDX[:, B:B + 1], axis=0),
                         bounds_check=4095, oob_is_err=False)
                    fw.D("sp", out=xb4[B % 2][:], in_=self.XROWS[B * BR:(B + 1) * BR, :].rearrange("(r p) c -> p r c", p=128),
                         _r=[self.tk["XROWS"]])

                def mAB(B):
                    x4, g_ = xb4[B % 2], gu[B % 2]
                    fw.D("pool", _meth="indirect_dma_start", out=dn[B % 2][:].rearrange("p k n -> p (k n)"), out_offset=None,
                         in_=dn_src, in_offset=bass.IndirectOffsetOnAxis(ap=WIDX[:, B:B + 1], axis=0),
                         bounds_check=4095, oob_is_err=False)
                    cnt = 0
                    for rt in range(RT):
                        for k0 in (0, 4):
                            pt = p_t[cnt % 2]
                            for k in range(4):
                                fw.I("pe", "transpose", out=pt[:, k, :], in_=x4[:, rt, (k0 + k) * 128:(k0 + k + 1) * 128],
                                     identity=self.ident)
                            eng, meth = ("act", "copy") if cnt % 2 else ("dve", "tensor_copy")
                            fw.I(eng, meth, out=xbT[:, k0:k0 + 4, rt * 128:(rt + 1) * 128], in_=pt[:])
                            cnt += 1
                    for fc in range(4):
                        b2 = fc % 2
                        for k in range(8):
                            fw.I("pe", "matmul", out=p_g[b2][:], lhsT=g_[:, k, fc * 128:(fc + 1) * 128], rhs=xbT[:, k, :],
                                 start=(k == 0), stop=(k == 7))
                        for k in range(8):
                            fw.I("pe", "matmul", out=p_u[b2][:], lhsT=g_[:, k, 512 + fc * 128:512 + (fc + 1) * 128],
                                 rhs=xbT[:, k, :], start=(k == 0), stop=(k == 7))
                        fw.I("act", "activation", out=sg[b2][:], in_=p_g[b2][:], func=AF.Silu)
                        fw.I("dve", "tensor_tensor", out=hT[B % 2][:, fc, :], in0=sg[b2][:], in1=p_u[b2][:], op=ALU.mult)

                def mC(B):
                    h_, d_ = hT[B % 2], dn[B % 2]
                    for rt in range(RT):
                        for n in range(2):
                            for k in range(4):
                                fw.I("pe", "matmul", out=p_y[:, n * 512:(n + 1) * 512], lhsT=h_[:, k, rt * 128:(rt + 1) * 128],
                                     rhs=d_[:, k, n * 512:(n + 1) * 512], start=(k == 0), stop=(k == 3))
                        fw.I("act" if rt % 2 else "dve", "copy" if rt % 2 else "tensor_copy", out=yb[rt % 2][:], in_=p_y[:])
                        u = B * RT + rt
                        fw.D("sp", out=self.YROWS[u * 128:(u + 1) * 128, :], in_=yb[rt % 2][:], _r=[self.tk["YROWS"]])
                pipeline(NB, [mL, mAB, mC])
            fw.barrier("sp", [self.tk["YROWS"]])
            with fw.scope():
                gbc = fw.T("m_g", [128, 1024])
                bbc = fw.T("m_b", [128, 1024])
                self.bc_load("sp", gbc[:], self.ln_g[li, 1])
                self.bc_load("sp", bbc[:], self.ln_b[li, 1])
                Y1 = [fw.T("m_y1%d" % i, [128, 1024]) for i in range(2)]
                Y2 = [fw.T("m_y2%d" % i, [128, 1024]) for i in range(2)]
                ffn = [fw.T("m_ffn%d" % i, [128, 1024]) for i in range(2)]
                ln = self.ln_alloc("m4")
                def cA(t):
                    a = t % 2
                    for k, Y in enumerate((Y1, Y2)):
                        fw.D("pool", _meth="indirect_dma_start", out=Y[a][:], out_offset=None, in_=self.YROWS[:, :],
                             in_offset=bass.IndirectOffsetOnAxis(ap=DESTi[:, k, t:t + 1], axis=0), _r=[self.tk["YROWS"]])
                    fw.I("dve", "tensor_scalar", out=ffn[a][:], in0=Y1[a][:], scalar1=GT[:, t, 0:1], scalar2=None,
                         op0=ALU.mult)
                    fw.I("dve", "scalar_tensor_tensor", out=ffn[a][:], in0=Y2[a][:], scalar=GT[:, t, 1:2], in1=ffn[a][:],
                         op0=ALU.mult, op1=ALU.add)
                pipeline(NT, [cA] + self.ln_stages(ln, lambda t: ffn[t % 2][:], gbc, bbc, XT_dst))

    def build(self):
        fw = self.fw
        self.initial()
        if self.with_moe:
            self.zero_xrows()
        if any(li % 2 == 1 for li in self.layers):
            self.rope_tables()
        cur = 0
        last_x = "XA"
        for li in self.layers:
            if li % 2 == 0:
                self.ssd_layer(li, cur, 1 - cur)
            else:
                self.attn_layer(li, cur, 1 - cur)
            cur = 1 - cur
            if self.stop_after == (li, 0):
                break
            self.direct_out = (li == self.layers[-1] and self.stop_after is None)
            self.moe_layer(li, cur, 1 - cur, self.moe_layers.index(li))
            wrote_out = self.direct_out
            self.direct_out = False
            cur = 1 - cur
            if self.stop_after == (li, 1):
                break
        if 'wrote_out' in dir() and wrote_out:
            fw.finish(["out"])
            return self.nc
        with fw.scope():
            ot = [fw.T("o_t%d" % i, [128, 1024]) for i in range(2)]
            for t in range(self.NT):
                fw.D("sp", out=ot[t % 2][:], in_=self.XA[t * 128:(t + 1) * 128, :], _r=[self.xa_tok[t]])
                fw.D("act", out=self.out[t * 128:(t + 1) * 128, :], in_=ot[t % 2][:])
        fw.finish(["out"])
        return self.nc


def _lay_gu(wg, wu):
    L = wg.shape[0]
    g = wg.reshape(L, 32, 8, 128, 512).transpose(0, 1, 3, 2, 4)
    u = wu.reshape(L, 32, 8, 128, 512).transpose(0, 1, 3, 2, 4)
    return np.ascontiguousarray(np.concatenate([g, u], -1)).reshape(L, 4096, 8192)


def _lay_dn(wd):
    L = wd.shape[0]
    return np.ascontiguousarray(wd.reshape(L, 32, 4, 128, 1024).transpose(0, 1, 3, 2, 4)).reshape(L, 4096, 4096)


def _run(inputs, S, ncores, layers=(0, 1, 2, 3)):
    f32 = lambda a: np.ascontiguousarray(np.asarray(a, dtype=np.float32))
    prog = Prog(S, layers=layers)
    nc = prog.build()
    base = {k: f32(inputs[k]) for k in ("ssd_w_in", "ssd_conv_w", "ssd_conv_b", "ssd_dt_bias", "ssd_a_log", "ssd_d",
                                        "ssd_norm_w", "ssd_w_out", "attn_w_qkv", "attn_w_o", "ln_g", "ln_b")}
    base["consts"] = make_consts(prog.NBLK)
    base["w_router"] = f32(np.concatenate([np.asarray(inputs["moe_w_router_group"]),
                                           np.asarray(inputs["moe_w_router_expert"])], -1))
    base["b_router"] = f32(np.concatenate([np.asarray(inputs["moe_b_router_group"]),
                                           np.asarray(inputs["moe_b_router_expert"])], -1))
    gu = _lay_gu(f32(inputs["moe_w_gate"]), f32(inputs["moe_w_up"]))
    dn = _lay_dn(f32(inputs["moe_w_down"]))
    for i, li in enumerate(prog.moe_layers):
        base["moe_gu%d" % i] = gu[li]
        base["moe_dn%d" % i] = dn[li]
    x = f32(inputs["x"])
    pos = np.ascontiguousarray(np.asarray(inputs["positions"]).astype(np.int32))
    in_maps = []
    for c in range(ncores):
        d = dict(base)
        d["x"] = np.ascontiguousarray(x[c, :S])
        d["pos"] = np.ascontiguousarray(pos[c:c + 1, :S])
        in_maps.append(d)
    res = run_bass_kernel_spmd(nc, in_maps, core_ids=list(range(ncores)))
    return np.stack([np.asarray(res.results[c]["out"]) for c in range(ncores)]).astype(np.float32)


def kernel(**inputs):
    x = np.asarray(inputs["x"])
    return _run(inputs, x.shape[1], x.shape[0])
```
